# Optimizing a Trainium2 kernel written in Bass

```python
import jax
import jax.numpy as jnp
from jax import lax
import numpy as np

D_MODEL = 1024
BATCH = 32
SEQ = 2048
DEPTH = 2

GRID_W = 64
CTX_LEN = 256
N_HEADS_ATTN = 8
N_KV = 2
HEAD_DIM = 64
AXIS_DIM = HEAD_DIM // 2
ATTN_W = N_HEADS_ATTN * HEAD_DIM
KV_W = N_KV * HEAD_DIM
WINDOW = 128
QBLOCK = 128
ROPE_BASE = 10000.0
LRU_W = 512
LRU_BLOCKS = 8
LRU_BS = LRU_W // LRU_BLOCKS
CONV_W = 4
CONV_LEFT = 2
LRU_C = 8.0
MIX_IN = ATTN_W + 2 * KV_W + 2 * LRU_W
MIX_OUT = ATTN_W + LRU_W
RWKV_H = 16
RWKV_HD = D_MODEL // RWKV_H
DECAY_LORA = 64
ICLR_LORA = 64
GATE_LORA = 160
GN_EPS = 64e-5
N_EXPERTS = 16
EXPERT_FF = 1024
CAPACITY = 2
NORM_EPS = 1e-6
NEG_INF = -1e30
N_A_LAYERS = (DEPTH + 1) // 2
N_C_LAYERS = DEPTH // 2

kernel_name = 'hybrid_dit_swa_rglru_rwkv7_ecmoe'


def rmsnorm(x, g):
    xf = x.astype(jnp.float32)
    y = xf * lax.rsqrt(jnp.mean(xf * xf, axis=-1, keepdims=True) + NORM_EPS)
    return (y * g.astype(jnp.float32)).astype(x.dtype)


def modulate(h, shift, scale):
    return h * (1 + scale) + shift


def axial_angles(rows):
    row = jnp.repeat(jnp.arange(rows), GRID_W).astype(jnp.float32)
    col = jnp.tile(jnp.arange(GRID_W), rows).astype(jnp.float32)
    inv = ROPE_BASE ** (-jnp.arange(0, AXIS_DIM, 2, dtype=jnp.float32) / AXIS_DIM)
    return row[:, None] * inv, col[:, None] * inv


def rope_axis(x, ang):
    c = jnp.cos(ang)[:, None, :]
    s = jnp.sin(ang)[:, None, :]
    x1, x2 = jnp.split(x.astype(jnp.float32), 2, axis=-1)
    return jnp.concatenate([x1 * c - x2 * s, x2 * c + x1 * s], axis=-1)


def axial_rope(x, ang_row, ang_col):
    return jnp.concatenate([rope_axis(x[..., :AXIS_DIM], ang_row),
                            rope_axis(x[..., AXIS_DIM:], ang_col)], axis=-1).astype(x.dtype)


def local_attention(q, k, v, kc, vc, sink):
    B, S, H, hd = q.shape
    G = H // N_KV
    Lc = kc.shape[1]
    span = QBLOCK + 2 * WINDOW
    qg = (q * hd ** -0.5).reshape(B, S, N_KV, G, hd)
    pad = ((0, 0), (WINDOW, WINDOW), (0, 0), (0, 0))
    kp = jnp.pad(k, pad)
    vp = jnp.pad(v, pad)
    rel = jnp.arange(span)[None, :] - WINDOW - jnp.arange(QBLOCK)[:, None]
    band = jnp.abs(rel) <= WINDOW
    sink_b = jnp.broadcast_to(sink.reshape(N_KV, G, 1, 1).astype(jnp.float32), (B, N_KV, G, QBLOCK, 1))

    def block(i):
        start = i * QBLOCK
        qb = lax.dynamic_slice_in_dim(qg, start, QBLOCK, axis=1)
        kb = lax.dynamic_slice_in_dim(kp, start, span, axis=1)
        vb = lax.dynamic_slice_in_dim(vp, start, span, axis=1)
        kpos = start - WINDOW + jnp.arange(span)
        valid = band & ((kpos >= 0) & (kpos < S))[None, :]
        s_ctx = jnp.einsum('bqkgd,bckd->bkgqc', qb, kc).astype(jnp.float32)
        s_win = jnp.where(valid, jnp.einsum('bqkgd,bjkd->bkgqj', qb, kb).astype(jnp.float32), NEG_INF)
        p = jax.nn.softmax(jnp.concatenate([s_ctx, s_win, sink_b], axis=-1), axis=-1).astype(v.dtype)
        return (jnp.einsum('bkgqc,bckd->bqkgd', p[..., :Lc], vc)
                + jnp.einsum('bkgqj,bjkd->bqkgd', p[..., Lc:Lc + span], vb))

    out = lax.map(block, jnp.arange(S // QBLOCK))
    return jnp.moveaxis(out, 0, 1).reshape(B, S, H * hd)


def context_attention(qc, kc, vc, sink):
    B, Lc, H, hd = qc.shape
    G = H // N_KV
    qg = (qc * hd ** -0.5).reshape(B, Lc, N_KV, G, hd)
    s = jnp.einsum('bqkgd,bckd->bkgqc', qg, kc).astype(jnp.float32)
    sink_b = jnp.broadcast_to(sink.reshape(N_KV, G, 1, 1).astype(jnp.float32), (B, N_KV, G, Lc, 1))
    p = jax.nn.softmax(jnp.concatenate([s, sink_b], axis=-1), axis=-1)[..., :Lc].astype(vc.dtype)
    return jnp.einsum('bkgqc,bckd->bqkgd', p, vc).reshape(B, Lc, H * hd)


def centred_dwconv(u, w, b):
    n = u.shape[1]
    up = jnp.pad(u, ((0, 0), (CONV_LEFT, CONV_W - 1 - CONV_LEFT), (0, 0)))
    out = up[:, 0:n] * w[0]
    for j in range(1, CONV_W):
        out = out + up[:, j:j + n] * w[j]
    return out + b


def block_diag_linear(u, w, b):
    B, N, _ = u.shape
    y = jnp.einsum('bnhi,hij->bnhj', u.reshape(B, N, LRU_BLOCKS, LRU_BS), w)
    return y.reshape(B, N, LRU_W) + b


def rglru_coeffs(u, wa, ba, wi, bi, lam):
    r = jax.nn.sigmoid(block_diag_linear(u, wa, ba))
    i = jax.nn.sigmoid(block_diag_linear(u, wi, bi))
    log_a = (-LRU_C * r * jax.nn.softplus(-lam)).astype(jnp.float32)
    a = jnp.exp(log_a)
    mult = jnp.sqrt(jnp.maximum(-jnp.expm1(2 * log_a), 0.0))
    return a, mult * (i * u).astype(jnp.float32)


def linear_scan(a, b, h0):
    def combine(e1, e2):
        a1, b1 = e1
        a2, b2 = e2
        return a1 * a2, a2 * b1 + b2
    A, Bc = lax.associative_scan(combine, (a, b), axis=1)
    return A * h0[:, None, :] + Bc


def rglru_bidir(ul, uc, wa, ba, wi, bi, lam, need_ctx_out):
    B = ul.shape[0]
    outs_l, outs_c = [], []
    for d in range(2):
        al, bl = rglru_coeffs(ul, wa[d], ba[d], wi[d], bi[d], lam[d])
        ac, bc = rglru_coeffs(uc, wa[d], ba[d], wi[d], bi[d], lam[d])
        if d == 1:
            al, bl, ac, bc = (jnp.flip(t, 1) for t in (al, bl, ac, bc))
        hc = linear_scan(ac, bc, jnp.zeros((B, LRU_W), jnp.float32))
        hl = linear_scan(al, bl, hc[:, -1])
        outs_l.append(jnp.flip(hl, 1) if d == 1 else hl)
        if need_ctx_out:
            outs_c.append(jnp.flip(hc, 1) if d == 1 else hc)
    yl = (outs_l[0] + outs_l[1]).astype(ul.dtype)
    yc = (outs_c[0] + outs_c[1]).astype(uc.dtype) if need_ctx_out else None
    return yl, yc


def attn_lru_mixer(hl, hc, ang_row, ang_col, w_in, w_out, sink, conv_w, conv_b,
                   wa, ba, wi, bi, lam, need_ctx_out):
    B, S, _ = hl.shape
    Lc = hc.shape[1]
    cuts = [ATTN_W, ATTN_W + KV_W, ATTN_W + 2 * KV_W, ATTN_W + 2 * KV_W + LRU_W]
    q, k, v, ul, gl = jnp.split(hl @ w_in, cuts, axis=-1)
    q = axial_rope(q.reshape(B, S, N_HEADS_ATTN, HEAD_DIM), ang_row, ang_col)
    k = axial_rope(k.reshape(B, S, N_KV, HEAD_DIM), ang_row, ang_col)
    v = v.reshape(B, S, N_KV, HEAD_DIM)
    if need_ctx_out:
        qc, kc, vc, uc, gc = jnp.split(hc @ w_in, cuts, axis=-1)
    else:
        kc, vc, uc = jnp.split(hc @ w_in[:, ATTN_W:cuts[3]], [KV_W, 2 * KV_W], axis=-1)
    kc = kc.reshape(B, Lc, N_KV, HEAD_DIM)
    vc = vc.reshape(B, Lc, N_KV, HEAD_DIM)
    attn_l = local_attention(q, k, v, kc, vc, sink)
    rl, rc = rglru_bidir(centred_dwconv(ul, conv_w, conv_b), centred_dwconv(uc, conv_w, conv_b),
                         wa, ba, wi, bi, lam, need_ctx_out)
    yl = jnp.concatenate([attn_l, rl * jax.nn.gelu(gl)], axis=-1) @ w_out
    if not need_ctx_out:
        return yl, None
    attn_c = context_attention(qc.reshape(B, Lc, N_HEADS_ATTN, HEAD_DIM), kc, vc, sink)
    yc = jnp.concatenate([attn_c, rc * jax.nn.gelu(gc)], axis=-1) @ w_out
    return yl, yc


def centred_shift(h):
    hp = jnp.pad(h, ((0, 0), (1, 1), (0, 0)))
    return 0.5 * (hp[:, :-2] + hp[:, 2:])


def l2_normalize(t):
    tf = t.astype(jnp.float32)
    return (tf * lax.rsqrt(jnp.maximum(jnp.sum(tf * tf, axis=-1, keepdims=True), 1e-24))).astype(t.dtype)


def head_groupnorm(y, g, b):
    B, N, H, hd = y.shape
    yf = y.astype(jnp.float32)
    mu = jnp.mean(yf, axis=-1, keepdims=True)
    var = jnp.mean(jnp.square(yf - mu), axis=-1, keepdims=True)
    yn = ((yf - mu) * lax.rsqrt(var + GN_EPS)).reshape(B, N, H * hd)
    return yn * g.astype(jnp.float32) + b.astype(jnp.float32)


def rwkv7_inputs(h, mu, w_rkv, w0, w1, w2, a0, a1, a2, g1, g2, k_k, k_a, with_out):
    B, N, _ = h.shape

    def heads(t):
        return t.reshape(B, N, RWKV_H, RWKV_HD)

    xx = centred_shift(h) - h
    xw = h + xx * mu[1]
    xa = h + xx * mu[4]
    k = (h + xx * mu[2]) @ w_rkv[1]
    v = heads((h + xx * mu[3]) @ w_rkv[2])
    kk = l2_normalize(heads(k * k_k))
    dirs = []
    for d in range(2):
        w_log = -jax.nn.softplus(-(w0[d] + jnp.tanh(xw @ w1[d]) @ w2[d]).astype(jnp.float32)) - 0.5
        decay = jnp.exp(-jnp.exp(w_log))
        a = jax.nn.sigmoid(a0[d] + (xa @ a1[d]) @ a2[d])
        kd = k * (1 + (a - 1) * k_a)
        dirs.append((heads(decay), heads(kd), heads(a)))
    if with_out:
        r = heads((h + xx * mu[0]) @ w_rkv[0])
        g = jax.nn.sigmoid((h + xx * mu[5]) @ g1) @ g2
    else:
        r = None
        g = None
    return r, g, v, kk, dirs


def wkv_scan(s0, decay, k, v, kk, a, r, reverse):
    def seq(t):
        return jnp.moveaxis(t.astype(jnp.float32), 1, 0)
    xs = (seq(decay), seq(k), seq(v), seq(-kk), seq(kk * a))
    if r is not None:
        xs = xs + (seq(r),)

    def step(s, inp):
        w_t, k_t, v_t, a_t, b_t = inp[:5]
        sa = jnp.einsum('bhvk,bhk->bhv', s, a_t)
        s = s * w_t[:, :, None, :] + sa[..., None] * b_t[:, :, None, :] + v_t[..., None] * k_t[:, :, None, :]
        y = jnp.einsum('bhvk,bhk->bhv', s, inp[5]) if r is not None else None
        return s, y

    s_fin, ys = lax.scan(step, s0, xs, reverse=reverse)
    return s_fin, (jnp.moveaxis(ys, 0, 1) if r is not None else None)


def rwkv7_mixer(hl, hc, mu, w_rkv, w0, w1, w2, a0, a1, a2, g1, g2, k_k, k_a, r_k,
                gn_g, gn_b, w_o, need_ctx_out):
    B = hl.shape[0]
    rl, gl, vl, kkl, dirs_l = rwkv7_inputs(hl, mu, w_rkv, w0, w1, w2, a0, a1, a2, g1, g2, k_k, k_a, True)
    rc, gc, vc, kkc, dirs_c = rwkv7_inputs(hc, mu, w_rkv, w0, w1, w2, a0, a1, a2, g1, g2, k_k, k_a, need_ctx_out)
    ys_l, bon_l, ys_c, bon_c = [], [], [], []
    for d in range(2):
        reverse = d == 1
        dec_c, k_c, a_c = dirs_c[d]
        s0 = jnp.zeros((B, RWKV_H, RWKV_HD, RWKV_HD), jnp.float32)
        s_ctx, y_c = wkv_scan(s0, dec_c, k_c, vc, kkc, a_c, rc, reverse)
        dec_l, k_l, a_l = dirs_l[d]
        _, y_l = wkv_scan(s_ctx, dec_l, k_l, vl, kkl, a_l, rl, reverse)
        ys_l.append(y_l)
        bon_l.append(jnp.sum(rl * k_l * r_k, axis=-1, keepdims=True) * vl)
        if need_ctx_out:
            ys_c.append(y_c)
            bon_c.append(jnp.sum(rc * k_c * r_k, axis=-1, keepdims=True) * vc)

    def finish(ys, bons, g):
        Bg, N, _ = g.shape
        o = head_groupnorm(ys[0] + ys[1], gn_g, gn_b).astype(g.dtype) + (bons[0] + bons[1]).reshape(Bg, N, D_MODEL)
        return (o * g) @ w_o

    yl = finish(ys_l, bon_l, gl)
    yc = finish(ys_c, bon_c, gc) if need_ctx_out else None
    return yl, yc


def expert_choice_moe(h, w_router, w1, w3, w2):
    B, N, _ = h.shape
    cap = CAPACITY * N // N_EXPERTS
    aff = jax.nn.softmax(jnp.einsum('bnd,de->ben', h, w_router).astype(jnp.float32), axis=1)
    gate, idx = lax.top_k(aff, cap)
    bidx = jnp.arange(B)[:, None, None]
    xin = h[bidx, idx]
    hid = jax.nn.silu(jnp.einsum('becd,edf->becf', xin, w1)) * jnp.einsum('becd,edf->becf', xin, w3)
    y = jnp.einsum('becf,efd->becd', hid, w2) * gate[..., None].astype(h.dtype)
    return jnp.zeros_like(h).at[bidx, idx].add(y)


def setup_inputs(seed: int = 0) -> dict:
    key = jax.random.key(seed)
    keys = iter(jax.random.split(key, 64))

    def nrm(shape, scale):
        return jax.random.normal(next(keys), shape, jnp.float32) * scale

    def uni(shape, lo, hi):
        return jax.random.uniform(next(keys), shape, jnp.float32, lo, hi)

    D = D_MODEL
    NA = N_A_LAYERS
    NC = N_C_LAYERS
    lru_a = uni((NA, 2, LRU_W), 0.9, 0.999) ** (1.0 / LRU_C)
    return {
        'x': nrm((BATCH, SEQ, D), 1.0),
        'c': nrm((BATCH, D), 1.0),
        'ctx': nrm((BATCH, CTX_LEN, D), 1.0),
        'c_ctx': nrm((D,), 1.0),
        'mod_w': nrm((DEPTH, D, 6 * D), 0.5 * D ** -0.5),
        'mod_b': nrm((DEPTH, 6 * D), 0.01),
        'norm_mix': 1.0 + nrm((DEPTH, D), 0.05),
        'norm_ffn': 1.0 + nrm((DEPTH, D), 0.05),
        'router_w': nrm((DEPTH, D, N_EXPERTS), D ** -0.5),
        'exp_w1': nrm((DEPTH, N_EXPERTS, D, EXPERT_FF), D ** -0.5),
        'exp_w3': nrm((DEPTH, N_EXPERTS, D, EXPERT_FF), D ** -0.5),
        'exp_w2': nrm((DEPTH, N_EXPERTS, EXPERT_FF, D), EXPERT_FF ** -0.5),
        'mix_in': nrm((NA, D, MIX_IN), D ** -0.5),
        'mix_out': nrm((NA, MIX_OUT, D), MIX_OUT ** -0.5),
        'attn_sink': nrm((NA, N_HEADS_ATTN), 1.0),
        'lru_conv_w': nrm((NA, CONV_W, LRU_W), CONV_W ** -0.5),
        'lru_conv_b': nrm((NA, LRU_W), 0.01),
        'lru_wa': nrm((NA, 2, LRU_BLOCKS, LRU_BS, LRU_BS), LRU_BS ** -0.5),
        'lru_ba': nrm((NA, 2, LRU_W), 0.01),
        'lru_wi': nrm((NA, 2, LRU_BLOCKS, LRU_BS, LRU_BS), LRU_BS ** -0.5),
        'lru_bi': nrm((NA, 2, LRU_W), 0.01),
        'lru_lam': jnp.log(lru_a) - jnp.log1p(-lru_a),
        'rw_mu': uni((NC, 6, D), 0.0, 1.0),
        'rw_rkv': nrm((NC, 3, D, D), D ** -0.5),
        'rw_w0': uni((NC, 2, D), -6.0, -1.0),
        'rw_w1': nrm((NC, 2, D, DECAY_LORA), D ** -0.5),
        'rw_w2': nrm((NC, 2, DECAY_LORA, D), 0.1 * DECAY_LORA ** -0.5),
        'rw_a0': nrm((NC, 2, D), 0.1),
        'rw_a1': nrm((NC, 2, D, ICLR_LORA), D ** -0.5),
        'rw_a2': nrm((NC, 2, ICLR_LORA, D), 0.1 * ICLR_LORA ** -0.5),
        'rw_g1': nrm((NC, D, GATE_LORA), D ** -0.5),
        'rw_g2': nrm((NC, GATE_LORA, D), GATE_LORA ** -0.5),
        'rw_kk': 1.0 + nrm((NC, D), 0.1),
        'rw_ka': 1.0 + nrm((NC, D), 0.1),
        'rw_rk': nrm((NC, RWKV_H, RWKV_HD), 0.1),
        'rw_gn_g': 1.0 + nrm((NC, D), 0.05),
        'rw_gn_b': nrm((NC, D), 0.01),
        'rw_wo': nrm((NC, D, D), D ** -0.5),
        'final_norm': 1.0 + nrm((D,), 0.05),
    }


def reference(x, c, ctx, c_ctx, mod_w, mod_b, norm_mix, norm_ffn, router_w, exp_w1, exp_w3, exp_w2,
              mix_in, mix_out, attn_sink, lru_conv_w, lru_conv_b, lru_wa, lru_ba, lru_wi, lru_bi, lru_lam,
              rw_mu, rw_rkv, rw_w0, rw_w1, rw_w2, rw_a0, rw_a1, rw_a2, rw_g1, rw_g2, rw_kk, rw_ka, rw_rk,
              rw_gn_g, rw_gn_b, rw_wo, final_norm):
    ROWS = x.shape[1] // GRID_W
    ang_row, ang_col = axial_angles(ROWS)
    s_lat = jax.nn.silu(c)
    s_ctx = jax.nn.silu(c_ctx)
    xl, xc = x, ctx
    for layer in range(DEPTH):
        last = layer == DEPTH - 1
        i = layer // 2
        m_l = jnp.split((s_lat @ mod_w[layer] + mod_b[layer])[:, None, :], 6, axis=-1)
        m_c = jnp.split(s_ctx @ mod_w[layer] + mod_b[layer], 6, axis=-1)
        hl = modulate(rmsnorm(xl, norm_mix[layer]), m_l[0], m_l[1])
        hc = modulate(rmsnorm(xc, norm_mix[layer]), m_c[0], m_c[1])
        if layer % 2 == 0:
            yl, yc = attn_lru_mixer(hl, hc, ang_row, ang_col, mix_in[i], mix_out[i], attn_sink[i],
                                    lru_conv_w[i], lru_conv_b[i], lru_wa[i], lru_ba[i], lru_wi[i],
                                    lru_bi[i], lru_lam[i], not last)
        else:
            yl, yc = rwkv7_mixer(hl, hc, rw_mu[i], rw_rkv[i], rw_w0[i], rw_w1[i], rw_w2[i], rw_a0[i],
                                 rw_a1[i], rw_a2[i], rw_g1[i], rw_g2[i], rw_kk[i], rw_ka[i], rw_rk[i],
                                 rw_gn_g[i], rw_gn_b[i], rw_wo[i], not last)
        xl = xl + m_l[2] * yl
        hl = modulate(rmsnorm(xl, norm_ffn[layer]), m_l[3], m_l[4])
        xl = xl + m_l[5] * expert_choice_moe(hl, router_w[layer], exp_w1[layer], exp_w3[layer], exp_w2[layer])
        if not last:
            xc = xc + m_c[2] * yc
            hc = modulate(rmsnorm(xc, norm_ffn[layer]), m_c[3], m_c[4])
            xc = xc + m_c[5] * expert_choice_moe(hc, router_w[layer], exp_w1[layer], exp_w3[layer], exp_w2[layer])
    return rmsnorm(xl, final_norm)
```

```python
from contextlib import ExitStack, contextmanager
import numpy as np
import concourse.bass as bass
import concourse.mybir as mybir
from concourse.bass_utils import run_bass_kernel_spmd

F32 = mybir.dt.float32
BF16 = mybir.dt.bfloat16
AF = mybir.ActivationFunctionType
ALU = mybir.AluOpType
AX = mybir.AxisListType

ENGS = ("pe", "dve", "act", "pool", "sp")

D = 1024
KC = 8
S = 2048
LC = 256
T = S + LC
NCORE = 8
BLOCKS = [(0, 256), (256, 512), (768, 512), (1280, 512), (1792, 512)]
LAT_BLOCKS = BLOCKS[1:]
EPS = 1e-6
ARENA_BYTES = 206 * 1024


class Buf:
    def __init__(self, prog, t, name, space):
        self.prog = prog
        self.t = t
        self.name = name
        self.space = space
        self.last_w = None
        self.reads = {}
        self.dsem = None

    def __getitem__(self, idx):
        return self.t[idx]

    def view(self, name):
        b = Buf(self.prog, self.t, name, self.space)
        self.prog._register(b)
        return b


class Prog:
    def __init__(self):
        self.nc = bass.Bass("TRN2", target_bir_lowering=False)
        self.gstack = ExitStack()
        self.ops = {e: [] for e in ENGS}
        self.cnt = {e: 0 for e in ENGS}
        self.waited = {e: {} for e in ENGS}
        self.sems = {}
        self.dpool = []
        self.dcnt = {}
        self.live_dsem = set()
        self.ndsem = 0
        self.live_bufs = []
        self.phase_bufs = None
        self.uid = 0
        for e in ENGS:
            self._sem("eng_" + e)
        self.arena = self.gstack.enter_context(self.nc.sbuf_tensor("arena", [128, ARENA_BYTES // 4], F32))
        self.top = 0
        self.limit = ARENA_BYTES
        self.hiwater = 0
        self.banks = []
        for i in range(8):
            t = self.gstack.enter_context(self.nc.psum_tensor("bank%d" % i, [128, 512], F32))
            b = Buf(self, t, "bank%d" % i, "psum")
            self.live_bufs.append(b)
            self.banks.append(b)

    def _sem(self, key):
        if key not in self.sems:
            self.sems[key] = self.gstack.enter_context(self.nc.semaphore("s_" + key))
        return self.sems[key]

    def _register(self, b):
        self.live_bufs.append(b)
        if self.phase_bufs is not None:
            self.phase_bufs.append(b)

    def _name(self, name):
        self.uid += 1
        return "%s_%d" % (name, self.uid)

    def sbuf(self, name, shape, dtype=F32):
        esz = 2 if dtype == BF16 else 4
        nel = 1
        for d in shape[1:]:
            nel *= d
        nb = (nel * esz + 63) // 64 * 64
        off = self.top
        self.top += nb
        assert self.top <= self.limit, "arena overflow %s: top=%d limit=%d" % (name, self.top, self.limit)
        self.hiwater = max(self.hiwater, self.top)
        ap = self.arena[:, off // 4:(off + nb) // 4]
        if dtype != F32:
            ap = ap.bitcast(dtype)
        ap = ap[:, :nel]
        if len(shape) > 2:
            names = ["d%d" % i for i in range(len(shape) - 1)]
            pat = "p (%s) -> p %s" % (" ".join(names), " ".join(names))
            ap = ap.rearrange(pat, **{n: v for n, v in zip(names, shape[1:])})
        if shape[0] < 128:
            ap = ap[0:shape[0]]
        b = Buf(self, ap, name, "sbuf")
        b.off = off
        b.nbytes = nb
        self._register(b)
        return b

    @contextmanager
    def overlay(self, buf):
        st = (self.top, self.limit)
        self.top, self.limit = buf.off, buf.off + buf.nbytes
        try:
            yield
        finally:
            self.top, self.limit = st

    def dram(self, name, shape, dtype=F32, kind="Internal"):
        t = self.nc.dram_tensor(name, list(shape), dtype, kind=kind)
        b = Buf(self, t.ap(), name, "dram")
        self.live_bufs.append(b)
        return b

    def _need(self, eng, key, val, waits):
        if key == "eng_pe" and eng == "pe":
            return
        if self.waited[eng].get(key, 0) >= val:
            return
        self.waited[eng][key] = val
        waits[key] = max(waits.get(key, 0), val)

    def _deps(self, eng, reads, writes):
        waits = {}
        for b in reads:
            if b.last_w is not None:
                self._need(eng, b.last_w[0], b.last_w[1], waits)
            if b.space == "psum":
                for k, v in b.reads.items():
                    if k != "eng_" + eng:
                        self._need(eng, k, v, waits)
        for b in writes:
            if b.last_w is not None:
                self._need(eng, b.last_w[0], b.last_w[1], waits)
            for k, v in b.reads.items():
                self._need(eng, k, v, waits)
        return list(waits.items())

    dbg_budget = None

    def op(self, eng, fn, reads=(), writes=()):
        if self.dbg_budget is not None:
            if self.dbg_budget <= 0:
                return
            self.dbg_budget -= 1
        waits = self._deps(eng, reads, writes)
        self.cnt[eng] += 1
        key = "eng_" + eng
        val = self.cnt[eng]
        for b in reads:
            if b.reads.get(key, 0) < val:
                b.reads[key] = val
        for b in writes:
            b.last_w = (key, val)
            b.reads = {}
        self.ops[eng].append((waits, fn, key, 1))

    def dma(self, eng, out_ap, in_ap, dst, src, **kw):
        waits = self._deps(eng, [src], [dst])
        if dst.dsem is None:
            if self.dpool:
                dst.dsem = self.dpool.pop()
            else:
                self.ndsem += 1
                dst.dsem = "d%d" % self.ndsem
                self._sem(dst.dsem)
                self.dcnt[dst.dsem] = 0
            self.live_dsem.add(dst.dsem)
        key = dst.dsem
        self.dcnt[key] += 16
        val = self.dcnt[key]
        if src.reads.get(key, 0) < val:
            src.reads[key] = val
        dst.last_w = (key, val)
        dst.reads = {}

        def fn(e, out_ap=out_ap, in_ap=in_ap, kw=kw):
            return e.dma_start(out=out_ap, in_=in_ap, **kw)

        self.ops[eng].append((waits, fn, key, 16))

    def barrier(self):
        toks = [("eng_" + e, self.cnt[e]) for e in ENGS if self.cnt[e] > 0]
        toks += [(k, self.dcnt[k]) for k in sorted(self.live_dsem) if self.dcnt[k] > 0]
        for e in ENGS:
            waits = {}
            for k, v in toks:
                if k == "eng_" + e:
                    continue
                self._need(e, k, v, waits)
            if waits:
                self.ops[e].append((list(waits.items()), None, None, 0))
        for b in self.live_bufs:
            b.last_w = None
            b.reads = {}

    @contextmanager
    def phase(self):
        prev_b = self.phase_bufs
        mark = (self.top, self.limit)
        self.phase_bufs = []
        try:
            yield
        finally:
            self.barrier()
            for b in self.phase_bufs:
                if b.dsem is not None:
                    if b.dsem in self.live_dsem:
                        self.live_dsem.discard(b.dsem)
                        self.dpool.append(b.dsem)
                    b.dsem = None
            dead = set(id(b) for b in self.phase_bufs)
            self.live_bufs = [b for b in self.live_bufs if id(b) not in dead]
            self.top, self.limit = mark
            self.phase_bufs = prev_b

    def final_wait(self, eng, bufs):
        waits = {}
        for b in bufs:
            if b.last_w is not None:
                self._need(eng, b.last_w[0], b.last_w[1], waits)
        self.ops[eng].append((list(waits.items()), None, None, 0))

    def emit(self):
        nc = self.nc
        sems = self.sems

        def run(e, lst):
            for waits, fn, key, inc in lst:
                for k, v in waits:
                    e.wait_ge(sems[k], v)
                if fn is not None:
                    fn(e).then_inc(sems[key], inc)

        with nc.Block() as block:
            @block.tensor
            def _(e):
                run(e, self.ops["pe"])

            @block.vector
            def _(e):
                run(e, self.ops["dve"])

            @block.scalar
            def _(e):
                run(e, self.ops["act"])

            @block.gpsimd
            def _(e):
                run(e, self.ops["pool"])

            @block.sync
            def _(e):
                run(e, self.ops["sp"])
        self.gstack.close()
        return nc

    def mm(self, out, lhsT, rhs, start, stop, R, W):
        self.op("pe", lambda e: e.matmul(out, lhsT=lhsT, rhs=rhs, start=start, stop=stop), R, W)

    def act(self, out, in_, func, R, W, scale=1.0, bias=0.0):
        self.op("act", lambda e: e.activation(out=out, in_=in_, func=func, bias=bias, scale=scale), R, W)

    def tt(self, out, in0, in1, op, R, W, eng="dve"):
        self.op(eng, lambda e: e.tensor_tensor(out=out, in0=in0, in1=in1, op=op), R, W)

    def ts(self, out, in0, s1, s2, op0, op1, R, W, eng="dve"):
        if op1 is None:
            self.op(eng, lambda e: e.tensor_scalar(out=out, in0=in0, scalar1=s1, scalar2=None, op0=op0), R, W)
        else:
            self.op(eng, lambda e: e.tensor_scalar(out=out, in0=in0, scalar1=s1, scalar2=s2, op0=op0, op1=op1), R, W)

    def stt(self, out, in0, scalar, in1, op0, op1, R, W):
        self.op("dve", lambda e: e.scalar_tensor_tensor(out=out, in0=in0, scalar=scalar, in1=in1, op0=op0, op1=op1), R, W)

    def copy(self, out, in_, R, W, eng="dve"):
        if eng == "act":
            self.op("act", lambda e: e.copy(out=out, in_=in_), R, W)
        else:
            self.op(eng, lambda e: e.tensor_copy(out=out, in_=in_), R, W)

    def memset(self, ap, val, W, eng="dve"):
        self.op(eng, lambda e: e.memset(ap, val), (), W)


def _rope_tables():
    rows = S // 64
    row = np.repeat(np.arange(rows), 64).astype(np.float32)
    col = np.tile(np.arange(64), rows).astype(np.float32)
    inv = (10000.0 ** (-np.arange(0, 32, 2, dtype=np.float32) / 32)).astype(np.float32)
    ar = row[:, None] * inv
    ac = col[:, None] * inv
    C = np.zeros((128, S), np.float32)
    Sg = np.zeros((128, S), np.float32)
    for p in range(128):
        d = p % 64
        ang = ar if d < 32 else ac
        i = d % 16
        C[p] = np.cos(ang[:, i])
        sgn = -1.0 if (d % 32) < 16 else 1.0
        Sg[p] = sgn * np.sin(ang[:, i])
    return C, Sg


def _perm_rot(cols64):
    idx = np.arange(64)
    out = idx.copy()
    for base in (0, 32):
        out[base:base + 16] = idx[base + 16:base + 32]
        out[base + 16:base + 32] = idx[base:base + 16]
    return cols64[out]


def prep_shared(inp):
    f = np.float32
    sh = {}
    sh["mod_w"] = np.ascontiguousarray(inp["mod_w"], f)
    sh["mod_bT"] = np.ascontiguousarray(inp["mod_b"].reshape(2, 48, 128).transpose(2, 0, 1), f)
    g = np.stack([inp["norm_mix"], inp["norm_ffn"]], 0)
    sh["gT"] = np.ascontiguousarray(g.reshape(2, 2, 8, 128).transpose(3, 0, 1, 2), f)
    sh["finT"] = np.ascontiguousarray(inp["final_norm"].reshape(8, 128).T, f)
    sh["router_w"] = np.ascontiguousarray(inp["router_w"], f)
    sh["exp_w1"] = inp["exp_w1"]
    sh["exp_w3"] = inp["exp_w3"]
    sh["exp_w2"] = inp["exp_w2"]
    w_in = inp["mix_in"][0]
    q = w_in[:, 0:512]
    k = w_in[:, 512:640]
    v = w_in[:, 640:768]
    ul = w_in[:, 768:1280]
    gl = w_in[:, 1280:1792]
    qcols, qpcols = [], []
    for j in range(4):
        for h in (j, 4 + j):
            c64 = np.arange(h * 64, (h + 1) * 64)
            qcols.append(c64)
            qpcols.append(_perm_rot(c64))
    qcols = np.concatenate(qcols)
    qpcols = np.concatenate(qpcols)
    kp = np.concatenate([_perm_rot(np.arange(0, 64)), _perm_rot(np.arange(64, 128))])
    sh["w_in"] = np.ascontiguousarray(np.concatenate([q[:, qcols], q[:, qpcols], k, k[:, kp], v, ul, gl], 1), f)
    w_out = inp["mix_out"][0]
    arows = []
    for j in range(4):
        for h in (j, 4 + j):
            arows.append(np.arange(h * 64, (h + 1) * 64))
    arows = np.concatenate(arows + [np.arange(512, 1024)])
    sh["w_out"] = np.ascontiguousarray(w_out[arows], f)
    sh["sinkb"] = np.ascontiguousarray(np.broadcast_to(inp["attn_sink"][0][None, :], (128, 8)), f)
    C, Sg = _rope_tables()
    sh["ropeC"] = C
    sh["ropeS"] = Sg
    kq = np.arange(128)
    mP = (kq[:, None] >= kq[None, :]).astype(f)
    mN = (kq[:, None] <= kq[None, :]).astype(f)
    sh["maskP"] = np.ascontiguousarray(np.tile(mP, (1, 4)))
    sh["maskN"] = np.ascontiguousarray(np.tile(mN, (1, 4)))
    sh["convw"] = np.ascontiguousarray(inp["lru_conv_w"][0].reshape(4, 4, 128).transpose(2, 1, 0), f)
    sh["convb"] = np.ascontiguousarray(inp["lru_conv_b"][0].reshape(4, 128).T, f)

    def bd(w):
        o = np.zeros((128, 2, 4, 128), f)
        for d in range(2):
            for c in range(4):
                for hb in range(2):
                    o[hb * 64:(hb + 1) * 64, d, c, hb * 64:(hb + 1) * 64] = w[d, 2 * c + hb]
        return o
    sh["lru_wa"] = bd(inp["lru_wa"][0])
    sh["lru_wi"] = bd(inp["lru_wi"][0])

    def vec(a):
        return np.ascontiguousarray(a.reshape(2, 4, 128).transpose(2, 0, 1), f)
    sh["lru_ba"] = vec(inp["lru_ba"][0])
    sh["lru_bi"] = vec(inp["lru_bi"][0])
    sh["lru_lam"] = vec(inp["lru_lam"][0])
    sh["id16"] = np.eye(16, dtype=f)
    def fm(v1024):
        return np.asarray(v1024, f).reshape(8, 128).T
    vecs = [inp["rw_mu"][0][i] for i in range(6)] + [inp["rw_w0"][0][0], inp["rw_w0"][0][1], inp["rw_a0"][0][0], inp["rw_a0"][0][1],
            inp["rw_kk"][0], inp["rw_ka"][0], inp["rw_rk"][0].reshape(1024), inp["rw_gn_g"][0], inp["rw_gn_b"][0]]
    sh["rw_vec"] = np.ascontiguousarray(np.stack([fm(v) for v in vecs], 1), f)
    sh["ident"] = np.eye(128, dtype=f)
    bo = np.zeros((128, 128), f)
    bo[:64, :64] = 1.0
    bo[64:, 64:] = 1.0
    sh["blockones"] = bo
    mc = np.ones((128, 256), f)
    mc[:, ::64] = 0.0
    sh["maskC"] = mc
    ii = np.arange(64)
    lt = (ii[:, None] < ii[None, :]).astype(f)
    le = (ii[:, None] <= ii[None, :]).astype(f)
    def bdm(m):
        o = np.zeros((128, 128), f)
        o[:64, :64] = m
        o[64:, 64:] = m
        return o
    sh["rmask4"] = np.ascontiguousarray(np.stack([np.concatenate([bdm(lt), bdm(lt.T), bdm(lt), bdm(le)], 1),
                                                  np.concatenate([bdm(lt.T), bdm(lt), bdm(lt.T), bdm(le.T)], 1)], 0), f)
    sh["rmask1"] = np.ascontiguousarray(np.stack([bdm(le), bdm(le.T)], 0), f)
    sh["rw_w1cat"] = np.ascontiguousarray(np.concatenate([inp["rw_w1"][0][0], inp["rw_w1"][0][1]], 1), f)
    sh["rw_a1cat"] = np.ascontiguousarray(np.concatenate([inp["rw_a1"][0][0], inp["rw_a1"][0][1]], 1), f)
    sh["rw_g1"] = np.ascontiguousarray(inp["rw_g1"][0], f)
    sh["rw_rkv"] = np.ascontiguousarray(inp["rw_rkv"][0], f)
    sh["rw_w2cat"] = np.ascontiguousarray(np.concatenate([inp["rw_w2"][0][0], inp["rw_w2"][0][1]], 0), f)
    sh["rw_a2cat"] = np.ascontiguousarray(np.concatenate([inp["rw_a2"][0][0], inp["rw_a2"][0][1]], 0), f)
    sh["rw_g2"] = np.ascontiguousarray(inp["rw_g2"][0], f)
    sh["rw_wo"] = np.ascontiguousarray(inp["rw_wo"][0], f)
    return sh


def prep_core(inp, core, ns):
    f = np.float32
    b0 = core * ns
    pc = {}
    pc["xT"] = np.ascontiguousarray(inp["x"][b0:b0 + ns].transpose(0, 2, 1), f)
    pc["cxT"] = np.ascontiguousarray(inp["ctx"][b0:b0 + ns].transpose(0, 2, 1), f)
    cc = np.zeros((8, 1024), f)
    cc[:ns] = inp["c"][b0:b0 + ns]
    cc[4] = inp["c_ctx"]
    pc["cT"] = np.ascontiguousarray(cc.reshape(8, 8, 128).transpose(2, 1, 0), f)
    return pc


class K:
    pass


GELU_C = 1.5957691216057308


def bank3(bank, a, b):
    return bank[:].rearrange("p (a b) -> p a b", a=a)


def build(ns=4, taps=None, stop_after=None, skip_l0=False):
    taps = taps or []
    P = Prog()
    k = K()
    k.P = P
    k.tap_outs = {}
    k.stop_after = stop_after
    k.skip_l0 = skip_l0
    IN = "ExternalInput"
    dr = {}
    k.dr = dr

    def din(name, shape):
        dr[name] = P.dram(name, shape, F32, IN)
        return dr[name]

    din("xT", [ns, D, S]); din("cxT", [ns, D, LC]); din("cT", [128, KC, 8])
    din("mod_w", [2, D, 6 * D]); din("mod_bT", [128, 2, 48]); din("gT", [128, 2, 2, 8]); din("finT", [128, 8])
    din("router_w", [2, D, 16])
    din("exp_w1", [2, 16, D, D]); din("exp_w3", [2, 16, D, D]); din("exp_w2", [2, 16, D, D])
    din("w_in", [D, 2432]); din("w_out", [D, D]); din("sinkb", [128, 8])
    din("ropeC", [128, S]); din("ropeS", [128, S]); din("maskP", [128, 512]); din("maskN", [128, 512])
    din("convw", [128, 4, 4]); din("convb", [128, 4])
    din("lru_wa", [128, 2, 4, 128]); din("lru_wi", [128, 2, 4, 128])
    din("lru_ba", [128, 2, 4]); din("lru_bi", [128, 2, 4]); din("lru_lam", [128, 2, 4])
    din("id16", [16, 16])
    din("rw_vec", [128, 15, KC]); din("ident", [128, 128]); din("blockones", [128, 128]); din("maskC", [128, 256])
    din("rmask4", [2, 128, 512]); din("rmask1", [2, 128, 128])
    din("rw_w1cat", [D, 128]); din("rw_a1cat", [D, 128]); din("rw_g1", [D, 160]); din("rw_rkv", [3, D, D])
    din("rw_w2cat", [128, D]); din("rw_a2cat", [128, D]); din("rw_g2", [160, D]); din("rw_wo", [D, D])
    k.xspill = P.dram("xspill", [D, S], F32, "Internal")
    k.outT = P.dram("outT", [ns, D, S], F32, "ExternalOutput")

    def tap(name, buf, ap, shape):
        if name not in taps:
            return
        o = P.dram("tap_" + name, list(shape), F32, "ExternalOutput")
        k.tap_outs[name] = o
        P.dma("pool", o[:], ap, o, buf)
    k.tap = tap

    k.ones_f = P.sbuf("ones_f", [128, 128], F32)
    P.memset(k.ones_f[:], 1.0, [k.ones_f])
    k.ones_b = P.sbuf("ones_b", [128, 64], BF16)
    P.memset(k.ones_b[:], 1.0, [k.ones_b])
    k.epsb = P.sbuf("epsb", [128, 1], F32)
    P.memset(k.epsb[:], EPS, [k.epsb])
    k.gneps = P.sbuf("gneps", [128, 1], F32)
    P.memset(k.gneps[:], 64e-5, [k.gneps])
    k.modv = modv = P.sbuf("modv", [128, 2, 48, 8], F32)
    k.Amod = Amod = P.sbuf("Amod", [128, 2, 2, 8, 8], F32)
    gT = P.sbuf("gT", [128, 2, 2, 8], F32)
    k.finT = P.sbuf("finT", [128, 8], F32)
    P.dma("sp", gT[:], dr["gT"][:], gT, dr["gT"])
    P.dma("sp", k.finT[:], dr["finT"][:], k.finT, dr["finT"])

    with P.phase():
        cT = P.sbuf("cT", [128, KC, 8], F32)
        P.dma("sp", cT[:], dr["cT"][:], cT, dr["cT"])
        sT = P.sbuf("sT", [128, KC, 8], F32)
        P.act(sT[:], cT[:], AF.Silu, [cT], [sT])
        mbT = P.sbuf("mbT", [128, 2, 48], F32)
        P.dma("sp", mbT[:], dr["mod_bT"][:], mbT, dr["mod_bT"])
        wm = [P.sbuf("wm%d" % i, [128, KC, 512], F32) for i in range(2)]
        it = 0
        for l in range(2):
            psm = P.banks[l]
            psm3 = psm[:, 0:384].rearrange("p (a b) -> p a b", a=48)
            for cb in range(12):
                w = wm[it % 2]
                it += 1
                src = dr["mod_w"][l].rearrange("(kc p) n -> p kc n", p=128)[:, :, cb * 512:(cb + 1) * 512]
                P.dma("sp", w[:], src, w, dr["mod_w"])
                for o4 in range(4):
                    oc = cb * 4 + o4
                    for kc in range(KC):
                        P.mm(psm3[:, oc, :], w[:, kc, o4 * 128:(o4 + 1) * 128], sT[:, kc, :], kc == 0, kc == KC - 1,
                             [w, sT], [psm])
            P.tt(modv[:, l], psm3, mbT[:, l, :, None].to_broadcast([128, 48, 8]), ALU.add, [psm, mbT], [modv])
            for kind in range(2):
                sc = modv[:, l, (1 + 3 * kind) * 8:(2 + 3 * kind) * 8, :]
                P.ts(Amod[:, l, kind], sc, 1.0, None, ALU.add, None, [modv], [Amod])
                P.tt(Amod[:, l, kind], Amod[:, l, kind], gT[:, kind, l, :, None].to_broadcast([128, 8, 8]), ALU.mult,
                     [Amod, gT], [Amod])
        tap("modv", modv, modv[:], [128, 2, 48, 8])

    for s in range(ns):
        sample_program(k, s)

    outs = [k.outT] + list(k.tap_outs.values())
    P.final_wait("sp", outs)
    P.final_wait("pool", outs)
    print("[build] arena hiwater %d / %d bytes, dma sems %d, instr %s" % (P.hiwater, ARENA_BYTES, P.ndsem,
                                                                        {e: len(P.ops[e]) for e in ENGS}))
    nc = P.emit()
    return nc, k


def mvec(k, l, j, kc, si):
    return k.modv[:, l, j * 8 + kc, si:si + 1]


def x_src(dr, s, b):
    t0, nb = BLOCKS[b]
    if b == 0:
        return dr["cxT"][s].rearrange("(kc p) t -> p kc t", p=128), dr["cxT"]
    return dr["xT"][s].rearrange("(kc p) t -> p kc t", p=128)[:, :, t0 - LC:t0 - LC + nb], dr["xT"]


class NormScratch:
    def __init__(self, k, bank):
        P = k.P
        self.sq = P.sbuf("sq", [128, KC, 512], F32)
        self.rs = P.sbuf("rs", [128, 512], F32)
        self.ps = bank


def rms_block(k, sc, x_ap, xbuf, nb):
    P = k.P
    P.act(sc.sq[:, :, :nb], x_ap, AF.Square, [xbuf], [sc.sq])
    for kc in range(KC):
        P.mm(sc.ps[:, :nb], k.ones_f[:], sc.sq[:, kc, :nb], kc == 0, kc == KC - 1, [k.ones_f, sc.sq], [sc.ps])
    P.act(sc.rs[:, :nb], sc.ps[:, :nb], AF.Ln, [sc.ps, k.epsb], [sc.rs], scale=1.0 / D, bias=k.epsb[:, 0:1])
    P.act(sc.rs[:, :nb], sc.rs[:, :nb], AF.Exp, [sc.rs], [sc.rs], scale=-0.5)
    P.tt(sc.sq[:, :, :nb], x_ap, sc.rs[:, None, :nb].to_broadcast([128, KC, nb]), ALU.mult, [xbuf, sc.rs], [sc.sq])
    return sc.sq


class WRing:
    def __init__(self, k, n):
        self.k = k
        self.slots = [k.P.sbuf("wslot%d" % i, [128, KC, 512], BF16) for i in range(n)]
        self.i = 0

    def load(self, w2d, wbuf, ncols=512):
        sl = self.slots[self.i % len(self.slots)]
        self.i += 1
        src = w2d.rearrange("(kc p) n -> p kc n", p=128)
        self.k.P.dma("pool", sl[:, :, :ncols], src, sl, wbuf)
        return sl


def sample_program(k, s):
    P = k.P
    dr = k.dr
    tap = k.tap if s == 0 else (lambda *a, **kw: None)
    SI = lambda b: 4 if b == 0 else s
    with P.phase():
        hT = P.sbuf("hT", [128, KC, T], BF16)
        xT = P.sbuf("xT", [128, KC, T], F32)
        if k.skip_l0:
            for b, (t0, nb) in enumerate(BLOCKS):
                src, sb_ = x_src(dr, s, b)
                P.dma("sp", xT[:, :, t0:t0 + nb], src, xT, sb_)
            layer1_and_out(k, s, hT, xT, tap)
            return
        with P.phase(), P.overlay(xT):
            sc = NormScratch(k, P.banks[0])
            xb = [P.sbuf("xb%d" % i, [128, KC, 512], F32) for i in range(2)]
            for b, (t0, nb) in enumerate(BLOCKS):
                xt = xb[b % 2]
                src, sb_ = x_src(dr, s, b)
                P.dma("sp", xt[:, :, :nb], src, xt, sb_)
                xn = rms_block(k, sc, xt[:, :, :nb], xt, nb)
                for kc in range(KC):
                    P.ts(hT[:, kc, t0:t0 + nb], xn[:, kc, :nb], k.Amod[:, 0, 0, kc, SI(b):SI(b) + 1],
                         mvec(k, 0, 0, kc, SI(b)), ALU.mult, ALU.add, [xn, k.Amod, k.modv], [hT])
            tap("L0_h", hT, hT[:], [128, KC, T])
        if k.stop_after == "L0A":
            return
        with P.phase():
            attnT = P.sbuf("attnT", [128, 4, T], BF16)
            lruT = P.sbuf("lruT", [128, 4, T], BF16)
            wr = WRing(k, 3)
            with P.phase(), P.overlay(xT):
                l0_attention(k, s, hT, attnT, wr, tap)
            if k.stop_after == "L0B":
                return
            with P.phase(), P.overlay(xT):
                l0_lru(k, s, hT, lruT, wr, tap)
            if k.stop_after == "L0C":
                return
            with P.phase():
                xb = [P.sbuf("xb%d" % i, [128, KC, 512], F32) for i in range(2)]
                wo = [wr.load(dr["w_out"][:, h * 512:(h + 1) * 512], dr["w_out"]) for h in range(2)]
                pi = 0
                for b, (t0, nb) in enumerate(BLOCKS):
                    xt = xb[b % 2]
                    src, sb_ = x_src(dr, s, b)
                    P.dma("sp", xt[:, :, :nb], src, xt, sb_)
                    for oc in range(KC):
                        ps = P.banks[pi % 4]
                        pi += 1
                        w = wo[oc // 4]
                        c0 = (oc % 4) * 128
                        for j in range(4):
                            P.mm(ps[:, :nb], w[:, j, c0:c0 + 128], attnT[:, j, t0:t0 + nb], j == 0, False, [w, attnT], [ps])
                        for c in range(4):
                            P.mm(ps[:, :nb], w[:, 4 + c, c0:c0 + 128], lruT[:, c, t0:t0 + nb], False, c == 3, [w, lruT], [ps])
                        P.stt(xT[:, oc, t0:t0 + nb], ps[:, :nb], mvec(k, 0, 2, oc, SI(b)), xt[:, oc, :nb], ALU.mult, ALU.add,
                              [ps, k.modv, xt], [xT])
                tap("L0_xmix", xT, xT[:], [128, KC, T])
        if k.stop_after == "L0D":
            return
        moe_layer(k, s, 0, hT, xT, True, tap)
        tap("L0_x", xT, xT[:], [128, KC, T])
        if k.stop_after == "L0F":
            return
        layer1_and_out(k, s, hT, xT, tap)


def layer1_and_out(k, s, hT, xT, tap):
    P = k.P
    layer1_mixer(k, s, hT, xT, tap)
    if k.stop_after in ("L1A", "L1B", "L1D", "L1L", "L1P"):
        return
    moe_layer(k, s, 1, hT, xT, False, tap)
    tap("L1_x", xT, xT[:], [128, KC, T])
    with P.phase():
        sc = NormScratch(k, P.banks[0])
        ob = [P.sbuf("ob%d" % i, [128, KC, 512], F32) for i in range(2)]
        for i, (t0, nb) in enumerate(LAT_BLOCKS):
            xn = rms_block(k, sc, xT[:, :, t0:t0 + nb], xT, nb)
            o = ob[i % 2]
            P.tt(o[:], xn[:, :, :nb], k.finT[:, :, None].to_broadcast([128, KC, nb]), ALU.mult, [xn, k.finT], [o])
            P.dma("sp", k.outT[s].rearrange("(kc p) t -> p kc t", p=128)[:, :, t0 - LC:t0 - LC + nb], o[:], k.outT, o)


def l0_attention(k, s, hT, qT, wr, tap):
    P = k.P
    dr = k.dr
    kT = P.sbuf("kT", [128, T], BF16)
    Vtok = P.sbuf("Vtok", [128, 18, 128], BF16)
    ropeC = P.sbuf("ropeC", [128, 512], F32)
    ropeS = P.sbuf("ropeS", [128, 512], F32)
    t1 = P.sbuf("rt1", [128, 512], F32)
    t2 = P.sbuf("rt2", [128, 512], F32)
    maskP = P.sbuf("maskP", [128, 4, 128], BF16)
    maskN = P.sbuf("maskN", [128, 4, 128], BF16)
    sinkb = P.sbuf("sinkb", [128, 8], F32)
    esink = P.sbuf("esink", [128, 8], F32)
    P.dma("pool", maskP[:], dr["maskP"][:].rearrange("p (a b) -> p a b", a=4), maskP, dr["maskP"])
    P.dma("pool", maskN[:], dr["maskN"][:].rearrange("p (a b) -> p a b", a=4), maskN, dr["maskN"])
    P.dma("sp", sinkb[:], dr["sinkb"][:], sinkb, dr["sinkb"])
    P.act(esink[:], sinkb[:], AF.Exp, [sinkb], [esink])
    wq = wr.load(dr["w_in"][:, 0:512], dr["w_in"])
    wqp = wr.load(dr["w_in"][:, 512:1024], dr["w_in"])
    wkv = wr.load(dr["w_in"][:, 1024:1408], dr["w_in"], 384)
    pi = 0
    for b, (t0, nb) in enumerate(BLOCKS):
        if b > 0:
            P.dma("sp", ropeC[:, :nb], dr["ropeC"][:, t0 - LC:t0 - LC + nb], ropeC, dr["ropeC"])
            P.dma("sp", ropeS[:, :nb], dr["ropeS"][:, t0 - LC:t0 - LC + nb], ropeS, dr["ropeS"])
        for j in range(5):
            w, wp, c0 = (wq, wqp, j * 128) if j < 4 else (wkv, wkv, 0)
            dst = qT[:, j, t0:t0 + nb] if j < 4 else kT[:, t0:t0 + nb]
            dbuf = qT if j < 4 else kT
            ps = P.banks[pi % 4]
            pi += 1
            for kc in range(KC):
                P.mm(ps[:, :nb], w[:, kc, c0:c0 + 128], hT[:, kc, t0:t0 + nb], kc == 0, kc == KC - 1, [w, hT], [ps])
            if b == 0:
                P.copy(dst, ps[:, :nb], [ps], [dbuf], eng="act")
            else:
                c1 = c0 if j < 4 else 128
                ps2 = P.banks[pi % 4]
                pi += 1
                for kc in range(KC):
                    P.mm(ps2[:, :nb], wp[:, kc, c1:c1 + 128], hT[:, kc, t0:t0 + nb], kc == 0, kc == KC - 1, [wp, hT], [ps2])
                P.tt(t1[:, :nb], ps[:, :nb], ropeC[:, :nb], ALU.mult, [ps, ropeC], [t1])
                P.tt(t2[:, :nb], ps2[:, :nb], ropeS[:, :nb], ALU.mult, [ps2, ropeS], [t2])
                P.tt(dst, t1[:, :nb], t2[:, :nb], ALU.add, [t1, t2], [dbuf])
        for tt_ in range(t0 // 128, (t0 + nb) // 128):
            ps = P.banks[pi % 4]
            pi += 1
            for kc in range(KC):
                P.mm(ps[:, 0:128], hT[:, kc, tt_ * 128:(tt_ + 1) * 128], wkv[:, kc, 256:384], kc == 0, kc == KC - 1,
                     [hT, wkv], [ps])
            P.copy(Vtok[:, tt_, :], ps[:, 0:128], [ps], [Vtok], eng="act")
    tap("L0_q", qT, qT[:], [128, 4, T])
    tap("L0_k", kT, kT[:], [128, T])
    tap("L0_v", Vtok, Vtok[:], [128, 18, 128])
    pT = [P.sbuf("pT%d" % i, [128, 4, 128], BF16) for i in range(2)]
    den = P.sbuf("den", [128, 4, 128], F32)
    ps_s = [P.banks[4], P.banks[5]]
    ps_num = P.banks[6]
    ps_den = P.banks[7]
    it = 0
    for qt in range(18):
        if qt < 2:
            keys = [(0, None), (1, None)]
        else:
            i = qt - 2
            keys = [(0, None), (1, None)]
            if i > 0:
                keys.append((qt - 1, maskP))
            keys.append((qt, None))
            if i < 15:
                keys.append((qt + 1, maskN))
        for g in range(2):
            pr = slice(g * 64, (g + 1) * 64)
            num3 = ps_num[pr, :].rearrange("p (a b) -> p a b", a=4)
            den3 = ps_den[pr, :].rearrange("p (a b) -> p a b", a=4)
            for idx, (kt, mk) in enumerate(keys):
                pss = ps_s[it % 2]
                pt = pT[it % 2]
                it += 1
                P.mm(pss[:].rearrange("p (a b) -> p a b", a=4), kT[pr, kt * 128:(kt + 1) * 128],
                     qT[pr, :, qt * 128:(qt + 1) * 128], True, True, [kT, qT], [pss])
                P.act(pt[:], pss[:].rearrange("p (a b) -> p a b", a=4), AF.Exp, [pss], [pt], scale=0.125)
                if mk is not None:
                    P.tt(pt[:], pt[:], mk[:], ALU.mult, [pt, mk], [pt])
                first, last = idx == 0, idx == len(keys) - 1
                P.mm(num3, Vtok[:, kt, pr], pt[:], first, last, [Vtok, pt], [ps_num])
                P.mm(den3, k.ones_b[:, 0:64], pt[:], first, last, [k.ones_b, pt], [ps_den])
            P.tt(den[pr], den3, esink[pr, g * 4:(g + 1) * 4, None].to_broadcast([64, 4, 128]), ALU.add,
                 [ps_den, esink], [den])
            P.op("dve", lambda e, pr=pr: e.reciprocal(out=den[pr], in_=den[pr]), [den], [den])
            P.tt(qT[pr, :, qt * 128:(qt + 1) * 128], num3, den[pr], ALU.mult, [ps_num, den], [qT])
    tap("L0_attn", qT, qT[:], [128, 4, T])


def l0_lru(k, s, hT, lruT, wr, tap):
    P = k.P
    dr = k.dr
    ul = P.sbuf("ul", [128, T], F32)
    u = P.sbuf("u", [128, T], F32)
    ub = P.sbuf("ub", [128, T], BF16)
    tmp = [P.sbuf("lt%d" % i, [128, 512], F32) for i in range(7)]
    t_r, t_i, t_a, t_y, t_b, t_h0, t_h1 = tmp
    convw = P.sbuf("convw", [128, 4, 4], F32)
    convb = P.sbuf("convb", [128, 4], F32)
    wa = P.sbuf("wa", [128, 2, 4, 128], BF16)
    wi = P.sbuf("wi", [128, 2, 4, 128], BF16)
    ba = P.sbuf("ba", [128, 2, 4], F32)
    bi = P.sbuf("bi", [128, 2, 4], F32)
    lam = P.sbuf("lam", [128, 2, 4], F32)
    kap = P.sbuf("kap", [128, 2, 4], F32)
    kap2 = P.sbuf("kap2", [128, 2, 4], F32)
    zz = P.sbuf("zz", [128, 2, 4], F32)
    zp = P.sbuf("zp", [128, 2, 4], F32)
    for nm, t_ in (("convw", convw), ("convb", convb), ("lru_ba", ba), ("lru_bi", bi), ("lru_lam", lam)):
        P.dma("sp", t_[:], dr[nm][:], t_, dr[nm])
    P.dma("pool", wa[:], dr["lru_wa"][:], wa, dr["lru_wa"])
    P.dma("pool", wi[:], dr["lru_wi"][:], wi, dr["lru_wi"])
    P.act(zz[:], lam[:], AF.Exp, [lam], [zz], scale=-1.0)
    P.ts(zp[:], zz[:], -0.2, 0.25, ALU.mult, ALU.add, [zz], [zp])
    for cst in (1.0 / 3.0, 0.5, 1.0):
        P.tt(zp[:], zp[:], zz[:], ALU.mult, [zp, zz], [zp])
        P.ts(zp[:], zp[:], -1.0, cst, ALU.mult, ALU.add, [zp], [zp])
    P.tt(zp[:], zp[:], zz[:], ALU.mult, [zp, zz], [zp])
    P.ts(kap[:], zp[:], -8.0, None, ALU.mult, None, [zp], [kap])
    P.ts(kap2[:], zp[:], -16.0, None, ALU.mult, None, [zp], [kap2])
    wul = wr.load(dr["w_in"][:, 1408:1920], dr["w_in"])
    wgl = wr.load(dr["w_in"][:, 1920:2432], dr["w_in"])
    pi = 0
    for c in range(4):
        for b, (t0, nb) in enumerate(BLOCKS):
            ps = P.banks[pi % 4]
            pi += 1
            for kc in range(KC):
                P.mm(ps[:, :nb], wul[:, kc, c * 128:(c + 1) * 128], hT[:, kc, t0:t0 + nb], kc == 0, kc == KC - 1, [wul, hT], [ps])
            P.copy(ul[:, t0:t0 + nb], ps[:, :nb], [ps], [ul], eng="act")
        P.act(u[:], ul[:], AF.Identity, [ul, convw, convb], [u], scale=convw[:, c, 2:3], bias=convb[:, c:c + 1])
        for (a_, e_) in ((0, LC), (LC, T)):
            P.stt(u[:, a_ + 2:e_], ul[:, a_:e_ - 2], convw[:, c, 0:1], u[:, a_ + 2:e_], ALU.mult, ALU.add, [ul, convw, u], [u])
            P.stt(u[:, a_ + 1:e_], ul[:, a_:e_ - 1], convw[:, c, 1:2], u[:, a_ + 1:e_], ALU.mult, ALU.add, [ul, convw, u], [u])
            P.stt(u[:, a_:e_ - 1], ul[:, a_ + 1:e_], convw[:, c, 3:4], u[:, a_:e_ - 1], ALU.mult, ALU.add, [ul, convw, u], [u])
        P.copy(ub[:], u[:], [u], [ub], eng="act")
        if c == 0:
            tap("L0_u0", u, u[:], [128, T])
        hsum = ul
        for d in range(2):
            order = [0, 1, 2, 3, 4] if d == 0 else [0, 4, 3, 2, 1]
            prev_h = None
            for oi, b in enumerate(order):
                t0, nb = BLOCKS[b]
                psr = P.banks[pi % 4]
                pi += 1
                psi = P.banks[pi % 4]
                pi += 1
                P.mm(psr[:, :nb], wa[:, d, c, :], ub[:, t0:t0 + nb], True, True, [wa, ub], [psr])
                P.mm(psi[:, :nb], wi[:, d, c, :], ub[:, t0:t0 + nb], True, True, [wi, ub], [psi])
                P.act(t_r[:, :nb], psr[:, :nb], AF.Sigmoid, [psr, ba], [t_r], bias=ba[:, d, c:c + 1])
                P.act(t_i[:, :nb], psi[:, :nb], AF.Sigmoid, [psi, bi], [t_i], bias=bi[:, d, c:c + 1])
                P.act(t_a[:, :nb], t_r[:, :nb], AF.Exp, [t_r, kap], [t_a], scale=kap[:, d, c:c + 1])
                P.act(t_y[:, :nb], t_r[:, :nb], AF.Exp, [t_r, kap2], [t_y], scale=kap2[:, d, c:c + 1])
                P.ts(t_y[:, :nb], t_y[:, :nb], -1.0, 1.0, ALU.mult, ALU.add, [t_y], [t_y])
                P.ts(t_y[:, :nb], t_y[:, :nb], 1e-30, None, ALU.max, None, [t_y], [t_y])
                P.act(t_y[:, :nb], t_y[:, :nb], AF.Ln, [t_y], [t_y])
                P.act(t_y[:, :nb], t_y[:, :nb], AF.Exp, [t_y], [t_y], scale=0.5)
                P.tt(t_b[:, :nb], t_y[:, :nb], t_i[:, :nb], ALU.mult, [t_y, t_i], [t_b])
                P.tt(t_b[:, :nb], t_b[:, :nb], u[:, t0:t0 + nb], ALU.mult, [t_b, u], [t_b])
                if d == 0:
                    init = 0.0 if oi == 0 else hsum[:, t0 - 1:t0]
                    P.op("dve", lambda e, t0=t0, nb=nb, init=init: e.tensor_tensor_scan(
                        out=hsum[:, t0:t0 + nb], data0=t_a[:, :nb], data1=t_b[:, :nb], initial=init,
                        op0=ALU.mult, op1=ALU.add), [t_a, t_b, hsum], [hsum])
                else:
                    th = t_h0 if oi % 2 == 0 else t_h1
                    if oi == 0:
                        init, rd = 0.0, []
                    else:
                        init, rd = prev_h[1], [prev_h[0]]
                    P.op("dve", lambda e, nb=nb, init=init, th=th: e.tensor_tensor_scan(
                        out=th[:, :nb][:, ::-1], data0=t_a[:, :nb][:, ::-1], data1=t_b[:, :nb][:, ::-1], initial=init,
                        op0=ALU.mult, op1=ALU.add), [t_a, t_b] + rd, [th])
                    prev_h = (th, th[:, 0:1])
                    P.tt(hsum[:, t0:t0 + nb], hsum[:, t0:t0 + nb], th[:, :nb], ALU.add, [hsum, th], [hsum])
        if c == 0:
            tap("L0_hsum0", hsum, hsum[:], [128, T])
        for b, (t0, nb) in enumerate(BLOCKS):
            ps = P.banks[pi % 4]
            pi += 1
            for kc in range(KC):
                P.mm(ps[:, :nb], wgl[:, kc, c * 128:(c + 1) * 128], hT[:, kc, t0:t0 + nb], kc == 0, kc == KC - 1, [wgl, hT], [ps])
            P.copy(t_r[:, :nb], ps[:, :nb], [ps], [t_r], eng="act")
            P.act(t_i[:, :nb], ps[:, :nb], AF.Square, [ps], [t_i])
            P.ts(t_i[:, :nb], t_i[:, :nb], 0.044715, 1.0, ALU.mult, ALU.add, [t_i], [t_i])
            P.tt(t_i[:, :nb], t_i[:, :nb], t_r[:, :nb], ALU.mult, [t_i, t_r], [t_i])
            P.act(t_i[:, :nb], t_i[:, :nb], AF.Sigmoid, [t_i], [t_i], scale=GELU_C)
            P.tt(t_i[:, :nb], t_i[:, :nb], t_r[:, :nb], ALU.mult, [t_i, t_r], [t_i])
            P.tt(lruT[:, c, t0:t0 + nb], t_i[:, :nb], hsum[:, t0:t0 + nb], ALU.mult, [t_i, hsum], [lruT])
    tap("L0_lru", lruT, lruT[:], [128, 4, T])


def moe_layer(k, s, l, hT, xT, with_ctx, tap):
    P = k.P
    dr = k.dr
    SI = lambda b: 4 if b == 0 else s
    blocks = list(enumerate(BLOCKS)) if with_ctx else list(enumerate(BLOCKS))[1:]
    with P.phase():
        gateT = P.sbuf("gateT", [16, T], F32)
        with P.phase():
            sc = NormScratch(k, P.banks[0])
            hf = P.sbuf("hf", [128, KC, 512], F32)
            wrt = P.sbuf("wrt", [128, KC, 16], F32)
            ones16 = P.sbuf("ones16", [16, 16], F32)
            ex = P.sbuf("ex", [16, 512], F32)
            P.memset(ones16[:], 1.0, [ones16])
            P.dma("sp", wrt[:], dr["router_w"][l].rearrange("(kc p) e -> p kc e", p=128), wrt, dr["router_w"])
            psl = P.banks[1]
            pss = P.banks[2]
            for b, (t0, nb) in blocks:
                xn = rms_block(k, sc, xT[:, :, t0:t0 + nb], xT, nb)
                for kc in range(KC):
                    P.ts(hf[:, kc, :nb], xn[:, kc, :nb], k.Amod[:, l, 1, kc, SI(b):SI(b) + 1], mvec(k, l, 3, kc, SI(b)),
                         ALU.mult, ALU.add, [xn, k.Amod, k.modv], [hf])
                P.copy(hT[:, :, t0:t0 + nb], hf[:, :, :nb], [hf], [hT], eng="act")
                for kc in range(KC):
                    P.mm(psl[0:16, :nb], wrt[:, kc, :], hf[:, kc, :nb], kc == 0, kc == KC - 1, [wrt, hf], [psl])
                P.act(ex[:, :nb], psl[0:16, :nb], AF.Exp, [psl], [ex])
                P.mm(pss[0:16, :nb], ones16[:], ex[:, :nb], True, True, [ones16, ex], [pss])
                P.op("dve", lambda e, nb=nb: e.reciprocal(out=sc.rs[0:16, :nb], in_=pss[0:16, :nb]), [pss], [sc.rs])
                P.tt(gateT[:, t0:t0 + nb], ex[:, :nb], sc.rs[0:16, :nb], ALU.mult, [ex, sc.rs], [gateT])
            tap("L%d_aff" % l, gateT, gateT[:], [16, T])
            work = P.sbuf("work", [16, S], F32)
            m8 = P.sbuf("m8", [16, 8], F32)
            segs = [(LC, S, 8)] + ([(0, LC, 8)] if with_ctx else [])
            for (a0, n, _) in segs:
                cap = 2 * n // 16
                P.copy(work[:, :n], gateT[:, a0:a0 + n], [gateT], [work])
                for r in range(cap // 8):
                    P.op("dve", lambda e, n=n: e.max(out=m8[:], in_=work[:, :n]), [work], [m8])
                    if r < cap // 8 - 1:
                        P.op("dve", lambda e, n=n: e.match_replace(out=work[:, :n], in_to_replace=m8[:], in_values=work[:, :n],
                                                                   imm_value=-1.0), [work, m8], [work])
                P.stt(gateT[:, a0:a0 + n], gateT[:, a0:a0 + n], m8[:, 7:8], gateT[:, a0:a0 + n], ALU.is_ge, ALU.mult,
                      [gateT, m8], [gateT])
            tap("L%d_gate" % l, gateT, gateT[:], [16, T])
            tap("L%d_hffn" % l, hT, hT[:], [128, KC, T])
        with P.phase():
            wr = WRing(k, 4)
            id16 = P.sbuf("id16", [16, 16], F32)
            P.dma("sp", id16[:], dr["id16"][:], id16, dr["id16"])
            hid = P.sbuf("hid", [128, KC, T], BF16)
            gb = [P.sbuf("gb%d" % i, [128, 512], F32) for i in range(2)]
            s1 = [P.sbuf("s1_%d" % i, [128, 512], F32) for i in range(2)]
            pi = 0
            it = 0
            for e in range(16):
                for h in range(2):
                    w1 = wr.load(dr["exp_w1"][l, e][:, h * 512:(h + 1) * 512], dr["exp_w1"])
                    w3 = wr.load(dr["exp_w3"][l, e][:, h * 512:(h + 1) * 512], dr["exp_w3"])
                    for b, (t0, nb) in blocks:
                        g_ = gb[it % 2]
                        it += 1
                        psg = P.banks[6 + it % 2]
                        P.mm(psg[:, :nb], id16[:, e:e + 1].to_broadcast([16, 128]), gateT[:, t0:t0 + nb], True, True,
                             [id16, gateT], [psg])
                        P.copy(g_[:, :nb], psg[:, :nb], [psg], [g_], eng="act")
                        for f4 in range(4):
                            f = h * 4 + f4
                            ps1 = P.banks[pi % 6]
                            ps3 = P.banks[(pi + 1) % 6]
                            pi += 2
                            st = s1[(pi // 2) % 2]
                            for kc in range(KC):
                                P.mm(ps1[:, :nb], w1[:, kc, f4 * 128:(f4 + 1) * 128], hT[:, kc, t0:t0 + nb], kc == 0, kc == KC - 1,
                                     [w1, hT], [ps1])
                            for kc in range(KC):
                                P.mm(ps3[:, :nb], w3[:, kc, f4 * 128:(f4 + 1) * 128], hT[:, kc, t0:t0 + nb], kc == 0, kc == KC - 1,
                                     [w3, hT], [ps3])
                            P.act(st[:, :nb], ps1[:, :nb], AF.Silu, [ps1], [st])
                            P.tt(st[:, :nb], st[:, :nb], ps3[:, :nb], ALU.mult, [st, ps3], [st])
                            P.tt(hid[:, f, t0:t0 + nb], st[:, :nb], g_[:, :nb], ALU.mult, [st, g_], [hid])
                for h2 in range(2):
                    w2 = wr.load(dr["exp_w2"][l, e][:, h2 * 512:(h2 + 1) * 512], dr["exp_w2"])
                    for b, (t0, nb) in blocks:
                        for o4 in range(4):
                            oc = h2 * 4 + o4
                            psy = P.banks[pi % 6]
                            pi += 1
                            for f in range(KC):
                                P.mm(psy[:, :nb], w2[:, f, o4 * 128:(o4 + 1) * 128], hid[:, f, t0:t0 + nb], f == 0, f == KC - 1,
                                     [w2, hid], [psy])
                            P.stt(xT[:, oc, t0:t0 + nb], psy[:, :nb], mvec(k, l, 5, oc, SI(b)), xT[:, oc, t0:t0 + nb],
                                  ALU.mult, ALU.add, [psy, k.modv, xT], [xT])


RB = [(0, 256)] + [(256 + 256 * i, 256) for i in range(8)]
V_MU, V_W0, V_A0, V_KK, V_KA, V_RK, V_GG, V_GB = 0, 6, 8, 10, 11, 12, 13, 14
NEG_EXP_HALF = -0.6065306597126334


def layer1_mixer(k, s, hT, xT, tap):
    P = k.P
    dr = k.dr
    SI = lambda b: 4 if b == 0 else s
    with P.phase():
        sc = NormScratch(k, P.banks[0])
        for b, (t0, nb) in enumerate(BLOCKS):
            xn = rms_block(k, sc, xT[:, :, t0:t0 + nb], xT, nb)
            for kc in range(KC):
                P.ts(hT[:, kc, t0:t0 + nb], xn[:, kc, :nb], k.Amod[:, 1, 0, kc, SI(b):SI(b) + 1],
                     mvec(k, 1, 0, kc, SI(b)), ALU.mult, ALU.add, [xn, k.Amod, k.modv], [hT])
        P.dma("sp", k.xspill[:].rearrange("(kc p) t -> p kc t", p=128), xT[:, :, LC:T], k.xspill, xT)
        tap("L1_h", hT, hT[:], [128, KC, T])
    if k.stop_after == "L1A":
        return
    with P.phase():
        outT = P.sbuf("outT", [128, KC, S], BF16)
        with P.phase():
            rwkv_core(k, s, hT, xT, outT, tap)
        if k.stop_after in ("L1B", "L1L", "L1P"):
            return
        with P.phase():
            wr = WRing(k, 2)
            wo = [wr.load(dr["rw_wo"][:, h * 512:(h + 1) * 512], dr["rw_wo"]) for h in range(2)]
            P.dma("sp", xT[:, :, LC:T], k.xspill[:].rearrange("(kc p) t -> p kc t", p=128), xT, k.xspill)
            pi = 0
            for b, (t0, nb) in list(enumerate(BLOCKS))[1:]:
                for oc in range(KC):
                    ps = P.banks[pi % 4]
                    pi += 1
                    w = wo[oc // 4]
                    c0 = (oc % 4) * 128
                    for c in range(KC):
                        P.mm(ps[:, :nb], w[:, c, c0:c0 + 128], outT[:, c, t0 - LC:t0 - LC + nb], c == 0, c == KC - 1,
                             [w, outT], [ps])
                    P.stt(xT[:, oc, t0:t0 + nb], ps[:, :nb], mvec(k, 1, 2, oc, s), xT[:, oc, t0:t0 + nb], ALU.mult, ALU.add,
                          [ps, k.modv, xT], [xT])
            tap("L1_xmix", xT, xT[:], [128, KC, T])


def rwkv_core(k, s, hT, xT, outT, tap):
    P = k.P
    dr = k.dr
    with P.overlay(xT):
        xxT = P.sbuf("xxT", [128, KC, T], BF16)
        lw1 = P.sbuf("lw1", [128, T], BF16)
        la1 = P.sbuf("la1", [128, T], BF16)
        lg1 = P.sbuf("lg1", [128, T], BF16)
        lg1b = P.sbuf("lg1b", [32, T], BF16)
        y_acc = P.sbuf("y_acc", [128, S], F32)
        bon = P.sbuf("bon", [128, S], F32)
    vec = P.sbuf("rwvec", [128, 15, KC], F32)
    P.dma("sp", vec[:], dr["rw_vec"][:], vec, dr["rw_vec"])
    omk = P.sbuf("omk", [128, KC], F32)
    P.ts(omk[:], vec[:, V_KA, :], -1.0, 1.0, ALU.mult, ALU.add, [vec], [omk])
    identb = P.sbuf("identb", [128, 128], BF16)
    P.dma("pool", identb[:], dr["ident"][:], identb, dr["ident"])
    bones = P.sbuf("bones", [128, 128], F32)
    P.dma("sp", bones[:], dr["blockones"][:], bones, dr["blockones"])
    maskC = P.sbuf("maskC", [128, 256], F32)
    P.dma("sp", maskC[:], dr["maskC"][:], maskC, dr["maskC"])
    m4 = [P.sbuf("m4_%d" % d, [128, 512], BF16) for d in range(2)]
    m1 = [P.sbuf("m1_%d" % d, [128, 128], BF16) for d in range(2)]
    for d in range(2):
        P.dma("pool", m4[d][:], dr["rmask4"][d], m4[d], dr["rmask4"])
        P.dma("pool", m1[d][:], dr["rmask1"][d], m1[d], dr["rmask1"])

    def vcol(i, c):
        return vec[:, i, c:c + 1]

    for (a_, e_) in ((0, LC), (LC, T)):
        P.tt(xxT[:, :, a_ + 1:e_ - 1], hT[:, :, a_:e_ - 2], hT[:, :, a_ + 2:e_], ALU.add, [hT], [xxT])
        P.copy(xxT[:, :, a_:a_ + 1], hT[:, :, a_ + 1:a_ + 2], [hT], [xxT])
        P.copy(xxT[:, :, e_ - 1:e_], hT[:, :, e_ - 2:e_ - 1], [hT], [xxT])
        P.stt(xxT[:, :, a_:e_], xxT[:, :, a_:e_], 0.5, hT[:, :, a_:e_], ALU.mult, ALU.subtract, [xxT, hT], [xxT])

    def load_scaled(name, src2d, ncols, mu_i):
        w = P.sbuf(name, [128, KC, ncols], BF16)
        ws = P.sbuf(name + "s", [128, KC, ncols], BF16)
        P.dma("pool", w[:], src2d.rearrange("(kc p) n -> p kc n", p=128), w, dr["rw_rkv"])
        for kc in range(KC):
            P.ts(ws[:, kc, :], w[:, kc, :], vcol(V_MU + mu_i, kc), None, ALU.mult, None, [w, vec], [ws])
        return w, ws

    def proj_mix(ps_ap, psbuf, w, ws, c0, m, t0, nb):
        for kc in range(KC):
            P.mm(ps_ap, w[:, kc, c0:c0 + m], hT[:, kc, t0:t0 + nb], kc == 0, False, [w, hT], [psbuf])
        for kc in range(KC):
            P.mm(ps_ap, ws[:, kc, c0:c0 + m], xxT[:, kc, t0:t0 + nb], False, kc == KC - 1, [ws, xxT], [psbuf])

    with P.phase():
        w1, w1s = load_scaled("w1c", dr["rw_w1cat"][:], 128, 1)
        a1, a1s = load_scaled("a1c", dr["rw_a1cat"][:], 128, 4)
        g1, g1s = load_scaled("g1c", dr["rw_g1"][:], 160, 5)
        pi = 0
        for (t0, nb) in BLOCKS:
            ps = P.banks[pi % 4]; pi += 1
            proj_mix(ps[:, :nb], ps, w1, w1s, 0, 128, t0, nb)
            P.act(lw1[:, t0:t0 + nb], ps[:, :nb], AF.Tanh, [ps], [lw1])
            ps = P.banks[pi % 4]; pi += 1
            proj_mix(ps[:, :nb], ps, a1, a1s, 0, 128, t0, nb)
            P.copy(la1[:, t0:t0 + nb], ps[:, :nb], [ps], [la1], eng="act")
            ps = P.banks[pi % 4]; pi += 1
            proj_mix(ps[:, :nb], ps, g1, g1s, 0, 128, t0, nb)
            P.act(lg1[:, t0:t0 + nb], ps[:, :nb], AF.Sigmoid, [ps], [lg1])
            ps = P.banks[pi % 4]; pi += 1
            proj_mix(ps[0:32, :nb], ps, g1, g1s, 128, 32, t0, nb)
            P.act(lg1b[:, t0:t0 + nb], ps[0:32, :nb], AF.Sigmoid, [ps], [lg1b])
    if k.stop_after == "L1L":
        tap("L1_lw1", lw1, lw1[:], [128, T])
        return
    for c in range(KC if k.stop_after != "L1P" else 1):
        with P.phase():
            rwkv_pair(k, s, c, hT, xxT, lw1, la1, lg1, lg1b, y_acc, bon, outT, vec, omk, identb, bones, maskC, m4, m1,
                      load_scaled, proj_mix, vcol, tap)
    tap("L1_og", outT, outT[:], [128, KC, S])


def rwkv_pair(k, s, c, hT, xxT, lw1, la1, lg1, lg1b, y_acc, bon, outT, vec, omk, identb, bones, maskC, m4, m1,
              load_scaled, proj_mix, vcol, tap):
    P = k.P
    dr = k.dr
    cs_ = slice(c * 128, (c + 1) * 128)
    wr_, wrs = load_scaled("wr", dr["rw_rkv"][0][:, cs_], 128, 0)
    wk_, wks = load_scaled("wk", dr["rw_rkv"][1][:, cs_], 128, 2)
    wv_, wvs = load_scaled("wv", dr["rw_rkv"][2][:, cs_], 128, 3)
    w2c = P.sbuf("w2c", [128, 128], BF16)
    a2c = P.sbuf("a2c", [128, 128], BF16)
    g2a = P.sbuf("g2a", [128, 128], BF16)
    g2b = P.sbuf("g2b", [32, 128], BF16)
    P.dma("pool", w2c[:], dr["rw_w2cat"][:, cs_], w2c, dr["rw_w2cat"])
    P.dma("pool", a2c[:], dr["rw_a2cat"][:, cs_], a2c, dr["rw_a2cat"])
    P.dma("pool", g2a[:], dr["rw_g2"][0:128, cs_], g2a, dr["rw_g2"])
    P.dma("pool", g2b[:], dr["rw_g2"][128:160, cs_], g2b, dr["rw_g2"])
    NB = 256
    f = [P.sbuf("rf%d" % i, [128, NB], F32) for i in range(10)]
    r_f, k_f, v_f, kk_f, ic_f, lw_f, cs_f, e1, e2, tmp = f
    Vb = P.sbuf("Vb", [128, NB], BF16)
    ATz, RTz, BTz, KTz = [[P.sbuf("z%d_%d" % (i, h), [128, NB], BF16) for h in range(2)] for i in range(4)]
    for lst in (ATz, RTz, BTz, KTz):
        for t_ in lst:
            P.memset(t_[:], 0.0, [t_])
    stk = [P.sbuf("stk%d" % i, [128, 192], BF16) for i in range(4)]
    Vz = [[P.sbuf("Vz%d_%d" % (i, h), [128, 64], BF16) for h in range(2)] for i in range(4)]
    Uz = [P.sbuf("Uz%d" % h, [128, 64], BF16) for h in range(2)]
    for t_ in [v_ for l_ in Vz for v_ in l_] + Uz:
        P.memset(t_[:], 0.0, [t_])
    mats = [P.sbuf("mats%d" % i, [128, 640], BF16) for i in range(4)]
    Lf = [P.sbuf("Lf%d" % i, [128, 256], F32) for i in range(4)]
    sq = [[P.sbuf("sq%d_%d" % (i, j), [128, 256], F32) for j in range(2)] for i in range(4)]
    ZT = [P.sbuf("ZT%d" % i, [128, 128], F32) for i in range(4)]
    identf = P.sbuf("identf", [128, 128], F32)
    P.dma("sp", identf[:], dr["ident"][:], identf, dr["ident"])
    T0 = P.sbuf("T0", [128, 64], F32)
    T0s = P.sbuf("T0s", [128, 64], F32)
    T0b = P.sbuf("T0b", [128, 64], BF16)
    Xb = P.sbuf("Xb", [128, 64], F32)
    sqb = P.sbuf("sqb", [128, 512], F32)
    B = P.banks
    import os as _os
    DBG_ND = int(_os.environ.get("DBG_ND", "2"))
    DBG_NBLK = int(_os.environ.get("DBG_NBLK", "9"))
    DBG_STAGE = int(_os.environ.get("DBG_STAGE", "9"))
    DBG_TAIL = int(_os.environ.get("DBG_TAIL", "1"))
    DBG_PREP = int(_os.environ.get("DBG_PREP", "99"))
    if DBG_STAGE < -1:
        return
    if DBG_PREP < 99:
        P.dbg_budget = DBG_PREP
    for d in range(DBG_ND):
        P.memset(T0[:], 0.0, [T0])
        P.memset(T0b[:], 0.0, [T0b])
        order = list(range(9)) if d == 0 else [0] + list(range(8, 0, -1))
        for b in order[:DBG_NBLK]:
            t0, nb = RB[b]
            lat = b > 0
            l0 = t0 - LC
            proj_mix(B[0][:, :nb], B[0], wr_, wrs, 0, 128, t0, nb)
            proj_mix(B[1][:, :nb], B[1], wk_, wks, 0, 128, t0, nb)
            proj_mix(B[2][:, :nb], B[2], wv_, wvs, 0, 128, t0, nb)
            pr_d = slice(d * 64, (d + 1) * 64)
            P.mm(B[3][:, 0:nb], w2c[pr_d, :], lw1[pr_d, t0:t0 + nb], True, True, [w2c, lw1], [B[3]])
            P.mm(B[3][:, 256:256 + nb], a2c[pr_d, :], la1[pr_d, t0:t0 + nb], True, True, [a2c, la1], [B[3]])
            P.copy(r_f[:], B[0][:, :nb], [B[0]], [r_f], eng="act")
            P.copy(k_f[:], B[1][:, :nb], [B[1]], [k_f], eng="act")
            P.copy(v_f[:], B[2][:, :nb], [B[2]], [v_f], eng="act")
            P.copy(Vb[:], B[2][:, :nb], [B[2]], [Vb])
            P.act(lw_f[:], B[3][:, 0:nb], AF.Sigmoid, [B[3], vec], [lw_f], bias=vcol(V_W0 + d, c))
            P.ts(lw_f[:], lw_f[:], NEG_EXP_HALF, None, ALU.mult, None, [lw_f], [lw_f])
            P.act(ic_f[:], B[3][:, 256:256 + nb], AF.Sigmoid, [B[3], vec], [ic_f], bias=vcol(V_A0 + d, c))
            P.ts(kk_f[:], k_f[:], vcol(V_KK, c), None, ALU.mult, None, [k_f, vec], [kk_f])
            P.tt(tmp[:], kk_f[:], kk_f[:], ALU.mult, [kk_f], [tmp])
            P.mm(B[0][:, :nb], bones[:], tmp[:], True, True, [bones, tmp], [B[0]])
            P.ts(e1[:], B[0][:, :nb], 1e-18, None, ALU.max, None, [B[0]], [e1])
            P.act(e1[:], e1[:], AF.Ln, [e1], [e1])
            P.act(e1[:], e1[:], AF.Exp, [e1], [e1], scale=-0.5)
            P.tt(kk_f[:], kk_f[:], e1[:], ALU.mult, [kk_f, e1], [kk_f])
            P.ts(tmp[:], ic_f[:], vcol(V_KA, c), omk[:, c:c + 1], ALU.mult, ALU.add, [ic_f, vec, omk], [tmp])
            P.tt(k_f[:], k_f[:], tmp[:], ALU.mult, [k_f, tmp], [k_f])
            if lat:
                P.stt(tmp[:], r_f[:], vcol(V_RK, c), k_f[:], ALU.mult, ALU.mult, [r_f, vec, k_f], [tmp])
                P.mm(B[1][:, :nb], bones[:], tmp[:], True, True, [bones, tmp], [B[1]])
                if d == 0:
                    P.tt(bon[:, l0:l0 + nb], B[1][:, :nb], v_f[:], ALU.mult, [B[1], v_f], [bon])
                else:
                    P.tt(tmp[:], B[1][:, :nb], v_f[:], ALU.mult, [B[1], v_f], [tmp])
                    P.tt(bon[:, l0:l0 + nb], bon[:, l0:l0 + nb], tmp[:], ALU.add, [bon, tmp], [bon])
            if d == 0:
                P.op("dve", lambda e: e.tensor_tensor_scan(out=cs_f[:], data0=maskC[:], data1=lw_f[:], initial=0.0,
                                                           op0=ALU.mult, op1=ALU.add), [maskC, lw_f], [cs_f])
            else:
                P.op("dve", lambda e: e.tensor_tensor_scan(out=cs_f[:, ::-1], data0=maskC[:], data1=lw_f[:, ::-1], initial=0.0,
                                                           op0=ALU.mult, op1=ALU.add), [maskC, lw_f], [cs_f])
            P.tt(e2[:], cs_f[:], lw_f[:], ALU.subtract, [cs_f, lw_f], [e2])
            P.act(e2[:], e2[:], AF.Exp, [e2], [e2])
            for h in range(2):
                pr = slice(h * 64, (h + 1) * 64)
                P.stt(ATz[h][pr], kk_f[pr], -1.0, e2[pr], ALU.mult, ALU.mult, [kk_f, e2], [ATz[h]])
            P.act(e1[:], cs_f[:], AF.Exp, [cs_f], [e1])
            for h in range(2):
                pr = slice(h * 64, (h + 1) * 64)
                P.tt(RTz[h][pr], r_f[pr], e1[pr], ALU.mult, [r_f, e1], [RTz[h]])
            P.act(e2[:], cs_f[:], AF.Exp, [cs_f], [e2], scale=-1.0)
            P.tt(tmp[:], kk_f[:], ic_f[:], ALU.mult, [kk_f, ic_f], [tmp])
            for h in range(2):
                pr = slice(h * 64, (h + 1) * 64)
                P.tt(BTz[h][pr], tmp[pr], e2[pr], ALU.mult, [tmp, e2], [BTz[h]])
                P.tt(KTz[h][pr], k_f[pr], e2[pr], ALU.mult, [k_f, e2], [KTz[h]])
            nch = nb // 64
            if DBG_STAGE < 1:
                continue
            for n in range(nch):
                cols = slice(n * 64, (n + 1) * 64)
                pb = B[4 + n % 2]
                for h in range(2):
                    pr = slice(h * 64, (h + 1) * 64)
                    for mi, src in enumerate((Vb, BTz[h], KTz[h])):
                        P.mm(pb[pr, mi * 64:(mi + 1) * 64], src[:, cols], identb[:, pr], True, True, [src, identb], [pb])
                P.copy(stk[n][:], pb[:, 0:192], [pb], [stk[n]], eng="act")
                for h in range(2):
                    pr = slice(h * 64, (h + 1) * 64)
                    P.copy(Vz[n][h][pr, :], pb[pr, 0:64], [pb], [Vz[n][h]])
            if DBG_STAGE < 2:
                continue
            for n in range(nch):
                cols = slice(n * 64, (n + 1) * 64)
                pa = B[6 + n % 2]
                pb = B[4 + n % 2]
                for h in range(2):
                    pr = slice(h * 64, (h + 1) * 64)
                    hc_ = slice(h * 64, (h + 1) * 64)
                    combos = ((BTz[h], ATz[h]), (ATz[h], BTz[h]), (KTz[h], ATz[h]), (BTz[h], RTz[h]))
                    for mi, (lh, rh) in enumerate(combos):
                        P.mm(pa[pr, mi * 128 + h * 64: mi * 128 + (h + 1) * 64], lh[:, cols], rh[:, cols], True, True,
                             [lh, rh], [pa])
                    P.mm(pb[pr, 256 + h * 64:256 + (h + 1) * 64], KTz[h][:, cols], RTz[h][:, cols], True, True,
                         [KTz[h], RTz[h]], [pb])
                P.tt(Lf[n][:, 0:256], pa[:, 0:256], m4[d][:, 0:256], ALU.mult, [pa, m4[d]], [Lf[n]])
                P.tt(mats[n][:, 256:512], pa[:, 256:512], m4[d][:, 256:512], ALU.mult, [pa, m4[d]], [mats[n]])
                P.tt(mats[n][:, 512:640], pb[:, 256:384], m1[d][:], ALU.mult, [pb, m1[d]], [mats[n]])
            if DBG_STAGE < 3:
                continue
            cur = []
            for n in range(nch):
                P.tt(ZT[n][:], Lf[n][:, 0:128], identf[:], ALU.add, [Lf[n], identf], [ZT[n]])
                cur.append((Lf[n], 128, 0))
            for lvl in range(5):
                last = lvl == 4
                for n in range(nch):
                    src, oL, oLT = cur[n]
                    dst = sq[n][lvl % 2]
                    pa = B[4 + n % 2]
                    P.mm(pa[:, 0:128], src[:, oLT:oLT + 128], src[:, oL:oL + 128], True, True, [src], [pa])
                    if not last:
                        P.mm(pa[:, 128:256], src[:, oL:oL + 128], src[:, oLT:oLT + 128], True, True, [src], [pa])
                        P.copy(dst[:, 0:256], pa[:, 0:256], [pa], [dst], eng="act")
                    else:
                        P.copy(dst[:, 0:128], pa[:, 0:128], [pa], [dst], eng="act")
                    cur[n] = (dst, 0, 128)
                for n in range(nch):
                    src, oL, oLT = cur[n]
                    pz = B[6 + n % 2]
                    P.mm(pz[:, 0:128], src[:, oL:oL + 128], ZT[n][:], True, True, [src, ZT[n]], [pz])
                    P.tt(ZT[n][:], ZT[n][:], pz[:, 0:128], ALU.add, [ZT[n], pz], [ZT[n]])
            if DBG_STAGE < 4:
                continue
            chunks = list(range(nch)) if d == 0 else list(range(nch - 1, -1, -1))
            for n in chunks:
                cols = slice(n * 64, (n + 1) * 64)
                dcol = (n * 64 + 63) if d == 0 else n * 64
                dC = e1[:, dcol:dcol + 1]
                pX = B[0]
                for h in range(2):
                    pr = slice(h * 64, (h + 1) * 64)
                    P.mm(pX[pr, 0:64], ATz[h][:, cols], T0b[:, :], True, False, [ATz[h], T0b], [pX])
                P.mm(pX[:, 0:64], mats[n][:, 256:384], stk[n][:, 0:64], False, True, [mats[n], stk[n]], [pX])
                P.ts(T0s[:], T0[:], dC, None, ALU.mult, None, [T0, e1], [T0s])
                P.copy(Xb[:], pX[:, 0:64], [pX], [Xb], eng="act")
                pU = B[1]
                P.mm(pU[:, 0:64], ZT[n][:], Xb[:], True, True, [ZT[n], Xb], [pU])
                P.copy(Uz[0][0:64, :], pU[0:64, 0:64], [pU], [Uz[0]], eng="act")
                P.copy(Uz[1][64:128, :], pU[64:128, 0:64], [pU], [Uz[1]], eng="act")
                if lat:
                    pY = B[2]
                    for h in range(2):
                        pr = slice(h * 64, (h + 1) * 64)
                        P.mm(pY[pr, 0:64], T0b[:, :], RTz[h][:, cols], True, False, [T0b, RTz[h]], [pY])
                        P.mm(pY[pr, 0:64], Uz[h][:, :], mats[n][:, 384 + h * 64:384 + (h + 1) * 64], False, False,
                             [Uz[h], mats[n]], [pY])
                        P.mm(pY[pr, 0:64], stk[n][:, 0:64], mats[n][:, 512 + h * 64:512 + (h + 1) * 64], False, True,
                             [stk[n], mats[n]], [pY])
                    yc = slice(l0 + n * 64, l0 + (n + 1) * 64)
                    if d == 0:
                        P.copy(y_acc[:, yc], pY[:, 0:64], [pY], [y_acc])
                    else:
                        P.tt(y_acc[:, yc], y_acc[:, yc], pY[:, 0:64], ALU.add, [y_acc, pY], [y_acc])
                pT = B[3]
                for h in range(2):
                    pr = slice(h * 64, (h + 1) * 64)
                    P.mm(pT[pr, 0:64], stk[n][:, 64:128], Uz[h][:, :], True, False, [stk[n], Uz[h]], [pT])
                    P.mm(pT[pr, 0:64], stk[n][:, 128:192], Vz[n][h][:, :], False, True, [stk[n], Vz[n][h]], [pT])
                P.stt(T0[:], pT[:, 0:64], dC, T0s[:], ALU.mult, ALU.add, [pT, e1, T0s], [T0])
                P.copy(T0b[:], T0[:], [T0], [T0b], eng="act")
    P.dbg_budget = None
    if c == 0:
        tap("L1_y0", y_acc, y_acc[:], [128, S])
        tap("L1_bon0", bon, bon[:], [128, S])
    for i in range(4 if DBG_TAIL else 0):
        l0 = i * 512
        t0 = LC + l0
        nb = 512
        yb = y_acc[:, l0:l0 + nb]
        P.mm(B[0][:, :nb], bones[:], yb, True, True, [bones, y_acc], [B[0]])
        P.stt(yb, B[0][:, :nb], -1.0 / 64, yb, ALU.mult, ALU.add, [B[0], y_acc], [y_acc])
        P.tt(sqb[:, :nb], yb, yb, ALU.mult, [y_acc], [sqb])
        P.mm(B[1][:, :nb], bones[:], sqb[:, :nb], True, True, [bones, sqb], [B[1]])
        P.act(sqb[:, :nb], B[1][:, :nb], AF.Ln, [B[1], k.gneps], [sqb], scale=1.0 / 64, bias=k.gneps[:, 0:1])
        P.act(sqb[:, :nb], sqb[:, :nb], AF.Exp, [sqb], [sqb], scale=-0.5)
        P.tt(yb, yb, sqb[:, :nb], ALU.mult, [y_acc, sqb], [y_acc])
        P.ts(yb, yb, vcol(V_GG, c), vcol(V_GB, c), ALU.mult, ALU.add, [y_acc, vec], [y_acc])
        P.tt(yb, yb, bon[:, l0:l0 + nb], ALU.add, [y_acc, bon], [y_acc])
        P.mm(B[2][:, :nb], g2a[:], lg1[:, t0:t0 + nb], True, False, [g2a, lg1], [B[2]])
        P.mm(B[2][:, :nb], g2b[:], lg1b[:, t0:t0 + nb], False, True, [g2b, lg1b], [B[2]])
        P.tt(outT[:, c, l0:l0 + nb], yb, B[2][:, :nb], ALU.mult, [y_acc, B[2]], [outT])


_CACHE = {}


def kernel(**inputs):
    inp = {k_: np.asarray(v) for k_, v in inputs.items()}
    B = inp["x"].shape[0]
    ns = B // NCORE
    if "nc" not in _CACHE:
        _CACHE["nc"] = build(ns=ns)[0]
    nc = _CACHE["nc"]
    sh = prep_shared(inp)
    in_maps = []
    for core in range(NCORE):
        m = dict(sh)
        m.update(prep_core(inp, core, ns))
        in_maps.append(m)
    res = run_bass_kernel_spmd(nc, in_maps, core_ids=list(range(NCORE)))
    out = np.empty((B, S, D), np.float32)
    for core in range(NCORE):
        o = res.results[core]["outT"]
        for s in range(ns):
            out[core * ns + s] = o[s].T
    return out
```

```python
from contextlib import ExitStack, contextmanager
import numpy as np
import concourse.bass as bass
import concourse.mybir as mybir
from concourse.bass_utils import run_bass_kernel_spmd

F32 = mybir.dt.float32
BF16 = mybir.dt.bfloat16
AF = mybir.ActivationFunctionType
ALU = mybir.AluOpType
AX = mybir.AxisListType

ENGS = ("pe", "dve", "act", "pool", "sp")

D = 1024
KC = 8
S = 2048
LC = 256
T = S + LC
NCORE = 8
BLOCKS = [(0, 256), (256, 512), (768, 512), (1280, 512), (1792, 512)]
LAT_BLOCKS = BLOCKS[1:]
EPS = 1e-6
ARENA_BYTES = 206 * 1024


class Buf:
    def __init__(self, prog, t, name, space):
        self.prog = prog
        self.t = t
        self.name = name
        self.space = space
        self.last_w = None
        self.reads = {}
        self.dsem = None

    def __getitem__(self, idx):
        return self.t[idx]

    def view(self, name):
        b = Buf(self.prog, self.t, name, self.space)
        self.prog._register(b)
        return b


class Prog:
    def __init__(self):
        self.nc = bass.Bass("TRN2", target_bir_lowering=False)
        self.gstack = ExitStack()
        self.ops = {e: [] for e in ENGS}
        self.cnt = {e: 0 for e in ENGS}
        self.waited = {e: {} for e in ENGS}
        self.sems = {}
        self.dpool = []
        self.dcnt = {}
        self.live_dsem = set()
        self.ndsem = 0
        self.live_bufs = []
        self.phase_bufs = None
        self.uid = 0
        for e in ENGS:
            self._sem("eng_" + e)
        self.arena = self.gstack.enter_context(self.nc.sbuf_tensor("arena", [128, ARENA_BYTES // 4], F32))
        self.top = 0
        self.limit = ARENA_BYTES
        self.hiwater = 0
        self.banks = []
        for i in range(8):
            t = self.gstack.enter_context(self.nc.psum_tensor("bank%d" % i, [128, 512], F32))
            b = Buf(self, t, "bank%d" % i, "psum")
            self.live_bufs.append(b)
            self.banks.append(b)

    def _sem(self, key):
        if key not in self.sems:
            self.sems[key] = self.gstack.enter_context(self.nc.semaphore("s_" + key))
        return self.sems[key]

    def _register(self, b):
        self.live_bufs.append(b)
        if self.phase_bufs is not None:
            self.phase_bufs.append(b)

    def _name(self, name):
        self.uid += 1
        return "%s_%d" % (name, self.uid)

    def sbuf(self, name, shape, dtype=F32):
        esz = 2 if dtype == BF16 else 4
        nel = 1
        for d in shape[1:]:
            nel *= d
        nb = (nel * esz + 63) // 64 * 64
        off = self.top
        self.top += nb
        assert self.top <= self.limit, "arena overflow %s: top=%d limit=%d" % (name, self.top, self.limit)
        self.hiwater = max(self.hiwater, self.top)
        ap = self.arena[:, off // 4:(off + nb) // 4]
        if dtype != F32:
            ap = ap.bitcast(dtype)
        ap = ap[:, :nel]
        if len(shape) > 2:
            names = ["d%d" % i for i in range(len(shape) - 1)]
            pat = "p (%s) -> p %s" % (" ".join(names), " ".join(names))
            ap = ap.rearrange(pat, **{n: v for n, v in zip(names, shape[1:])})
        if shape[0] < 128:
            ap = ap[0:shape[0]]
        b = Buf(self, ap, name, "sbuf")
        b.off = off
        b.nbytes = nb
        self._register(b)
        return b

    @contextmanager
    def overlay(self, buf, start=None):
        st = (self.top, self.limit)
        self.top, self.limit = (buf.off if start is None else start), buf.off + buf.nbytes
        try:
            yield
        finally:
            buf.ov_top = self.top
            self.top, self.limit = st

    def dram(self, name, shape, dtype=F32, kind="Internal"):
        t = self.nc.dram_tensor(name, list(shape), dtype, kind=kind)
        b = Buf(self, t.ap(), name, "dram")
        self.live_bufs.append(b)
        return b

    def _need(self, eng, key, val, waits):
        if key == "eng_pe" and eng == "pe":
            return
        if self.waited[eng].get(key, 0) >= val:
            return
        self.waited[eng][key] = val
        waits[key] = max(waits.get(key, 0), val)

    def _deps(self, eng, reads, writes):
        waits = {}
        for b in reads:
            if b.last_w is not None:
                self._need(eng, b.last_w[0], b.last_w[1], waits)
            if b.space == "psum":
                for k, v in b.reads.items():
                    if k != "eng_" + eng:
                        self._need(eng, k, v, waits)
        for b in writes:
            if b.last_w is not None:
                self._need(eng, b.last_w[0], b.last_w[1], waits)
            for k, v in b.reads.items():
                self._need(eng, k, v, waits)
        return list(waits.items())

    dbg_budget = None

    def op(self, eng, fn, reads=(), writes=()):
        if self.dbg_budget is not None:
            if self.dbg_budget <= 0:
                return
            self.dbg_budget -= 1
        waits = self._deps(eng, reads, writes)
        self.cnt[eng] += 1
        key = "eng_" + eng
        val = self.cnt[eng]
        for b in reads:
            if b.reads.get(key, 0) < val:
                b.reads[key] = val
        for b in writes:
            b.last_w = (key, val)
            b.reads = {}
        self.ops[eng].append((waits, fn, key, 1))

    def dma(self, eng, out_ap, in_ap, dst, src, **kw):
        waits = self._deps(eng, [src], [dst])
        if dst.dsem is None:
            if self.dpool:
                dst.dsem = self.dpool.pop()
            else:
                self.ndsem += 1
                dst.dsem = "d%d" % self.ndsem
                self._sem(dst.dsem)
                self.dcnt[dst.dsem] = 0
            self.live_dsem.add(dst.dsem)
        key = dst.dsem
        self.dcnt[key] += 16
        val = self.dcnt[key]
        if src.reads.get(key, 0) < val:
            src.reads[key] = val
        dst.last_w = (key, val)
        dst.reads = {}

        def fn(e, out_ap=out_ap, in_ap=in_ap, kw=kw):
            return e.dma_start(out=out_ap, in_=in_ap, **kw)

        self.ops[eng].append((waits, fn, key, 16))

    def barrier(self):
        toks = [("eng_" + e, self.cnt[e]) for e in ENGS if self.cnt[e] > 0]
        toks += [(k, self.dcnt[k]) for k in sorted(self.live_dsem) if self.dcnt[k] > 0]
        for e in ENGS:
            waits = {}
            for k, v in toks:
                if k == "eng_" + e:
                    continue
                self._need(e, k, v, waits)
            if waits:
                self.ops[e].append((list(waits.items()), None, None, 0))
        for b in self.live_bufs:
            b.last_w = None
            b.reads = {}

    @contextmanager
    def phase(self):
        prev_b = self.phase_bufs
        mark = (self.top, self.limit)
        self.phase_bufs = []
        try:
            yield
        finally:
            self.barrier()
            for b in self.phase_bufs:
                if b.dsem is not None:
                    if b.dsem in self.live_dsem:
                        self.live_dsem.discard(b.dsem)
                        self.dpool.append(b.dsem)
                    b.dsem = None
            dead = set(id(b) for b in self.phase_bufs)
            self.live_bufs = [b for b in self.live_bufs if id(b) not in dead]
            self.top, self.limit = mark
            self.phase_bufs = prev_b

    def final_wait(self, eng, bufs):
        waits = {}
        for b in bufs:
            if b.last_w is not None:
                self._need(eng, b.last_w[0], b.last_w[1], waits)
        self.ops[eng].append((list(waits.items()), None, None, 0))

    def emit(self):
        nc = self.nc
        sems = self.sems

        def run(e, lst):
            for waits, fn, key, inc in lst:
                for k, v in waits:
                    e.wait_ge(sems[k], v)
                if fn is not None:
                    fn(e).then_inc(sems[key], inc)

        with nc.Block() as block:
            @block.tensor
            def _(e):
                run(e, self.ops["pe"])

            @block.vector
            def _(e):
                run(e, self.ops["dve"])

            @block.scalar
            def _(e):
                run(e, self.ops["act"])

            @block.gpsimd
            def _(e):
                run(e, self.ops["pool"])

            @block.sync
            def _(e):
                run(e, self.ops["sp"])
        self.gstack.close()
        return nc

    def mm(self, out, lhsT, rhs, start, stop, R, W):
        self.op("pe", lambda e: e.matmul(out, lhsT=lhsT, rhs=rhs, start=start, stop=stop), R, W)

    def act(self, out, in_, func, R, W, scale=1.0, bias=0.0):
        self.op("act", lambda e: e.activation(out=out, in_=in_, func=func, bias=bias, scale=scale), R, W)

    def tt(self, out, in0, in1, op, R, W, eng="dve"):
        self.op(eng, lambda e: e.tensor_tensor(out=out, in0=in0, in1=in1, op=op), R, W)

    def ts(self, out, in0, s1, s2, op0, op1, R, W, eng="dve"):
        if op1 is None:
            self.op(eng, lambda e: e.tensor_scalar(out=out, in0=in0, scalar1=s1, scalar2=None, op0=op0), R, W)
        else:
            self.op(eng, lambda e: e.tensor_scalar(out=out, in0=in0, scalar1=s1, scalar2=s2, op0=op0, op1=op1), R, W)

    def stt(self, out, in0, scalar, in1, op0, op1, R, W):
        self.op("dve", lambda e: e.scalar_tensor_tensor(out=out, in0=in0, scalar=scalar, in1=in1, op0=op0, op1=op1), R, W)

    def copy(self, out, in_, R, W, eng="dve"):
        if eng == "act":
            self.op("act", lambda e: e.copy(out=out, in_=in_), R, W)
        else:
            self.op(eng, lambda e: e.tensor_copy(out=out, in_=in_), R, W)

    def memset(self, ap, val, W, eng="dve"):
        self.op(eng, lambda e: e.memset(ap, val), (), W)


def _rope_tables():
    rows = S // 64
    row = np.repeat(np.arange(rows), 64).astype(np.float32)
    col = np.tile(np.arange(64), rows).astype(np.float32)
    inv = (10000.0 ** (-np.arange(0, 32, 2, dtype=np.float32) / 32)).astype(np.float32)
    ar = row[:, None] * inv
    ac = col[:, None] * inv
    C = np.zeros((128, S), np.float32)
    Sg = np.zeros((128, S), np.float32)
    for p in range(128):
        d = p % 64
        ang = ar if d < 32 else ac
        i = d % 16
        C[p] = np.cos(ang[:, i])
        sgn = -1.0 if (d % 32) < 16 else 1.0
        Sg[p] = sgn * np.sin(ang[:, i])
    return C, Sg


def _perm_rot(cols64):
    idx = np.arange(64)
    out = idx.copy()
    for base in (0, 32):
        out[base:base + 16] = idx[base + 16:base + 32]
        out[base + 16:base + 32] = idx[base:base + 16]
    return cols64[out]


def prep_shared(inp):
    f = np.float32
    sh = {}
    sh["mod_w"] = np.ascontiguousarray(inp["mod_w"], f)
    sh["mod_bT"] = np.ascontiguousarray(inp["mod_b"].reshape(2, 48, 128).transpose(2, 0, 1), f)
    g = np.stack([inp["norm_mix"], inp["norm_ffn"]], 0)
    sh["gT"] = np.ascontiguousarray(g.reshape(2, 2, 8, 128).transpose(3, 0, 1, 2), f)
    sh["finT"] = np.ascontiguousarray(inp["final_norm"].reshape(8, 128).T, f)
    sh["router_w"] = np.ascontiguousarray(inp["router_w"], f)
    sh["exp_w1"] = inp["exp_w1"]
    sh["exp_w3"] = inp["exp_w3"]
    sh["exp_w2"] = inp["exp_w2"]
    w_in = inp["mix_in"][0]
    q = w_in[:, 0:512]
    k = w_in[:, 512:640]
    v = w_in[:, 640:768]
    ul = w_in[:, 768:1280]
    gl = w_in[:, 1280:1792]
    qcols, qpcols = [], []
    for j in range(4):
        for h in (j, 4 + j):
            c64 = np.arange(h * 64, (h + 1) * 64)
            qcols.append(c64)
            qpcols.append(_perm_rot(c64))
    qcols = np.concatenate(qcols)
    qpcols = np.concatenate(qpcols)
    kp = np.concatenate([_perm_rot(np.arange(0, 64)), _perm_rot(np.arange(64, 128))])
    sh["w_in"] = np.ascontiguousarray(np.concatenate([q[:, qcols], q[:, qpcols], k, k[:, kp], v, ul, gl], 1), f)
    w_out = inp["mix_out"][0]
    arows = []
    for j in range(4):
        for h in (j, 4 + j):
            arows.append(np.arange(h * 64, (h + 1) * 64))
    arows = np.concatenate(arows + [np.arange(512, 1024)])
    sh["w_out"] = np.ascontiguousarray(w_out[arows], f)
    sh["sinkb"] = np.ascontiguousarray(np.broadcast_to(inp["attn_sink"][0][None, :], (128, 8)), f)
    C, Sg = _rope_tables()
    sh["ropeC"] = C
    sh["ropeS"] = Sg
    kq = np.arange(128)
    mP = (kq[:, None] >= kq[None, :]).astype(f)
    mN = (kq[:, None] <= kq[None, :]).astype(f)
    sh["maskP"] = np.ascontiguousarray(np.tile(mP, (1, 4)))
    sh["maskN"] = np.ascontiguousarray(np.tile(mN, (1, 4)))
    sh["convw"] = np.ascontiguousarray(inp["lru_conv_w"][0].reshape(4, 4, 128).transpose(2, 1, 0), f)
    sh["convb"] = np.ascontiguousarray(inp["lru_conv_b"][0].reshape(4, 128).T, f)

    def bd(w):
        o = np.zeros((128, 2, 4, 128), f)
        for d in range(2):
            for c in range(4):
                for hb in range(2):
                    o[hb * 64:(hb + 1) * 64, d, c, hb * 64:(hb + 1) * 64] = w[d, 2 * c + hb]
        return o
    sh["lru_wa"] = bd(inp["lru_wa"][0])
    sh["lru_wi"] = bd(inp["lru_wi"][0])

    def vec(a):
        return np.ascontiguousarray(a.reshape(2, 4, 128).transpose(2, 0, 1), f)
    sh["lru_ba"] = vec(inp["lru_ba"][0])
    sh["lru_bi"] = vec(inp["lru_bi"][0])
    sh["lru_lam"] = vec(inp["lru_lam"][0])
    sh["id16"] = np.eye(16, dtype=f)
    def fm(v1024):
        return np.asarray(v1024, f).reshape(8, 128).T
    vecs = [inp["rw_mu"][0][i] for i in range(6)] + [inp["rw_w0"][0][0], inp["rw_w0"][0][1], inp["rw_a0"][0][0], inp["rw_a0"][0][1],
            inp["rw_kk"][0], inp["rw_ka"][0], inp["rw_rk"][0].reshape(1024), inp["rw_gn_g"][0], inp["rw_gn_b"][0]]
    sh["rw_vec"] = np.ascontiguousarray(np.stack([fm(v) for v in vecs], 1), f)
    sh["ident"] = np.eye(128, dtype=f)
    bo = np.zeros((128, 128), f)
    bo[:64, :64] = 1.0
    bo[64:, 64:] = 1.0
    sh["blockones"] = bo
    mc = np.ones((128, 256), f)
    mc[:, ::64] = 0.0
    sh["maskC"] = mc
    ii = np.arange(64)
    lt = (ii[:, None] < ii[None, :]).astype(f)
    le = (ii[:, None] <= ii[None, :]).astype(f)
    def bdm(m):
        o = np.zeros((128, 128), f)
        o[:64, :64] = m
        o[64:, 64:] = m
        return o
    sh["rmask4"] = np.ascontiguousarray(np.stack([np.concatenate([bdm(lt), bdm(lt.T), bdm(lt), bdm(le)], 1),
                                                  np.concatenate([bdm(lt.T), bdm(lt), bdm(lt.T), bdm(le.T)], 1)], 0), f)
    sh["rmask1"] = np.ascontiguousarray(np.stack([bdm(le), bdm(le.T)], 0), f)
    sh["rw_w1cat"] = np.ascontiguousarray(np.concatenate([inp["rw_w1"][0][0], inp["rw_w1"][0][1]], 1), f)
    sh["rw_a1cat"] = np.ascontiguousarray(np.concatenate([inp["rw_a1"][0][0], inp["rw_a1"][0][1]], 1), f)
    sh["rw_g1"] = np.ascontiguousarray(inp["rw_g1"][0], f)
    sh["rw_rkv"] = np.ascontiguousarray(inp["rw_rkv"][0], f)
    sh["rw_w2cat"] = np.ascontiguousarray(np.concatenate([inp["rw_w2"][0][0], inp["rw_w2"][0][1]], 0), f)
    sh["rw_a2cat"] = np.ascontiguousarray(np.concatenate([inp["rw_a2"][0][0], inp["rw_a2"][0][1]], 0), f)
    sh["rw_g2"] = np.ascontiguousarray(inp["rw_g2"][0], f)
    sh["rw_wo"] = np.ascontiguousarray(inp["rw_wo"][0], f)
    return sh


def prep_core(inp, core, ns):
    f = np.float32
    b0 = core * ns
    pc = {}
    pc["xT"] = np.ascontiguousarray(inp["x"][b0:b0 + ns].transpose(0, 2, 1), f)
    pc["cxT"] = np.ascontiguousarray(inp["ctx"][b0:b0 + ns].transpose(0, 2, 1), f)
    cc = np.zeros((8, 1024), f)
    cc[:ns] = inp["c"][b0:b0 + ns]
    cc[4] = inp["c_ctx"]
    pc["cT"] = np.ascontiguousarray(cc.reshape(8, 8, 128).transpose(2, 1, 0), f)
    return pc


class K:
    pass


GELU_C = 1.5957691216057308


def bank3(bank, a, b):
    return bank[:].rearrange("p (a b) -> p a b", a=a)


def build(ns=4, taps=None, stop_after=None, skip_l0=False):
    taps = taps or []
    P = Prog()
    k = K()
    k.P = P
    k.tap_outs = {}
    k.stop_after = stop_after
    k.skip_l0 = skip_l0
    IN = "ExternalInput"
    dr = {}
    k.dr = dr

    def din(name, shape):
        dr[name] = P.dram(name, shape, F32, IN)
        return dr[name]

    din("xT", [ns, D, S]); din("cxT", [ns, D, LC]); din("cT", [128, KC, 8])
    din("mod_w", [2, D, 6 * D]); din("mod_bT", [128, 2, 48]); din("gT", [128, 2, 2, 8]); din("finT", [128, 8])
    din("router_w", [2, D, 16])
    din("exp_w1", [2, 16, D, D]); din("exp_w3", [2, 16, D, D]); din("exp_w2", [2, 16, D, D])
    din("w_in", [D, 2432]); din("w_out", [D, D]); din("sinkb", [128, 8])
    din("ropeC", [128, S]); din("ropeS", [128, S]); din("maskP", [128, 512]); din("maskN", [128, 512])
    din("convw", [128, 4, 4]); din("convb", [128, 4])
    din("lru_wa", [128, 2, 4, 128]); din("lru_wi", [128, 2, 4, 128])
    din("lru_ba", [128, 2, 4]); din("lru_bi", [128, 2, 4]); din("lru_lam", [128, 2, 4])
    din("id16", [16, 16])
    din("rw_vec", [128, 15, KC]); din("ident", [128, 128]); din("blockones", [128, 128]); din("maskC", [128, 256])
    din("rmask4", [2, 128, 512]); din("rmask1", [2, 128, 128])
    din("rw_w1cat", [D, 128]); din("rw_a1cat", [D, 128]); din("rw_g1", [D, 160]); din("rw_rkv", [3, D, D])
    din("rw_w2cat", [128, D]); din("rw_a2cat", [128, D]); din("rw_g2", [160, D]); din("rw_wo", [D, D])
    k.xspill = P.dram("xspill", [D, S], F32, "Internal")
    k.outT = P.dram("outT", [ns, D, S], F32, "ExternalOutput")

    def tap(name, buf, ap, shape):
        if name not in taps:
            return
        o = P.dram("tap_" + name, list(shape), F32, "ExternalOutput")
        k.tap_outs[name] = o
        P.dma("pool", o[:], ap, o, buf)
    k.tap = tap

    k.ones_f = P.sbuf("ones_f", [128, 128], F32)
    P.memset(k.ones_f[:], 1.0, [k.ones_f])
    k.ones_b = P.sbuf("ones_b", [128, 64], BF16)
    P.memset(k.ones_b[:], 1.0, [k.ones_b])
    k.epsb = P.sbuf("epsb", [128, 1], F32)
    P.memset(k.epsb[:], EPS, [k.epsb])
    k.gneps = P.sbuf("gneps", [128, 1], F32)
    P.memset(k.gneps[:], 64e-5, [k.gneps])
    k.modv = modv = P.sbuf("modv", [128, 2, 48, 8], F32)
    k.Amod = Amod = P.sbuf("Amod", [128, 2, 2, 8, 8], F32)
    gT = P.sbuf("gT", [128, 2, 2, 8], F32)
    k.finT = P.sbuf("finT", [128, 8], F32)
    P.dma("sp", gT[:], dr["gT"][:], gT, dr["gT"])
    P.dma("sp", k.finT[:], dr["finT"][:], k.finT, dr["finT"])

    with P.phase():
        cT = P.sbuf("cT", [128, KC, 8], F32)
        P.dma("sp", cT[:], dr["cT"][:], cT, dr["cT"])
        sT = P.sbuf("sT", [128, KC, 8], F32)
        P.act(sT[:], cT[:], AF.Silu, [cT], [sT])
        mbT = P.sbuf("mbT", [128, 2, 48], F32)
        P.dma("sp", mbT[:], dr["mod_bT"][:], mbT, dr["mod_bT"])
        wm = [P.sbuf("wm%d" % i, [128, KC, 512], F32) for i in range(2)]
        it = 0
        for l in range(2):
            psm = P.banks[l]
            psm3 = psm[:, 0:384].rearrange("p (a b) -> p a b", a=48)
            for cb in range(12):
                w = wm[it % 2]
                it += 1
                src = dr["mod_w"][l].rearrange("(kc p) n -> p kc n", p=128)[:, :, cb * 512:(cb + 1) * 512]
                P.dma("sp", w[:], src, w, dr["mod_w"])
                for o4 in range(4):
                    oc = cb * 4 + o4
                    for kc in range(KC):
                        P.mm(psm3[:, oc, :], w[:, kc, o4 * 128:(o4 + 1) * 128], sT[:, kc, :], kc == 0, kc == KC - 1,
                             [w, sT], [psm])
            P.tt(modv[:, l], psm3, mbT[:, l, :, None].to_broadcast([128, 48, 8]), ALU.add, [psm, mbT], [modv])
            for kind in range(2):
                sc = modv[:, l, (1 + 3 * kind) * 8:(2 + 3 * kind) * 8, :]
                P.ts(Amod[:, l, kind], sc, 1.0, None, ALU.add, None, [modv], [Amod])
                P.tt(Amod[:, l, kind], Amod[:, l, kind], gT[:, kind, l, :, None].to_broadcast([128, 8, 8]), ALU.mult,
                     [Amod, gT], [Amod])
        tap("modv", modv, modv[:], [128, 2, 48, 8])

    for s in range(ns):
        sample_program(k, s)

    outs = [k.outT] + list(k.tap_outs.values())
    P.final_wait("sp", outs)
    P.final_wait("pool", outs)
    print("[build] arena hiwater %d / %d bytes, dma sems %d, instr %s" % (P.hiwater, ARENA_BYTES, P.ndsem,
                                                                        {e: len(P.ops[e]) for e in ENGS}))
    nc = P.emit()
    return nc, k


def mvec(k, l, j, kc, si):
    return k.modv[:, l, j * 8 + kc, si:si + 1]


def x_src(dr, s, b):
    t0, nb = BLOCKS[b]
    if b == 0:
        return dr["cxT"][s].rearrange("(kc p) t -> p kc t", p=128), dr["cxT"]
    return dr["xT"][s].rearrange("(kc p) t -> p kc t", p=128)[:, :, t0 - LC:t0 - LC + nb], dr["xT"]


class NormScratch:
    def __init__(self, k, bank):
        P = k.P
        self.sq = P.sbuf("sq", [128, KC, 512], F32)
        self.rs = P.sbuf("rs", [128, 512], F32)
        self.ps = bank


def rms_block(k, sc, x_ap, xbuf, nb):
    P = k.P
    P.act(sc.sq[:, :, :nb], x_ap, AF.Square, [xbuf], [sc.sq])
    for kc in range(KC):
        P.mm(sc.ps[:, :nb], k.ones_f[:], sc.sq[:, kc, :nb], kc == 0, kc == KC - 1, [k.ones_f, sc.sq], [sc.ps])
    P.act(sc.rs[:, :nb], sc.ps[:, :nb], AF.Ln, [sc.ps, k.epsb], [sc.rs], scale=1.0 / D, bias=k.epsb[:, 0:1])
    P.act(sc.rs[:, :nb], sc.rs[:, :nb], AF.Exp, [sc.rs], [sc.rs], scale=-0.5)
    P.tt(sc.sq[:, :, :nb], x_ap, sc.rs[:, None, :nb].to_broadcast([128, KC, nb]), ALU.mult, [xbuf, sc.rs], [sc.sq])
    return sc.sq


class WRing:
    def __init__(self, k, n):
        self.k = k
        self.slots = [k.P.sbuf("wslot%d" % i, [128, KC, 512], BF16) for i in range(n)]
        self.i = 0

    def load(self, w2d, wbuf, ncols=512):
        sl = self.slots[self.i % len(self.slots)]
        self.i += 1
        src = w2d.rearrange("(kc p) n -> p kc n", p=128)
        self.k.P.dma("pool", sl[:, :, :ncols], src, sl, wbuf)
        return sl


def sample_program(k, s):
    P = k.P
    dr = k.dr
    tap = k.tap if s == 0 else (lambda *a, **kw: None)
    SI = lambda b: 4 if b == 0 else s
    with P.phase():
        hT = P.sbuf("hT", [128, KC, T], BF16)
        xT = P.sbuf("xT", [128, KC, T], F32)
        if k.skip_l0:
            for b, (t0, nb) in enumerate(BLOCKS):
                src, sb_ = x_src(dr, s, b)
                P.dma("sp", xT[:, :, t0:t0 + nb], src, xT, sb_)
            layer1_and_out(k, s, hT, xT, tap)
            return
        with P.phase(), P.overlay(xT):
            sc = NormScratch(k, P.banks[0])
            xb = [P.sbuf("xb%d" % i, [128, KC, 512], F32) for i in range(2)]
            for b, (t0, nb) in enumerate(BLOCKS):
                xt = xb[b % 2]
                src, sb_ = x_src(dr, s, b)
                P.dma("sp", xt[:, :, :nb], src, xt, sb_)
                xn = rms_block(k, sc, xt[:, :, :nb], xt, nb)
                for kc in range(KC):
                    P.ts(hT[:, kc, t0:t0 + nb], xn[:, kc, :nb], k.Amod[:, 0, 0, kc, SI(b):SI(b) + 1],
                         mvec(k, 0, 0, kc, SI(b)), ALU.mult, ALU.add, [xn, k.Amod, k.modv], [hT])
            tap("L0_h", hT, hT[:], [128, KC, T])
        if k.stop_after == "L0A":
            return
        with P.phase():
            attnT = P.sbuf("attnT", [128, 4, T], BF16)
            lruT = P.sbuf("lruT", [128, 4, T], BF16)
            wr = WRing(k, 3)
            with P.phase(), P.overlay(xT):
                l0_attention(k, s, hT, attnT, wr, tap)
            if k.stop_after == "L0B":
                return
            with P.phase(), P.overlay(xT):
                l0_lru(k, s, hT, lruT, wr, tap)
            if k.stop_after == "L0C":
                return
            with P.phase():
                xb = [P.sbuf("xb%d" % i, [128, KC, 512], F32) for i in range(2)]
                wo = [wr.load(dr["w_out"][:, h * 512:(h + 1) * 512], dr["w_out"]) for h in range(2)]
                pi = 0
                for b, (t0, nb) in enumerate(BLOCKS):
                    xt = xb[b % 2]
                    src, sb_ = x_src(dr, s, b)
                    P.dma("sp", xt[:, :, :nb], src, xt, sb_)
                    for oc in range(KC):
                        ps = P.banks[pi % 4]
                        pi += 1
                        w = wo[oc // 4]
                        c0 = (oc % 4) * 128
                        for j in range(4):
                            P.mm(ps[:, :nb], w[:, j, c0:c0 + 128], attnT[:, j, t0:t0 + nb], j == 0, False, [w, attnT], [ps])
                        for c in range(4):
                            P.mm(ps[:, :nb], w[:, 4 + c, c0:c0 + 128], lruT[:, c, t0:t0 + nb], False, c == 3, [w, lruT], [ps])
                        P.stt(xT[:, oc, t0:t0 + nb], ps[:, :nb], mvec(k, 0, 2, oc, SI(b)), xt[:, oc, :nb], ALU.mult, ALU.add,
                              [ps, k.modv, xt], [xT])
                tap("L0_xmix", xT, xT[:], [128, KC, T])
        if k.stop_after == "L0D":
            return
        moe_layer(k, s, 0, hT, xT, True, tap)
        tap("L0_x", xT, xT[:], [128, KC, T])
        if k.stop_after == "L0F":
            return
        layer1_and_out(k, s, hT, xT, tap)


def layer1_and_out(k, s, hT, xT, tap):
    P = k.P
    layer1_mixer(k, s, hT, xT, tap)
    if k.stop_after in ("L1A", "L1B", "L1D", "L1L", "L1P"):
        return
    moe_layer(k, s, 1, hT, xT, False, tap)
    tap("L1_x", xT, xT[:], [128, KC, T])
    with P.phase():
        sc = NormScratch(k, P.banks[0])
        ob = [P.sbuf("ob%d" % i, [128, KC, 512], F32) for i in range(2)]
        for i, (t0, nb) in enumerate(LAT_BLOCKS):
            xn = rms_block(k, sc, xT[:, :, t0:t0 + nb], xT, nb)
            o = ob[i % 2]
            P.tt(o[:], xn[:, :, :nb], k.finT[:, :, None].to_broadcast([128, KC, nb]), ALU.mult, [xn, k.finT], [o])
            P.dma("sp", k.outT[s].rearrange("(kc p) t -> p kc t", p=128)[:, :, t0 - LC:t0 - LC + nb], o[:], k.outT, o)


def l0_attention(k, s, hT, qT, wr, tap):
    P = k.P
    dr = k.dr
    kT = P.sbuf("kT", [128, T], BF16)
    Vtok = P.sbuf("Vtok", [128, 18, 128], BF16)
    ropeC = P.sbuf("ropeC", [128, 512], F32)
    ropeS = P.sbuf("ropeS", [128, 512], F32)
    t1 = P.sbuf("rt1", [128, 512], F32)
    t2 = P.sbuf("rt2", [128, 512], F32)
    maskP = P.sbuf("maskP", [128, 4, 128], BF16)
    maskN = P.sbuf("maskN", [128, 4, 128], BF16)
    sinkb = P.sbuf("sinkb", [128, 8], F32)
    esink = P.sbuf("esink", [128, 8], F32)
    P.dma("pool", maskP[:], dr["maskP"][:].rearrange("p (a b) -> p a b", a=4), maskP, dr["maskP"])
    P.dma("pool", maskN[:], dr["maskN"][:].rearrange("p (a b) -> p a b", a=4), maskN, dr["maskN"])
    P.dma("sp", sinkb[:], dr["sinkb"][:], sinkb, dr["sinkb"])
    P.act(esink[:], sinkb[:], AF.Exp, [sinkb], [esink])
    wq = wr.load(dr["w_in"][:, 0:512], dr["w_in"])
    wqp = wr.load(dr["w_in"][:, 512:1024], dr["w_in"])
    wkv = wr.load(dr["w_in"][:, 1024:1408], dr["w_in"], 384)
    pi = 0
    for b, (t0, nb) in enumerate(BLOCKS):
        if b > 0:
            P.dma("sp", ropeC[:, :nb], dr["ropeC"][:, t0 - LC:t0 - LC + nb], ropeC, dr["ropeC"])
            P.dma("sp", ropeS[:, :nb], dr["ropeS"][:, t0 - LC:t0 - LC + nb], ropeS, dr["ropeS"])
        for j in range(5):
            w, wp, c0 = (wq, wqp, j * 128) if j < 4 else (wkv, wkv, 0)
            dst = qT[:, j, t0:t0 + nb] if j < 4 else kT[:, t0:t0 + nb]
            dbuf = qT if j < 4 else kT
            ps = P.banks[pi % 4]
            pi += 1
            for kc in range(KC):
                P.mm(ps[:, :nb], w[:, kc, c0:c0 + 128], hT[:, kc, t0:t0 + nb], kc == 0, kc == KC - 1, [w, hT], [ps])
            if b == 0:
                P.copy(dst, ps[:, :nb], [ps], [dbuf], eng="act")
            else:
                c1 = c0 if j < 4 else 128
                ps2 = P.banks[pi % 4]
                pi += 1
                for kc in range(KC):
                    P.mm(ps2[:, :nb], wp[:, kc, c1:c1 + 128], hT[:, kc, t0:t0 + nb], kc == 0, kc == KC - 1, [wp, hT], [ps2])
                P.tt(t1[:, :nb], ps[:, :nb], ropeC[:, :nb], ALU.mult, [ps, ropeC], [t1])
                P.tt(t2[:, :nb], ps2[:, :nb], ropeS[:, :nb], ALU.mult, [ps2, ropeS], [t2])
                P.tt(dst, t1[:, :nb], t2[:, :nb], ALU.add, [t1, t2], [dbuf])
        for tt_ in range(t0 // 128, (t0 + nb) // 128):
            ps = P.banks[pi % 4]
            pi += 1
            for kc in range(KC):
                P.mm(ps[:, 0:128], hT[:, kc, tt_ * 128:(tt_ + 1) * 128], wkv[:, kc, 256:384], kc == 0, kc == KC - 1,
                     [hT, wkv], [ps])
            P.copy(Vtok[:, tt_, :], ps[:, 0:128], [ps], [Vtok], eng="act")
    tap("L0_q", qT, qT[:], [128, 4, T])
    tap("L0_k", kT, kT[:], [128, T])
    tap("L0_v", Vtok, Vtok[:], [128, 18, 128])
    pT = [P.sbuf("pT%d" % i, [128, 4, 128], BF16) for i in range(2)]
    den = P.sbuf("den", [128, 4, 128], F32)
    ps_s = [P.banks[4], P.banks[5]]
    ps_num = P.banks[6]
    ps_den = P.banks[7]
    it = 0
    for qt in range(18):
        if qt < 2:
            keys = [(0, None), (1, None)]
        else:
            i = qt - 2
            keys = [(0, None), (1, None)]
            if i > 0:
                keys.append((qt - 1, maskP))
            keys.append((qt, None))
            if i < 15:
                keys.append((qt + 1, maskN))
        for g in range(2):
            pr = slice(g * 64, (g + 1) * 64)
            num3 = ps_num[pr, :].rearrange("p (a b) -> p a b", a=4)
            den3 = ps_den[pr, :].rearrange("p (a b) -> p a b", a=4)
            for idx, (kt, mk) in enumerate(keys):
                pss = ps_s[it % 2]
                pt = pT[it % 2]
                it += 1
                P.mm(pss[:].rearrange("p (a b) -> p a b", a=4), kT[pr, kt * 128:(kt + 1) * 128],
                     qT[pr, :, qt * 128:(qt + 1) * 128], True, True, [kT, qT], [pss])
                P.act(pt[:], pss[:].rearrange("p (a b) -> p a b", a=4), AF.Exp, [pss], [pt], scale=0.125)
                if mk is not None:
                    P.tt(pt[:], pt[:], mk[:], ALU.mult, [pt, mk], [pt])
                first, last = idx == 0, idx == len(keys) - 1
                P.mm(num3, Vtok[:, kt, pr], pt[:], first, last, [Vtok, pt], [ps_num])
                P.mm(den3, k.ones_b[:, 0:64], pt[:], first, last, [k.ones_b, pt], [ps_den])
            P.tt(den[pr], den3, esink[pr, g * 4:(g + 1) * 4, None].to_broadcast([64, 4, 128]), ALU.add,
                 [ps_den, esink], [den])
            P.op("dve", lambda e, pr=pr: e.reciprocal(out=den[pr], in_=den[pr]), [den], [den])
            P.tt(qT[pr, :, qt * 128:(qt + 1) * 128], num3, den[pr], ALU.mult, [ps_num, den], [qT])
    tap("L0_attn", qT, qT[:], [128, 4, T])


def l0_lru(k, s, hT, lruT, wr, tap):
    P = k.P
    dr = k.dr
    ul = P.sbuf("ul", [128, T], F32)
    u = P.sbuf("u", [128, T], F32)
    ub = P.sbuf("ub", [128, T], BF16)
    tmp = [P.sbuf("lt%d" % i, [128, 512], F32) for i in range(7)]
    t_r, t_i, t_a, t_y, t_b, t_h0, t_h1 = tmp
    convw = P.sbuf("convw", [128, 4, 4], F32)
    convb = P.sbuf("convb", [128, 4], F32)
    wa = P.sbuf("wa", [128, 2, 4, 128], BF16)
    wi = P.sbuf("wi", [128, 2, 4, 128], BF16)
    ba = P.sbuf("ba", [128, 2, 4], F32)
    bi = P.sbuf("bi", [128, 2, 4], F32)
    lam = P.sbuf("lam", [128, 2, 4], F32)
    kap = P.sbuf("kap", [128, 2, 4], F32)
    kap2 = P.sbuf("kap2", [128, 2, 4], F32)
    zz = P.sbuf("zz", [128, 2, 4], F32)
    zp = P.sbuf("zp", [128, 2, 4], F32)
    for nm, t_ in (("convw", convw), ("convb", convb), ("lru_ba", ba), ("lru_bi", bi), ("lru_lam", lam)):
        P.dma("sp", t_[:], dr[nm][:], t_, dr[nm])
    P.dma("pool", wa[:], dr["lru_wa"][:], wa, dr["lru_wa"])
    P.dma("pool", wi[:], dr["lru_wi"][:], wi, dr["lru_wi"])
    P.act(zz[:], lam[:], AF.Exp, [lam], [zz], scale=-1.0)
    P.ts(zp[:], zz[:], -0.2, 0.25, ALU.mult, ALU.add, [zz], [zp])
    for cst in (1.0 / 3.0, 0.5, 1.0):
        P.tt(zp[:], zp[:], zz[:], ALU.mult, [zp, zz], [zp])
        P.ts(zp[:], zp[:], -1.0, cst, ALU.mult, ALU.add, [zp], [zp])
    P.tt(zp[:], zp[:], zz[:], ALU.mult, [zp, zz], [zp])
    P.ts(kap[:], zp[:], -8.0, None, ALU.mult, None, [zp], [kap])
    P.ts(kap2[:], zp[:], -16.0, None, ALU.mult, None, [zp], [kap2])
    wul = wr.load(dr["w_in"][:, 1408:1920], dr["w_in"])
    wgl = wr.load(dr["w_in"][:, 1920:2432], dr["w_in"])
    pi = 0
    for c in range(4):
        for b, (t0, nb) in enumerate(BLOCKS):
            ps = P.banks[pi % 4]
            pi += 1
            for kc in range(KC):
                P.mm(ps[:, :nb], wul[:, kc, c * 128:(c + 1) * 128], hT[:, kc, t0:t0 + nb], kc == 0, kc == KC - 1, [wul, hT], [ps])
            P.copy(ul[:, t0:t0 + nb], ps[:, :nb], [ps], [ul], eng="act")
        P.act(u[:], ul[:], AF.Identity, [ul, convw, convb], [u], scale=convw[:, c, 2:3], bias=convb[:, c:c + 1])
        for (a_, e_) in ((0, LC), (LC, T)):
            P.stt(u[:, a_ + 2:e_], ul[:, a_:e_ - 2], convw[:, c, 0:1], u[:, a_ + 2:e_], ALU.mult, ALU.add, [ul, convw, u], [u])
            P.stt(u[:, a_ + 1:e_], ul[:, a_:e_ - 1], convw[:, c, 1:2], u[:, a_ + 1:e_], ALU.mult, ALU.add, [ul, convw, u], [u])
            P.stt(u[:, a_:e_ - 1], ul[:, a_ + 1:e_], convw[:, c, 3:4], u[:, a_:e_ - 1], ALU.mult, ALU.add, [ul, convw, u], [u])
        P.copy(ub[:], u[:], [u], [ub], eng="act")
        if c == 0:
            tap("L0_u0", u, u[:], [128, T])
        hsum = ul
        for d in range(2):
            order = [0, 1, 2, 3, 4] if d == 0 else [0, 4, 3, 2, 1]
            prev_h = None
            for oi, b in enumerate(order):
                t0, nb = BLOCKS[b]
                psr = P.banks[pi % 4]
                pi += 1
                psi = P.banks[pi % 4]
                pi += 1
                P.mm(psr[:, :nb], wa[:, d, c, :], ub[:, t0:t0 + nb], True, True, [wa, ub], [psr])
                P.mm(psi[:, :nb], wi[:, d, c, :], ub[:, t0:t0 + nb], True, True, [wi, ub], [psi])
                P.act(t_r[:, :nb], psr[:, :nb], AF.Sigmoid, [psr, ba], [t_r], bias=ba[:, d, c:c + 1])
                P.act(t_i[:, :nb], psi[:, :nb], AF.Sigmoid, [psi, bi], [t_i], bias=bi[:, d, c:c + 1])
                P.act(t_a[:, :nb], t_r[:, :nb], AF.Exp, [t_r, kap], [t_a], scale=kap[:, d, c:c + 1])
                P.act(t_y[:, :nb], t_r[:, :nb], AF.Exp, [t_r, kap2], [t_y], scale=kap2[:, d, c:c + 1])
                P.ts(t_y[:, :nb], t_y[:, :nb], -1.0, 1.0, ALU.mult, ALU.add, [t_y], [t_y])
                P.ts(t_y[:, :nb], t_y[:, :nb], 1e-30, None, ALU.max, None, [t_y], [t_y])
                P.act(t_y[:, :nb], t_y[:, :nb], AF.Ln, [t_y], [t_y])
                P.act(t_y[:, :nb], t_y[:, :nb], AF.Exp, [t_y], [t_y], scale=0.5)
                P.tt(t_b[:, :nb], t_y[:, :nb], t_i[:, :nb], ALU.mult, [t_y, t_i], [t_b])
                P.tt(t_b[:, :nb], t_b[:, :nb], u[:, t0:t0 + nb], ALU.mult, [t_b, u], [t_b])
                if d == 0:
                    init = 0.0 if oi == 0 else hsum[:, t0 - 1:t0]
                    P.op("dve", lambda e, t0=t0, nb=nb, init=init: e.tensor_tensor_scan(
                        out=hsum[:, t0:t0 + nb], data0=t_a[:, :nb], data1=t_b[:, :nb], initial=init,
                        op0=ALU.mult, op1=ALU.add), [t_a, t_b, hsum], [hsum])
                else:
                    th = t_h0 if oi % 2 == 0 else t_h1
                    if oi == 0:
                        init, rd = 0.0, []
                    else:
                        init, rd = prev_h[1], [prev_h[0]]
                    P.op("dve", lambda e, nb=nb, init=init, th=th: e.tensor_tensor_scan(
                        out=th[:, :nb][:, ::-1], data0=t_a[:, :nb][:, ::-1], data1=t_b[:, :nb][:, ::-1], initial=init,
                        op0=ALU.mult, op1=ALU.add), [t_a, t_b] + rd, [th])
                    prev_h = (th, th[:, 0:1])
                    P.tt(hsum[:, t0:t0 + nb], hsum[:, t0:t0 + nb], th[:, :nb], ALU.add, [hsum, th], [hsum])
        if c == 0:
            tap("L0_hsum0", hsum, hsum[:], [128, T])
        for b, (t0, nb) in enumerate(BLOCKS):
            ps = P.banks[pi % 4]
            pi += 1
            for kc in range(KC):
                P.mm(ps[:, :nb], wgl[:, kc, c * 128:(c + 1) * 128], hT[:, kc, t0:t0 + nb], kc == 0, kc == KC - 1, [wgl, hT], [ps])
            P.copy(t_r[:, :nb], ps[:, :nb], [ps], [t_r], eng="act")
            P.act(t_i[:, :nb], ps[:, :nb], AF.Square, [ps], [t_i])
            P.ts(t_i[:, :nb], t_i[:, :nb], 0.044715, 1.0, ALU.mult, ALU.add, [t_i], [t_i])
            P.tt(t_i[:, :nb], t_i[:, :nb], t_r[:, :nb], ALU.mult, [t_i, t_r], [t_i])
            P.act(t_i[:, :nb], t_i[:, :nb], AF.Sigmoid, [t_i], [t_i], scale=GELU_C)
            P.tt(t_i[:, :nb], t_i[:, :nb], t_r[:, :nb], ALU.mult, [t_i, t_r], [t_i])
            P.tt(lruT[:, c, t0:t0 + nb], t_i[:, :nb], hsum[:, t0:t0 + nb], ALU.mult, [t_i, hsum], [lruT])
    tap("L0_lru", lruT, lruT[:], [128, 4, T])


def moe_layer(k, s, l, hT, xT, with_ctx, tap):
    P = k.P
    dr = k.dr
    SI = lambda b: 4 if b == 0 else s
    blocks = list(enumerate(BLOCKS)) if with_ctx else list(enumerate(BLOCKS))[1:]
    with P.phase():
        gateT = P.sbuf("gateT", [16, T], F32)
        with P.phase():
            sc = NormScratch(k, P.banks[0])
            hf = P.sbuf("hf", [128, KC, 512], F32)
            wrt = P.sbuf("wrt", [128, KC, 16], F32)
            ones16 = P.sbuf("ones16", [16, 16], F32)
            ex = P.sbuf("ex", [16, 512], F32)
            P.memset(ones16[:], 1.0, [ones16])
            P.dma("sp", wrt[:], dr["router_w"][l].rearrange("(kc p) e -> p kc e", p=128), wrt, dr["router_w"])
            psl = P.banks[1]
            pss = P.banks[2]
            for b, (t0, nb) in blocks:
                xn = rms_block(k, sc, xT[:, :, t0:t0 + nb], xT, nb)
                for kc in range(KC):
                    P.ts(hf[:, kc, :nb], xn[:, kc, :nb], k.Amod[:, l, 1, kc, SI(b):SI(b) + 1], mvec(k, l, 3, kc, SI(b)),
                         ALU.mult, ALU.add, [xn, k.Amod, k.modv], [hf])
                P.copy(hT[:, :, t0:t0 + nb], hf[:, :, :nb], [hf], [hT], eng="act")
                for kc in range(KC):
                    P.mm(psl[0:16, :nb], wrt[:, kc, :], hf[:, kc, :nb], kc == 0, kc == KC - 1, [wrt, hf], [psl])
                P.act(ex[:, :nb], psl[0:16, :nb], AF.Exp, [psl], [ex])
                P.mm(pss[0:16, :nb], ones16[:], ex[:, :nb], True, True, [ones16, ex], [pss])
                P.op("dve", lambda e, nb=nb: e.reciprocal(out=sc.rs[0:16, :nb], in_=pss[0:16, :nb]), [pss], [sc.rs])
                P.tt(gateT[:, t0:t0 + nb], ex[:, :nb], sc.rs[0:16, :nb], ALU.mult, [ex, sc.rs], [gateT])
            tap("L%d_aff" % l, gateT, gateT[:], [16, T])
            work = P.sbuf("work", [16, S], F32)
            m8 = P.sbuf("m8", [16, 8], F32)
            segs = [(LC, S, 8)] + ([(0, LC, 8)] if with_ctx else [])
            for (a0, n, _) in segs:
                cap = 2 * n // 16
                P.copy(work[:, :n], gateT[:, a0:a0 + n], [gateT], [work])
                for r in range(cap // 8):
                    P.op("dve", lambda e, n=n: e.max(out=m8[:], in_=work[:, :n]), [work], [m8])
                    if r < cap // 8 - 1:
                        P.op("dve", lambda e, n=n: e.match_replace(out=work[:, :n], in_to_replace=m8[:], in_values=work[:, :n],
                                                                   imm_value=-1.0), [work, m8], [work])
                P.stt(gateT[:, a0:a0 + n], gateT[:, a0:a0 + n], m8[:, 7:8], gateT[:, a0:a0 + n], ALU.is_ge, ALU.mult,
                      [gateT, m8], [gateT])
            tap("L%d_gate" % l, gateT, gateT[:], [16, T])
            tap("L%d_hffn" % l, hT, hT[:], [128, KC, T])
        with P.phase():
            wr = WRing(k, 4)
            id16 = P.sbuf("id16", [16, 16], F32)
            P.dma("sp", id16[:], dr["id16"][:], id16, dr["id16"])
            hid = P.sbuf("hid", [128, KC, T], BF16)
            gb = [P.sbuf("gb%d" % i, [128, 512], F32) for i in range(2)]
            s1 = [P.sbuf("s1_%d" % i, [128, 512], F32) for i in range(2)]
            pi = 0
            it = 0
            for e in range(16):
                for h in range(2):
                    w1 = wr.load(dr["exp_w1"][l, e][:, h * 512:(h + 1) * 512], dr["exp_w1"])
                    w3 = wr.load(dr["exp_w3"][l, e][:, h * 512:(h + 1) * 512], dr["exp_w3"])
                    for b, (t0, nb) in blocks:
                        g_ = gb[it % 2]
                        it += 1
                        psg = P.banks[6 + it % 2]
                        P.mm(psg[:, :nb], id16[:, e:e + 1].to_broadcast([16, 128]), gateT[:, t0:t0 + nb], True, True,
                             [id16, gateT], [psg])
                        P.copy(g_[:, :nb], psg[:, :nb], [psg], [g_], eng="act")
                        for f4 in range(4):
                            f = h * 4 + f4
                            ps1 = P.banks[pi % 6]
                            ps3 = P.banks[(pi + 1) % 6]
                            pi += 2
                            st = s1[(pi // 2) % 2]
                            for kc in range(KC):
                                P.mm(ps1[:, :nb], w1[:, kc, f4 * 128:(f4 + 1) * 128], hT[:, kc, t0:t0 + nb], kc == 0, kc == KC - 1,
                                     [w1, hT], [ps1])
                            for kc in range(KC):
                                P.mm(ps3[:, :nb], w3[:, kc, f4 * 128:(f4 + 1) * 128], hT[:, kc, t0:t0 + nb], kc == 0, kc == KC - 1,
                                     [w3, hT], [ps3])
                            P.act(st[:, :nb], ps1[:, :nb], AF.Silu, [ps1], [st])
                            P.tt(st[:, :nb], st[:, :nb], ps3[:, :nb], ALU.mult, [st, ps3], [st])
                            P.tt(hid[:, f, t0:t0 + nb], st[:, :nb], g_[:, :nb], ALU.mult, [st, g_], [hid])
                for h2 in range(2):
                    w2 = wr.load(dr["exp_w2"][l, e][:, h2 * 512:(h2 + 1) * 512], dr["exp_w2"])
                    for b, (t0, nb) in blocks:
                        for o4 in range(4):
                            oc = h2 * 4 + o4
                            psy = P.banks[pi % 6]
                            pi += 1
                            for f in range(KC):
                                P.mm(psy[:, :nb], w2[:, f, o4 * 128:(o4 + 1) * 128], hid[:, f, t0:t0 + nb], f == 0, f == KC - 1,
                                     [w2, hid], [psy])
                            P.stt(xT[:, oc, t0:t0 + nb], psy[:, :nb], mvec(k, l, 5, oc, SI(b)), xT[:, oc, t0:t0 + nb],
                                  ALU.mult, ALU.add, [psy, k.modv, xT], [xT])


RB = [(0, 256)] + [(256 + 256 * i, 256) for i in range(8)]
V_MU, V_W0, V_A0, V_KK, V_KA, V_RK, V_GG, V_GB = 0, 6, 8, 10, 11, 12, 13, 14
NEG_EXP_HALF = -0.6065306597126334


def layer1_mixer(k, s, hT, xT, tap):
    P = k.P
    dr = k.dr
    SI = lambda b: 4 if b == 0 else s
    with P.phase():
        sc = NormScratch(k, P.banks[0])
        for b, (t0, nb) in enumerate(BLOCKS):
            xn = rms_block(k, sc, xT[:, :, t0:t0 + nb], xT, nb)
            for kc in range(KC):
                P.ts(hT[:, kc, t0:t0 + nb], xn[:, kc, :nb], k.Amod[:, 1, 0, kc, SI(b):SI(b) + 1],
                     mvec(k, 1, 0, kc, SI(b)), ALU.mult, ALU.add, [xn, k.Amod, k.modv], [hT])
        P.dma("sp", k.xspill[:].rearrange("(kc p) t -> p kc t", p=128), xT[:, :, LC:T], k.xspill, xT)
        tap("L1_h", hT, hT[:], [128, KC, T])
    if k.stop_after == "L1A":
        return
    with P.phase():
        outT = P.sbuf("outT", [128, KC, S], BF16)
        with P.phase():
            rwkv_core(k, s, hT, xT, outT, tap)
        if k.stop_after in ("L1B", "L1L", "L1P"):
            return
        with P.phase():
            wr = WRing(k, 2)
            wo = [wr.load(dr["rw_wo"][:, h * 512:(h + 1) * 512], dr["rw_wo"]) for h in range(2)]
            P.dma("sp", xT[:, :, LC:T], k.xspill[:].rearrange("(kc p) t -> p kc t", p=128), xT, k.xspill)
            pi = 0
            for b, (t0, nb) in list(enumerate(BLOCKS))[1:]:
                for oc in range(KC):
                    ps = P.banks[pi % 4]
                    pi += 1
                    w = wo[oc // 4]
                    c0 = (oc % 4) * 128
                    for c in range(KC):
                        P.mm(ps[:, :nb], w[:, c, c0:c0 + 128], outT[:, c, t0 - LC:t0 - LC + nb], c == 0, c == KC - 1,
                             [w, outT], [ps])
                    P.stt(xT[:, oc, t0:t0 + nb], ps[:, :nb], mvec(k, 1, 2, oc, s), xT[:, oc, t0:t0 + nb], ALU.mult, ALU.add,
                          [ps, k.modv, xT], [xT])
            tap("L1_xmix", xT, xT[:], [128, KC, T])


def rwkv_core(k, s, hT, xT, outT, tap):
    P = k.P
    dr = k.dr
    with P.overlay(xT):
        lw1 = P.sbuf("lw1", [128, T], BF16)
        la1 = P.sbuf("la1", [128, T], BF16)
        lg1 = P.sbuf("lg1", [128, T], BF16)
        lg1b = P.sbuf("lg1b", [32, T], BF16)
        y_acc = P.sbuf("y_acc", [128, S], F32)
        bon = P.sbuf("bon", [128, S], F32)
    ov_mark = xT.ov_top
    vec = P.sbuf("rwvec", [128, 15, KC], F32)
    P.dma("sp", vec[:], dr["rw_vec"][:], vec, dr["rw_vec"])
    omk = P.sbuf("omk", [128, KC], F32)
    P.ts(omk[:], vec[:, V_KA, :], -1.0, 1.0, ALU.mult, ALU.add, [vec], [omk])
    omu = P.sbuf("omu", [128, 6, KC], F32)
    hmu = P.sbuf("hmu", [128, 6, KC], F32)
    P.ts(omu[:], vec[:, 0:6, :], -1.0, 1.0, ALU.mult, ALU.add, [vec], [omu])
    P.ts(hmu[:], vec[:, 0:6, :], 0.5, None, ALU.mult, None, [vec], [hmu])
    identb = P.sbuf("identb", [128, 128], BF16)
    P.dma("pool", identb[:], dr["ident"][:], identb, dr["ident"])
    identf = P.sbuf("identf", [128, 128], F32)
    P.dma("sp", identf[:], dr["ident"][:], identf, dr["ident"])
    bones = P.sbuf("bones", [128, 128], F32)
    P.dma("sp", bones[:], dr["blockones"][:], bones, dr["blockones"])
    maskC = P.sbuf("maskC", [128, 256], F32)
    P.dma("sp", maskC[:], dr["maskC"][:], maskC, dr["maskC"])
    m4 = [P.sbuf("m4_%d" % d, [128, 512], BF16) for d in range(2)]
    m1 = [P.sbuf("m1_%d" % d, [128, 128], BF16) for d in range(2)]
    for d in range(2):
        P.dma("pool", m4[d][:], dr["rmask4"][d], m4[d], dr["rmask4"])
        P.dma("pool", m1[d][:], dr["rmask1"][d], m1[d], dr["rmask1"])

    def vcol(i, c):
        return vec[:, i, c:c + 1]

    def load_scaled(name, src2d, ncols, mu_i):
        wa = P.sbuf(name + "a", [128, KC, ncols], BF16)
        wb = P.sbuf(name + "b", [128, KC, ncols], BF16)
        P.dma("pool", wa[:], src2d.rearrange("(kc p) n -> p kc n", p=128), wa, dr["rw_rkv"])
        for kc in range(KC):
            P.ts(wb[:, kc, :], wa[:, kc, :], hmu[:, mu_i, kc:kc + 1], None, ALU.mult, None, [wa, hmu], [wb])
        for kc in range(KC):
            P.ts(wa[:, kc, :], wa[:, kc, :], omu[:, mu_i, kc:kc + 1], None, ALU.mult, None, [wa, omu], [wa])
        return wa, wb

    def proj_mix(ps_ap, psbuf, wa, wb, c0, m, t0, nb):
        lo = 1 if t0 in (0, LC) else 0
        hi = nb - 1 if (t0 + nb) in (LC, T) else nb
        for kc in range(KC):
            P.mm(ps_ap, wa[:, kc, c0:c0 + m], hT[:, kc, t0:t0 + nb], kc == 0, False, [wa, hT], [psbuf])
        for kc in range(KC):
            P.mm(ps_ap[:, lo:nb], wb[:, kc, c0:c0 + m], hT[:, kc, t0 + lo - 1:t0 + nb - 1], False, False, [wb, hT], [psbuf])
        for kc in range(KC):
            P.mm(ps_ap[:, 0:hi], wb[:, kc, c0:c0 + m], hT[:, kc, t0 + 1:t0 + hi + 1], False, kc == KC - 1, [wb, hT], [psbuf])

    with P.phase():
        w1, w1s = load_scaled("w1c", dr["rw_w1cat"][:], 128, 1)
        a1, a1s = load_scaled("a1c", dr["rw_a1cat"][:], 128, 4)
        g1, g1s = load_scaled("g1c", dr["rw_g1"][:], 160, 5)
        pi = 0
        for (t0, nb) in BLOCKS:
            ps = P.banks[pi % 4]; pi += 1
            proj_mix(ps[:, :nb], ps, w1, w1s, 0, 128, t0, nb)
            P.act(lw1[:, t0:t0 + nb], ps[:, :nb], AF.Tanh, [ps], [lw1])
            ps = P.banks[pi % 4]; pi += 1
            proj_mix(ps[:, :nb], ps, a1, a1s, 0, 128, t0, nb)
            P.copy(la1[:, t0:t0 + nb], ps[:, :nb], [ps], [la1], eng="act")
            ps = P.banks[pi % 4]; pi += 1
            proj_mix(ps[:, :nb], ps, g1, g1s, 0, 128, t0, nb)
            P.act(lg1[:, t0:t0 + nb], ps[:, :nb], AF.Sigmoid, [ps], [lg1])
            ps = P.banks[pi % 4]; pi += 1
            proj_mix(ps[0:32, :nb], ps, g1, g1s, 128, 32, t0, nb)
            P.act(lg1b[:, t0:t0 + nb], ps[0:32, :nb], AF.Sigmoid, [ps], [lg1b])
    if k.stop_after == "L1L":
        tap("L1_lw1", lw1, lw1[:], [128, T])
        return
    for c in range(KC if k.stop_after != "L1P" else 1):
        with P.phase():
            rwkv_pair(k, s, c, hT, xT, ov_mark, lw1, la1, lg1, lg1b, y_acc, bon, outT, vec, omk, identb, identf, bones, maskC,
                      m4, m1, load_scaled, proj_mix, vcol, tap)
    tap("L1_og", outT, outT[:], [128, KC, S])


class DirBufs:
    def __init__(self, P, d):
        NB = 256
        self.f = [P.sbuf("rf%d_%d" % (d, i), [128, NB], F32) for i in range(10)]
        self.Vb = P.sbuf("Vb%d" % d, [128, NB], BF16)
        self.ATz, self.RTz, self.BTz, self.KTz = [[P.sbuf("z%d_%d_%d" % (d, i, h), [128, NB], BF16) for h in range(2)]
                                                  for i in range(4)]
        self.stk = [P.sbuf("stk%d_%d" % (d, i), [128, 192], BF16) for i in range(4)]
        self.Vz = [[P.sbuf("Vz%d_%d_%d" % (d, i, h), [128, 64], BF16) for h in range(2)] for i in range(4)]
        self.Uz = [P.sbuf("Uz%d_%d" % (d, h), [128, 64], BF16) for h in range(2)]
        self.mats = [P.sbuf("mats%d_%d" % (d, i), [128, 640], BF16) for i in range(4)]
        self.Lf = [P.sbuf("Lf%d_%d" % (d, i), [128, 256], F32) for i in range(4)]
        self.sq = [[P.sbuf("sq%d_%d_%d" % (d, i, j), [128, 256], F32) for j in range(2)] for i in range(4)]
        self.ZT = [P.sbuf("ZT%d_%d" % (d, i), [128, 128], F32) for i in range(4)]
        self.T0 = P.sbuf("T0_%d" % d, [128, 64], F32)
        self.T0s = P.sbuf("T0s_%d" % d, [128, 64], F32)
        self.T0b = P.sbuf("T0b_%d" % d, [128, 64], BF16)
        self.Xb = P.sbuf("Xb_%d" % d, [128, 64], F32)
        zs = [t_ for l_ in (self.ATz, self.RTz, self.BTz, self.KTz) for t_ in l_] + [v_ for l_ in self.Vz for v_ in l_] + self.Uz
        for t_ in zs + [self.T0, self.T0b]:
            P.memset(t_[:], 0.0, [t_])


def rwkv_pair(k, s, c, hT, xT, ov_mark, lw1, la1, lg1, lg1b, y_acc, bon, outT, vec, omk, identb, identf, bones, maskC, m4, m1,
              load_scaled, proj_mix, vcol, tap):
    P = k.P
    dr = k.dr
    cs_ = slice(c * 128, (c + 1) * 128)
    wr_, wrs = load_scaled("wr", dr["rw_rkv"][0][:, cs_], 128, 0)
    wk_, wks = load_scaled("wk", dr["rw_rkv"][1][:, cs_], 128, 2)
    wv_, wvs = load_scaled("wv", dr["rw_rkv"][2][:, cs_], 128, 3)
    w2c = P.sbuf("w2c", [128, 128], BF16)
    a2c = P.sbuf("a2c", [128, 128], BF16)
    g2a = P.sbuf("g2a", [128, 128], BF16)
    g2b = P.sbuf("g2b", [32, 128], BF16)
    P.dma("pool", w2c[:], dr["rw_w2cat"][:, cs_], w2c, dr["rw_w2cat"])
    P.dma("pool", a2c[:], dr["rw_a2cat"][:, cs_], a2c, dr["rw_a2cat"])
    P.dma("pool", g2a[:], dr["rw_g2"][0:128, cs_], g2a, dr["rw_g2"])
    P.dma("pool", g2b[:], dr["rw_g2"][128:160, cs_], g2b, dr["rw_g2"])
    P.memset(y_acc[:], 0.0, [y_acc])
    P.memset(bon[:], 0.0, [bon])
    bufs = [DirBufs(P, 0)]
    with P.overlay(xT, start=ov_mark):
        bufs.append(DirBufs(P, 1))
    sqb = bufs[0].f[0]

    def dir_gen(d):
        W = bufs[d]
        B = P.banks[4 * d:4 * d + 4]
        r_f, k_f, v_f, kk_f, ic_f, lw_f, cs_f, e1, e2, tmp = W.f
        ATz, RTz, BTz, KTz, Vb, stk, Vz, Uz, mats, Lf, sq, ZT = (W.ATz, W.RTz, W.BTz, W.KTz, W.Vb, W.stk, W.Vz, W.Uz, W.mats,
                                                                W.Lf, W.sq, W.ZT)
        T0, T0s, T0b, Xb = W.T0, W.T0s, W.T0b, W.Xb
        order = list(range(9)) if d == 0 else [0] + list(range(8, 0, -1))
        for b in order:
            t0, nb = RB[b]
            lat = b > 0
            l0 = t0 - LC
            proj_mix(B[0][:, :nb], B[0], wr_, wrs, 0, 128, t0, nb)
            yield
            proj_mix(B[1][:, :nb], B[1], wk_, wks, 0, 128, t0, nb)
            yield
            proj_mix(B[2][:, :nb], B[2], wv_, wvs, 0, 128, t0, nb)
            pr_d = slice(d * 64, (d + 1) * 64)
            P.mm(B[3][:, 0:nb], w2c[pr_d, :], lw1[pr_d, t0:t0 + nb], True, True, [w2c, lw1], [B[3]])
            P.mm(B[3][:, 256:256 + nb], a2c[pr_d, :], la1[pr_d, t0:t0 + nb], True, True, [a2c, la1], [B[3]])
            P.copy(r_f[:], B[0][:, :nb], [B[0]], [r_f], eng="act")
            P.copy(k_f[:], B[1][:, :nb], [B[1]], [k_f], eng="act")
            P.copy(v_f[:], B[2][:, :nb], [B[2]], [v_f], eng="act")
            P.copy(Vb[:], v_f[:], [v_f], [Vb])
            yield
            P.act(lw_f[:], B[3][:, 0:nb], AF.Sigmoid, [B[3], vec], [lw_f], bias=vcol(V_W0 + d, c))
            P.ts(lw_f[:], lw_f[:], NEG_EXP_HALF, None, ALU.mult, None, [lw_f], [lw_f])
            P.act(ic_f[:], B[3][:, 256:256 + nb], AF.Sigmoid, [B[3], vec], [ic_f], bias=vcol(V_A0 + d, c))
            P.ts(kk_f[:], k_f[:], vcol(V_KK, c), None, ALU.mult, None, [k_f, vec], [kk_f])
            P.tt(tmp[:], kk_f[:], kk_f[:], ALU.mult, [kk_f], [tmp])
            P.mm(B[0][:, :nb], bones[:], tmp[:], True, True, [bones, tmp], [B[0]])
            yield
            P.ts(e1[:], B[0][:, :nb], 1e-18, None, ALU.max, None, [B[0]], [e1])
            P.act(e1[:], e1[:], AF.Ln, [e1], [e1])
            P.act(e1[:], e1[:], AF.Exp, [e1], [e1], scale=-0.5)
            P.tt(kk_f[:], kk_f[:], e1[:], ALU.mult, [kk_f, e1], [kk_f])
            P.ts(tmp[:], ic_f[:], vcol(V_KA, c), omk[:, c:c + 1], ALU.mult, ALU.add, [ic_f, vec, omk], [tmp])
            P.tt(k_f[:], k_f[:], tmp[:], ALU.mult, [k_f, tmp], [k_f])
            if lat:
                P.stt(tmp[:], r_f[:], vcol(V_RK, c), k_f[:], ALU.mult, ALU.mult, [r_f, vec, k_f], [tmp])
                P.mm(B[1][:, :nb], bones[:], tmp[:], True, True, [bones, tmp], [B[1]])
                yield
                P.tt(tmp[:], B[1][:, :nb], v_f[:], ALU.mult, [B[1], v_f], [tmp])
                P.tt(bon[:, l0:l0 + nb], bon[:, l0:l0 + nb], tmp[:], ALU.add, [bon, tmp], [bon])
            if d == 0:
                P.op("dve", lambda e: e.tensor_tensor_scan(out=cs_f[:], data0=maskC[:], data1=lw_f[:], initial=0.0,
                                                           op0=ALU.mult, op1=ALU.add), [maskC, lw_f], [cs_f])
            else:
                P.op("dve", lambda e: e.tensor_tensor_scan(out=cs_f[:, ::-1], data0=maskC[:], data1=lw_f[:, ::-1], initial=0.0,
                                                           op0=ALU.mult, op1=ALU.add), [maskC, lw_f], [cs_f])
            P.tt(e2[:], cs_f[:], lw_f[:], ALU.subtract, [cs_f, lw_f], [e2])
            P.act(e2[:], e2[:], AF.Exp, [e2], [e2])
            for h in range(2):
                pr = slice(h * 64, (h + 1) * 64)
                P.stt(ATz[h][pr], kk_f[pr], -1.0, e2[pr], ALU.mult, ALU.mult, [kk_f, e2], [ATz[h]])
            P.act(e1[:], cs_f[:], AF.Exp, [cs_f], [e1])
            for h in range(2):
                pr = slice(h * 64, (h + 1) * 64)
                P.tt(RTz[h][pr], r_f[pr], e1[pr], ALU.mult, [r_f, e1], [RTz[h]])
            P.act(e2[:], cs_f[:], AF.Exp, [cs_f], [e2], scale=-1.0)
            P.tt(tmp[:], kk_f[:], ic_f[:], ALU.mult, [kk_f, ic_f], [tmp])
            for h in range(2):
                pr = slice(h * 64, (h + 1) * 64)
                P.tt(BTz[h][pr], tmp[pr], e2[pr], ALU.mult, [tmp, e2], [BTz[h]])
                P.tt(KTz[h][pr], k_f[pr], e2[pr], ALU.mult, [k_f, e2], [KTz[h]])
            yield
            nch = nb // 64
            for n in range(nch):
                cols = slice(n * 64, (n + 1) * 64)
                pb = B[n % 2]
                for h in range(2):
                    pr = slice(h * 64, (h + 1) * 64)
                    for mi, src in enumerate((Vb, BTz[h], KTz[h])):
                        P.mm(pb[pr, mi * 64:(mi + 1) * 64], src[:, cols], identb[:, pr], True, True, [src, identb], [pb])
                P.copy(stk[n][:], pb[:, 0:192], [pb], [stk[n]], eng="act")
                for h in range(2):
                    pr = slice(h * 64, (h + 1) * 64)
                    P.copy(Vz[n][h][pr, :], stk[n][pr, 0:64], [stk[n]], [Vz[n][h]])
                yield
            for n in range(nch):
                cols = slice(n * 64, (n + 1) * 64)
                pa = B[2 + n % 2]
                pb = B[n % 2]
                for h in range(2):
                    pr = slice(h * 64, (h + 1) * 64)
                    combos = ((BTz[h], ATz[h]), (ATz[h], BTz[h]), (KTz[h], ATz[h]), (BTz[h], RTz[h]))
                    for mi, (lh, rh) in enumerate(combos):
                        P.mm(pa[pr, mi * 128 + h * 64: mi * 128 + (h + 1) * 64], lh[:, cols], rh[:, cols], True, True,
                             [lh, rh], [pa])
                    P.mm(pb[pr, 256 + h * 64:256 + (h + 1) * 64], KTz[h][:, cols], RTz[h][:, cols], True, True,
                         [KTz[h], RTz[h]], [pb])
                P.tt(Lf[n][:, 0:256], pa[:, 0:256], m4[d][:, 0:256], ALU.mult, [pa, m4[d]], [Lf[n]])
                P.tt(mats[n][:, 256:512], pa[:, 256:512], m4[d][:, 256:512], ALU.mult, [pa, m4[d]], [mats[n]])
                P.tt(mats[n][:, 512:640], pb[:, 256:384], m1[d][:], ALU.mult, [pb, m1[d]], [mats[n]])
                yield
            cur = []
            for n in range(nch):
                P.tt(ZT[n][:], Lf[n][:, 0:128], identf[:], ALU.add, [Lf[n], identf], [ZT[n]])
                cur.append((Lf[n], 128, 0))
            for lvl in range(5):
                last = lvl == 4
                for n in range(nch):
                    src, oL, oLT = cur[n]
                    dst = sq[n][lvl % 2]
                    pa = B[n % 2]
                    P.mm(pa[:, 0:128], src[:, oLT:oLT + 128], src[:, oL:oL + 128], True, True, [src], [pa])
                    if not last:
                        P.mm(pa[:, 128:256], src[:, oL:oL + 128], src[:, oLT:oLT + 128], True, True, [src], [pa])
                        P.copy(dst[:, 0:256], pa[:, 0:256], [pa], [dst], eng="act")
                    else:
                        P.copy(dst[:, 0:128], pa[:, 0:128], [pa], [dst], eng="act")
                    cur[n] = (dst, 0, 128)
                    if n % 2 == 1:
                        yield
                for n in range(nch):
                    src, oL, oLT = cur[n]
                    pz = B[2 + n % 2]
                    P.mm(pz[:, 0:128], src[:, oL:oL + 128], ZT[n][:], True, True, [src, ZT[n]], [pz])
                    P.tt(ZT[n][:], ZT[n][:], pz[:, 0:128], ALU.add, [ZT[n], pz], [ZT[n]])
                    if n % 2 == 1:
                        yield
            chunks = list(range(nch)) if d == 0 else list(range(nch - 1, -1, -1))
            for n in chunks:
                cols = slice(n * 64, (n + 1) * 64)
                dcol = (n * 64 + 63) if d == 0 else n * 64
                dC = e1[:, dcol:dcol + 1]
                pX = B[0]
                for h in range(2):
                    pr = slice(h * 64, (h + 1) * 64)
                    P.mm(pX[pr, 0:64], ATz[h][:, cols], T0b[:, :], True, False, [ATz[h], T0b], [pX])
                P.mm(pX[:, 0:64], mats[n][:, 256:384], stk[n][:, 0:64], False, True, [mats[n], stk[n]], [pX])
                P.ts(T0s[:], T0[:], dC, None, ALU.mult, None, [T0, e1], [T0s])
                yield
                P.copy(Xb[:], pX[:, 0:64], [pX], [Xb], eng="act")
                pU = B[1]
                P.mm(pU[:, 0:64], ZT[n][:], Xb[:], True, True, [ZT[n], Xb], [pU])
                yield
                P.copy(Uz[0][0:64, :], pU[0:64, 0:64], [pU], [Uz[0]], eng="act")
                P.copy(Uz[1][64:128, :], pU[64:128, 0:64], [pU], [Uz[1]])
                pT = B[3]
                for h in range(2):
                    pr = slice(h * 64, (h + 1) * 64)
                    P.mm(pT[pr, 0:64], stk[n][:, 64:128], Uz[h][:, :], True, False, [stk[n], Uz[h]], [pT])
                    P.mm(pT[pr, 0:64], stk[n][:, 128:192], Vz[n][h][:, :], False, True, [stk[n], Vz[n][h]], [pT])
                if lat:
                    pY = B[2]
                    for h in range(2):
                        pr = slice(h * 64, (h + 1) * 64)
                        P.mm(pY[pr, 0:64], T0b[:, :], RTz[h][:, cols], True, False, [T0b, RTz[h]], [pY])
                        P.mm(pY[pr, 0:64], Uz[h][:, :], mats[n][:, 384 + h * 64:384 + (h + 1) * 64], False, False,
                             [Uz[h], mats[n]], [pY])
                        P.mm(pY[pr, 0:64], stk[n][:, 0:64], mats[n][:, 512 + h * 64:512 + (h + 1) * 64], False, True,
                             [stk[n], mats[n]], [pY])
                yield
                P.stt(T0[:], pT[:, 0:64], dC, T0s[:], ALU.mult, ALU.add, [pT, e1, T0s], [T0])
                P.copy(T0b[:], T0[:], [T0], [T0b], eng="act")
                if lat:
                    yc = slice(l0 + n * 64, l0 + (n + 1) * 64)
                    P.tt(y_acc[:, yc], y_acc[:, yc], pY[:, 0:64], ALU.add, [y_acc, pY], [y_acc])
                yield

    gens = [dir_gen(0), dir_gen(1)]
    alive = [True, True]
    while any(alive):
        for gi, g in enumerate(gens):
            if alive[gi]:
                try:
                    next(g)
                except StopIteration:
                    alive[gi] = False
    if c == 0:
        tap("L1_y0", y_acc, y_acc[:], [128, S])
        tap("L1_bon0", bon, bon[:], [128, S])
    B = P.banks
    sqb2 = bufs[1].f[0]
    for i in range(4):
        l0 = i * 512
        t0 = LC + l0
        nb = 512
        yb = y_acc[:, l0:l0 + nb]
        sq_ = [bufs[0].f[0], bufs[0].f[1]]
        for hf in range(2):
            c0_ = l0 + hf * 256
            ybh = y_acc[:, c0_:c0_ + 256]
            sc_ = bufs[hf].f[0]
            P.mm(B[0][:, hf * 256:(hf + 1) * 256], bones[:], ybh, True, True, [bones, y_acc], [B[0]])
            P.stt(ybh, B[0][:, hf * 256:(hf + 1) * 256], -1.0 / 64, ybh, ALU.mult, ALU.add, [B[0], y_acc], [y_acc])
            P.tt(sc_[:], ybh, ybh, ALU.mult, [y_acc], [sc_])
            P.mm(B[1][:, hf * 256:(hf + 1) * 256], bones[:], sc_[:], True, True, [bones, sc_], [B[1]])
            P.act(sc_[:], B[1][:, hf * 256:(hf + 1) * 256], AF.Ln, [B[1], k.gneps], [sc_], scale=1.0 / 64, bias=k.gneps[:, 0:1])
            P.act(sc_[:], sc_[:], AF.Exp, [sc_], [sc_], scale=-0.5)
            P.tt(ybh, ybh, sc_[:], ALU.mult, [y_acc, sc_], [y_acc])
        P.ts(yb, yb, vcol(V_GG, c), vcol(V_GB, c), ALU.mult, ALU.add, [y_acc, vec], [y_acc])
        P.tt(yb, yb, bon[:, l0:l0 + nb], ALU.add, [y_acc, bon], [y_acc])
        P.mm(B[2][:, :nb], g2a[:], lg1[:, t0:t0 + nb], True, False, [g2a, lg1], [B[2]])
        P.mm(B[2][:, :nb], g2b[:], lg1b[:, t0:t0 + nb], False, True, [g2b, lg1b], [B[2]])
        P.tt(outT[:, c, l0:l0 + nb], yb, B[2][:, :nb], ALU.mult, [y_acc, B[2]], [outT])


_CACHE = {}


def kernel(**inputs):
    inp = {k_: np.asarray(v) for k_, v in inputs.items()}
    B = inp["x"].shape[0]
    ns = B // NCORE
    if "nc" not in _CACHE:
        _CACHE["nc"] = build(ns=ns)[0]
    nc = _CACHE["nc"]
    sh = prep_shared(inp)
    in_maps = []
    for core in range(NCORE):
        m = dict(sh)
        m.update(prep_core(inp, core, ns))
        in_maps.append(m)
    res = run_bass_kernel_spmd(nc, in_maps, core_ids=list(range(NCORE)))
    out = np.empty((B, S, D), np.float32)
    for core in range(NCORE):
        o = res.results[core]["outT"]
        for s in range(ns):
            out[core * ns + s] = o[s].T
    return out
```

```python
from contextlib import ExitStack, contextmanager
import numpy as np
import concourse.bass as bass
import concourse.mybir as mybir
from concourse.bass_utils import run_bass_kernel_spmd

F32 = mybir.dt.float32
BF16 = mybir.dt.bfloat16
AF = mybir.ActivationFunctionType
ALU = mybir.AluOpType
AX = mybir.AxisListType

ENGS = ("pe", "dve", "act", "pool", "sp")

D = 1024
KC = 8
S = 2048
LC = 256
T = S + LC
NCORE = 8
BLOCKS = [(0, 256), (256, 512), (768, 512), (1280, 512), (1792, 512)]
LAT_BLOCKS = BLOCKS[1:]
EPS = 1e-6
ARENA_BYTES = 206 * 1024


class Buf:
    def __init__(self, prog, t, name, space):
        self.prog = prog
        self.t = t
        self.name = name
        self.space = space
        self.last_w = None
        self.reads = {}
        self.dsem = None

    def __getitem__(self, idx):
        return self.t[idx]

    def view(self, name):
        b = Buf(self.prog, self.t, name, self.space)
        self.prog._register(b)
        return b


class Prog:
    def __init__(self):
        self.nc = bass.Bass("TRN2", target_bir_lowering=False)
        self.gstack = ExitStack()
        self.ops = {e: [] for e in ENGS}
        self.cnt = {e: 0 for e in ENGS}
        self.waited = {e: {} for e in ENGS}
        self.sems = {}
        self.dpool = []
        self.dcnt = {}
        self.live_dsem = set()
        self.ndsem = 0
        self.live_bufs = []
        self.phase_bufs = None
        self.uid = 0
        for e in ENGS:
            self._sem("eng_" + e)
        self.arena = self.gstack.enter_context(self.nc.sbuf_tensor("arena", [128, ARENA_BYTES // 4], F32))
        self.top = 0
        self.limit = ARENA_BYTES
        self.hiwater = 0
        self.banks = []
        for i in range(8):
            t = self.gstack.enter_context(self.nc.psum_tensor("bank%d" % i, [128, 512], F32))
            b = Buf(self, t, "bank%d" % i, "psum")
            self.live_bufs.append(b)
            self.banks.append(b)

    def _sem(self, key):
        if key not in self.sems:
            self.sems[key] = self.gstack.enter_context(self.nc.semaphore("s_" + key))
        return self.sems[key]

    def _register(self, b):
        self.live_bufs.append(b)
        if self.phase_bufs is not None:
            self.phase_bufs.append(b)

    def _name(self, name):
        self.uid += 1
        return "%s_%d" % (name, self.uid)

    def sbuf(self, name, shape, dtype=F32):
        esz = 2 if dtype == BF16 else 4
        nel = 1
        for d in shape[1:]:
            nel *= d
        nb = (nel * esz + 63) // 64 * 64
        off = self.top
        self.top += nb
        assert self.top <= self.limit, "arena overflow %s: top=%d limit=%d" % (name, self.top, self.limit)
        self.hiwater = max(self.hiwater, self.top)
        ap = self.arena[:, off // 4:(off + nb) // 4]
        if dtype != F32:
            ap = ap.bitcast(dtype)
        ap = ap[:, :nel]
        if len(shape) > 2:
            names = ["d%d" % i for i in range(len(shape) - 1)]
            pat = "p (%s) -> p %s" % (" ".join(names), " ".join(names))
            ap = ap.rearrange(pat, **{n: v for n, v in zip(names, shape[1:])})
        if shape[0] < 128:
            ap = ap[0:shape[0]]
        b = Buf(self, ap, name, "sbuf")
        b.off = off
        b.nbytes = nb
        self._register(b)
        return b

    @contextmanager
    def overlay(self, buf, start=None):
        st = (self.top, self.limit)
        self.top, self.limit = (buf.off if start is None else start), buf.off + buf.nbytes
        try:
            yield
        finally:
            buf.ov_top = self.top
            self.top, self.limit = st

    def dram(self, name, shape, dtype=F32, kind="Internal"):
        t = self.nc.dram_tensor(name, list(shape), dtype, kind=kind)
        b = Buf(self, t.ap(), name, "dram")
        self.live_bufs.append(b)
        return b

    def _need(self, eng, key, val, waits):
        if key == "eng_pe" and eng == "pe":
            return
        if self.waited[eng].get(key, 0) >= val:
            return
        self.waited[eng][key] = val
        waits[key] = max(waits.get(key, 0), val)

    def _deps(self, eng, reads, writes):
        waits = {}
        for b in reads:
            if b.last_w is not None:
                self._need(eng, b.last_w[0], b.last_w[1], waits)
            if b.space == "psum":
                for k, v in b.reads.items():
                    if k != "eng_" + eng:
                        self._need(eng, k, v, waits)
        for b in writes:
            if b.last_w is not None:
                self._need(eng, b.last_w[0], b.last_w[1], waits)
            for k, v in b.reads.items():
                self._need(eng, k, v, waits)
        return list(waits.items())

    dbg_budget = None

    def op(self, eng, fn, reads=(), writes=()):
        if self.dbg_budget is not None:
            if self.dbg_budget <= 0:
                return
            self.dbg_budget -= 1
        waits = self._deps(eng, reads, writes)
        self.cnt[eng] += 1
        key = "eng_" + eng
        val = self.cnt[eng]
        for b in reads:
            if b.reads.get(key, 0) < val:
                b.reads[key] = val
        for b in writes:
            b.last_w = (key, val)
            b.reads = {}
        self.ops[eng].append((waits, fn, key, 1))

    def dma(self, eng, out_ap, in_ap, dst, src, **kw):
        waits = self._deps(eng, [src], [dst])
        if dst.dsem is None:
            if self.dpool:
                dst.dsem = self.dpool.pop()
            else:
                self.ndsem += 1
                dst.dsem = "d%d" % self.ndsem
                self._sem(dst.dsem)
                self.dcnt[dst.dsem] = 0
            self.live_dsem.add(dst.dsem)
        key = dst.dsem
        self.dcnt[key] += 16
        val = self.dcnt[key]
        if src.reads.get(key, 0) < val:
            src.reads[key] = val
        dst.last_w = (key, val)
        dst.reads = {}

        def fn(e, out_ap=out_ap, in_ap=in_ap, kw=kw):
            return e.dma_start(out=out_ap, in_=in_ap, **kw)

        self.ops[eng].append((waits, fn, key, 16))

    def barrier(self):
        toks = [("eng_" + e, self.cnt[e]) for e in ENGS if self.cnt[e] > 0]
        toks += [(k, self.dcnt[k]) for k in sorted(self.live_dsem) if self.dcnt[k] > 0]
        for e in ENGS:
            waits = {}
            for k, v in toks:
                if k == "eng_" + e:
                    continue
                self._need(e, k, v, waits)
            if waits:
                self.ops[e].append((list(waits.items()), None, None, 0))
        for b in self.live_bufs:
            b.last_w = None
            b.reads = {}

    @contextmanager
    def phase(self):
        prev_b = self.phase_bufs
        mark = (self.top, self.limit)
        self.phase_bufs = []
        try:
            yield
        finally:
            self.barrier()
            for b in self.phase_bufs:
                if b.dsem is not None:
                    if b.dsem in self.live_dsem:
                        self.live_dsem.discard(b.dsem)
                        self.dpool.append(b.dsem)
                    b.dsem = None
            dead = set(id(b) for b in self.phase_bufs)
            self.live_bufs = [b for b in self.live_bufs if id(b) not in dead]
            self.top, self.limit = mark
            self.phase_bufs = prev_b

    def final_wait(self, eng, bufs):
        waits = {}
        for b in bufs:
            if b.last_w is not None:
                self._need(eng, b.last_w[0], b.last_w[1], waits)
        self.ops[eng].append((list(waits.items()), None, None, 0))

    def emit(self):
        nc = self.nc
        sems = self.sems

        def run(e, lst):
            for waits, fn, key, inc in lst:
                for k, v in waits:
                    e.wait_ge(sems[k], v)
                if fn is not None:
                    fn(e).then_inc(sems[key], inc)

        with nc.Block() as block:
            @block.tensor
            def _(e):
                run(e, self.ops["pe"])

            @block.vector
            def _(e):
                run(e, self.ops["dve"])

            @block.scalar
            def _(e):
                run(e, self.ops["act"])

            @block.gpsimd
            def _(e):
                run(e, self.ops["pool"])

            @block.sync
            def _(e):
                run(e, self.ops["sp"])
        self.gstack.close()
        return nc

    def mm(self, out, lhsT, rhs, start, stop, R, W):
        self.op("pe", lambda e: e.matmul(out, lhsT=lhsT, rhs=rhs, start=start, stop=stop), R, W)

    def act(self, out, in_, func, R, W, scale=1.0, bias=0.0):
        self.op("act", lambda e: e.activation(out=out, in_=in_, func=func, bias=bias, scale=scale), R, W)

    def tt(self, out, in0, in1, op, R, W, eng="dve"):
        self.op(eng, lambda e: e.tensor_tensor(out=out, in0=in0, in1=in1, op=op), R, W)

    def ts(self, out, in0, s1, s2, op0, op1, R, W, eng="dve"):
        if op1 is None:
            self.op(eng, lambda e: e.tensor_scalar(out=out, in0=in0, scalar1=s1, scalar2=None, op0=op0), R, W)
        else:
            self.op(eng, lambda e: e.tensor_scalar(out=out, in0=in0, scalar1=s1, scalar2=s2, op0=op0, op1=op1), R, W)

    def stt(self, out, in0, scalar, in1, op0, op1, R, W):
        self.op("dve", lambda e: e.scalar_tensor_tensor(out=out, in0=in0, scalar=scalar, in1=in1, op0=op0, op1=op1), R, W)

    def copy(self, out, in_, R, W, eng="dve"):
        if eng == "act":
            self.op("act", lambda e: e.copy(out=out, in_=in_), R, W)
        else:
            self.op(eng, lambda e: e.tensor_copy(out=out, in_=in_), R, W)

    def memset(self, ap, val, W, eng="dve"):
        self.op(eng, lambda e: e.memset(ap, val), (), W)


def _rope_tables():
    rows = S // 64
    row = np.repeat(np.arange(rows), 64).astype(np.float32)
    col = np.tile(np.arange(64), rows).astype(np.float32)
    inv = (10000.0 ** (-np.arange(0, 32, 2, dtype=np.float32) / 32)).astype(np.float32)
    ar = row[:, None] * inv
    ac = col[:, None] * inv
    C = np.zeros((128, S), np.float32)
    Sg = np.zeros((128, S), np.float32)
    for p in range(128):
        d = p % 64
        ang = ar if d < 32 else ac
        i = d % 16
        C[p] = np.cos(ang[:, i])
        sgn = -1.0 if (d % 32) < 16 else 1.0
        Sg[p] = sgn * np.sin(ang[:, i])
    return C, Sg


def _perm_rot(cols64):
    idx = np.arange(64)
    out = idx.copy()
    for base in (0, 32):
        out[base:base + 16] = idx[base + 16:base + 32]
        out[base + 16:base + 32] = idx[base:base + 16]
    return cols64[out]


def prep_shared(inp):
    f = np.float32
    sh = {}
    sh["mod_w"] = np.ascontiguousarray(inp["mod_w"], f)
    sh["mod_bT"] = np.ascontiguousarray(inp["mod_b"].reshape(2, 48, 128).transpose(2, 0, 1), f)
    g = np.stack([inp["norm_mix"], inp["norm_ffn"]], 0)
    sh["gT"] = np.ascontiguousarray(g.reshape(2, 2, 8, 128).transpose(3, 0, 1, 2), f)
    sh["finT"] = np.ascontiguousarray(inp["final_norm"].reshape(8, 128).T, f)
    sh["router_w"] = np.ascontiguousarray(inp["router_w"], f)
    sh["exp_w1"] = inp["exp_w1"]
    sh["exp_w3"] = inp["exp_w3"]
    sh["exp_w2"] = inp["exp_w2"]
    w_in = inp["mix_in"][0]
    q = w_in[:, 0:512]
    k = w_in[:, 512:640]
    v = w_in[:, 640:768]
    ul = w_in[:, 768:1280]
    gl = w_in[:, 1280:1792]
    qcols, qpcols = [], []
    for j in range(4):
        for h in (j, 4 + j):
            c64 = np.arange(h * 64, (h + 1) * 64)
            qcols.append(c64)
            qpcols.append(_perm_rot(c64))
    qcols = np.concatenate(qcols)
    qpcols = np.concatenate(qpcols)
    kp = np.concatenate([_perm_rot(np.arange(0, 64)), _perm_rot(np.arange(64, 128))])
    sh["w_in"] = np.ascontiguousarray(np.concatenate([q[:, qcols], q[:, qpcols], k, k[:, kp], v, ul, gl], 1), f)
    w_out = inp["mix_out"][0]
    arows = []
    for j in range(4):
        for h in (j, 4 + j):
            arows.append(np.arange(h * 64, (h + 1) * 64))
    arows = np.concatenate(arows + [np.arange(512, 1024)])
    sh["w_out"] = np.ascontiguousarray(w_out[arows], f)
    sh["sinkb"] = np.ascontiguousarray(np.broadcast_to(inp["attn_sink"][0][None, :], (128, 8)), f)
    C, Sg = _rope_tables()
    sh["ropeC"] = C
    sh["ropeS"] = Sg
    kq = np.arange(128)
    mP = (kq[:, None] >= kq[None, :]).astype(f)
    mN = (kq[:, None] <= kq[None, :]).astype(f)
    sh["maskP"] = np.ascontiguousarray(np.tile(mP, (1, 4)))
    sh["maskN"] = np.ascontiguousarray(np.tile(mN, (1, 4)))
    sh["convw"] = np.ascontiguousarray(inp["lru_conv_w"][0].reshape(4, 4, 128).transpose(2, 1, 0), f)
    sh["convb"] = np.ascontiguousarray(inp["lru_conv_b"][0].reshape(4, 128).T, f)

    def bd(w):
        o = np.zeros((128, 2, 4, 128), f)
        for d in range(2):
            for c in range(4):
                for hb in range(2):
                    o[hb * 64:(hb + 1) * 64, d, c, hb * 64:(hb + 1) * 64] = w[d, 2 * c + hb]
        return o
    sh["lru_wa"] = bd(inp["lru_wa"][0])
    sh["lru_wi"] = bd(inp["lru_wi"][0])

    def vec(a):
        return np.ascontiguousarray(a.reshape(2, 4, 128).transpose(2, 0, 1), f)
    sh["lru_ba"] = vec(inp["lru_ba"][0])
    sh["lru_bi"] = vec(inp["lru_bi"][0])
    sh["lru_lam"] = vec(inp["lru_lam"][0])
    sh["id16"] = np.eye(16, dtype=f)
    rw48 = np.zeros((2, 1024, 48), f)
    rw48[:, :, 0:16] = inp["router_w"]
    rw48[:, :, 32:48] = inp["router_w"]
    sh["router_w48"] = rw48
    bd = np.zeros((48, 48), f)
    for g_ in range(3):
        bd[g_ * 16:(g_ + 1) * 16, g_ * 16:(g_ + 1) * 16] = 1.0
    sh["bd48"] = bd
    sh["iota_j"] = np.ascontiguousarray(np.broadcast_to(np.arange(288, dtype=f)[None, :], (128, 288)))
    sh["iota_p"] = np.ascontiguousarray(np.arange(128, dtype=f)[:, None] + np.array([0, 128, 256], f)[None, :])
    def fm(v1024):
        return np.asarray(v1024, f).reshape(8, 128).T
    vecs = [inp["rw_mu"][0][i] for i in range(6)] + [inp["rw_w0"][0][0], inp["rw_w0"][0][1], inp["rw_a0"][0][0], inp["rw_a0"][0][1],
            inp["rw_kk"][0], inp["rw_ka"][0], inp["rw_rk"][0].reshape(1024), inp["rw_gn_g"][0], inp["rw_gn_b"][0]]
    sh["rw_vec"] = np.ascontiguousarray(np.stack([fm(v) for v in vecs], 1), f)
    sh["ident"] = np.eye(128, dtype=f)
    bo = np.zeros((128, 128), f)
    bo[:64, :64] = 1.0
    bo[64:, 64:] = 1.0
    sh["blockones"] = bo
    mc = np.ones((128, 256), f)
    mc[:, ::64] = 0.0
    sh["maskC"] = mc
    ii = np.arange(64)
    lt = (ii[:, None] < ii[None, :]).astype(f)
    le = (ii[:, None] <= ii[None, :]).astype(f)
    def bdm(m):
        o = np.zeros((128, 128), f)
        o[:64, :64] = m
        o[64:, 64:] = m
        return o
    sh["rmask4"] = np.ascontiguousarray(np.stack([np.concatenate([bdm(lt), bdm(lt.T), bdm(lt), bdm(le)], 1),
                                                  np.concatenate([bdm(lt.T), bdm(lt), bdm(lt.T), bdm(le.T)], 1)], 0), f)
    sh["rmask1"] = np.ascontiguousarray(np.stack([bdm(le), bdm(le.T)], 0), f)
    sh["rw_w1cat"] = np.ascontiguousarray(np.concatenate([inp["rw_w1"][0][0], inp["rw_w1"][0][1]], 1), f)
    sh["rw_a1cat"] = np.ascontiguousarray(np.concatenate([inp["rw_a1"][0][0], inp["rw_a1"][0][1]], 1), f)
    sh["rw_g1"] = np.ascontiguousarray(inp["rw_g1"][0], f)
    sh["rw_rkv"] = np.ascontiguousarray(inp["rw_rkv"][0], f)
    sh["rw_w2cat"] = np.ascontiguousarray(np.concatenate([inp["rw_w2"][0][0], inp["rw_w2"][0][1]], 0), f)
    sh["rw_a2cat"] = np.ascontiguousarray(np.concatenate([inp["rw_a2"][0][0], inp["rw_a2"][0][1]], 0), f)
    sh["rw_g2"] = np.ascontiguousarray(inp["rw_g2"][0], f)
    sh["rw_wo"] = np.ascontiguousarray(inp["rw_wo"][0], f)
    return sh


def prep_core(inp, core, ns):
    f = np.float32
    b0 = core * ns
    pc = {}
    pc["xT"] = np.ascontiguousarray(inp["x"][b0:b0 + ns].transpose(0, 2, 1), f)
    pc["cxT"] = np.ascontiguousarray(inp["ctx"][b0:b0 + ns].transpose(0, 2, 1), f)
    cc = np.zeros((8, 1024), f)
    cc[:ns] = inp["c"][b0:b0 + ns]
    cc[4] = inp["c_ctx"]
    pc["cT"] = np.ascontiguousarray(cc.reshape(8, 8, 128).transpose(2, 1, 0), f)
    return pc


class K:
    pass


GELU_C = 1.5957691216057308


def bank3(bank, a, b):
    return bank[:].rearrange("p (a b) -> p a b", a=a)


def build(ns=4, taps=None, stop_after=None, skip_l0=False):
    taps = taps or []
    P = Prog()
    k = K()
    k.P = P
    k.tap_outs = {}
    k.stop_after = stop_after
    k.skip_l0 = skip_l0
    import os as _os
    k.sparse = _os.environ.get("MOE_DENSE", "0") != "1"
    IN = "ExternalInput"
    dr = {}
    k.dr = dr

    def din(name, shape):
        dr[name] = P.dram(name, shape, F32, IN)
        return dr[name]

    din("xT", [ns, D, S]); din("cxT", [ns, D, LC]); din("cT", [128, KC, 8])
    din("mod_w", [2, D, 6 * D]); din("mod_bT", [128, 2, 48]); din("gT", [128, 2, 2, 8]); din("finT", [128, 8])
    din("router_w", [2, D, 16])
    din("exp_w1", [2, 16, D, D]); din("exp_w3", [2, 16, D, D]); din("exp_w2", [2, 16, D, D])
    din("w_in", [D, 2432]); din("w_out", [D, D]); din("sinkb", [128, 8])
    din("ropeC", [128, S]); din("ropeS", [128, S]); din("maskP", [128, 512]); din("maskN", [128, 512])
    din("convw", [128, 4, 4]); din("convb", [128, 4])
    din("lru_wa", [128, 2, 4, 128]); din("lru_wi", [128, 2, 4, 128])
    din("lru_ba", [128, 2, 4]); din("lru_bi", [128, 2, 4]); din("lru_lam", [128, 2, 4])
    din("id16", [16, 16]); din("iota_j", [128, 288]); din("iota_p", [128, 3]); din("router_w48", [2, D, 48]); din("bd48", [48, 48])
    din("rw_vec", [128, 15, KC]); din("ident", [128, 128]); din("blockones", [128, 128]); din("maskC", [128, 256])
    din("rmask4", [2, 128, 512]); din("rmask1", [2, 128, 128])
    din("rw_w1cat", [D, 128]); din("rw_a1cat", [D, 128]); din("rw_g1", [D, 160]); din("rw_rkv", [3, D, D])
    din("rw_w2cat", [128, D]); din("rw_a2cat", [128, D]); din("rw_g2", [160, D]); din("rw_wo", [D, D])
    k.xspill = P.dram("xspill", [D, S], F32, "Internal")
    k.outT = P.dram("outT", [ns, D, S], F32, "ExternalOutput")

    def tap(name, buf, ap, shape):
        if name not in taps:
            return
        o = P.dram("tap_" + name, list(shape), F32, "ExternalOutput")
        k.tap_outs[name] = o
        P.dma("pool", o[:], ap, o, buf)
    k.tap = tap

    k.ones_f = P.sbuf("ones_f", [128, 128], F32)
    P.memset(k.ones_f[:], 1.0, [k.ones_f])
    k.ones_b = P.sbuf("ones_b", [128, 64], BF16)
    P.memset(k.ones_b[:], 1.0, [k.ones_b])
    k.epsb = P.sbuf("epsb", [128, 1], F32)
    P.memset(k.epsb[:], EPS, [k.epsb])
    k.gneps = P.sbuf("gneps", [128, 1], F32)
    P.memset(k.gneps[:], 64e-5, [k.gneps])
    k.modv = modv = P.sbuf("modv", [128, 2, 48, 8], F32)
    k.Amod = Amod = P.sbuf("Amod", [128, 2, 2, 8, 8], F32)
    gT = P.sbuf("gT", [128, 2, 2, 8], F32)
    k.finT = P.sbuf("finT", [128, 8], F32)
    P.dma("sp", gT[:], dr["gT"][:], gT, dr["gT"])
    P.dma("sp", k.finT[:], dr["finT"][:], k.finT, dr["finT"])

    with P.phase():
        cT = P.sbuf("cT", [128, KC, 8], F32)
        P.dma("sp", cT[:], dr["cT"][:], cT, dr["cT"])
        sT = P.sbuf("sT", [128, KC, 8], F32)
        P.act(sT[:], cT[:], AF.Silu, [cT], [sT])
        mbT = P.sbuf("mbT", [128, 2, 48], F32)
        P.dma("sp", mbT[:], dr["mod_bT"][:], mbT, dr["mod_bT"])
        wm = [P.sbuf("wm%d" % i, [128, KC, 512], F32) for i in range(2)]
        it = 0
        for l in range(2):
            psm = P.banks[l]
            psm3 = psm[:, 0:384].rearrange("p (a b) -> p a b", a=48)
            for cb in range(12):
                w = wm[it % 2]
                it += 1
                src = dr["mod_w"][l].rearrange("(kc p) n -> p kc n", p=128)[:, :, cb * 512:(cb + 1) * 512]
                P.dma("sp", w[:], src, w, dr["mod_w"])
                for o4 in range(4):
                    oc = cb * 4 + o4
                    for kc in range(KC):
                        P.mm(psm3[:, oc, :], w[:, kc, o4 * 128:(o4 + 1) * 128], sT[:, kc, :], kc == 0, kc == KC - 1,
                             [w, sT], [psm])
            P.tt(modv[:, l], psm3, mbT[:, l, :, None].to_broadcast([128, 48, 8]), ALU.add, [psm, mbT], [modv])
            for kind in range(2):
                sc = modv[:, l, (1 + 3 * kind) * 8:(2 + 3 * kind) * 8, :]
                P.ts(Amod[:, l, kind], sc, 1.0, None, ALU.add, None, [modv], [Amod])
                P.tt(Amod[:, l, kind], Amod[:, l, kind], gT[:, kind, l, :, None].to_broadcast([128, 8, 8]), ALU.mult,
                     [Amod, gT], [Amod])
        tap("modv", modv, modv[:], [128, 2, 48, 8])

    for s in range(ns):
        sample_program(k, s)

    outs = [k.outT] + list(k.tap_outs.values())
    P.final_wait("sp", outs)
    P.final_wait("pool", outs)
    print("[build] arena hiwater %d / %d bytes, dma sems %d, instr %s" % (P.hiwater, ARENA_BYTES, P.ndsem,
                                                                        {e: len(P.ops[e]) for e in ENGS}))
    nc = P.emit()
    return nc, k


def mvec(k, l, j, kc, si):
    return k.modv[:, l, j * 8 + kc, si:si + 1]


def x_src(dr, s, b):
    t0, nb = BLOCKS[b]
    if b == 0:
        return dr["cxT"][s].rearrange("(kc p) t -> p kc t", p=128), dr["cxT"]
    return dr["xT"][s].rearrange("(kc p) t -> p kc t", p=128)[:, :, t0 - LC:t0 - LC + nb], dr["xT"]


class NormScratch:
    def __init__(self, k, bank):
        P = k.P
        self.sq = P.sbuf("sq", [128, KC, 512], F32)
        self.rs = P.sbuf("rs", [128, 512], F32)
        self.ps = bank


def rms_block(k, sc, x_ap, xbuf, nb):
    P = k.P
    P.act(sc.sq[:, :, :nb], x_ap, AF.Square, [xbuf], [sc.sq])
    for kc in range(KC):
        P.mm(sc.ps[:, :nb], k.ones_f[:], sc.sq[:, kc, :nb], kc == 0, kc == KC - 1, [k.ones_f, sc.sq], [sc.ps])
    P.act(sc.rs[:, :nb], sc.ps[:, :nb], AF.Ln, [sc.ps, k.epsb], [sc.rs], scale=1.0 / D, bias=k.epsb[:, 0:1])
    P.act(sc.rs[:, :nb], sc.rs[:, :nb], AF.Exp, [sc.rs], [sc.rs], scale=-0.5)
    P.tt(sc.sq[:, :, :nb], x_ap, sc.rs[:, None, :nb].to_broadcast([128, KC, nb]), ALU.mult, [xbuf, sc.rs], [sc.sq])
    return sc.sq


class WRing:
    def __init__(self, k, n):
        self.k = k
        self.slots = [k.P.sbuf("wslot%d" % i, [128, KC, 512], BF16) for i in range(n)]
        self.i = 0

    def load(self, w2d, wbuf, ncols=512):
        sl = self.slots[self.i % len(self.slots)]
        self.i += 1
        src = w2d.rearrange("(kc p) n -> p kc n", p=128)
        self.k.P.dma("pool", sl[:, :, :ncols], src, sl, wbuf)
        return sl


def sample_program(k, s):
    P = k.P
    dr = k.dr
    tap = k.tap if s == 0 else (lambda *a, **kw: None)
    SI = lambda b: 4 if b == 0 else s
    with P.phase():
        hT = P.sbuf("hT", [128, KC, T], BF16)
        xT = P.sbuf("xT", [128, KC, T], F32)
        if k.skip_l0:
            for b, (t0, nb) in enumerate(BLOCKS):
                src, sb_ = x_src(dr, s, b)
                P.dma("sp", xT[:, :, t0:t0 + nb], src, xT, sb_)
            layer1_and_out(k, s, hT, xT, tap)
            return
        with P.phase(), P.overlay(xT):
            sc = NormScratch(k, P.banks[0])
            xb = [P.sbuf("xb%d" % i, [128, KC, 512], F32) for i in range(2)]
            for b, (t0, nb) in enumerate(BLOCKS):
                xt = xb[b % 2]
                src, sb_ = x_src(dr, s, b)
                P.dma("sp", xt[:, :, :nb], src, xt, sb_)
                xn = rms_block(k, sc, xt[:, :, :nb], xt, nb)
                for kc in range(KC):
                    P.ts(hT[:, kc, t0:t0 + nb], xn[:, kc, :nb], k.Amod[:, 0, 0, kc, SI(b):SI(b) + 1],
                         mvec(k, 0, 0, kc, SI(b)), ALU.mult, ALU.add, [xn, k.Amod, k.modv], [hT])
            tap("L0_h", hT, hT[:], [128, KC, T])
        if k.stop_after == "L0A":
            return
        with P.phase():
            attnT = P.sbuf("attnT", [128, 4, T], BF16)
            lruT = P.sbuf("lruT", [128, 4, T], BF16)
            wr = WRing(k, 3)
            with P.phase(), P.overlay(xT):
                l0_attention(k, s, hT, attnT, wr, tap)
            if k.stop_after == "L0B":
                return
            with P.phase(), P.overlay(xT):
                l0_lru(k, s, hT, lruT, wr, tap)
            if k.stop_after == "L0C":
                return
            with P.phase():
                xb = [P.sbuf("xb%d" % i, [128, KC, 512], F32) for i in range(2)]
                wo = [wr.load(dr["w_out"][:, h * 512:(h + 1) * 512], dr["w_out"]) for h in range(2)]
                pi = 0
                for b, (t0, nb) in enumerate(BLOCKS):
                    xt = xb[b % 2]
                    src, sb_ = x_src(dr, s, b)
                    P.dma("sp", xt[:, :, :nb], src, xt, sb_)
                    for oc in range(KC):
                        ps = P.banks[pi % 4]
                        pi += 1
                        w = wo[oc // 4]
                        c0 = (oc % 4) * 128
                        for j in range(4):
                            P.mm(ps[:, :nb], w[:, j, c0:c0 + 128], attnT[:, j, t0:t0 + nb], j == 0, False, [w, attnT], [ps])
                        for c in range(4):
                            P.mm(ps[:, :nb], w[:, 4 + c, c0:c0 + 128], lruT[:, c, t0:t0 + nb], False, c == 3, [w, lruT], [ps])
                        P.stt(xT[:, oc, t0:t0 + nb], ps[:, :nb], mvec(k, 0, 2, oc, SI(b)), xt[:, oc, :nb], ALU.mult, ALU.add,
                              [ps, k.modv, xt], [xT])
                tap("L0_xmix", xT, xT[:], [128, KC, T])
        if k.stop_after == "L0D":
            return
        (moe_layer_sparse if k.sparse else moe_layer)(k, s, 0, hT, xT, True, tap)
        tap("L0_x", xT, xT[:], [128, KC, T])
        if k.stop_after == "L0F":
            return
        layer1_and_out(k, s, hT, xT, tap)


def layer1_and_out(k, s, hT, xT, tap):
    P = k.P
    layer1_mixer(k, s, hT, xT, tap)
    if k.stop_after in ("L1A", "L1B", "L1D", "L1L", "L1P"):
        return
    (moe_layer_sparse if k.sparse else moe_layer)(k, s, 1, hT, xT, False, tap)
    tap("L1_x", xT, xT[:], [128, KC, T])
    with P.phase():
        sc = NormScratch(k, P.banks[0])
        ob = [P.sbuf("ob%d" % i, [128, KC, 512], F32) for i in range(2)]
        for i, (t0, nb) in enumerate(LAT_BLOCKS):
            xn = rms_block(k, sc, xT[:, :, t0:t0 + nb], xT, nb)
            o = ob[i % 2]
            P.tt(o[:], xn[:, :, :nb], k.finT[:, :, None].to_broadcast([128, KC, nb]), ALU.mult, [xn, k.finT], [o])
            P.dma("sp", k.outT[s].rearrange("(kc p) t -> p kc t", p=128)[:, :, t0 - LC:t0 - LC + nb], o[:], k.outT, o)


def l0_attention(k, s, hT, qT, wr, tap):
    P = k.P
    dr = k.dr
    kT = P.sbuf("kT", [128, T], BF16)
    Vtok = P.sbuf("Vtok", [128, 18, 128], BF16)
    ropeC = P.sbuf("ropeC", [128, 512], F32)
    ropeS = P.sbuf("ropeS", [128, 512], F32)
    t1 = P.sbuf("rt1", [128, 512], F32)
    t2 = P.sbuf("rt2", [128, 512], F32)
    maskP = P.sbuf("maskP", [128, 4, 128], BF16)
    maskN = P.sbuf("maskN", [128, 4, 128], BF16)
    sinkb = P.sbuf("sinkb", [128, 8], F32)
    esink = P.sbuf("esink", [128, 8], F32)
    P.dma("pool", maskP[:], dr["maskP"][:].rearrange("p (a b) -> p a b", a=4), maskP, dr["maskP"])
    P.dma("pool", maskN[:], dr["maskN"][:].rearrange("p (a b) -> p a b", a=4), maskN, dr["maskN"])
    P.dma("sp", sinkb[:], dr["sinkb"][:], sinkb, dr["sinkb"])
    P.act(esink[:], sinkb[:], AF.Exp, [sinkb], [esink])
    wq = wr.load(dr["w_in"][:, 0:512], dr["w_in"])
    wqp = wr.load(dr["w_in"][:, 512:1024], dr["w_in"])
    wkv = wr.load(dr["w_in"][:, 1024:1408], dr["w_in"], 384)
    pi = 0
    for b, (t0, nb) in enumerate(BLOCKS):
        if b > 0:
            P.dma("sp", ropeC[:, :nb], dr["ropeC"][:, t0 - LC:t0 - LC + nb], ropeC, dr["ropeC"])
            P.dma("sp", ropeS[:, :nb], dr["ropeS"][:, t0 - LC:t0 - LC + nb], ropeS, dr["ropeS"])
        for j in range(5):
            w, wp, c0 = (wq, wqp, j * 128) if j < 4 else (wkv, wkv, 0)
            dst = qT[:, j, t0:t0 + nb] if j < 4 else kT[:, t0:t0 + nb]
            dbuf = qT if j < 4 else kT
            ps = P.banks[pi % 4]
            pi += 1
            for kc in range(KC):
                P.mm(ps[:, :nb], w[:, kc, c0:c0 + 128], hT[:, kc, t0:t0 + nb], kc == 0, kc == KC - 1, [w, hT], [ps])
            if b == 0:
                P.copy(dst, ps[:, :nb], [ps], [dbuf], eng="act")
            else:
                c1 = c0 if j < 4 else 128
                ps2 = P.banks[pi % 4]
                pi += 1
                for kc in range(KC):
                    P.mm(ps2[:, :nb], wp[:, kc, c1:c1 + 128], hT[:, kc, t0:t0 + nb], kc == 0, kc == KC - 1, [wp, hT], [ps2])
                P.tt(t1[:, :nb], ps[:, :nb], ropeC[:, :nb], ALU.mult, [ps, ropeC], [t1])
                P.tt(t2[:, :nb], ps2[:, :nb], ropeS[:, :nb], ALU.mult, [ps2, ropeS], [t2])
                P.tt(dst, t1[:, :nb], t2[:, :nb], ALU.add, [t1, t2], [dbuf])
        for tt_ in range(t0 // 128, (t0 + nb) // 128):
            ps = P.banks[pi % 4]
            pi += 1
            for kc in range(KC):
                P.mm(ps[:, 0:128], hT[:, kc, tt_ * 128:(tt_ + 1) * 128], wkv[:, kc, 256:384], kc == 0, kc == KC - 1,
                     [hT, wkv], [ps])
            P.copy(Vtok[:, tt_, :], ps[:, 0:128], [ps], [Vtok], eng="act")
    tap("L0_q", qT, qT[:], [128, 4, T])
    tap("L0_k", kT, kT[:], [128, T])
    tap("L0_v", Vtok, Vtok[:], [128, 18, 128])
    pT = [P.sbuf("pT%d" % i, [128, 4, 128], BF16) for i in range(2)]
    den = P.sbuf("den", [128, 4, 128], F32)
    ps_s = [P.banks[4], P.banks[5]]
    ps_num = P.banks[6]
    ps_den = P.banks[7]
    it = 0
    for qt in range(18):
        if qt < 2:
            keys = [(0, None), (1, None)]
        else:
            i = qt - 2
            keys = [(0, None), (1, None)]
            if i > 0:
                keys.append((qt - 1, maskP))
            keys.append((qt, None))
            if i < 15:
                keys.append((qt + 1, maskN))
        for g in range(2):
            pr = slice(g * 64, (g + 1) * 64)
            num3 = ps_num[pr, :].rearrange("p (a b) -> p a b", a=4)
            den3 = ps_den[pr, :].rearrange("p (a b) -> p a b", a=4)
            for idx, (kt, mk) in enumerate(keys):
                pss = ps_s[it % 2]
                pt = pT[it % 2]
                it += 1
                P.mm(pss[:].rearrange("p (a b) -> p a b", a=4), kT[pr, kt * 128:(kt + 1) * 128],
                     qT[pr, :, qt * 128:(qt + 1) * 128], True, True, [kT, qT], [pss])
                P.act(pt[:], pss[:].rearrange("p (a b) -> p a b", a=4), AF.Exp, [pss], [pt], scale=0.125)
                if mk is not None:
                    P.tt(pt[:], pt[:], mk[:], ALU.mult, [pt, mk], [pt])
                first, last = idx == 0, idx == len(keys) - 1
                P.mm(num3, Vtok[:, kt, pr], pt[:], first, last, [Vtok, pt], [ps_num])
                P.mm(den3, k.ones_b[:, 0:64], pt[:], first, last, [k.ones_b, pt], [ps_den])
            P.tt(den[pr], den3, esink[pr, g * 4:(g + 1) * 4, None].to_broadcast([64, 4, 128]), ALU.add,
                 [ps_den, esink], [den])
            P.op("dve", lambda e, pr=pr: e.reciprocal(out=den[pr], in_=den[pr]), [den], [den])
            P.tt(qT[pr, :, qt * 128:(qt + 1) * 128], num3, den[pr], ALU.mult, [ps_num, den], [qT])
    tap("L0_attn", qT, qT[:], [128, 4, T])


def l0_lru(k, s, hT, lruT, wr, tap):
    P = k.P
    dr = k.dr
    ul = P.sbuf("ul", [128, T], F32)
    u = P.sbuf("u", [128, T], F32)
    ub = P.sbuf("ub", [128, T], BF16)
    tmp = [P.sbuf("lt%d" % i, [128, 512], F32) for i in range(7)]
    t_r, t_i, t_a, t_y, t_b, t_h0, t_h1 = tmp
    convw = P.sbuf("convw", [128, 4, 4], F32)
    convb = P.sbuf("convb", [128, 4], F32)
    wa = P.sbuf("wa", [128, 2, 4, 128], BF16)
    wi = P.sbuf("wi", [128, 2, 4, 128], BF16)
    ba = P.sbuf("ba", [128, 2, 4], F32)
    bi = P.sbuf("bi", [128, 2, 4], F32)
    lam = P.sbuf("lam", [128, 2, 4], F32)
    kap = P.sbuf("kap", [128, 2, 4], F32)
    kap2 = P.sbuf("kap2", [128, 2, 4], F32)
    zz = P.sbuf("zz", [128, 2, 4], F32)
    zp = P.sbuf("zp", [128, 2, 4], F32)
    for nm, t_ in (("convw", convw), ("convb", convb), ("lru_ba", ba), ("lru_bi", bi), ("lru_lam", lam)):
        P.dma("sp", t_[:], dr[nm][:], t_, dr[nm])
    P.dma("pool", wa[:], dr["lru_wa"][:], wa, dr["lru_wa"])
    P.dma("pool", wi[:], dr["lru_wi"][:], wi, dr["lru_wi"])
    P.act(zz[:], lam[:], AF.Exp, [lam], [zz], scale=-1.0)
    P.ts(zp[:], zz[:], -0.2, 0.25, ALU.mult, ALU.add, [zz], [zp])
    for cst in (1.0 / 3.0, 0.5, 1.0):
        P.tt(zp[:], zp[:], zz[:], ALU.mult, [zp, zz], [zp])
        P.ts(zp[:], zp[:], -1.0, cst, ALU.mult, ALU.add, [zp], [zp])
    P.tt(zp[:], zp[:], zz[:], ALU.mult, [zp, zz], [zp])
    P.ts(kap[:], zp[:], -8.0, None, ALU.mult, None, [zp], [kap])
    P.ts(kap2[:], zp[:], -16.0, None, ALU.mult, None, [zp], [kap2])
    wul = wr.load(dr["w_in"][:, 1408:1920], dr["w_in"])
    wgl = wr.load(dr["w_in"][:, 1920:2432], dr["w_in"])
    pi = 0
    for c in range(4):
        for b, (t0, nb) in enumerate(BLOCKS):
            ps = P.banks[pi % 4]
            pi += 1
            for kc in range(KC):
                P.mm(ps[:, :nb], wul[:, kc, c * 128:(c + 1) * 128], hT[:, kc, t0:t0 + nb], kc == 0, kc == KC - 1, [wul, hT], [ps])
            P.copy(ul[:, t0:t0 + nb], ps[:, :nb], [ps], [ul], eng="act")
        P.act(u[:], ul[:], AF.Identity, [ul, convw, convb], [u], scale=convw[:, c, 2:3], bias=convb[:, c:c + 1])
        for (a_, e_) in ((0, LC), (LC, T)):
            P.stt(u[:, a_ + 2:e_], ul[:, a_:e_ - 2], convw[:, c, 0:1], u[:, a_ + 2:e_], ALU.mult, ALU.add, [ul, convw, u], [u])
            P.stt(u[:, a_ + 1:e_], ul[:, a_:e_ - 1], convw[:, c, 1:2], u[:, a_ + 1:e_], ALU.mult, ALU.add, [ul, convw, u], [u])
            P.stt(u[:, a_:e_ - 1], ul[:, a_ + 1:e_], convw[:, c, 3:4], u[:, a_:e_ - 1], ALU.mult, ALU.add, [ul, convw, u], [u])
        P.copy(ub[:], u[:], [u], [ub], eng="act")
        if c == 0:
            tap("L0_u0", u, u[:], [128, T])
        hsum = ul
        for d in range(2):
            order = [0, 1, 2, 3, 4] if d == 0 else [0, 4, 3, 2, 1]
            prev_h = None
            for oi, b in enumerate(order):
                t0, nb = BLOCKS[b]
                psr = P.banks[pi % 4]
                pi += 1
                psi = P.banks[pi % 4]
                pi += 1
                P.mm(psr[:, :nb], wa[:, d, c, :], ub[:, t0:t0 + nb], True, True, [wa, ub], [psr])
                P.mm(psi[:, :nb], wi[:, d, c, :], ub[:, t0:t0 + nb], True, True, [wi, ub], [psi])
                P.act(t_r[:, :nb], psr[:, :nb], AF.Sigmoid, [psr, ba], [t_r], bias=ba[:, d, c:c + 1])
                P.act(t_i[:, :nb], psi[:, :nb], AF.Sigmoid, [psi, bi], [t_i], bias=bi[:, d, c:c + 1])
                P.act(t_a[:, :nb], t_r[:, :nb], AF.Exp, [t_r, kap], [t_a], scale=kap[:, d, c:c + 1])
                P.act(t_y[:, :nb], t_r[:, :nb], AF.Exp, [t_r, kap2], [t_y], scale=kap2[:, d, c:c + 1])
                P.ts(t_y[:, :nb], t_y[:, :nb], -1.0, 1.0, ALU.mult, ALU.add, [t_y], [t_y])
                P.ts(t_y[:, :nb], t_y[:, :nb], 1e-30, None, ALU.max, None, [t_y], [t_y])
                P.act(t_y[:, :nb], t_y[:, :nb], AF.Ln, [t_y], [t_y])
                P.act(t_y[:, :nb], t_y[:, :nb], AF.Exp, [t_y], [t_y], scale=0.5)
                P.tt(t_b[:, :nb], t_y[:, :nb], t_i[:, :nb], ALU.mult, [t_y, t_i], [t_b])
                P.tt(t_b[:, :nb], t_b[:, :nb], u[:, t0:t0 + nb], ALU.mult, [t_b, u], [t_b])
                if d == 0:
                    init = 0.0 if oi == 0 else hsum[:, t0 - 1:t0]
                    P.op("dve", lambda e, t0=t0, nb=nb, init=init: e.tensor_tensor_scan(
                        out=hsum[:, t0:t0 + nb], data0=t_a[:, :nb], data1=t_b[:, :nb], initial=init,
                        op0=ALU.mult, op1=ALU.add), [t_a, t_b, hsum], [hsum])
                else:
                    th = t_h0 if oi % 2 == 0 else t_h1
                    if oi == 0:
                        init, rd = 0.0, []
                    else:
                        init, rd = prev_h[1], [prev_h[0]]
                    P.op("dve", lambda e, nb=nb, init=init, th=th: e.tensor_tensor_scan(
                        out=th[:, :nb][:, ::-1], data0=t_a[:, :nb][:, ::-1], data1=t_b[:, :nb][:, ::-1], initial=init,
                        op0=ALU.mult, op1=ALU.add), [t_a, t_b] + rd, [th])
                    prev_h = (th, th[:, 0:1])
                    P.tt(hsum[:, t0:t0 + nb], hsum[:, t0:t0 + nb], th[:, :nb], ALU.add, [hsum, th], [hsum])
        if c == 0:
            tap("L0_hsum0", hsum, hsum[:], [128, T])
        for b, (t0, nb) in enumerate(BLOCKS):
            ps = P.banks[pi % 4]
            pi += 1
            for kc in range(KC):
                P.mm(ps[:, :nb], wgl[:, kc, c * 128:(c + 1) * 128], hT[:, kc, t0:t0 + nb], kc == 0, kc == KC - 1, [wgl, hT], [ps])
            P.copy(t_r[:, :nb], ps[:, :nb], [ps], [t_r], eng="act")
            P.act(t_i[:, :nb], ps[:, :nb], AF.Square, [ps], [t_i])
            P.ts(t_i[:, :nb], t_i[:, :nb], 0.044715, 1.0, ALU.mult, ALU.add, [t_i], [t_i])
            P.tt(t_i[:, :nb], t_i[:, :nb], t_r[:, :nb], ALU.mult, [t_i, t_r], [t_i])
            P.act(t_i[:, :nb], t_i[:, :nb], AF.Sigmoid, [t_i], [t_i], scale=GELU_C)
            P.tt(t_i[:, :nb], t_i[:, :nb], t_r[:, :nb], ALU.mult, [t_i, t_r], [t_i])
            P.tt(lruT[:, c, t0:t0 + nb], t_i[:, :nb], hsum[:, t0:t0 + nb], ALU.mult, [t_i, hsum], [lruT])
    tap("L0_lru", lruT, lruT[:], [128, 4, T])


def moe_layer(k, s, l, hT, xT, with_ctx, tap):
    P = k.P
    dr = k.dr
    SI = lambda b: 4 if b == 0 else s
    blocks = list(enumerate(BLOCKS)) if with_ctx else list(enumerate(BLOCKS))[1:]
    with P.phase():
        gateT = P.sbuf("gateT", [16, T], F32)
        with P.phase():
            sc = NormScratch(k, P.banks[0])
            hf = P.sbuf("hf", [128, KC, 512], F32)
            wrt = P.sbuf("wrt", [128, KC, 16], F32)
            ones16 = P.sbuf("ones16", [16, 16], F32)
            ex = P.sbuf("ex", [16, 512], F32)
            P.memset(ones16[:], 1.0, [ones16])
            P.dma("sp", wrt[:], dr["router_w"][l].rearrange("(kc p) e -> p kc e", p=128), wrt, dr["router_w"])
            psl = P.banks[1]
            pss = P.banks[2]
            for b, (t0, nb) in blocks:
                xn = rms_block(k, sc, xT[:, :, t0:t0 + nb], xT, nb)
                for kc in range(KC):
                    P.ts(hf[:, kc, :nb], xn[:, kc, :nb], k.Amod[:, l, 1, kc, SI(b):SI(b) + 1], mvec(k, l, 3, kc, SI(b)),
                         ALU.mult, ALU.add, [xn, k.Amod, k.modv], [hf])
                P.copy(hT[:, :, t0:t0 + nb], hf[:, :, :nb], [hf], [hT], eng="act")
                for kc in range(KC):
                    P.mm(psl[0:16, :nb], wrt[:, kc, :], hf[:, kc, :nb], kc == 0, kc == KC - 1, [wrt, hf], [psl])
                P.act(ex[:, :nb], psl[0:16, :nb], AF.Exp, [psl], [ex])
                P.mm(pss[0:16, :nb], ones16[:], ex[:, :nb], True, True, [ones16, ex], [pss])
                P.op("dve", lambda e, nb=nb: e.reciprocal(out=sc.rs[0:16, :nb], in_=pss[0:16, :nb]), [pss], [sc.rs])
                P.tt(gateT[:, t0:t0 + nb], ex[:, :nb], sc.rs[0:16, :nb], ALU.mult, [ex, sc.rs], [gateT])
            tap("L%d_aff" % l, gateT, gateT[:], [16, T])
            work = P.sbuf("work", [16, S], F32)
            m8 = P.sbuf("m8", [16, 8], F32)
            segs = [(LC, S, 8)] + ([(0, LC, 8)] if with_ctx else [])
            for (a0, n, _) in segs:
                cap = 2 * n // 16
                P.copy(work[:, :n], gateT[:, a0:a0 + n], [gateT], [work])
                for r in range(cap // 8):
                    P.op("dve", lambda e, n=n: e.max(out=m8[:], in_=work[:, :n]), [work], [m8])
                    if r < cap // 8 - 1:
                        P.op("dve", lambda e, n=n: e.match_replace(out=work[:, :n], in_to_replace=m8[:], in_values=work[:, :n],
                                                                   imm_value=-1.0), [work, m8], [work])
                P.stt(gateT[:, a0:a0 + n], gateT[:, a0:a0 + n], m8[:, 7:8], gateT[:, a0:a0 + n], ALU.is_ge, ALU.mult,
                      [gateT, m8], [gateT])
            tap("L%d_gate" % l, gateT, gateT[:], [16, T])
            tap("L%d_hffn" % l, hT, hT[:], [128, KC, T])
        with P.phase():
            wr = WRing(k, 4)
            id16 = P.sbuf("id16", [16, 16], F32)
            P.dma("sp", id16[:], dr["id16"][:], id16, dr["id16"])
            hid = P.sbuf("hid", [128, KC, T], BF16)
            gb = [P.sbuf("gb%d" % i, [128, 512], F32) for i in range(2)]
            s1 = [P.sbuf("s1_%d" % i, [128, 512], F32) for i in range(2)]
            pi = 0
            it = 0
            for e in range(16):
                for h in range(2):
                    w1 = wr.load(dr["exp_w1"][l, e][:, h * 512:(h + 1) * 512], dr["exp_w1"])
                    w3 = wr.load(dr["exp_w3"][l, e][:, h * 512:(h + 1) * 512], dr["exp_w3"])
                    for b, (t0, nb) in blocks:
                        g_ = gb[it % 2]
                        it += 1
                        psg = P.banks[6 + it % 2]
                        P.mm(psg[:, :nb], id16[:, e:e + 1].to_broadcast([16, 128]), gateT[:, t0:t0 + nb], True, True,
                             [id16, gateT], [psg])
                        P.copy(g_[:, :nb], psg[:, :nb], [psg], [g_], eng="act")
                        for f4 in range(4):
                            f = h * 4 + f4
                            ps1 = P.banks[pi % 6]
                            ps3 = P.banks[(pi + 1) % 6]
                            pi += 2
                            st = s1[(pi // 2) % 2]
                            for kc in range(KC):
                                P.mm(ps1[:, :nb], w1[:, kc, f4 * 128:(f4 + 1) * 128], hT[:, kc, t0:t0 + nb], kc == 0, kc == KC - 1,
                                     [w1, hT], [ps1])
                            for kc in range(KC):
                                P.mm(ps3[:, :nb], w3[:, kc, f4 * 128:(f4 + 1) * 128], hT[:, kc, t0:t0 + nb], kc == 0, kc == KC - 1,
                                     [w3, hT], [ps3])
                            P.act(st[:, :nb], ps1[:, :nb], AF.Silu, [ps1], [st])
                            P.tt(st[:, :nb], st[:, :nb], ps3[:, :nb], ALU.mult, [st, ps3], [st])
                            P.tt(hid[:, f, t0:t0 + nb], st[:, :nb], g_[:, :nb], ALU.mult, [st, g_], [hid])
                for h2 in range(2):
                    w2 = wr.load(dr["exp_w2"][l, e][:, h2 * 512:(h2 + 1) * 512], dr["exp_w2"])
                    for b, (t0, nb) in blocks:
                        for o4 in range(4):
                            oc = h2 * 4 + o4
                            psy = P.banks[pi % 6]
                            pi += 1
                            for f in range(KC):
                                P.mm(psy[:, :nb], w2[:, f, o4 * 128:(o4 + 1) * 128], hid[:, f, t0:t0 + nb], f == 0, f == KC - 1,
                                     [w2, hid], [psy])
                            P.stt(xT[:, oc, t0:t0 + nb], psy[:, :nb], mvec(k, l, 5, oc, SI(b)), xT[:, oc, t0:t0 + nb],
                                  ALU.mult, ALU.add, [psy, k.modv, xT], [xT])


def moe_layer_sparse(k, s, l, hT, xT, with_ctx, tap):
    P = k.P
    dr = k.dr
    SI = lambda b: 4 if b == 0 else s
    blocks = list(enumerate(BLOCKS)) if with_ctx else list(enumerate(BLOCKS))[1:]
    nslot = 288 if with_ctx else 256
    jcs = [(0, 128), (128, 128)] + ([(256, 32)] if with_ctx else [])
    tiles = list(range(18)) if with_ctx else list(range(2, 18))
    with P.phase():
        gp = P.sbuf("gp", [48, T], F32)
        pos_tok = P.sbuf("pos_tok", [128, 18, 16], F32)
        with P.overlay(hT):
            h_tok = P.sbuf("h_tok", [128, 18, D], BF16)
        gateT = gp
        with P.phase():
            sc = NormScratch(k, P.banks[0])
            hf = P.sbuf("hf", [128, KC, 512], F32)
            wrt = P.sbuf("wrt", [128, KC, 48], F32)
            ones16 = P.sbuf("ones48", [48, 48], F32)
            ex = P.sbuf("ex", [48, 512], F32)
            identf = P.sbuf("identf", [128, 128], F32)
            P.dma("sp", identf[:], dr["ident"][:], identf, dr["ident"])
            P.dma("sp", ones16[:], dr["bd48"][:], ones16, dr["bd48"])
            P.dma("sp", wrt[:], dr["router_w48"][l].rearrange("(kc p) e -> p kc e", p=128), wrt, dr["router_w48"])
            psl = P.banks[1]
            pss = P.banks[2]
            ti = 0
            for b, (t0, nb) in blocks:
                xn = rms_block(k, sc, xT[:, :, t0:t0 + nb], xT, nb)
                for kc in range(KC):
                    P.ts(hf[:, kc, :nb], xn[:, kc, :nb], k.Amod[:, l, 1, kc, SI(b):SI(b) + 1], mvec(k, l, 3, kc, SI(b)),
                         ALU.mult, ALU.add, [xn, k.Amod, k.modv], [hf])
                for kc in range(KC):
                    P.mm(psl[0:48, :nb], wrt[:, kc, :], hf[:, kc, :nb], kc == 0, kc == KC - 1, [wrt, hf], [psl])
                P.act(ex[:, :nb], psl[0:48, :nb], AF.Exp, [psl], [ex])
                P.mm(pss[0:48, :nb], ones16[:], ex[:, :nb], True, True, [ones16, ex], [pss])
                P.op("dve", lambda e, nb=nb: e.reciprocal(out=sc.rs[0:48, :nb], in_=pss[0:48, :nb]), [pss], [sc.rs])
                P.tt(gateT[0:48, t0:t0 + nb], ex[:, :nb], sc.rs[0:48, :nb], ALU.mult, [ex, sc.rs], [gateT])
                for tl in range(nb // 128):
                    tile = t0 // 128 + tl
                    for half in range(2):
                        bk = P.banks[3 + ti % 4]
                        ti += 1
                        for q in range(4):
                            kc = half * 4 + q
                            P.op("pe", lambda e, bk=bk, q=q, kc=kc, tl=tl: e.transpose(
                                out=bk[:, q * 128:(q + 1) * 128], in_=hf[:, kc, tl * 128:(tl + 1) * 128], identity=identf[:]),
                                [hf, identf], [bk])
                        P.copy(h_tok[:, tile, half * 512:(half + 1) * 512], bk[:, :], [bk], [h_tok],
                               eng="act" if ti % 2 == 0 else "dve")
            work = P.sbuf("work", [48, S], F32)
            m8 = P.sbuf("m8", [48, 8], F32)
            segs = [(LC, S)] + ([(0, LC)] if with_ctx else [])
            for (a0, n) in segs:
                cap = 2 * n // 16
                P.copy(work[:, :n], gateT[0:48, a0:a0 + n], [gateT], [work])
                for r in range(cap // 8):
                    P.op("dve", lambda e, n=n: e.max(out=m8[:], in_=work[:, :n]), [work], [m8])
                    if r < cap // 8 - 1:
                        P.op("dve", lambda e, n=n: e.match_replace(out=work[:, :n], in_to_replace=m8[:], in_values=work[:, :n],
                                                                   imm_value=-1.0), [work, m8], [work])
                P.stt(gateT[0:48, a0:a0 + n], gateT[0:48, a0:a0 + n], m8[:, 7:8], gateT[0:48, a0:a0 + n], ALU.is_ge, ALU.mult,
                      [gateT, m8], [gateT])
                P.ts(work[32:48, :n], gateT[32:48, a0:a0 + n], 0.0, None, ALU.is_gt, None, [gateT], [work])
                P.op("dve", lambda e, a0=a0, n=n: e.tensor_tensor_scan(out=gp[32:48, a0:a0 + n], data0=work[32:48, :n], data1=work[32:48, :n],
                                                                       initial=0.0, op0=ALU.add, op1=ALU.max), [work], [gp])
                if a0 == 0:
                    P.ts(gp[32:48, a0:a0 + n], gp[32:48, a0:a0 + n], 256.0, None, ALU.add, None, [gp], [gp])
                P.tt(gp[32:48, a0:a0 + n], gp[32:48, a0:a0 + n], work[32:48, :n], ALU.mult, [gp, work], [gp])
                P.ts(gp[32:48, a0:a0 + n], gp[32:48, a0:a0 + n], -1.0, None, ALU.add, None, [gp], [gp])
            tap("L%d_gate" % l, gp, gp[0:16, :], [16, T])
            bk = P.banks[1]
            bk3 = bk[:, 0:288].rearrange("p (a b) -> p a b", a=18)
            for tile in tiles:
                P.op("pe", lambda e, tile=tile: e.transpose(out=bk3[:, tile, :], in_=gp[32:48, tile * 128:(tile + 1) * 128],
                                                            identity=identf[32:48, 32:48]), [gp, identf], [bk])
            P.copy(pos_tok[:, tiles[0]:18, :], bk3[:, tiles[0]:18, :], [bk], [pos_tok])
        with P.phase():
            wr = WRing(k, 3)
            id48 = P.sbuf("id48", [48, 48], F32)
            P.dma("sp", id48[:], dr["ident"][0:48, 0:48], id48, dr["ident"])
            iota_j = P.sbuf("iota_j", [128, 288], F32)
            iota_p = P.sbuf("iota_p", [128, 3], F32)
            P.dma("sp", iota_j[:], dr["iota_j"][:], iota_j, dr["iota_j"])
            P.dma("sp", iota_p[:], dr["iota_p"][:], iota_p, dr["iota_p"])
            Sel = P.sbuf("Sel", [128, 18, nslot], BF16)
            SelT = P.sbuf("SelT", [128, len(jcs), T], BF16)
            xin = P.sbuf("xin", [128, KC, nslot], BF16)
            hid = P.sbuf("hid", [128, KC, nslot], BF16)
            y_tok = P.sbuf("y_tok", [128, len(jcs), D], BF16)
            gb = P.sbuf("gb", [128, 512], F32)
            s1 = [P.sbuf("s1_%d" % i, [128, nslot], F32) for i in range(2)]
            pi = 0
            it = 0
            for e in range(16):
                for tile in tiles:
                    P.ts(Sel[:, tile, :], iota_j[:, :nslot], pos_tok[:, tile, e:e + 1], None, ALU.is_equal, None,
                         [iota_j, pos_tok], [Sel])
                for b, (t0, nb) in blocks:
                    psp = P.banks[6]
                    psg = P.banks[7]
                    P.mm(psp[:, :nb], id48[:, 32 + e:33 + e].to_broadcast([48, 128]), gp[:, t0:t0 + nb], True, True, [id48, gp], [psp])
                    P.mm(psg[:, :nb], id48[:, e:e + 1].to_broadcast([48, 128]), gp[:, t0:t0 + nb], True, True, [id48, gp], [psg])
                    P.copy(gb[:, :nb], psg[:, :nb], [psg], [gb], eng="act")
                    for ji, (j0, nj) in enumerate(jcs):
                        P.stt(SelT[0:nj, ji, t0:t0 + nb], psp[0:nj, :nb], iota_p[0:nj, ji:ji + 1], gb[0:nj, :nb], ALU.is_equal, ALU.mult,
                              [psp, iota_p, gb], [SelT])
                for kc in range(KC):
                    ps = P.banks[pi % 6]
                    pi += 1
                    for i_, tile in enumerate(tiles):
                        P.mm(ps[:, :nslot], h_tok[:, tile, kc * 128:(kc + 1) * 128], Sel[:, tile, :], i_ == 0, i_ == len(tiles) - 1,
                             [h_tok, Sel], [ps])
                    P.copy(xin[:, kc, :], ps[:, :nslot], [ps], [xin], eng="act" if kc % 2 == 0 else "dve")
                for h in range(2):
                    w1 = wr.load(dr["exp_w1"][l, e][:, h * 512:(h + 1) * 512], dr["exp_w1"])
                    w3 = wr.load(dr["exp_w3"][l, e][:, h * 512:(h + 1) * 512], dr["exp_w3"])
                    for f4 in range(4):
                        f = h * 4 + f4
                        ps1 = P.banks[pi % 6]
                        ps3 = P.banks[(pi + 1) % 6]
                        pi += 2
                        st = s1[(pi // 2) % 2]
                        for kc in range(KC):
                            P.mm(ps1[:, :nslot], w1[:, kc, f4 * 128:(f4 + 1) * 128], xin[:, kc, :], kc == 0, kc == KC - 1, [w1, xin], [ps1])
                        for kc in range(KC):
                            P.mm(ps3[:, :nslot], w3[:, kc, f4 * 128:(f4 + 1) * 128], xin[:, kc, :], kc == 0, kc == KC - 1, [w3, xin], [ps3])
                        P.act(st[:], ps1[:, :nslot], AF.Silu, [ps1], [st])
                        P.tt(hid[:, f, :], st[:], ps3[:, :nslot], ALU.mult, [st, ps3], [hid])
                for h2 in range(2):
                    w2 = wr.load(dr["exp_w2"][l, e][:, h2 * 512:(h2 + 1) * 512], dr["exp_w2"])
                    for ji, (j0, nj) in enumerate(jcs):
                        psy = P.banks[pi % 6]
                        pi += 1
                        for f in range(KC):
                            P.mm(psy[0:nj, :], hid[:, f, j0:j0 + nj], w2[:, f, :], f == 0, f == KC - 1, [hid, w2], [psy])
                        P.copy(y_tok[0:nj, ji, h2 * 512:(h2 + 1) * 512], psy[0:nj, :], [psy], [y_tok], eng="act" if ji % 2 == 0 else "dve")
                for b, (t0, nb) in blocks:
                    for oc in range(KC):
                        pso = P.banks[pi % 6]
                        pi += 1
                        for ji, (j0, nj) in enumerate(jcs):
                            P.mm(pso[:, :nb], y_tok[0:nj, ji, oc * 128:(oc + 1) * 128], SelT[0:nj, ji, t0:t0 + nb], ji == 0, ji == len(jcs) - 1,
                                 [y_tok, SelT], [pso])
                        P.stt(xT[:, oc, t0:t0 + nb], pso[:, :nb], mvec(k, l, 5, oc, SI(b)), xT[:, oc, t0:t0 + nb],
                              ALU.mult, ALU.add, [pso, k.modv, xT], [xT])


RB = [(0, 256)] + [(256 + 256 * i, 256) for i in range(8)]
V_MU, V_W0, V_A0, V_KK, V_KA, V_RK, V_GG, V_GB = 0, 6, 8, 10, 11, 12, 13, 14
NEG_EXP_HALF = -0.6065306597126334


def layer1_mixer(k, s, hT, xT, tap):
    P = k.P
    dr = k.dr
    SI = lambda b: 4 if b == 0 else s
    with P.phase():
        sc = NormScratch(k, P.banks[0])
        for b, (t0, nb) in enumerate(BLOCKS):
            xn = rms_block(k, sc, xT[:, :, t0:t0 + nb], xT, nb)
            for kc in range(KC):
                P.ts(hT[:, kc, t0:t0 + nb], xn[:, kc, :nb], k.Amod[:, 1, 0, kc, SI(b):SI(b) + 1],
                     mvec(k, 1, 0, kc, SI(b)), ALU.mult, ALU.add, [xn, k.Amod, k.modv], [hT])
        P.dma("sp", k.xspill[:].rearrange("(kc p) t -> p kc t", p=128), xT[:, :, LC:T], k.xspill, xT)
        tap("L1_h", hT, hT[:], [128, KC, T])
    if k.stop_after == "L1A":
        return
    with P.phase():
        outT = P.sbuf("outT", [128, KC, S], BF16)
        with P.phase():
            rwkv_core(k, s, hT, xT, outT, tap)
        if k.stop_after in ("L1B", "L1L", "L1P"):
            return
        with P.phase():
            wr = WRing(k, 2)
            wo = [wr.load(dr["rw_wo"][:, h * 512:(h + 1) * 512], dr["rw_wo"]) for h in range(2)]
            P.dma("sp", xT[:, :, LC:T], k.xspill[:].rearrange("(kc p) t -> p kc t", p=128), xT, k.xspill)
            pi = 0
            for b, (t0, nb) in list(enumerate(BLOCKS))[1:]:
                for oc in range(KC):
                    ps = P.banks[pi % 4]
                    pi += 1
                    w = wo[oc // 4]
                    c0 = (oc % 4) * 128
                    for c in range(KC):
                        P.mm(ps[:, :nb], w[:, c, c0:c0 + 128], outT[:, c, t0 - LC:t0 - LC + nb], c == 0, c == KC - 1,
                             [w, outT], [ps])
                    P.stt(xT[:, oc, t0:t0 + nb], ps[:, :nb], mvec(k, 1, 2, oc, s), xT[:, oc, t0:t0 + nb], ALU.mult, ALU.add,
                          [ps, k.modv, xT], [xT])
            tap("L1_xmix", xT, xT[:], [128, KC, T])


def rwkv_core(k, s, hT, xT, outT, tap):
    P = k.P
    dr = k.dr
    with P.overlay(xT):
        lw1 = P.sbuf("lw1", [128, T], BF16)
        la1 = P.sbuf("la1", [128, T], BF16)
        lg1 = P.sbuf("lg1", [128, T], BF16)
        lg1b = P.sbuf("lg1b", [32, T], BF16)
        y_acc = P.sbuf("y_acc", [128, S], F32)
        bon = P.sbuf("bon", [128, S], F32)
    ov_mark = xT.ov_top
    vec = P.sbuf("rwvec", [128, 15, KC], F32)
    P.dma("sp", vec[:], dr["rw_vec"][:], vec, dr["rw_vec"])
    omk = P.sbuf("omk", [128, KC], F32)
    P.ts(omk[:], vec[:, V_KA, :], -1.0, 1.0, ALU.mult, ALU.add, [vec], [omk])
    omu = P.sbuf("omu", [128, 6, KC], F32)
    hmu = P.sbuf("hmu", [128, 6, KC], F32)
    P.ts(omu[:], vec[:, 0:6, :], -1.0, 1.0, ALU.mult, ALU.add, [vec], [omu])
    P.ts(hmu[:], vec[:, 0:6, :], 0.5, None, ALU.mult, None, [vec], [hmu])
    identb = P.sbuf("identb", [128, 128], BF16)
    P.dma("pool", identb[:], dr["ident"][:], identb, dr["ident"])
    identf = P.sbuf("identf", [128, 128], F32)
    P.dma("sp", identf[:], dr["ident"][:], identf, dr["ident"])
    bones = P.sbuf("bones", [128, 128], F32)
    P.dma("sp", bones[:], dr["blockones"][:], bones, dr["blockones"])
    maskC = P.sbuf("maskC", [128, 256], F32)
    P.dma("sp", maskC[:], dr["maskC"][:], maskC, dr["maskC"])
    m4 = [P.sbuf("m4_%d" % d, [128, 512], BF16) for d in range(2)]
    m1 = [P.sbuf("m1_%d" % d, [128, 128], BF16) for d in range(2)]
    for d in range(2):
        P.dma("pool", m4[d][:], dr["rmask4"][d], m4[d], dr["rmask4"])
        P.dma("pool", m1[d][:], dr["rmask1"][d], m1[d], dr["rmask1"])

    def vcol(i, c):
        return vec[:, i, c:c + 1]

    def load_scaled(name, src2d, ncols, mu_i):
        wa = P.sbuf(name + "a", [128, KC, ncols], BF16)
        wb = P.sbuf(name + "b", [128, KC, ncols], BF16)
        P.dma("pool", wa[:], src2d.rearrange("(kc p) n -> p kc n", p=128), wa, dr["rw_rkv"])
        for kc in range(KC):
            P.ts(wb[:, kc, :], wa[:, kc, :], hmu[:, mu_i, kc:kc + 1], None, ALU.mult, None, [wa, hmu], [wb])
        for kc in range(KC):
            P.ts(wa[:, kc, :], wa[:, kc, :], omu[:, mu_i, kc:kc + 1], None, ALU.mult, None, [wa, omu], [wa])
        return wa, wb

    def proj_mix(ps_ap, psbuf, wa, wb, c0, m, t0, nb):
        lo = 1 if t0 in (0, LC) else 0
        hi = nb - 1 if (t0 + nb) in (LC, T) else nb
        for kc in range(KC):
            P.mm(ps_ap, wa[:, kc, c0:c0 + m], hT[:, kc, t0:t0 + nb], kc == 0, False, [wa, hT], [psbuf])
        for kc in range(KC):
            P.mm(ps_ap[:, lo:nb], wb[:, kc, c0:c0 + m], hT[:, kc, t0 + lo - 1:t0 + nb - 1], False, False, [wb, hT], [psbuf])
        for kc in range(KC):
            P.mm(ps_ap[:, 0:hi], wb[:, kc, c0:c0 + m], hT[:, kc, t0 + 1:t0 + hi + 1], False, kc == KC - 1, [wb, hT], [psbuf])

    with P.phase():
        w1, w1s = load_scaled("w1c", dr["rw_w1cat"][:], 128, 1)
        a1, a1s = load_scaled("a1c", dr["rw_a1cat"][:], 128, 4)
        g1, g1s = load_scaled("g1c", dr["rw_g1"][:], 160, 5)
        pi = 0
        for (t0, nb) in BLOCKS:
            ps = P.banks[pi % 4]; pi += 1
            proj_mix(ps[:, :nb], ps, w1, w1s, 0, 128, t0, nb)
            P.act(lw1[:, t0:t0 + nb], ps[:, :nb], AF.Tanh, [ps], [lw1])
            ps = P.banks[pi % 4]; pi += 1
            proj_mix(ps[:, :nb], ps, a1, a1s, 0, 128, t0, nb)
            P.copy(la1[:, t0:t0 + nb], ps[:, :nb], [ps], [la1], eng="act")
            ps = P.banks[pi % 4]; pi += 1
            proj_mix(ps[:, :nb], ps, g1, g1s, 0, 128, t0, nb)
            P.act(lg1[:, t0:t0 + nb], ps[:, :nb], AF.Sigmoid, [ps], [lg1])
            ps = P.banks[pi % 4]; pi += 1
            proj_mix(ps[0:32, :nb], ps, g1, g1s, 128, 32, t0, nb)
            P.act(lg1b[:, t0:t0 + nb], ps[0:32, :nb], AF.Sigmoid, [ps], [lg1b])
    if k.stop_after == "L1L":
        tap("L1_lw1", lw1, lw1[:], [128, T])
        return
    for c in range(KC if k.stop_after != "L1P" else 1):
        with P.phase():
            rwkv_pair(k, s, c, hT, xT, ov_mark, lw1, la1, lg1, lg1b, y_acc, bon, outT, vec, omk, identb, identf, bones, maskC,
                      m4, m1, load_scaled, proj_mix, vcol, tap)
    tap("L1_og", outT, outT[:], [128, KC, S])


class DirBufs:
    def __init__(self, P, d):
        NB = 256
        self.f = [P.sbuf("rf%d_%d" % (d, i), [128, NB], F32) for i in range(10)]
        self.Vb = P.sbuf("Vb%d" % d, [128, NB], BF16)
        self.ATz, self.RTz, self.BTz, self.KTz = [[P.sbuf("z%d_%d_%d" % (d, i, h), [128, NB], BF16) for h in range(2)]
                                                  for i in range(4)]
        self.stk = [P.sbuf("stk%d_%d" % (d, i), [128, 192], BF16) for i in range(4)]
        self.Vz = [[P.sbuf("Vz%d_%d_%d" % (d, i, h), [128, 64], BF16) for h in range(2)] for i in range(4)]
        self.Uz = [P.sbuf("Uz%d_%d" % (d, h), [128, 64], BF16) for h in range(2)]
        self.mats = [P.sbuf("mats%d_%d" % (d, i), [128, 640], BF16) for i in range(4)]
        self.Lf = [P.sbuf("Lf%d_%d" % (d, i), [128, 256], F32) for i in range(4)]
        self.sq = [[P.sbuf("sq%d_%d_%d" % (d, i, j), [128, 256], F32) for j in range(2)] for i in range(4)]
        self.ZT = [P.sbuf("ZT%d_%d" % (d, i), [128, 128], F32) for i in range(4)]
        self.T0 = P.sbuf("T0_%d" % d, [128, 64], F32)
        self.T0s = P.sbuf("T0s_%d" % d, [128, 64], F32)
        self.T0b = P.sbuf("T0b_%d" % d, [128, 64], BF16)
        self.Xb = P.sbuf("Xb_%d" % d, [128, 64], F32)
        zs = [t_ for l_ in (self.ATz, self.RTz, self.BTz, self.KTz) for t_ in l_] + [v_ for l_ in self.Vz for v_ in l_] + self.Uz
        for t_ in zs + [self.T0, self.T0b]:
            P.memset(t_[:], 0.0, [t_])


def rwkv_pair(k, s, c, hT, xT, ov_mark, lw1, la1, lg1, lg1b, y_acc, bon, outT, vec, omk, identb, identf, bones, maskC, m4, m1,
              load_scaled, proj_mix, vcol, tap):
    P = k.P
    dr = k.dr
    cs_ = slice(c * 128, (c + 1) * 128)
    wr_, wrs = load_scaled("wr", dr["rw_rkv"][0][:, cs_], 128, 0)
    wk_, wks = load_scaled("wk", dr["rw_rkv"][1][:, cs_], 128, 2)
    wv_, wvs = load_scaled("wv", dr["rw_rkv"][2][:, cs_], 128, 3)
    w2c = P.sbuf("w2c", [128, 128], BF16)
    a2c = P.sbuf("a2c", [128, 128], BF16)
    g2a = P.sbuf("g2a", [128, 128], BF16)
    g2b = P.sbuf("g2b", [32, 128], BF16)
    P.dma("pool", w2c[:], dr["rw_w2cat"][:, cs_], w2c, dr["rw_w2cat"])
    P.dma("pool", a2c[:], dr["rw_a2cat"][:, cs_], a2c, dr["rw_a2cat"])
    P.dma("pool", g2a[:], dr["rw_g2"][0:128, cs_], g2a, dr["rw_g2"])
    P.dma("pool", g2b[:], dr["rw_g2"][128:160, cs_], g2b, dr["rw_g2"])
    P.memset(y_acc[:], 0.0, [y_acc])
    P.memset(bon[:], 0.0, [bon])
    bufs = [DirBufs(P, 0)]
    with P.overlay(xT, start=ov_mark):
        bufs.append(DirBufs(P, 1))
    sqb = bufs[0].f[0]

    def dir_gen(d):
        W = bufs[d]
        B = P.banks[4 * d:4 * d + 4]
        r_f, k_f, v_f, kk_f, ic_f, lw_f, cs_f, e1, e2, tmp = W.f
        ATz, RTz, BTz, KTz, Vb, stk, Vz, Uz, mats, Lf, sq, ZT = (W.ATz, W.RTz, W.BTz, W.KTz, W.Vb, W.stk, W.Vz, W.Uz, W.mats,
                                                                W.Lf, W.sq, W.ZT)
        T0, T0s, T0b, Xb = W.T0, W.T0s, W.T0b, W.Xb
        order = list(range(9)) if d == 0 else [0] + list(range(8, 0, -1))
        for b in order:
            t0, nb = RB[b]
            lat = b > 0
            l0 = t0 - LC
            proj_mix(B[0][:, :nb], B[0], wr_, wrs, 0, 128, t0, nb)
            yield
            proj_mix(B[1][:, :nb], B[1], wk_, wks, 0, 128, t0, nb)
            yield
            proj_mix(B[2][:, :nb], B[2], wv_, wvs, 0, 128, t0, nb)
            pr_d = slice(d * 64, (d + 1) * 64)
            P.mm(B[3][:, 0:nb], w2c[pr_d, :], lw1[pr_d, t0:t0 + nb], True, True, [w2c, lw1], [B[3]])
            P.mm(B[3][:, 256:256 + nb], a2c[pr_d, :], la1[pr_d, t0:t0 + nb], True, True, [a2c, la1], [B[3]])
            P.copy(r_f[:], B[0][:, :nb], [B[0]], [r_f], eng="act")
            P.copy(k_f[:], B[1][:, :nb], [B[1]], [k_f], eng="act")
            P.copy(v_f[:], B[2][:, :nb], [B[2]], [v_f], eng="act")
            P.copy(Vb[:], v_f[:], [v_f], [Vb])
            yield
            P.act(lw_f[:], B[3][:, 0:nb], AF.Sigmoid, [B[3], vec], [lw_f], bias=vcol(V_W0 + d, c))
            P.ts(lw_f[:], lw_f[:], NEG_EXP_HALF, None, ALU.mult, None, [lw_f], [lw_f])
            P.act(ic_f[:], B[3][:, 256:256 + nb], AF.Sigmoid, [B[3], vec], [ic_f], bias=vcol(V_A0 + d, c))
            P.ts(kk_f[:], k_f[:], vcol(V_KK, c), None, ALU.mult, None, [k_f, vec], [kk_f])
            P.tt(tmp[:], kk_f[:], kk_f[:], ALU.mult, [kk_f], [tmp])
            P.mm(B[0][:, :nb], bones[:], tmp[:], True, True, [bones, tmp], [B[0]])
            yield
            P.ts(e1[:], B[0][:, :nb], 1e-18, None, ALU.max, None, [B[0]], [e1])
            P.act(e1[:], e1[:], AF.Ln, [e1], [e1])
            P.act(e1[:], e1[:], AF.Exp, [e1], [e1], scale=-0.5)
            P.tt(kk_f[:], kk_f[:], e1[:], ALU.mult, [kk_f, e1], [kk_f])
            P.ts(tmp[:], ic_f[:], vcol(V_KA, c), omk[:, c:c + 1], ALU.mult, ALU.add, [ic_f, vec, omk], [tmp])
            P.tt(k_f[:], k_f[:], tmp[:], ALU.mult, [k_f, tmp], [k_f])
            if lat:
                P.stt(tmp[:], r_f[:], vcol(V_RK, c), k_f[:], ALU.mult, ALU.mult, [r_f, vec, k_f], [tmp])
                P.mm(B[1][:, :nb], bones[:], tmp[:], True, True, [bones, tmp], [B[1]])
                yield
                P.tt(tmp[:], B[1][:, :nb], v_f[:], ALU.mult, [B[1], v_f], [tmp])
                P.tt(bon[:, l0:l0 + nb], bon[:, l0:l0 + nb], tmp[:], ALU.add, [bon, tmp], [bon])
            if d == 0:
                P.op("dve", lambda e: e.tensor_tensor_scan(out=cs_f[:], data0=maskC[:], data1=lw_f[:], initial=0.0,
                                                           op0=ALU.mult, op1=ALU.add), [maskC, lw_f], [cs_f])
            else:
                P.op("dve", lambda e: e.tensor_tensor_scan(out=cs_f[:, ::-1], data0=maskC[:], data1=lw_f[:, ::-1], initial=0.0,
                                                           op0=ALU.mult, op1=ALU.add), [maskC, lw_f], [cs_f])
            P.tt(e2[:], cs_f[:], lw_f[:], ALU.subtract, [cs_f, lw_f], [e2])
            P.act(e2[:], e2[:], AF.Exp, [e2], [e2])
            for h in range(2):
                pr = slice(h * 64, (h + 1) * 64)
                P.stt(ATz[h][pr], kk_f[pr], -1.0, e2[pr], ALU.mult, ALU.mult, [kk_f, e2], [ATz[h]])
            P.act(e1[:], cs_f[:], AF.Exp, [cs_f], [e1])
            for h in range(2):
                pr = slice(h * 64, (h + 1) * 64)
                P.tt(RTz[h][pr], r_f[pr], e1[pr], ALU.mult, [r_f, e1], [RTz[h]])
            P.act(e2[:], cs_f[:], AF.Exp, [cs_f], [e2], scale=-1.0)
            P.tt(tmp[:], kk_f[:], ic_f[:], ALU.mult, [kk_f, ic_f], [tmp])
            for h in range(2):
                pr = slice(h * 64, (h + 1) * 64)
                P.tt(BTz[h][pr], tmp[pr], e2[pr], ALU.mult, [tmp, e2], [BTz[h]])
                P.tt(KTz[h][pr], k_f[pr], e2[pr], ALU.mult, [k_f, e2], [KTz[h]])
            yield
            nch = nb // 64
            for n in range(nch):
                cols = slice(n * 64, (n + 1) * 64)
                pb = B[n % 2]
                for h in range(2):
                    pr = slice(h * 64, (h + 1) * 64)
                    for mi, src in enumerate((Vb, BTz[h], KTz[h])):
                        P.mm(pb[pr, mi * 64:(mi + 1) * 64], src[:, cols], identb[:, pr], True, True, [src, identb], [pb])
                P.copy(stk[n][:], pb[:, 0:192], [pb], [stk[n]], eng="act")
                for h in range(2):
                    pr = slice(h * 64, (h + 1) * 64)
                    P.copy(Vz[n][h][pr, :], stk[n][pr, 0:64], [stk[n]], [Vz[n][h]])
                yield
            for n in range(nch):
                cols = slice(n * 64, (n + 1) * 64)
                pa = B[2 + n % 2]
                pb = B[n % 2]
                for h in range(2):
                    pr = slice(h * 64, (h + 1) * 64)
                    combos = ((BTz[h], ATz[h]), (ATz[h], BTz[h]), (KTz[h], ATz[h]), (BTz[h], RTz[h]))
                    for mi, (lh, rh) in enumerate(combos):
                        P.mm(pa[pr, mi * 128 + h * 64: mi * 128 + (h + 1) * 64], lh[:, cols], rh[:, cols], True, True,
                             [lh, rh], [pa])
                    P.mm(pb[pr, 256 + h * 64:256 + (h + 1) * 64], KTz[h][:, cols], RTz[h][:, cols], True, True,
                         [KTz[h], RTz[h]], [pb])
                P.tt(Lf[n][:, 0:256], pa[:, 0:256], m4[d][:, 0:256], ALU.mult, [pa, m4[d]], [Lf[n]])
                P.tt(mats[n][:, 256:512], pa[:, 256:512], m4[d][:, 256:512], ALU.mult, [pa, m4[d]], [mats[n]])
                P.tt(mats[n][:, 512:640], pb[:, 256:384], m1[d][:], ALU.mult, [pb, m1[d]], [mats[n]])
                yield
            cur = []
            for n in range(nch):
                P.tt(ZT[n][:], Lf[n][:, 0:128], identf[:], ALU.add, [Lf[n], identf], [ZT[n]])
                cur.append((Lf[n], 128, 0))
            for lvl in range(5):
                last = lvl == 4
                for n in range(nch):
                    src, oL, oLT = cur[n]
                    dst = sq[n][lvl % 2]
                    pa = B[n % 2]
                    P.mm(pa[:, 0:128], src[:, oLT:oLT + 128], src[:, oL:oL + 128], True, True, [src], [pa])
                    if not last:
                        P.mm(pa[:, 128:256], src[:, oL:oL + 128], src[:, oLT:oLT + 128], True, True, [src], [pa])
                        P.copy(dst[:, 0:256], pa[:, 0:256], [pa], [dst], eng="act")
                    else:
                        P.copy(dst[:, 0:128], pa[:, 0:128], [pa], [dst], eng="act")
                    cur[n] = (dst, 0, 128)
                    if n % 2 == 1:
                        yield
                for n in range(nch):
                    src, oL, oLT = cur[n]
                    pz = B[2 + n % 2]
                    P.mm(pz[:, 0:128], src[:, oL:oL + 128], ZT[n][:], True, True, [src, ZT[n]], [pz])
                    P.tt(ZT[n][:], ZT[n][:], pz[:, 0:128], ALU.add, [ZT[n], pz], [ZT[n]])
                    if n % 2 == 1:
                        yield
            chunks = list(range(nch)) if d == 0 else list(range(nch - 1, -1, -1))
            for n in chunks:
                cols = slice(n * 64, (n + 1) * 64)
                dcol = (n * 64 + 63) if d == 0 else n * 64
                dC = e1[:, dcol:dcol + 1]
                pX = B[0]
                for h in range(2):
                    pr = slice(h * 64, (h + 1) * 64)
                    P.mm(pX[pr, 0:64], ATz[h][:, cols], T0b[:, :], True, False, [ATz[h], T0b], [pX])
                P.mm(pX[:, 0:64], mats[n][:, 256:384], stk[n][:, 0:64], False, True, [mats[n], stk[n]], [pX])
                P.ts(T0s[:], T0[:], dC, None, ALU.mult, None, [T0, e1], [T0s])
                yield
                P.copy(Xb[:], pX[:, 0:64], [pX], [Xb], eng="act")
                pU = B[1]
                P.mm(pU[:, 0:64], ZT[n][:], Xb[:], True, True, [ZT[n], Xb], [pU])
                yield
                P.copy(Uz[0][0:64, :], pU[0:64, 0:64], [pU], [Uz[0]], eng="act")
                P.copy(Uz[1][64:128, :], pU[64:128, 0:64], [pU], [Uz[1]])
                pT = B[3]
                for h in range(2):
                    pr = slice(h * 64, (h + 1) * 64)
                    P.mm(pT[pr, 0:64], stk[n][:, 64:128], Uz[h][:, :], True, False, [stk[n], Uz[h]], [pT])
                    P.mm(pT[pr, 0:64], stk[n][:, 128:192], Vz[n][h][:, :], False, True, [stk[n], Vz[n][h]], [pT])
                if lat:
                    pY = B[2]
                    for h in range(2):
                        pr = slice(h * 64, (h + 1) * 64)
                        P.mm(pY[pr, 0:64], T0b[:, :], RTz[h][:, cols], True, False, [T0b, RTz[h]], [pY])
                        P.mm(pY[pr, 0:64], Uz[h][:, :], mats[n][:, 384 + h * 64:384 + (h + 1) * 64], False, False,
                             [Uz[h], mats[n]], [pY])
                        P.mm(pY[pr, 0:64], stk[n][:, 0:64], mats[n][:, 512 + h * 64:512 + (h + 1) * 64], False, True,
                             [stk[n], mats[n]], [pY])
                yield
                P.stt(T0[:], pT[:, 0:64], dC, T0s[:], ALU.mult, ALU.add, [pT, e1, T0s], [T0])
                P.copy(T0b[:], T0[:], [T0], [T0b], eng="act")
                if lat:
                    yc = slice(l0 + n * 64, l0 + (n + 1) * 64)
                    P.tt(y_acc[:, yc], y_acc[:, yc], pY[:, 0:64], ALU.add, [y_acc, pY], [y_acc])
                yield

    gens = [dir_gen(0), dir_gen(1)]
    alive = [True, True]
    while any(alive):
        for gi, g in enumerate(gens):
            if alive[gi]:
                try:
                    next(g)
                except StopIteration:
                    alive[gi] = False
    if c == 0:
        tap("L1_y0", y_acc, y_acc[:], [128, S])
        tap("L1_bon0", bon, bon[:], [128, S])
    B = P.banks
    sqb2 = bufs[1].f[0]
    for i in range(4):
        l0 = i * 512
        t0 = LC + l0
        nb = 512
        yb = y_acc[:, l0:l0 + nb]
        sq_ = [bufs[0].f[0], bufs[0].f[1]]
        for hf in range(2):
            c0_ = l0 + hf * 256
            ybh = y_acc[:, c0_:c0_ + 256]
            sc_ = bufs[hf].f[0]
            P.mm(B[0][:, hf * 256:(hf + 1) * 256], bones[:], ybh, True, True, [bones, y_acc], [B[0]])
            P.stt(ybh, B[0][:, hf * 256:(hf + 1) * 256], -1.0 / 64, ybh, ALU.mult, ALU.add, [B[0], y_acc], [y_acc])
            P.tt(sc_[:], ybh, ybh, ALU.mult, [y_acc], [sc_])
            P.mm(B[1][:, hf * 256:(hf + 1) * 256], bones[:], sc_[:], True, True, [bones, sc_], [B[1]])
            P.act(sc_[:], B[1][:, hf * 256:(hf + 1) * 256], AF.Ln, [B[1], k.gneps], [sc_], scale=1.0 / 64, bias=k.gneps[:, 0:1])
            P.act(sc_[:], sc_[:], AF.Exp, [sc_], [sc_], scale=-0.5)
            P.tt(ybh, ybh, sc_[:], ALU.mult, [y_acc, sc_], [y_acc])
        P.ts(yb, yb, vcol(V_GG, c), vcol(V_GB, c), ALU.mult, ALU.add, [y_acc, vec], [y_acc])
        P.tt(yb, yb, bon[:, l0:l0 + nb], ALU.add, [y_acc, bon], [y_acc])
        P.mm(B[2][:, :nb], g2a[:], lg1[:, t0:t0 + nb], True, False, [g2a, lg1], [B[2]])
        P.mm(B[2][:, :nb], g2b[:], lg1b[:, t0:t0 + nb], False, True, [g2b, lg1b], [B[2]])
        P.tt(outT[:, c, l0:l0 + nb], yb, B[2][:, :nb], ALU.mult, [y_acc, B[2]], [outT])


_CACHE = {}


def kernel(**inputs):
    inp = {k_: np.asarray(v) for k_, v in inputs.items()}
    B = inp["x"].shape[0]
    ns = B // NCORE
    if "nc" not in _CACHE:
        _CACHE["nc"] = build(ns=ns)[0]
    nc = _CACHE["nc"]
    sh = prep_shared(inp)
    in_maps = []
    for core in range(NCORE):
        m = dict(sh)
        m.update(prep_core(inp, core, ns))
        in_maps.append(m)
    res = run_bass_kernel_spmd(nc, in_maps, core_ids=list(range(NCORE)))
    out = np.empty((B, S, D), np.float32)
    for core in range(NCORE):
        o = res.results[core]["outT"]
        for s in range(ns):
            out[core * ns + s] = o[s].T
    return out
```

```python
from contextlib import ExitStack, contextmanager
import numpy as np
import concourse.bass as bass
import concourse.mybir as mybir
from concourse.bass_utils import run_bass_kernel_spmd

F32 = mybir.dt.float32
BF16 = mybir.dt.bfloat16
AF = mybir.ActivationFunctionType
ALU = mybir.AluOpType
AX = mybir.AxisListType

ENGS = ("pe", "dve", "act", "pool", "sp")

D = 1024
KC = 8
S = 2048
LC = 256
T = S + LC
NCORE = 8
BLOCKS = [(0, 256), (256, 512), (768, 512), (1280, 512), (1792, 512)]
LAT_BLOCKS = BLOCKS[1:]
EPS = 1e-6
ARENA_BYTES = 206 * 1024


class Buf:
    def __init__(self, prog, t, name, space):
        self.prog = prog
        self.t = t
        self.name = name
        self.space = space
        self.last_w = None
        self.reads = {}
        self.dsem = None

    def __getitem__(self, idx):
        return self.t[idx]

    def view(self, name):
        b = Buf(self.prog, self.t, name, self.space)
        self.prog._register(b)
        return b


class Prog:
    def __init__(self):
        self.nc = bass.Bass("TRN2", target_bir_lowering=False)
        self.gstack = ExitStack()
        self.ops = {e: [] for e in ENGS}
        self.cnt = {e: 0 for e in ENGS}
        self.waited = {e: {} for e in ENGS}
        self.sems = {}
        self.dpool = []
        self.dcnt = {}
        self.live_dsem = set()
        self.ndsem = 0
        self.live_bufs = []
        self.phase_bufs = None
        self.uid = 0
        for e in ENGS:
            self._sem("eng_" + e)
        self.arena = self.gstack.enter_context(self.nc.sbuf_tensor("arena", [128, ARENA_BYTES // 4], F32))
        self.top = 0
        self.limit = ARENA_BYTES
        self.hiwater = 0
        self.banks = []
        for i in range(8):
            t = self.gstack.enter_context(self.nc.psum_tensor("bank%d" % i, [128, 512], F32))
            b = Buf(self, t, "bank%d" % i, "psum")
            self.live_bufs.append(b)
            self.banks.append(b)

    def _sem(self, key):
        if key not in self.sems:
            self.sems[key] = self.gstack.enter_context(self.nc.semaphore("s_" + key))
        return self.sems[key]

    def _register(self, b):
        self.live_bufs.append(b)
        if self.phase_bufs is not None:
            self.phase_bufs.append(b)

    def _name(self, name):
        self.uid += 1
        return "%s_%d" % (name, self.uid)

    def sbuf(self, name, shape, dtype=F32):
        esz = 2 if dtype == BF16 else 4
        nel = 1
        for d in shape[1:]:
            nel *= d
        nb = (nel * esz + 63) // 64 * 64
        off = self.top
        self.top += nb
        assert self.top <= self.limit, "arena overflow %s: top=%d limit=%d" % (name, self.top, self.limit)
        self.hiwater = max(self.hiwater, self.top)
        ap = self.arena[:, off // 4:(off + nb) // 4]
        if dtype != F32:
            ap = ap.bitcast(dtype)
        ap = ap[:, :nel]
        if len(shape) > 2:
            names = ["d%d" % i for i in range(len(shape) - 1)]
            pat = "p (%s) -> p %s" % (" ".join(names), " ".join(names))
            ap = ap.rearrange(pat, **{n: v for n, v in zip(names, shape[1:])})
        if shape[0] < 128:
            ap = ap[0:shape[0]]
        b = Buf(self, ap, name, "sbuf")
        b.off = off
        b.nbytes = nb
        self._register(b)
        return b

    @contextmanager
    def overlay(self, buf, start=None):
        st = (self.top, self.limit)
        self.top, self.limit = (buf.off if start is None else start), buf.off + buf.nbytes
        try:
            yield
        finally:
            buf.ov_top = self.top
            self.top, self.limit = st

    def dram(self, name, shape, dtype=F32, kind="Internal"):
        t = self.nc.dram_tensor(name, list(shape), dtype, kind=kind)
        b = Buf(self, t.ap(), name, "dram")
        self.live_bufs.append(b)
        return b

    def _need(self, eng, key, val, waits):
        if key == "eng_pe" and eng == "pe":
            return
        if self.waited[eng].get(key, 0) >= val:
            return
        self.waited[eng][key] = val
        waits[key] = max(waits.get(key, 0), val)

    def _deps(self, eng, reads, writes):
        waits = {}
        for b in reads:
            if b.last_w is not None:
                self._need(eng, b.last_w[0], b.last_w[1], waits)
            if b.space == "psum":
                for k, v in b.reads.items():
                    if k != "eng_" + eng:
                        self._need(eng, k, v, waits)
        for b in writes:
            if b.last_w is not None:
                self._need(eng, b.last_w[0], b.last_w[1], waits)
            for k, v in b.reads.items():
                self._need(eng, k, v, waits)
        return list(waits.items())

    dbg_budget = None

    def op(self, eng, fn, reads=(), writes=()):
        if self.dbg_budget is not None:
            if self.dbg_budget <= 0:
                return
            self.dbg_budget -= 1
        waits = self._deps(eng, reads, writes)
        self.cnt[eng] += 1
        key = "eng_" + eng
        val = self.cnt[eng]
        for b in reads:
            if b.reads.get(key, 0) < val:
                b.reads[key] = val
        for b in writes:
            b.last_w = (key, val)
            b.reads = {}
        self.ops[eng].append((waits, fn, key, 1))

    def dma(self, eng, out_ap, in_ap, dst, src, **kw):
        waits = self._deps(eng, [src], [dst])
        if dst.dsem is None:
            if self.dpool:
                dst.dsem = self.dpool.pop()
            else:
                self.ndsem += 1
                dst.dsem = "d%d" % self.ndsem
                self._sem(dst.dsem)
                self.dcnt[dst.dsem] = 0
            self.live_dsem.add(dst.dsem)
        key = dst.dsem
        self.dcnt[key] += 16
        val = self.dcnt[key]
        if src.reads.get(key, 0) < val:
            src.reads[key] = val
        dst.last_w = (key, val)
        dst.reads = {}

        def fn(e, out_ap=out_ap, in_ap=in_ap, kw=kw):
            return e.dma_start(out=out_ap, in_=in_ap, **kw)

        self.ops[eng].append((waits, fn, key, 16))

    def barrier(self):
        toks = [("eng_" + e, self.cnt[e]) for e in ENGS if self.cnt[e] > 0]
        toks += [(k, self.dcnt[k]) for k in sorted(self.live_dsem) if self.dcnt[k] > 0]
        for e in ENGS:
            waits = {}
            for k, v in toks:
                if k == "eng_" + e:
                    continue
                self._need(e, k, v, waits)
            if waits:
                self.ops[e].append((list(waits.items()), None, None, 0))
        for b in self.live_bufs:
            b.last_w = None
            b.reads = {}

    @contextmanager
    def phase(self):
        prev_b = self.phase_bufs
        mark = (self.top, self.limit)
        self.phase_bufs = []
        try:
            yield
        finally:
            self.barrier()
            for b in self.phase_bufs:
                if b.dsem is not None:
                    if b.dsem in self.live_dsem:
                        self.live_dsem.discard(b.dsem)
                        self.dpool.append(b.dsem)
                    b.dsem = None
            dead = set(id(b) for b in self.phase_bufs)
            self.live_bufs = [b for b in self.live_bufs if id(b) not in dead]
            self.top, self.limit = mark
            self.phase_bufs = prev_b

    def final_wait(self, eng, bufs):
        waits = {}
        for b in bufs:
            if b.last_w is not None:
                self._need(eng, b.last_w[0], b.last_w[1], waits)
        self.ops[eng].append((list(waits.items()), None, None, 0))

    def emit(self):
        nc = self.nc
        sems = self.sems

        def run(e, lst):
            for waits, fn, key, inc in lst:
                for k, v in waits:
                    e.wait_ge(sems[k], v)
                if fn is not None:
                    fn(e).then_inc(sems[key], inc)

        with nc.Block() as block:
            @block.tensor
            def _(e):
                run(e, self.ops["pe"])

            @block.vector
            def _(e):
                run(e, self.ops["dve"])

            @block.scalar
            def _(e):
                run(e, self.ops["act"])

            @block.gpsimd
            def _(e):
                run(e, self.ops["pool"])

            @block.sync
            def _(e):
                run(e, self.ops["sp"])
        self.gstack.close()
        return nc

    def mm(self, out, lhsT, rhs, start, stop, R, W):
        self.op("pe", lambda e: e.matmul(out, lhsT=lhsT, rhs=rhs, start=start, stop=stop), R, W)

    def act(self, out, in_, func, R, W, scale=1.0, bias=0.0):
        self.op("act", lambda e: e.activation(out=out, in_=in_, func=func, bias=bias, scale=scale), R, W)

    def tt(self, out, in0, in1, op, R, W, eng="dve"):
        self.op(eng, lambda e: e.tensor_tensor(out=out, in0=in0, in1=in1, op=op), R, W)

    def ts(self, out, in0, s1, s2, op0, op1, R, W, eng="dve"):
        if op1 is None:
            self.op(eng, lambda e: e.tensor_scalar(out=out, in0=in0, scalar1=s1, scalar2=None, op0=op0), R, W)
        else:
            self.op(eng, lambda e: e.tensor_scalar(out=out, in0=in0, scalar1=s1, scalar2=s2, op0=op0, op1=op1), R, W)

    def stt(self, out, in0, scalar, in1, op0, op1, R, W):
        self.op("dve", lambda e: e.scalar_tensor_tensor(out=out, in0=in0, scalar=scalar, in1=in1, op0=op0, op1=op1), R, W)

    def copy(self, out, in_, R, W, eng="dve"):
        if eng == "act":
            self.op("act", lambda e: e.copy(out=out, in_=in_), R, W)
        else:
            self.op(eng, lambda e: e.tensor_copy(out=out, in_=in_), R, W)

    def memset(self, ap, val, W, eng="dve"):
        self.op(eng, lambda e: e.memset(ap, val), (), W)


def _rope_tables():
    rows = S // 64
    row = np.repeat(np.arange(rows), 64).astype(np.float32)
    col = np.tile(np.arange(64), rows).astype(np.float32)
    inv = (10000.0 ** (-np.arange(0, 32, 2, dtype=np.float32) / 32)).astype(np.float32)
    ar = row[:, None] * inv
    ac = col[:, None] * inv
    C = np.zeros((128, S), np.float32)
    Sg = np.zeros((128, S), np.float32)
    for p in range(128):
        d = p % 64
        ang = ar if d < 32 else ac
        i = d % 16
        C[p] = np.cos(ang[:, i])
        sgn = -1.0 if (d % 32) < 16 else 1.0
        Sg[p] = sgn * np.sin(ang[:, i])
    return C, Sg


def _perm_rot(cols64):
    idx = np.arange(64)
    out = idx.copy()
    for base in (0, 32):
        out[base:base + 16] = idx[base + 16:base + 32]
        out[base + 16:base + 32] = idx[base:base + 16]
    return cols64[out]


def prep_shared(inp):
    f = np.float32
    sh = {}
    sh["mod_w"] = np.ascontiguousarray(inp["mod_w"], f)
    sh["mod_bT"] = np.ascontiguousarray(inp["mod_b"].reshape(2, 48, 128).transpose(2, 0, 1), f)
    g = np.stack([inp["norm_mix"], inp["norm_ffn"]], 0)
    sh["gT"] = np.ascontiguousarray(g.reshape(2, 2, 8, 128).transpose(3, 0, 1, 2), f)
    sh["finT"] = np.ascontiguousarray(inp["final_norm"].reshape(8, 128).T, f)
    sh["router_w"] = np.ascontiguousarray(inp["router_w"], f)
    sh["exp_w1"] = inp["exp_w1"]
    sh["exp_w3"] = inp["exp_w3"]
    sh["exp_w2"] = inp["exp_w2"]
    w_in = inp["mix_in"][0]
    q = w_in[:, 0:512]
    k = w_in[:, 512:640]
    v = w_in[:, 640:768]
    ul = w_in[:, 768:1280]
    gl = w_in[:, 1280:1792]
    qcols, qpcols = [], []
    for j in range(4):
        for h in (j, 4 + j):
            c64 = np.arange(h * 64, (h + 1) * 64)
            qcols.append(c64)
            qpcols.append(_perm_rot(c64))
    qcols = np.concatenate(qcols)
    qpcols = np.concatenate(qpcols)
    kp = np.concatenate([_perm_rot(np.arange(0, 64)), _perm_rot(np.arange(64, 128))])
    sh["w_in"] = np.ascontiguousarray(np.concatenate([q[:, qcols], q[:, qpcols], k, k[:, kp], v, ul, gl], 1), f)
    w_out = inp["mix_out"][0]
    arows = []
    for j in range(4):
        for h in (j, 4 + j):
            arows.append(np.arange(h * 64, (h + 1) * 64))
    arows = np.concatenate(arows + [np.arange(512, 1024)])
    sh["w_out"] = np.ascontiguousarray(w_out[arows], f)
    sh["sinkb"] = np.ascontiguousarray(np.broadcast_to(inp["attn_sink"][0][None, :], (128, 8)), f)
    C, Sg = _rope_tables()
    sh["ropeC"] = C
    sh["ropeS"] = Sg
    kq = np.arange(128)
    mP = (kq[:, None] >= kq[None, :]).astype(f)
    mN = (kq[:, None] <= kq[None, :]).astype(f)
    sh["maskP"] = np.ascontiguousarray(np.tile(mP, (1, 4)))
    sh["maskN"] = np.ascontiguousarray(np.tile(mN, (1, 4)))
    sh["convw"] = np.ascontiguousarray(inp["lru_conv_w"][0].reshape(4, 4, 128).transpose(2, 1, 0), f)
    sh["convb"] = np.ascontiguousarray(inp["lru_conv_b"][0].reshape(4, 128).T, f)

    def bd(w):
        o = np.zeros((128, 2, 4, 128), f)
        for d in range(2):
            for c in range(4):
                for hb in range(2):
                    o[hb * 64:(hb + 1) * 64, d, c, hb * 64:(hb + 1) * 64] = w[d, 2 * c + hb]
        return o
    sh["lru_wa"] = bd(inp["lru_wa"][0])
    sh["lru_wi"] = bd(inp["lru_wi"][0])

    def vec(a):
        return np.ascontiguousarray(a.reshape(2, 4, 128).transpose(2, 0, 1), f)
    sh["lru_ba"] = vec(inp["lru_ba"][0])
    sh["lru_bi"] = vec(inp["lru_bi"][0])
    sh["lru_lam"] = vec(inp["lru_lam"][0])
    sh["id16"] = np.eye(16, dtype=f)
    rw48 = np.zeros((2, 1024, 48), f)
    rw48[:, :, 0:16] = inp["router_w"]
    rw48[:, :, 32:48] = inp["router_w"]
    sh["router_w48"] = rw48
    bd = np.zeros((48, 48), f)
    for g_ in range(3):
        bd[g_ * 16:(g_ + 1) * 16, g_ * 16:(g_ + 1) * 16] = 1.0
    sh["bd48"] = bd
    sh["iota_j"] = np.ascontiguousarray(np.broadcast_to(np.arange(288, dtype=f)[None, :], (128, 288)))
    sh["iota_p"] = np.ascontiguousarray(np.arange(128, dtype=f)[:, None] + np.array([0, 128, 256], f)[None, :])
    def fm(v1024):
        return np.asarray(v1024, f).reshape(8, 128).T
    vecs = [inp["rw_mu"][0][i] for i in range(6)] + [inp["rw_w0"][0][0], inp["rw_w0"][0][1], inp["rw_a0"][0][0], inp["rw_a0"][0][1],
            inp["rw_kk"][0], inp["rw_ka"][0], inp["rw_rk"][0].reshape(1024), inp["rw_gn_g"][0], inp["rw_gn_b"][0]]
    sh["rw_vec"] = np.ascontiguousarray(np.stack([fm(v) for v in vecs], 1), f)
    sh["ident"] = np.eye(128, dtype=f)
    bo = np.zeros((128, 128), f)
    bo[:64, :64] = 1.0
    bo[64:, 64:] = 1.0
    sh["blockones"] = bo
    mc = np.ones((128, 256), f)
    mc[:, ::64] = 0.0
    sh["maskC"] = mc
    ii = np.arange(64)
    lt = (ii[:, None] < ii[None, :]).astype(f)
    le = (ii[:, None] <= ii[None, :]).astype(f)
    def bdm(m):
        o = np.zeros((128, 128), f)
        o[:64, :64] = m
        o[64:, 64:] = m
        return o
    sh["rmask4"] = np.ascontiguousarray(np.stack([np.concatenate([bdm(lt), bdm(le), bdm(lt), bdm(le)], 1),
                                                  np.concatenate([bdm(lt.T), bdm(le.T), bdm(lt.T), bdm(le.T)], 1)], 0), f)
    sh["rmask1"] = np.ascontiguousarray(np.stack([bdm(lt.T), bdm(lt)], 0), f)
    sh["rw_w1cat"] = np.ascontiguousarray(np.concatenate([inp["rw_w1"][0][0], inp["rw_w1"][0][1]], 1), f)
    sh["rw_a1cat"] = np.ascontiguousarray(np.concatenate([inp["rw_a1"][0][0], inp["rw_a1"][0][1]], 1), f)
    sh["rw_g1"] = np.ascontiguousarray(inp["rw_g1"][0], f)
    sh["rw_rkv"] = np.ascontiguousarray(inp["rw_rkv"][0], f)
    sh["rw_w2cat"] = np.ascontiguousarray(np.concatenate([inp["rw_w2"][0][0], inp["rw_w2"][0][1]], 0), f)
    sh["rw_a2cat"] = np.ascontiguousarray(np.concatenate([inp["rw_a2"][0][0], inp["rw_a2"][0][1]], 0), f)
    sh["rw_g2"] = np.ascontiguousarray(inp["rw_g2"][0], f)
    sh["rw_wo"] = np.ascontiguousarray(inp["rw_wo"][0], f)
    return sh


def prep_core(inp, core, ns):
    f = np.float32
    b0 = core * ns
    pc = {}
    pc["xT"] = np.ascontiguousarray(inp["x"][b0:b0 + ns].transpose(0, 2, 1), f)
    pc["cxT"] = np.ascontiguousarray(inp["ctx"][b0:b0 + ns].transpose(0, 2, 1), f)
    cc = np.zeros((8, 1024), f)
    cc[:ns] = inp["c"][b0:b0 + ns]
    cc[4] = inp["c_ctx"]
    pc["cT"] = np.ascontiguousarray(cc.reshape(8, 8, 128).transpose(2, 1, 0), f)
    return pc


class K:
    pass


GELU_C = 1.5957691216057308


def bank3(bank, a, b):
    return bank[:].rearrange("p (a b) -> p a b", a=a)


def build(ns=4, taps=None, stop_after=None, skip_l0=False):
    taps = taps or []
    P = Prog()
    k = K()
    k.P = P
    k.tap_outs = {}
    k.stop_after = stop_after
    k.skip_l0 = skip_l0
    import os as _os
    k.sparse = _os.environ.get("MOE_DENSE", "0") != "1"
    IN = "ExternalInput"
    dr = {}
    k.dr = dr

    def din(name, shape):
        dr[name] = P.dram(name, shape, F32, IN)
        return dr[name]

    din("xT", [ns, D, S]); din("cxT", [ns, D, LC]); din("cT", [128, KC, 8])
    din("mod_w", [2, D, 6 * D]); din("mod_bT", [128, 2, 48]); din("gT", [128, 2, 2, 8]); din("finT", [128, 8])
    din("router_w", [2, D, 16])
    din("exp_w1", [2, 16, D, D]); din("exp_w3", [2, 16, D, D]); din("exp_w2", [2, 16, D, D])
    din("w_in", [D, 2432]); din("w_out", [D, D]); din("sinkb", [128, 8])
    din("ropeC", [128, S]); din("ropeS", [128, S]); din("maskP", [128, 512]); din("maskN", [128, 512])
    din("convw", [128, 4, 4]); din("convb", [128, 4])
    din("lru_wa", [128, 2, 4, 128]); din("lru_wi", [128, 2, 4, 128])
    din("lru_ba", [128, 2, 4]); din("lru_bi", [128, 2, 4]); din("lru_lam", [128, 2, 4])
    din("id16", [16, 16]); din("iota_j", [128, 288]); din("iota_p", [128, 3]); din("router_w48", [2, D, 48]); din("bd48", [48, 48])
    din("rw_vec", [128, 15, KC]); din("ident", [128, 128]); din("blockones", [128, 128]); din("maskC", [128, 256])
    din("rmask4", [2, 128, 512]); din("rmask1", [2, 128, 128])
    din("rw_w1cat", [D, 128]); din("rw_a1cat", [D, 128]); din("rw_g1", [D, 160]); din("rw_rkv", [3, D, D])
    din("rw_w2cat", [128, D]); din("rw_a2cat", [128, D]); din("rw_g2", [160, D]); din("rw_wo", [D, D])
    k.xspill = P.dram("xspill", [D, S], F32, "Internal")
    k.outT = P.dram("outT", [ns, D, S], F32, "ExternalOutput")

    def tap(name, buf, ap, shape):
        if name not in taps:
            return
        o = P.dram("tap_" + name, list(shape), F32, "ExternalOutput")
        k.tap_outs[name] = o
        P.dma("pool", o[:], ap, o, buf)
    k.tap = tap

    k.ones_f = P.sbuf("ones_f", [128, 128], F32)
    P.memset(k.ones_f[:], 1.0, [k.ones_f])
    k.ones_b = P.sbuf("ones_b", [128, 64], BF16)
    P.memset(k.ones_b[:], 1.0, [k.ones_b])
    k.epsb = P.sbuf("epsb", [128, 1], F32)
    P.memset(k.epsb[:], EPS, [k.epsb])
    k.gneps = P.sbuf("gneps", [128, 1], F32)
    P.memset(k.gneps[:], 64e-5, [k.gneps])
    k.modv = modv = P.sbuf("modv", [128, 2, 48, 8], F32)
    k.Amod = Amod = P.sbuf("Amod", [128, 2, 2, 8, 8], F32)
    gT = P.sbuf("gT", [128, 2, 2, 8], F32)
    k.finT = P.sbuf("finT", [128, 8], F32)
    P.dma("sp", gT[:], dr["gT"][:], gT, dr["gT"])
    P.dma("sp", k.finT[:], dr["finT"][:], k.finT, dr["finT"])

    with P.phase():
        cT = P.sbuf("cT", [128, KC, 8], F32)
        P.dma("sp", cT[:], dr["cT"][:], cT, dr["cT"])
        sT = P.sbuf("sT", [128, KC, 8], F32)
        P.act(sT[:], cT[:], AF.Silu, [cT], [sT])
        mbT = P.sbuf("mbT", [128, 2, 48], F32)
        P.dma("sp", mbT[:], dr["mod_bT"][:], mbT, dr["mod_bT"])
        wm = [P.sbuf("wm%d" % i, [128, KC, 512], F32) for i in range(2)]
        it = 0
        for l in range(2):
            psm = P.banks[l]
            psm3 = psm[:, 0:384].rearrange("p (a b) -> p a b", a=48)
            for cb in range(12):
                w = wm[it % 2]
                it += 1
                src = dr["mod_w"][l].rearrange("(kc p) n -> p kc n", p=128)[:, :, cb * 512:(cb + 1) * 512]
                P.dma("sp", w[:], src, w, dr["mod_w"])
                for o4 in range(4):
                    oc = cb * 4 + o4
                    for kc in range(KC):
                        P.mm(psm3[:, oc, :], w[:, kc, o4 * 128:(o4 + 1) * 128], sT[:, kc, :], kc == 0, kc == KC - 1,
                             [w, sT], [psm])
            P.tt(modv[:, l], psm3, mbT[:, l, :, None].to_broadcast([128, 48, 8]), ALU.add, [psm, mbT], [modv])
            for kind in range(2):
                sc = modv[:, l, (1 + 3 * kind) * 8:(2 + 3 * kind) * 8, :]
                P.ts(Amod[:, l, kind], sc, 1.0, None, ALU.add, None, [modv], [Amod])
                P.tt(Amod[:, l, kind], Amod[:, l, kind], gT[:, kind, l, :, None].to_broadcast([128, 8, 8]), ALU.mult,
                     [Amod, gT], [Amod])
        tap("modv", modv, modv[:], [128, 2, 48, 8])

    for s in range(ns):
        sample_program(k, s)

    outs = [k.outT] + list(k.tap_outs.values())
    P.final_wait("sp", outs)
    P.final_wait("pool", outs)
    print("[build] arena hiwater %d / %d bytes, dma sems %d, instr %s" % (P.hiwater, ARENA_BYTES, P.ndsem,
                                                                        {e: len(P.ops[e]) for e in ENGS}))
    nc = P.emit()
    return nc, k


def mvec(k, l, j, kc, si):
    return k.modv[:, l, j * 8 + kc, si:si + 1]


def x_src(dr, s, b):
    t0, nb = BLOCKS[b]
    if b == 0:
        return dr["cxT"][s].rearrange("(kc p) t -> p kc t", p=128), dr["cxT"]
    return dr["xT"][s].rearrange("(kc p) t -> p kc t", p=128)[:, :, t0 - LC:t0 - LC + nb], dr["xT"]


class NormScratch:
    def __init__(self, k, bank):
        P = k.P
        self.sq = P.sbuf("sq", [128, KC, 512], F32)
        self.rs = P.sbuf("rs", [128, 512], F32)
        self.ps = bank


def rms_block(k, sc, x_ap, xbuf, nb):
    P = k.P
    P.act(sc.sq[:, :, :nb], x_ap, AF.Square, [xbuf], [sc.sq])
    for kc in range(KC):
        P.mm(sc.ps[:, :nb], k.ones_f[:], sc.sq[:, kc, :nb], kc == 0, kc == KC - 1, [k.ones_f, sc.sq], [sc.ps])
    P.act(sc.rs[:, :nb], sc.ps[:, :nb], AF.Ln, [sc.ps, k.epsb], [sc.rs], scale=1.0 / D, bias=k.epsb[:, 0:1])
    P.act(sc.rs[:, :nb], sc.rs[:, :nb], AF.Exp, [sc.rs], [sc.rs], scale=-0.5)
    P.tt(sc.sq[:, :, :nb], x_ap, sc.rs[:, None, :nb].to_broadcast([128, KC, nb]), ALU.mult, [xbuf, sc.rs], [sc.sq])
    return sc.sq


class WRing:
    def __init__(self, k, n):
        self.k = k
        self.slots = [k.P.sbuf("wslot%d" % i, [128, KC, 512], BF16) for i in range(n)]
        self.i = 0

    def load(self, w2d, wbuf, ncols=512):
        sl = self.slots[self.i % len(self.slots)]
        self.i += 1
        src = w2d.rearrange("(kc p) n -> p kc n", p=128)
        self.k.P.dma("pool", sl[:, :, :ncols], src, sl, wbuf)
        return sl


def sample_program(k, s):
    P = k.P
    dr = k.dr
    tap = k.tap if s == 0 else (lambda *a, **kw: None)
    SI = lambda b: 4 if b == 0 else s
    with P.phase():
        hT = P.sbuf("hT", [128, KC, T], BF16)
        xT = P.sbuf("xT", [128, KC, T], F32)
        if k.skip_l0:
            for b, (t0, nb) in enumerate(BLOCKS):
                src, sb_ = x_src(dr, s, b)
                P.dma("sp", xT[:, :, t0:t0 + nb], src, xT, sb_)
            layer1_and_out(k, s, hT, xT, tap)
            return
        with P.phase(), P.overlay(xT):
            sc = NormScratch(k, P.banks[0])
            xb = [P.sbuf("xb%d" % i, [128, KC, 512], F32) for i in range(2)]
            for b, (t0, nb) in enumerate(BLOCKS):
                xt = xb[b % 2]
                src, sb_ = x_src(dr, s, b)
                P.dma("sp", xt[:, :, :nb], src, xt, sb_)
                xn = rms_block(k, sc, xt[:, :, :nb], xt, nb)
                for kc in range(KC):
                    P.ts(hT[:, kc, t0:t0 + nb], xn[:, kc, :nb], k.Amod[:, 0, 0, kc, SI(b):SI(b) + 1],
                         mvec(k, 0, 0, kc, SI(b)), ALU.mult, ALU.add, [xn, k.Amod, k.modv], [hT])
            tap("L0_h", hT, hT[:], [128, KC, T])
        if k.stop_after == "L0A":
            return
        with P.phase():
            attnT = P.sbuf("attnT", [128, 4, T], BF16)
            lruT = P.sbuf("lruT", [128, 4, T], BF16)
            wr = WRing(k, 3)
            with P.phase(), P.overlay(xT):
                l0_attention(k, s, hT, attnT, wr, tap)
            if k.stop_after == "L0B":
                return
            with P.phase(), P.overlay(xT):
                l0_lru(k, s, hT, lruT, wr, tap)
            if k.stop_after == "L0C":
                return
            with P.phase():
                xb = [P.sbuf("xb%d" % i, [128, KC, 512], F32) for i in range(2)]
                wo = [wr.load(dr["w_out"][:, h * 512:(h + 1) * 512], dr["w_out"]) for h in range(2)]
                pi = 0
                for b, (t0, nb) in enumerate(BLOCKS):
                    xt = xb[b % 2]
                    src, sb_ = x_src(dr, s, b)
                    P.dma("sp", xt[:, :, :nb], src, xt, sb_)
                    for oc in range(KC):
                        ps = P.banks[pi % 4]
                        pi += 1
                        w = wo[oc // 4]
                        c0 = (oc % 4) * 128
                        for j in range(4):
                            P.mm(ps[:, :nb], w[:, j, c0:c0 + 128], attnT[:, j, t0:t0 + nb], j == 0, False, [w, attnT], [ps])
                        for c in range(4):
                            P.mm(ps[:, :nb], w[:, 4 + c, c0:c0 + 128], lruT[:, c, t0:t0 + nb], False, c == 3, [w, lruT], [ps])
                        P.stt(xT[:, oc, t0:t0 + nb], ps[:, :nb], mvec(k, 0, 2, oc, SI(b)), xt[:, oc, :nb], ALU.mult, ALU.add,
                              [ps, k.modv, xt], [xT])
                tap("L0_xmix", xT, xT[:], [128, KC, T])
        if k.stop_after == "L0D":
            return
        (moe_layer_sparse if k.sparse else moe_layer)(k, s, 0, hT, xT, True, tap)
        tap("L0_x", xT, xT[:], [128, KC, T])
        if k.stop_after == "L0F":
            return
        layer1_and_out(k, s, hT, xT, tap)


def layer1_and_out(k, s, hT, xT, tap):
    P = k.P
    layer1_mixer(k, s, hT, xT, tap)
    if k.stop_after in ("L1A", "L1B", "L1D", "L1L", "L1P"):
        return
    (moe_layer_sparse if k.sparse else moe_layer)(k, s, 1, hT, xT, False, tap)
    tap("L1_x", xT, xT[:], [128, KC, T])
    with P.phase():
        sc = NormScratch(k, P.banks[0])
        ob = [P.sbuf("ob%d" % i, [128, KC, 512], F32) for i in range(2)]
        for i, (t0, nb) in enumerate(LAT_BLOCKS):
            xn = rms_block(k, sc, xT[:, :, t0:t0 + nb], xT, nb)
            o = ob[i % 2]
            P.tt(o[:], xn[:, :, :nb], k.finT[:, :, None].to_broadcast([128, KC, nb]), ALU.mult, [xn, k.finT], [o])
            P.dma("sp", k.outT[s].rearrange("(kc p) t -> p kc t", p=128)[:, :, t0 - LC:t0 - LC + nb], o[:], k.outT, o)


def l0_attention(k, s, hT, qT, wr, tap):
    P = k.P
    dr = k.dr
    kT = P.sbuf("kT", [128, T], BF16)
    Vtok = P.sbuf("Vtok", [128, 18, 128], BF16)
    ropeC = P.sbuf("ropeC", [128, 512], F32)
    ropeS = P.sbuf("ropeS", [128, 512], F32)
    t1 = P.sbuf("rt1", [128, 512], F32)
    t2 = P.sbuf("rt2", [128, 512], F32)
    maskP = P.sbuf("maskP", [128, 4, 128], BF16)
    maskN = P.sbuf("maskN", [128, 4, 128], BF16)
    sinkb = P.sbuf("sinkb", [128, 8], F32)
    esink = P.sbuf("esink", [128, 8], F32)
    P.dma("pool", maskP[:], dr["maskP"][:].rearrange("p (a b) -> p a b", a=4), maskP, dr["maskP"])
    P.dma("pool", maskN[:], dr["maskN"][:].rearrange("p (a b) -> p a b", a=4), maskN, dr["maskN"])
    P.dma("sp", sinkb[:], dr["sinkb"][:], sinkb, dr["sinkb"])
    P.act(esink[:], sinkb[:], AF.Exp, [sinkb], [esink])
    wq = wr.load(dr["w_in"][:, 0:512], dr["w_in"])
    wqp = wr.load(dr["w_in"][:, 512:1024], dr["w_in"])
    wkv = wr.load(dr["w_in"][:, 1024:1408], dr["w_in"], 384)
    pi = 0
    for b, (t0, nb) in enumerate(BLOCKS):
        if b > 0:
            P.dma("sp", ropeC[:, :nb], dr["ropeC"][:, t0 - LC:t0 - LC + nb], ropeC, dr["ropeC"])
            P.dma("sp", ropeS[:, :nb], dr["ropeS"][:, t0 - LC:t0 - LC + nb], ropeS, dr["ropeS"])
        for j in range(5):
            w, wp, c0 = (wq, wqp, j * 128) if j < 4 else (wkv, wkv, 0)
            dst = qT[:, j, t0:t0 + nb] if j < 4 else kT[:, t0:t0 + nb]
            dbuf = qT if j < 4 else kT
            ps = P.banks[pi % 4]
            pi += 1
            for kc in range(KC):
                P.mm(ps[:, :nb], w[:, kc, c0:c0 + 128], hT[:, kc, t0:t0 + nb], kc == 0, kc == KC - 1, [w, hT], [ps])
            if b == 0:
                P.copy(dst, ps[:, :nb], [ps], [dbuf], eng="act")
            else:
                c1 = c0 if j < 4 else 128
                ps2 = P.banks[pi % 4]
                pi += 1
                for kc in range(KC):
                    P.mm(ps2[:, :nb], wp[:, kc, c1:c1 + 128], hT[:, kc, t0:t0 + nb], kc == 0, kc == KC - 1, [wp, hT], [ps2])
                P.tt(t1[:, :nb], ps[:, :nb], ropeC[:, :nb], ALU.mult, [ps, ropeC], [t1])
                P.tt(t2[:, :nb], ps2[:, :nb], ropeS[:, :nb], ALU.mult, [ps2, ropeS], [t2])
                P.tt(dst, t1[:, :nb], t2[:, :nb], ALU.add, [t1, t2], [dbuf])
        for tt_ in range(t0 // 128, (t0 + nb) // 128):
            ps = P.banks[pi % 4]
            pi += 1
            for kc in range(KC):
                P.mm(ps[:, 0:128], hT[:, kc, tt_ * 128:(tt_ + 1) * 128], wkv[:, kc, 256:384], kc == 0, kc == KC - 1,
                     [hT, wkv], [ps])
            P.copy(Vtok[:, tt_, :], ps[:, 0:128], [ps], [Vtok], eng="act")
    tap("L0_q", qT, qT[:], [128, 4, T])
    tap("L0_k", kT, kT[:], [128, T])
    tap("L0_v", Vtok, Vtok[:], [128, 18, 128])
    pT = [P.sbuf("pT%d" % i, [128, 4, 128], BF16) for i in range(2)]
    den = P.sbuf("den", [128, 4, 128], F32)
    ps_s = [P.banks[4], P.banks[5]]
    ps_num = P.banks[6]
    ps_den = P.banks[7]
    it = 0
    for qt in range(18):
        if qt < 2:
            keys = [(0, None), (1, None)]
        else:
            i = qt - 2
            keys = [(0, None), (1, None)]
            if i > 0:
                keys.append((qt - 1, maskP))
            keys.append((qt, None))
            if i < 15:
                keys.append((qt + 1, maskN))
        for g in range(2):
            pr = slice(g * 64, (g + 1) * 64)
            num3 = ps_num[pr, :].rearrange("p (a b) -> p a b", a=4)
            den3 = ps_den[pr, :].rearrange("p (a b) -> p a b", a=4)
            for idx, (kt, mk) in enumerate(keys):
                pss = ps_s[it % 2]
                pt = pT[it % 2]
                it += 1
                P.mm(pss[:].rearrange("p (a b) -> p a b", a=4), kT[pr, kt * 128:(kt + 1) * 128],
                     qT[pr, :, qt * 128:(qt + 1) * 128], True, True, [kT, qT], [pss])
                P.act(pt[:], pss[:].rearrange("p (a b) -> p a b", a=4), AF.Exp, [pss], [pt], scale=0.125)
                if mk is not None:
                    P.tt(pt[:], pt[:], mk[:], ALU.mult, [pt, mk], [pt])
                first, last = idx == 0, idx == len(keys) - 1
                P.mm(num3, Vtok[:, kt, pr], pt[:], first, last, [Vtok, pt], [ps_num])
                P.mm(den3, k.ones_b[:, 0:64], pt[:], first, last, [k.ones_b, pt], [ps_den])
            P.tt(den[pr], den3, esink[pr, g * 4:(g + 1) * 4, None].to_broadcast([64, 4, 128]), ALU.add,
                 [ps_den, esink], [den])
            P.op("dve", lambda e, pr=pr: e.reciprocal(out=den[pr], in_=den[pr]), [den], [den])
            P.tt(qT[pr, :, qt * 128:(qt + 1) * 128], num3, den[pr], ALU.mult, [ps_num, den], [qT])
    tap("L0_attn", qT, qT[:], [128, 4, T])


def l0_lru(k, s, hT, lruT, wr, tap):
    P = k.P
    dr = k.dr
    ul = P.sbuf("ul", [128, T], F32)
    u = P.sbuf("u", [128, T], F32)
    ub = P.sbuf("ub", [128, T], BF16)
    tmp = [P.sbuf("lt%d" % i, [128, 512], F32) for i in range(7)]
    t_r, t_i, t_a, t_y, t_b, t_h0, t_h1 = tmp
    convw = P.sbuf("convw", [128, 4, 4], F32)
    convb = P.sbuf("convb", [128, 4], F32)
    wa = P.sbuf("wa", [128, 2, 4, 128], BF16)
    wi = P.sbuf("wi", [128, 2, 4, 128], BF16)
    ba = P.sbuf("ba", [128, 2, 4], F32)
    bi = P.sbuf("bi", [128, 2, 4], F32)
    lam = P.sbuf("lam", [128, 2, 4], F32)
    kap = P.sbuf("kap", [128, 2, 4], F32)
    kap2 = P.sbuf("kap2", [128, 2, 4], F32)
    zz = P.sbuf("zz", [128, 2, 4], F32)
    zp = P.sbuf("zp", [128, 2, 4], F32)
    for nm, t_ in (("convw", convw), ("convb", convb), ("lru_ba", ba), ("lru_bi", bi), ("lru_lam", lam)):
        P.dma("sp", t_[:], dr[nm][:], t_, dr[nm])
    P.dma("pool", wa[:], dr["lru_wa"][:], wa, dr["lru_wa"])
    P.dma("pool", wi[:], dr["lru_wi"][:], wi, dr["lru_wi"])
    P.act(zz[:], lam[:], AF.Exp, [lam], [zz], scale=-1.0)
    P.ts(zp[:], zz[:], -0.2, 0.25, ALU.mult, ALU.add, [zz], [zp])
    for cst in (1.0 / 3.0, 0.5, 1.0):
        P.tt(zp[:], zp[:], zz[:], ALU.mult, [zp, zz], [zp])
        P.ts(zp[:], zp[:], -1.0, cst, ALU.mult, ALU.add, [zp], [zp])
    P.tt(zp[:], zp[:], zz[:], ALU.mult, [zp, zz], [zp])
    P.ts(kap[:], zp[:], -8.0, None, ALU.mult, None, [zp], [kap])
    P.ts(kap2[:], zp[:], -16.0, None, ALU.mult, None, [zp], [kap2])
    wul = wr.load(dr["w_in"][:, 1408:1920], dr["w_in"])
    wgl = wr.load(dr["w_in"][:, 1920:2432], dr["w_in"])
    pi = 0
    for c in range(4):
        for b, (t0, nb) in enumerate(BLOCKS):
            ps = P.banks[pi % 4]
            pi += 1
            for kc in range(KC):
                P.mm(ps[:, :nb], wul[:, kc, c * 128:(c + 1) * 128], hT[:, kc, t0:t0 + nb], kc == 0, kc == KC - 1, [wul, hT], [ps])
            P.copy(ul[:, t0:t0 + nb], ps[:, :nb], [ps], [ul], eng="act")
        P.act(u[:], ul[:], AF.Identity, [ul, convw, convb], [u], scale=convw[:, c, 2:3], bias=convb[:, c:c + 1])
        for (a_, e_) in ((0, LC), (LC, T)):
            P.stt(u[:, a_ + 2:e_], ul[:, a_:e_ - 2], convw[:, c, 0:1], u[:, a_ + 2:e_], ALU.mult, ALU.add, [ul, convw, u], [u])
            P.stt(u[:, a_ + 1:e_], ul[:, a_:e_ - 1], convw[:, c, 1:2], u[:, a_ + 1:e_], ALU.mult, ALU.add, [ul, convw, u], [u])
            P.stt(u[:, a_:e_ - 1], ul[:, a_ + 1:e_], convw[:, c, 3:4], u[:, a_:e_ - 1], ALU.mult, ALU.add, [ul, convw, u], [u])
        P.copy(ub[:], u[:], [u], [ub], eng="act")
        if c == 0:
            tap("L0_u0", u, u[:], [128, T])
        hsum = ul
        for d in range(2):
            order = [0, 1, 2, 3, 4] if d == 0 else [0, 4, 3, 2, 1]
            prev_h = None
            for oi, b in enumerate(order):
                t0, nb = BLOCKS[b]
                psr = P.banks[pi % 4]
                pi += 1
                psi = P.banks[pi % 4]
                pi += 1
                P.mm(psr[:, :nb], wa[:, d, c, :], ub[:, t0:t0 + nb], True, True, [wa, ub], [psr])
                P.mm(psi[:, :nb], wi[:, d, c, :], ub[:, t0:t0 + nb], True, True, [wi, ub], [psi])
                P.act(t_r[:, :nb], psr[:, :nb], AF.Sigmoid, [psr, ba], [t_r], bias=ba[:, d, c:c + 1])
                P.act(t_i[:, :nb], psi[:, :nb], AF.Sigmoid, [psi, bi], [t_i], bias=bi[:, d, c:c + 1])
                P.act(t_a[:, :nb], t_r[:, :nb], AF.Exp, [t_r, kap], [t_a], scale=kap[:, d, c:c + 1])
                P.act(t_y[:, :nb], t_r[:, :nb], AF.Exp, [t_r, kap2], [t_y], scale=kap2[:, d, c:c + 1])
                P.ts(t_y[:, :nb], t_y[:, :nb], -1.0, 1.0, ALU.mult, ALU.add, [t_y], [t_y])
                P.ts(t_y[:, :nb], t_y[:, :nb], 1e-30, None, ALU.max, None, [t_y], [t_y])
                P.act(t_y[:, :nb], t_y[:, :nb], AF.Ln, [t_y], [t_y])
                P.act(t_y[:, :nb], t_y[:, :nb], AF.Exp, [t_y], [t_y], scale=0.5)
                P.tt(t_b[:, :nb], t_y[:, :nb], t_i[:, :nb], ALU.mult, [t_y, t_i], [t_b])
                P.tt(t_b[:, :nb], t_b[:, :nb], u[:, t0:t0 + nb], ALU.mult, [t_b, u], [t_b])
                if d == 0:
                    init = 0.0 if oi == 0 else hsum[:, t0 - 1:t0]
                    P.op("dve", lambda e, t0=t0, nb=nb, init=init: e.tensor_tensor_scan(
                        out=hsum[:, t0:t0 + nb], data0=t_a[:, :nb], data1=t_b[:, :nb], initial=init,
                        op0=ALU.mult, op1=ALU.add), [t_a, t_b, hsum], [hsum])
                else:
                    th = t_h0 if oi % 2 == 0 else t_h1
                    if oi == 0:
                        init, rd = 0.0, []
                    else:
                        init, rd = prev_h[1], [prev_h[0]]
                    P.op("dve", lambda e, nb=nb, init=init, th=th: e.tensor_tensor_scan(
                        out=th[:, :nb][:, ::-1], data0=t_a[:, :nb][:, ::-1], data1=t_b[:, :nb][:, ::-1], initial=init,
                        op0=ALU.mult, op1=ALU.add), [t_a, t_b] + rd, [th])
                    prev_h = (th, th[:, 0:1])
                    P.tt(hsum[:, t0:t0 + nb], hsum[:, t0:t0 + nb], th[:, :nb], ALU.add, [hsum, th], [hsum])
        if c == 0:
            tap("L0_hsum0", hsum, hsum[:], [128, T])
        for b, (t0, nb) in enumerate(BLOCKS):
            ps = P.banks[pi % 4]
            pi += 1
            for kc in range(KC):
                P.mm(ps[:, :nb], wgl[:, kc, c * 128:(c + 1) * 128], hT[:, kc, t0:t0 + nb], kc == 0, kc == KC - 1, [wgl, hT], [ps])
            P.copy(t_r[:, :nb], ps[:, :nb], [ps], [t_r], eng="act")
            P.act(t_i[:, :nb], ps[:, :nb], AF.Square, [ps], [t_i])
            P.ts(t_i[:, :nb], t_i[:, :nb], 0.044715, 1.0, ALU.mult, ALU.add, [t_i], [t_i])
            P.tt(t_i[:, :nb], t_i[:, :nb], t_r[:, :nb], ALU.mult, [t_i, t_r], [t_i])
            P.act(t_i[:, :nb], t_i[:, :nb], AF.Sigmoid, [t_i], [t_i], scale=GELU_C)
            P.tt(t_i[:, :nb], t_i[:, :nb], t_r[:, :nb], ALU.mult, [t_i, t_r], [t_i])
            P.tt(lruT[:, c, t0:t0 + nb], t_i[:, :nb], hsum[:, t0:t0 + nb], ALU.mult, [t_i, hsum], [lruT])
    tap("L0_lru", lruT, lruT[:], [128, 4, T])


def moe_layer(k, s, l, hT, xT, with_ctx, tap):
    P = k.P
    dr = k.dr
    SI = lambda b: 4 if b == 0 else s
    blocks = list(enumerate(BLOCKS)) if with_ctx else list(enumerate(BLOCKS))[1:]
    with P.phase():
        gateT = P.sbuf("gateT", [16, T], F32)
        with P.phase():
            sc = NormScratch(k, P.banks[0])
            hf = P.sbuf("hf", [128, KC, 512], F32)
            wrt = P.sbuf("wrt", [128, KC, 16], F32)
            ones16 = P.sbuf("ones16", [16, 16], F32)
            ex = P.sbuf("ex", [16, 512], F32)
            P.memset(ones16[:], 1.0, [ones16])
            P.dma("sp", wrt[:], dr["router_w"][l].rearrange("(kc p) e -> p kc e", p=128), wrt, dr["router_w"])
            psl = P.banks[1]
            pss = P.banks[2]
            for b, (t0, nb) in blocks:
                xn = rms_block(k, sc, xT[:, :, t0:t0 + nb], xT, nb)
                for kc in range(KC):
                    P.ts(hf[:, kc, :nb], xn[:, kc, :nb], k.Amod[:, l, 1, kc, SI(b):SI(b) + 1], mvec(k, l, 3, kc, SI(b)),
                         ALU.mult, ALU.add, [xn, k.Amod, k.modv], [hf])
                P.copy(hT[:, :, t0:t0 + nb], hf[:, :, :nb], [hf], [hT], eng="act")
                for kc in range(KC):
                    P.mm(psl[0:16, :nb], wrt[:, kc, :], hf[:, kc, :nb], kc == 0, kc == KC - 1, [wrt, hf], [psl])
                P.act(ex[:, :nb], psl[0:16, :nb], AF.Exp, [psl], [ex])
                P.mm(pss[0:16, :nb], ones16[:], ex[:, :nb], True, True, [ones16, ex], [pss])
                P.op("dve", lambda e, nb=nb: e.reciprocal(out=sc.rs[0:16, :nb], in_=pss[0:16, :nb]), [pss], [sc.rs])
                P.tt(gateT[:, t0:t0 + nb], ex[:, :nb], sc.rs[0:16, :nb], ALU.mult, [ex, sc.rs], [gateT])
            tap("L%d_aff" % l, gateT, gateT[:], [16, T])
            work = P.sbuf("work", [16, S], F32)
            m8 = P.sbuf("m8", [16, 8], F32)
            segs = [(LC, S, 8)] + ([(0, LC, 8)] if with_ctx else [])
            for (a0, n, _) in segs:
                cap = 2 * n // 16
                P.copy(work[:, :n], gateT[:, a0:a0 + n], [gateT], [work])
                for r in range(cap // 8):
                    P.op("dve", lambda e, n=n: e.max(out=m8[:], in_=work[:, :n]), [work], [m8])
                    if r < cap // 8 - 1:
                        P.op("dve", lambda e, n=n: e.match_replace(out=work[:, :n], in_to_replace=m8[:], in_values=work[:, :n],
                                                                   imm_value=-1.0), [work, m8], [work])
                P.stt(gateT[:, a0:a0 + n], gateT[:, a0:a0 + n], m8[:, 7:8], gateT[:, a0:a0 + n], ALU.is_ge, ALU.mult,
                      [gateT, m8], [gateT])
            tap("L%d_gate" % l, gateT, gateT[:], [16, T])
            tap("L%d_hffn" % l, hT, hT[:], [128, KC, T])
        with P.phase():
            wr = WRing(k, 4)
            id16 = P.sbuf("id16", [16, 16], F32)
            P.dma("sp", id16[:], dr["id16"][:], id16, dr["id16"])
            hid = P.sbuf("hid", [128, KC, T], BF16)
            gb = [P.sbuf("gb%d" % i, [128, 512], F32) for i in range(2)]
            s1 = [P.sbuf("s1_%d" % i, [128, 512], F32) for i in range(2)]
            pi = 0
            it = 0
            for e in range(16):
                for h in range(2):
                    w1 = wr.load(dr["exp_w1"][l, e][:, h * 512:(h + 1) * 512], dr["exp_w1"])
                    w3 = wr.load(dr["exp_w3"][l, e][:, h * 512:(h + 1) * 512], dr["exp_w3"])
                    for b, (t0, nb) in blocks:
                        g_ = gb[it % 2]
                        it += 1
                        psg = P.banks[6 + it % 2]
                        P.mm(psg[:, :nb], id16[:, e:e + 1].to_broadcast([16, 128]), gateT[:, t0:t0 + nb], True, True,
                             [id16, gateT], [psg])
                        P.copy(g_[:, :nb], psg[:, :nb], [psg], [g_], eng="act")
                        for f4 in range(4):
                            f = h * 4 + f4
                            ps1 = P.banks[pi % 6]
                            ps3 = P.banks[(pi + 1) % 6]
                            pi += 2
                            st = s1[(pi // 2) % 2]
                            for kc in range(KC):
                                P.mm(ps1[:, :nb], w1[:, kc, f4 * 128:(f4 + 1) * 128], hT[:, kc, t0:t0 + nb], kc == 0, kc == KC - 1,
                                     [w1, hT], [ps1])
                            for kc in range(KC):
                                P.mm(ps3[:, :nb], w3[:, kc, f4 * 128:(f4 + 1) * 128], hT[:, kc, t0:t0 + nb], kc == 0, kc == KC - 1,
                                     [w3, hT], [ps3])
                            P.act(st[:, :nb], ps1[:, :nb], AF.Silu, [ps1], [st])
                            P.tt(st[:, :nb], st[:, :nb], ps3[:, :nb], ALU.mult, [st, ps3], [st])
                            P.tt(hid[:, f, t0:t0 + nb], st[:, :nb], g_[:, :nb], ALU.mult, [st, g_], [hid])
                for h2 in range(2):
                    w2 = wr.load(dr["exp_w2"][l, e][:, h2 * 512:(h2 + 1) * 512], dr["exp_w2"])
                    for b, (t0, nb) in blocks:
                        for o4 in range(4):
                            oc = h2 * 4 + o4
                            psy = P.banks[pi % 6]
                            pi += 1
                            for f in range(KC):
                                P.mm(psy[:, :nb], w2[:, f, o4 * 128:(o4 + 1) * 128], hid[:, f, t0:t0 + nb], f == 0, f == KC - 1,
                                     [w2, hid], [psy])
                            P.stt(xT[:, oc, t0:t0 + nb], psy[:, :nb], mvec(k, l, 5, oc, SI(b)), xT[:, oc, t0:t0 + nb],
                                  ALU.mult, ALU.add, [psy, k.modv, xT], [xT])


def moe_layer_sparse(k, s, l, hT, xT, with_ctx, tap):
    P = k.P
    dr = k.dr
    SI = lambda b: 4 if b == 0 else s
    blocks = list(enumerate(BLOCKS)) if with_ctx else list(enumerate(BLOCKS))[1:]
    nslot = 288 if with_ctx else 256
    jcs = [(0, 128), (128, 128)] + ([(256, 32)] if with_ctx else [])
    tiles = list(range(18)) if with_ctx else list(range(2, 18))
    with P.phase():
        gp = P.sbuf("gp", [48, T], F32)
        pos_tok = P.sbuf("pos_tok", [128, 18, 16], F32)
        with P.overlay(hT):
            h_tok = P.sbuf("h_tok", [128, 18, D], BF16)
        gateT = gp
        with P.phase():
            sc = NormScratch(k, P.banks[0])
            hf = P.sbuf("hf", [128, KC, 512], F32)
            wrt = P.sbuf("wrt", [128, KC, 48], F32)
            ones16 = P.sbuf("ones48", [48, 48], F32)
            ex = P.sbuf("ex", [48, 512], F32)
            identf = P.sbuf("identf", [128, 128], F32)
            P.dma("sp", identf[:], dr["ident"][:], identf, dr["ident"])
            P.dma("sp", ones16[:], dr["bd48"][:], ones16, dr["bd48"])
            P.dma("sp", wrt[:], dr["router_w48"][l].rearrange("(kc p) e -> p kc e", p=128), wrt, dr["router_w48"])
            psl = P.banks[1]
            pss = P.banks[2]
            ti = 0
            for b, (t0, nb) in blocks:
                xn = rms_block(k, sc, xT[:, :, t0:t0 + nb], xT, nb)
                for kc in range(KC):
                    P.ts(hf[:, kc, :nb], xn[:, kc, :nb], k.Amod[:, l, 1, kc, SI(b):SI(b) + 1], mvec(k, l, 3, kc, SI(b)),
                         ALU.mult, ALU.add, [xn, k.Amod, k.modv], [hf])
                for kc in range(KC):
                    P.mm(psl[0:48, :nb], wrt[:, kc, :], hf[:, kc, :nb], kc == 0, kc == KC - 1, [wrt, hf], [psl])
                P.act(ex[:, :nb], psl[0:48, :nb], AF.Exp, [psl], [ex])
                P.mm(pss[0:48, :nb], ones16[:], ex[:, :nb], True, True, [ones16, ex], [pss])
                P.op("dve", lambda e, nb=nb: e.reciprocal(out=sc.rs[0:48, :nb], in_=pss[0:48, :nb]), [pss], [sc.rs])
                P.tt(gateT[0:48, t0:t0 + nb], ex[:, :nb], sc.rs[0:48, :nb], ALU.mult, [ex, sc.rs], [gateT])
                for tl in range(nb // 128):
                    tile = t0 // 128 + tl
                    for half in range(2):
                        bk = P.banks[3 + ti % 4]
                        ti += 1
                        for q in range(4):
                            kc = half * 4 + q
                            P.op("pe", lambda e, bk=bk, q=q, kc=kc, tl=tl: e.transpose(
                                out=bk[:, q * 128:(q + 1) * 128], in_=hf[:, kc, tl * 128:(tl + 1) * 128], identity=identf[:]),
                                [hf, identf], [bk])
                        P.copy(h_tok[:, tile, half * 512:(half + 1) * 512], bk[:, :], [bk], [h_tok],
                               eng="act" if ti % 2 == 0 else "dve")
            work = P.sbuf("work", [48, S], F32)
            m8 = P.sbuf("m8", [48, 8], F32)
            segs = [(LC, S)] + ([(0, LC)] if with_ctx else [])
            for (a0, n) in segs:
                cap = 2 * n // 16
                P.copy(work[:, :n], gateT[0:48, a0:a0 + n], [gateT], [work])
                for r in range(cap // 8):
                    P.op("dve", lambda e, n=n: e.max(out=m8[:], in_=work[:, :n]), [work], [m8])
                    if r < cap // 8 - 1:
                        P.op("dve", lambda e, n=n: e.match_replace(out=work[:, :n], in_to_replace=m8[:], in_values=work[:, :n],
                                                                   imm_value=-1.0), [work, m8], [work])
                P.stt(gateT[0:48, a0:a0 + n], gateT[0:48, a0:a0 + n], m8[:, 7:8], gateT[0:48, a0:a0 + n], ALU.is_ge, ALU.mult,
                      [gateT, m8], [gateT])
                P.ts(work[32:48, :n], gateT[32:48, a0:a0 + n], 0.0, None, ALU.is_gt, None, [gateT], [work])
                P.op("dve", lambda e, a0=a0, n=n: e.tensor_tensor_scan(out=gp[32:48, a0:a0 + n], data0=work[32:48, :n], data1=work[32:48, :n],
                                                                       initial=0.0, op0=ALU.add, op1=ALU.max), [work], [gp])
                if a0 == 0:
                    P.ts(gp[32:48, a0:a0 + n], gp[32:48, a0:a0 + n], 256.0, None, ALU.add, None, [gp], [gp])
                P.tt(gp[32:48, a0:a0 + n], gp[32:48, a0:a0 + n], work[32:48, :n], ALU.mult, [gp, work], [gp])
                P.ts(gp[32:48, a0:a0 + n], gp[32:48, a0:a0 + n], -1.0, None, ALU.add, None, [gp], [gp])
            tap("L%d_gate" % l, gp, gp[0:16, :], [16, T])
            bk = P.banks[1]
            bk3 = bk[:, 0:288].rearrange("p (a b) -> p a b", a=18)
            for tile in tiles:
                P.op("pe", lambda e, tile=tile: e.transpose(out=bk3[:, tile, :], in_=gp[32:48, tile * 128:(tile + 1) * 128],
                                                            identity=identf[32:48, 32:48]), [gp, identf], [bk])
            P.copy(pos_tok[:, tiles[0]:18, :], bk3[:, tiles[0]:18, :], [bk], [pos_tok])
        with P.phase():
            wr = WRing(k, 3)
            id48 = P.sbuf("id48", [48, 48], F32)
            P.dma("sp", id48[:], dr["ident"][0:48, 0:48], id48, dr["ident"])
            iota_j = P.sbuf("iota_j", [128, 288], F32)
            iota_p = P.sbuf("iota_p", [128, 3], F32)
            P.dma("sp", iota_j[:], dr["iota_j"][:], iota_j, dr["iota_j"])
            P.dma("sp", iota_p[:], dr["iota_p"][:], iota_p, dr["iota_p"])
            Sel = P.sbuf("Sel", [128, 18, nslot], BF16)
            SelT = P.sbuf("SelT", [128, len(jcs), T], BF16)
            xin = P.sbuf("xin", [128, KC, nslot], BF16)
            hid = P.sbuf("hid", [128, KC, nslot], BF16)
            y_tok = P.sbuf("y_tok", [128, len(jcs), D], BF16)
            gb = P.sbuf("gb", [128, 512], F32)
            s1 = [P.sbuf("s1_%d" % i, [128, nslot], F32) for i in range(2)]
            pi = 0
            it = 0
            for e in range(16):
                for tile in tiles:
                    P.ts(Sel[:, tile, :], iota_j[:, :nslot], pos_tok[:, tile, e:e + 1], None, ALU.is_equal, None,
                         [iota_j, pos_tok], [Sel])
                for b, (t0, nb) in blocks:
                    psp = P.banks[6]
                    psg = P.banks[7]
                    P.mm(psp[:, :nb], id48[:, 32 + e:33 + e].to_broadcast([48, 128]), gp[:, t0:t0 + nb], True, True, [id48, gp], [psp])
                    P.mm(psg[:, :nb], id48[:, e:e + 1].to_broadcast([48, 128]), gp[:, t0:t0 + nb], True, True, [id48, gp], [psg])
                    P.copy(gb[:, :nb], psg[:, :nb], [psg], [gb], eng="act")
                    for ji, (j0, nj) in enumerate(jcs):
                        P.stt(SelT[0:nj, ji, t0:t0 + nb], psp[0:nj, :nb], iota_p[0:nj, ji:ji + 1], gb[0:nj, :nb], ALU.is_equal, ALU.mult,
                              [psp, iota_p, gb], [SelT])
                for kc in range(KC):
                    ps = P.banks[pi % 6]
                    pi += 1
                    for i_, tile in enumerate(tiles):
                        P.mm(ps[:, :nslot], h_tok[:, tile, kc * 128:(kc + 1) * 128], Sel[:, tile, :], i_ == 0, i_ == len(tiles) - 1,
                             [h_tok, Sel], [ps])
                    P.copy(xin[:, kc, :], ps[:, :nslot], [ps], [xin], eng="act" if kc % 2 == 0 else "dve")
                for h in range(2):
                    w1 = wr.load(dr["exp_w1"][l, e][:, h * 512:(h + 1) * 512], dr["exp_w1"])
                    w3 = wr.load(dr["exp_w3"][l, e][:, h * 512:(h + 1) * 512], dr["exp_w3"])
                    for f4 in range(4):
                        f = h * 4 + f4
                        ps1 = P.banks[pi % 6]
                        ps3 = P.banks[(pi + 1) % 6]
                        pi += 2
                        st = s1[(pi // 2) % 2]
                        for kc in range(KC):
                            P.mm(ps1[:, :nslot], w1[:, kc, f4 * 128:(f4 + 1) * 128], xin[:, kc, :], kc == 0, kc == KC - 1, [w1, xin], [ps1])
                        for kc in range(KC):
                            P.mm(ps3[:, :nslot], w3[:, kc, f4 * 128:(f4 + 1) * 128], xin[:, kc, :], kc == 0, kc == KC - 1, [w3, xin], [ps3])
                        P.act(st[:], ps1[:, :nslot], AF.Silu, [ps1], [st])
                        P.tt(hid[:, f, :], st[:], ps3[:, :nslot], ALU.mult, [st, ps3], [hid])
                for h2 in range(2):
                    w2 = wr.load(dr["exp_w2"][l, e][:, h2 * 512:(h2 + 1) * 512], dr["exp_w2"])
                    for ji, (j0, nj) in enumerate(jcs):
                        psy = P.banks[pi % 6]
                        pi += 1
                        for f in range(KC):
                            P.mm(psy[0:nj, :], hid[:, f, j0:j0 + nj], w2[:, f, :], f == 0, f == KC - 1, [hid, w2], [psy])
                        P.copy(y_tok[0:nj, ji, h2 * 512:(h2 + 1) * 512], psy[0:nj, :], [psy], [y_tok], eng="act" if ji % 2 == 0 else "dve")
                for b, (t0, nb) in blocks:
                    for oc in range(KC):
                        pso = P.banks[pi % 6]
                        pi += 1
                        for ji, (j0, nj) in enumerate(jcs):
                            P.mm(pso[:, :nb], y_tok[0:nj, ji, oc * 128:(oc + 1) * 128], SelT[0:nj, ji, t0:t0 + nb], ji == 0, ji == len(jcs) - 1,
                                 [y_tok, SelT], [pso])
                        P.stt(xT[:, oc, t0:t0 + nb], pso[:, :nb], mvec(k, l, 5, oc, SI(b)), xT[:, oc, t0:t0 + nb],
                              ALU.mult, ALU.add, [pso, k.modv, xT], [xT])


RB = [(0, 256)] + [(256 + 256 * i, 256) for i in range(8)]
V_MU, V_W0, V_A0, V_KK, V_KA, V_RK, V_GG, V_GB = 0, 6, 8, 10, 11, 12, 13, 14
NEG_EXP_HALF = -0.6065306597126334


def layer1_mixer(k, s, hT, xT, tap):
    P = k.P
    dr = k.dr
    SI = lambda b: 4 if b == 0 else s
    with P.phase():
        sc = NormScratch(k, P.banks[0])
        for b, (t0, nb) in enumerate(BLOCKS):
            xn = rms_block(k, sc, xT[:, :, t0:t0 + nb], xT, nb)
            for kc in range(KC):
                P.ts(hT[:, kc, t0:t0 + nb], xn[:, kc, :nb], k.Amod[:, 1, 0, kc, SI(b):SI(b) + 1],
                     mvec(k, 1, 0, kc, SI(b)), ALU.mult, ALU.add, [xn, k.Amod, k.modv], [hT])
        P.dma("sp", k.xspill[:].rearrange("(kc p) t -> p kc t", p=128), xT[:, :, LC:T], k.xspill, xT)
        tap("L1_h", hT, hT[:], [128, KC, T])
    if k.stop_after == "L1A":
        return
    with P.phase():
        outT = P.sbuf("outT", [128, KC, S], BF16)
        with P.phase():
            rwkv_core(k, s, hT, xT, outT, tap)
        if k.stop_after in ("L1B", "L1L", "L1P"):
            return
        with P.phase():
            wr = WRing(k, 2)
            wo = [wr.load(dr["rw_wo"][:, h * 512:(h + 1) * 512], dr["rw_wo"]) for h in range(2)]
            P.dma("sp", xT[:, :, LC:T], k.xspill[:].rearrange("(kc p) t -> p kc t", p=128), xT, k.xspill)
            pi = 0
            for b, (t0, nb) in list(enumerate(BLOCKS))[1:]:
                for oc in range(KC):
                    ps = P.banks[pi % 4]
                    pi += 1
                    w = wo[oc // 4]
                    c0 = (oc % 4) * 128
                    for c in range(KC):
                        P.mm(ps[:, :nb], w[:, c, c0:c0 + 128], outT[:, c, t0 - LC:t0 - LC + nb], c == 0, c == KC - 1,
                             [w, outT], [ps])
                    P.stt(xT[:, oc, t0:t0 + nb], ps[:, :nb], mvec(k, 1, 2, oc, s), xT[:, oc, t0:t0 + nb], ALU.mult, ALU.add,
                          [ps, k.modv, xT], [xT])
            tap("L1_xmix", xT, xT[:], [128, KC, T])


def rwkv_core(k, s, hT, xT, outT, tap):
    P = k.P
    dr = k.dr
    with P.overlay(xT):
        lw1 = P.sbuf("lw1", [128, T], BF16)
        la1 = P.sbuf("la1", [128, T], BF16)
        lg1 = P.sbuf("lg1", [128, T], BF16)
        lg1b = P.sbuf("lg1b", [32, T], BF16)
        y_acc = P.sbuf("y_acc", [128, S], F32)
        bon = P.sbuf("bon", [128, S], F32)
    ov_mark = xT.ov_top
    vec = P.sbuf("rwvec", [128, 15, KC], F32)
    P.dma("sp", vec[:], dr["rw_vec"][:], vec, dr["rw_vec"])
    omk = P.sbuf("omk", [128, KC], F32)
    P.ts(omk[:], vec[:, V_KA, :], -1.0, 1.0, ALU.mult, ALU.add, [vec], [omk])
    omu = P.sbuf("omu", [128, 6, KC], F32)
    hmu = P.sbuf("hmu", [128, 6, KC], F32)
    P.ts(omu[:], vec[:, 0:6, :], -1.0, 1.0, ALU.mult, ALU.add, [vec], [omu])
    P.ts(hmu[:], vec[:, 0:6, :], 0.5, None, ALU.mult, None, [vec], [hmu])
    identb = P.sbuf("identb", [128, 128], BF16)
    P.dma("pool", identb[:], dr["ident"][:], identb, dr["ident"])
    identf = P.sbuf("identf", [128, 128], F32)
    P.dma("sp", identf[:], dr["ident"][:], identf, dr["ident"])
    bones = P.sbuf("bones", [128, 128], F32)
    P.dma("sp", bones[:], dr["blockones"][:], bones, dr["blockones"])
    maskC = P.sbuf("maskC", [128, 256], F32)
    P.dma("sp", maskC[:], dr["maskC"][:], maskC, dr["maskC"])
    m4 = [P.sbuf("m4_%d" % d, [128, 512], BF16) for d in range(2)]
    m1 = [P.sbuf("m1_%d" % d, [128, 128], BF16) for d in range(2)]
    for d in range(2):
        P.dma("pool", m4[d][:], dr["rmask4"][d], m4[d], dr["rmask4"])
        P.dma("pool", m1[d][:], dr["rmask1"][d], m1[d], dr["rmask1"])

    def vcol(i, c):
        return vec[:, i, c:c + 1]

    def load_scaled(name, src2d, ncols, mu_i):
        wa = P.sbuf(name + "a", [128, KC, ncols], BF16)
        wb = P.sbuf(name + "b", [128, KC, ncols], BF16)
        P.dma("pool", wa[:], src2d.rearrange("(kc p) n -> p kc n", p=128), wa, dr["rw_rkv"])
        for kc in range(KC):
            P.ts(wb[:, kc, :], wa[:, kc, :], hmu[:, mu_i, kc:kc + 1], None, ALU.mult, None, [wa, hmu], [wb])
        for kc in range(KC):
            P.ts(wa[:, kc, :], wa[:, kc, :], omu[:, mu_i, kc:kc + 1], None, ALU.mult, None, [wa, omu], [wa])
        return wa, wb

    def proj_mix(ps_ap, psbuf, wa, wb, c0, m, t0, nb):
        lo = 1 if t0 in (0, LC) else 0
        hi = nb - 1 if (t0 + nb) in (LC, T) else nb
        for kc in range(KC):
            P.mm(ps_ap, wa[:, kc, c0:c0 + m], hT[:, kc, t0:t0 + nb], kc == 0, False, [wa, hT], [psbuf])
        for kc in range(KC):
            P.mm(ps_ap[:, lo:nb], wb[:, kc, c0:c0 + m], hT[:, kc, t0 + lo - 1:t0 + nb - 1], False, False, [wb, hT], [psbuf])
        for kc in range(KC):
            P.mm(ps_ap[:, 0:hi], wb[:, kc, c0:c0 + m], hT[:, kc, t0 + 1:t0 + hi + 1], False, kc == KC - 1, [wb, hT], [psbuf])

    with P.phase():
        w1, w1s = load_scaled("w1c", dr["rw_w1cat"][:], 128, 1)
        a1, a1s = load_scaled("a1c", dr["rw_a1cat"][:], 128, 4)
        g1, g1s = load_scaled("g1c", dr["rw_g1"][:], 160, 5)
        pi = 0
        for (t0, nb) in BLOCKS:
            ps = P.banks[pi % 4]; pi += 1
            proj_mix(ps[:, :nb], ps, w1, w1s, 0, 128, t0, nb)
            P.act(lw1[:, t0:t0 + nb], ps[:, :nb], AF.Tanh, [ps], [lw1])
            ps = P.banks[pi % 4]; pi += 1
            proj_mix(ps[:, :nb], ps, a1, a1s, 0, 128, t0, nb)
            P.copy(la1[:, t0:t0 + nb], ps[:, :nb], [ps], [la1], eng="act")
            ps = P.banks[pi % 4]; pi += 1
            proj_mix(ps[:, :nb], ps, g1, g1s, 0, 128, t0, nb)
            P.act(lg1[:, t0:t0 + nb], ps[:, :nb], AF.Sigmoid, [ps], [lg1])
            ps = P.banks[pi % 4]; pi += 1
            proj_mix(ps[0:32, :nb], ps, g1, g1s, 128, 32, t0, nb)
            P.act(lg1b[:, t0:t0 + nb], ps[0:32, :nb], AF.Sigmoid, [ps], [lg1b])
    if k.stop_after == "L1L":
        tap("L1_lw1", lw1, lw1[:], [128, T])
        return
    for c in range(KC if k.stop_after != "L1P" else 1):
        with P.phase():
            rwkv_pair(k, s, c, hT, xT, ov_mark, lw1, la1, lg1, lg1b, y_acc, bon, outT, vec, omk, identb, identf, bones, maskC,
                      m4, m1, load_scaled, proj_mix, vcol, tap)
    tap("L1_og", outT, outT[:], [128, KC, S])


class DirBufs:
    def __init__(self, P, d):
        NB = 256
        self.f = [P.sbuf("rf%d_%d" % (d, i), [128, NB], F32) for i in range(10)]
        self.Vb = P.sbuf("Vb%d" % d, [128, NB], BF16)
        self.ARz = [P.sbuf("zar%d_%d" % (d, h), [128, 2, NB], BF16) for h in range(2)]
        self.BTz, self.KTz = [[P.sbuf("z%d_%d_%d" % (d, i, h), [128, NB], BF16) for h in range(2)] for i in range(2)]
        self.stk = [P.sbuf("stk%d_%d" % (d, i), [128, 192], BF16) for i in range(4)]
        self.Vz = [[P.sbuf("Vz%d_%d_%d" % (d, i, h), [128, 64], BF16) for h in range(2)] for i in range(4)]
        self.Uz = [P.sbuf("Uz%d_%d" % (d, h), [128, 64], BF16) for h in range(2)]
        self.mats = [P.sbuf("mats%d_%d" % (d, i), [128, 640], BF16) for i in range(4)]
        self.Lf = [P.sbuf("Lf%d_%d" % (d, i), [128, 256], F32) for i in range(4)]
        self.sq = [[P.sbuf("sq%d_%d_%d" % (d, i, j), [128, 256], F32) for j in range(2)] for i in range(4)]
        self.ZT = [P.sbuf("ZT%d_%d" % (d, i), [128, 128], F32) for i in range(4)]
        self.T0 = P.sbuf("T0_%d" % d, [128, 64], F32)
        self.T0s = P.sbuf("T0s_%d" % d, [128, 64], F32)
        self.T0b = P.sbuf("T0b_%d" % d, [128, 64], BF16)
        self.Xb = P.sbuf("Xb_%d" % d, [128, 64], F32)
        zs = [t_ for l_ in (self.ARz, self.BTz, self.KTz) for t_ in l_] + [v_ for l_ in self.Vz for v_ in l_] + self.Uz
        for t_ in zs + [self.T0, self.T0b]:
            P.memset(t_[:], 0.0, [t_])


def rwkv_pair(k, s, c, hT, xT, ov_mark, lw1, la1, lg1, lg1b, y_acc, bon, outT, vec, omk, identb, identf, bones, maskC, m4, m1,
              load_scaled, proj_mix, vcol, tap):
    P = k.P
    dr = k.dr
    cs_ = slice(c * 128, (c + 1) * 128)
    wr_, wrs = load_scaled("wr", dr["rw_rkv"][0][:, cs_], 128, 0)
    wk_, wks = load_scaled("wk", dr["rw_rkv"][1][:, cs_], 128, 2)
    wv_, wvs = load_scaled("wv", dr["rw_rkv"][2][:, cs_], 128, 3)
    w2c = P.sbuf("w2c", [128, 128], BF16)
    a2c = P.sbuf("a2c", [128, 128], BF16)
    g2a = P.sbuf("g2a", [128, 128], BF16)
    g2b = P.sbuf("g2b", [32, 128], BF16)
    P.dma("pool", w2c[:], dr["rw_w2cat"][:, cs_], w2c, dr["rw_w2cat"])
    P.dma("pool", a2c[:], dr["rw_a2cat"][:, cs_], a2c, dr["rw_a2cat"])
    P.dma("pool", g2a[:], dr["rw_g2"][0:128, cs_], g2a, dr["rw_g2"])
    P.dma("pool", g2b[:], dr["rw_g2"][128:160, cs_], g2b, dr["rw_g2"])
    P.memset(y_acc[:], 0.0, [y_acc])
    P.memset(bon[:], 0.0, [bon])
    bufs = [DirBufs(P, 0)]
    with P.overlay(xT, start=ov_mark):
        bufs.append(DirBufs(P, 1))
    sqb = bufs[0].f[0]

    def dir_gen(d):
        W = bufs[d]
        B = P.banks[4 * d:4 * d + 4]
        r_f, k_f, v_f, kk_f, ic_f, lw_f, cs_f, e1, e2, tmp = W.f
        ARz, BTz, KTz, Vb, stk, Vz, Uz, mats, Lf, sq, ZT = (W.ARz, W.BTz, W.KTz, W.Vb, W.stk, W.Vz, W.Uz, W.mats, W.Lf, W.sq, W.ZT)
        T0, T0s, T0b, Xb = W.T0, W.T0s, W.T0b, W.Xb
        order = list(range(9)) if d == 0 else [0] + list(range(8, 0, -1))
        for b in order:
            t0, nb = RB[b]
            lat = b > 0
            l0 = t0 - LC
            proj_mix(B[0][:, :nb], B[0], wr_, wrs, 0, 128, t0, nb)
            yield
            proj_mix(B[1][:, :nb], B[1], wk_, wks, 0, 128, t0, nb)
            yield
            proj_mix(B[2][:, :nb], B[2], wv_, wvs, 0, 128, t0, nb)
            pr_d = slice(d * 64, (d + 1) * 64)
            P.mm(B[3][:, 0:nb], w2c[pr_d, :], lw1[pr_d, t0:t0 + nb], True, True, [w2c, lw1], [B[3]])
            P.mm(B[3][:, 256:256 + nb], a2c[pr_d, :], la1[pr_d, t0:t0 + nb], True, True, [a2c, la1], [B[3]])
            P.copy(r_f[:], B[0][:, :nb], [B[0]], [r_f], eng="act")
            P.copy(k_f[:], B[1][:, :nb], [B[1]], [k_f], eng="act")
            P.copy(v_f[:], B[2][:, :nb], [B[2]], [v_f], eng="act")
            P.copy(Vb[:], v_f[:], [v_f], [Vb])
            yield
            P.act(lw_f[:], B[3][:, 0:nb], AF.Sigmoid, [B[3], vec], [lw_f], bias=vcol(V_W0 + d, c))
            P.ts(lw_f[:], lw_f[:], NEG_EXP_HALF, None, ALU.mult, None, [lw_f], [lw_f])
            P.act(ic_f[:], B[3][:, 256:256 + nb], AF.Sigmoid, [B[3], vec], [ic_f], bias=vcol(V_A0 + d, c))
            P.ts(kk_f[:], k_f[:], vcol(V_KK, c), None, ALU.mult, None, [k_f, vec], [kk_f])
            P.tt(tmp[:], kk_f[:], kk_f[:], ALU.mult, [kk_f], [tmp])
            P.mm(B[0][:, :nb], bones[:], tmp[:], True, True, [bones, tmp], [B[0]])
            yield
            P.ts(e1[:], B[0][:, :nb], 1e-18, None, ALU.max, None, [B[0]], [e1])
            P.act(e1[:], e1[:], AF.Ln, [e1], [e1])
            P.act(e1[:], e1[:], AF.Exp, [e1], [e1], scale=-0.5)
            P.tt(kk_f[:], kk_f[:], e1[:], ALU.mult, [kk_f, e1], [kk_f])
            P.ts(tmp[:], ic_f[:], vcol(V_KA, c), omk[:, c:c + 1], ALU.mult, ALU.add, [ic_f, vec, omk], [tmp])
            P.tt(k_f[:], k_f[:], tmp[:], ALU.mult, [k_f, tmp], [k_f])
            if lat:
                P.stt(tmp[:], r_f[:], vcol(V_RK, c), k_f[:], ALU.mult, ALU.mult, [r_f, vec, k_f], [tmp])
                P.mm(B[1][:, :nb], bones[:], tmp[:], True, True, [bones, tmp], [B[1]])
                yield
                P.tt(tmp[:], B[1][:, :nb], v_f[:], ALU.mult, [B[1], v_f], [tmp])
                P.tt(bon[:, l0:l0 + nb], bon[:, l0:l0 + nb], tmp[:], ALU.add, [bon, tmp], [bon])
            if d == 0:
                P.op("dve", lambda e: e.tensor_tensor_scan(out=cs_f[:], data0=maskC[:], data1=lw_f[:], initial=0.0,
                                                           op0=ALU.mult, op1=ALU.add), [maskC, lw_f], [cs_f])
            else:
                P.op("dve", lambda e: e.tensor_tensor_scan(out=cs_f[:, ::-1], data0=maskC[:], data1=lw_f[:, ::-1], initial=0.0,
                                                           op0=ALU.mult, op1=ALU.add), [maskC, lw_f], [cs_f])
            P.tt(e2[:], cs_f[:], lw_f[:], ALU.subtract, [cs_f, lw_f], [e2])
            P.act(e2[:], e2[:], AF.Exp, [e2], [e2])
            for h in range(2):
                pr = slice(h * 64, (h + 1) * 64)
                P.stt(ARz[h][pr, 0, :], kk_f[pr], -1.0, e2[pr], ALU.mult, ALU.mult, [kk_f, e2], [ARz[h]])
            P.act(e1[:], cs_f[:], AF.Exp, [cs_f], [e1])
            for h in range(2):
                pr = slice(h * 64, (h + 1) * 64)
                P.tt(ARz[h][pr, 1, :], r_f[pr], e1[pr], ALU.mult, [r_f, e1], [ARz[h]])
            P.act(e2[:], cs_f[:], AF.Exp, [cs_f], [e2], scale=-1.0)
            P.tt(tmp[:], kk_f[:], ic_f[:], ALU.mult, [kk_f, ic_f], [tmp])
            for h in range(2):
                pr = slice(h * 64, (h + 1) * 64)
                P.tt(BTz[h][pr], tmp[pr], e2[pr], ALU.mult, [tmp, e2], [BTz[h]])
                P.tt(KTz[h][pr], k_f[pr], e2[pr], ALU.mult, [k_f, e2], [KTz[h]])
            yield
            nch = nb // 64
            for n in range(nch):
                cols = slice(n * 64, (n + 1) * 64)
                pb = B[n % 2]
                for h in range(2):
                    pr = slice(h * 64, (h + 1) * 64)
                    for mi, src in enumerate((Vb, BTz[h], KTz[h])):
                        P.mm(pb[pr, mi * 64:(mi + 1) * 64], src[:, cols], identb[:, pr], True, True, [src, identb], [pb])
                P.copy(stk[n][:], pb[:, 0:192], [pb], [stk[n]], eng="act")
                for h in range(2):
                    pr = slice(h * 64, (h + 1) * 64)
                    P.copy(Vz[n][h][pr, :], stk[n][pr, 0:64], [stk[n]], [Vz[n][h]])
                yield
            for n in range(nch):
                cols = slice(n * 64, (n + 1) * 64)
                pa = B[2 + n % 2]
                pb = B[n % 2]
                for h in range(2):
                    pr = slice(h * 64, (h + 1) * 64)
                    hc2 = slice(h * 64, (h + 1) * 64)
                    P.mm(pa[pr, 0:256].rearrange("p (m c) -> p m c", m=2)[:, :, hc2], BTz[h][:, cols], ARz[h][:, :, cols], True, True,
                         [BTz[h], ARz[h]], [pa])
                    P.mm(pa[pr, 256:512].rearrange("p (m c) -> p m c", m=2)[:, :, hc2], KTz[h][:, cols], ARz[h][:, :, cols], True, True,
                         [KTz[h], ARz[h]], [pa])
                    P.mm(pb[pr, 256 + h * 64:256 + (h + 1) * 64], ARz[h][:, 0, cols], BTz[h][:, cols], True, True,
                         [ARz[h], BTz[h]], [pb])
                P.tt(Lf[n][:, 0:128], pa[:, 0:128], m4[d][:, 0:128], ALU.mult, [pa, m4[d]], [Lf[n]])
                P.tt(Lf[n][:, 128:256], pb[:, 256:384], m1[d][:], ALU.mult, [pb, m1[d]], [Lf[n]])
                P.tt(mats[n][:, 128:512], pa[:, 128:512], m4[d][:, 128:512], ALU.mult, [pa, m4[d]], [mats[n]])
                yield
            cur = []
            for n in range(nch):
                P.tt(ZT[n][:], Lf[n][:, 0:128], identf[:], ALU.add, [Lf[n], identf], [ZT[n]])
                cur.append((Lf[n], 128, 0))
            for lvl in range(5):
                last = lvl == 4
                for n in range(nch):
                    src, oL, oLT = cur[n]
                    dst = sq[n][lvl % 2]
                    pa = B[n % 2]
                    P.mm(pa[:, 0:128], src[:, oLT:oLT + 128], src[:, oL:oL + 128], True, True, [src], [pa])
                    if not last:
                        P.mm(pa[:, 128:256], src[:, oL:oL + 128], src[:, oLT:oLT + 128], True, True, [src], [pa])
                        P.copy(dst[:, 0:256], pa[:, 0:256], [pa], [dst], eng="act")
                    else:
                        P.copy(dst[:, 0:128], pa[:, 0:128], [pa], [dst], eng="act")
                    cur[n] = (dst, 0, 128)
                    yield
                for n in range(nch):
                    src, oL, oLT = cur[n]
                    pz = B[2 + n % 2]
                    P.mm(pz[:, 0:128], src[:, oL:oL + 128], ZT[n][:], True, True, [src, ZT[n]], [pz])
                    P.tt(ZT[n][:], ZT[n][:], pz[:, 0:128], ALU.add, [ZT[n], pz], [ZT[n]])
                    yield
            chunks = list(range(nch)) if d == 0 else list(range(nch - 1, -1, -1))
            for n in chunks:
                cols = slice(n * 64, (n + 1) * 64)
                dcol = (n * 64 + 63) if d == 0 else n * 64
                dC = e1[:, dcol:dcol + 1]
                pX = B[0]
                for h in range(2):
                    pr = slice(h * 64, (h + 1) * 64)
                    P.mm(pX[pr, 0:64], ARz[h][:, 0, cols], T0b[:, :], True, False, [ARz[h], T0b], [pX])
                P.mm(pX[:, 0:64], mats[n][:, 256:384], stk[n][:, 0:64], False, True, [mats[n], stk[n]], [pX])
                P.ts(T0s[:], T0[:], dC, None, ALU.mult, None, [T0, e1], [T0s])
                yield
                P.copy(Xb[:], pX[:, 0:64], [pX], [Xb], eng="act")
                pU = B[1]
                P.mm(pU[:, 0:64], ZT[n][:], Xb[:], True, True, [ZT[n], Xb], [pU])
                yield
                P.copy(Uz[0][0:64, :], pU[0:64, 0:64], [pU], [Uz[0]], eng="act")
                P.copy(Uz[1][64:128, :], pU[64:128, 0:64], [pU], [Uz[1]])
                pT = B[3]
                for h in range(2):
                    pr = slice(h * 64, (h + 1) * 64)
                    P.mm(pT[pr, 0:64], stk[n][:, 64:128], Uz[h][:, :], True, False, [stk[n], Uz[h]], [pT])
                    P.mm(pT[pr, 0:64], stk[n][:, 128:192], Vz[n][h][:, :], False, True, [stk[n], Vz[n][h]], [pT])
                if lat:
                    pY = B[2]
                    for h in range(2):
                        pr = slice(h * 64, (h + 1) * 64)
                        P.mm(pY[pr, 0:64], T0b[:, :], ARz[h][:, 1, cols], True, False, [T0b, ARz[h]], [pY])
                        P.mm(pY[pr, 0:64], Uz[h][:, :], mats[n][:, 128 + h * 64:128 + (h + 1) * 64], False, False,
                             [Uz[h], mats[n]], [pY])
                        P.mm(pY[pr, 0:64], stk[n][:, 0:64], mats[n][:, 384 + h * 64:384 + (h + 1) * 64], False, True,
                             [stk[n], mats[n]], [pY])
                yield
                P.stt(T0[:], pT[:, 0:64], dC, T0s[:], ALU.mult, ALU.add, [pT, e1, T0s], [T0])
                P.copy(T0b[:], T0[:], [T0], [T0b], eng="act")
                if lat:
                    yc = slice(l0 + n * 64, l0 + (n + 1) * 64)
                    P.tt(y_acc[:, yc], y_acc[:, yc], pY[:, 0:64], ALU.add, [y_acc, pY], [y_acc])
                yield

    gens = [dir_gen(0), dir_gen(1)]
    alive = [True, True]
    while any(alive):
        for gi, g in enumerate(gens):
            if alive[gi]:
                try:
                    next(g)
                except StopIteration:
                    alive[gi] = False
    if c == 0:
        tap("L1_y0", y_acc, y_acc[:], [128, S])
        tap("L1_bon0", bon, bon[:], [128, S])
    B = P.banks
    sqb2 = bufs[1].f[0]
    for i in range(4):
        l0 = i * 512
        t0 = LC + l0
        nb = 512
        yb = y_acc[:, l0:l0 + nb]
        sq_ = [bufs[0].f[0], bufs[0].f[1]]
        for hf in range(2):
            c0_ = l0 + hf * 256
            ybh = y_acc[:, c0_:c0_ + 256]
            sc_ = bufs[hf].f[0]
            P.mm(B[0][:, hf * 256:(hf + 1) * 256], bones[:], ybh, True, True, [bones, y_acc], [B[0]])
            P.stt(ybh, B[0][:, hf * 256:(hf + 1) * 256], -1.0 / 64, ybh, ALU.mult, ALU.add, [B[0], y_acc], [y_acc])
            P.tt(sc_[:], ybh, ybh, ALU.mult, [y_acc], [sc_])
            P.mm(B[1][:, hf * 256:(hf + 1) * 256], bones[:], sc_[:], True, True, [bones, sc_], [B[1]])
            P.act(sc_[:], B[1][:, hf * 256:(hf + 1) * 256], AF.Ln, [B[1], k.gneps], [sc_], scale=1.0 / 64, bias=k.gneps[:, 0:1])
            P.act(sc_[:], sc_[:], AF.Exp, [sc_], [sc_], scale=-0.5)
            P.tt(ybh, ybh, sc_[:], ALU.mult, [y_acc, sc_], [y_acc])
        P.ts(yb, yb, vcol(V_GG, c), vcol(V_GB, c), ALU.mult, ALU.add, [y_acc, vec], [y_acc])
        P.tt(yb, yb, bon[:, l0:l0 + nb], ALU.add, [y_acc, bon], [y_acc])
        P.mm(B[2][:, :nb], g2a[:], lg1[:, t0:t0 + nb], True, False, [g2a, lg1], [B[2]])
        P.mm(B[2][:, :nb], g2b[:], lg1b[:, t0:t0 + nb], False, True, [g2b, lg1b], [B[2]])
        P.tt(outT[:, c, l0:l0 + nb], yb, B[2][:, :nb], ALU.mult, [y_acc, B[2]], [outT])


_CACHE = {}


def kernel(**inputs):
    inp = {k_: np.asarray(v) for k_, v in inputs.items()}
    B = inp["x"].shape[0]
    ns = B // NCORE
    if "nc" not in _CACHE:
        _CACHE["nc"] = build(ns=ns)[0]
    nc = _CACHE["nc"]
    sh = prep_shared(inp)
    in_maps = []
    for core in range(NCORE):
        m = dict(sh)
        m.update(prep_core(inp, core, ns))
        in_maps.append(m)
    res = run_bass_kernel_spmd(nc, in_maps, core_ids=list(range(NCORE)))
    out = np.empty((B, S, D), np.float32)
    for core in range(NCORE):
        o = res.results[core]["outT"]
        for s in range(ns):
            out[core * ns + s] = o[s].T
    return out
```

```python
from contextlib import ExitStack, contextmanager
import numpy as np
import concourse.bass as bass
import concourse.mybir as mybir
from concourse.bass_utils import run_bass_kernel_spmd

F32 = mybir.dt.float32
BF16 = mybir.dt.bfloat16
AF = mybir.ActivationFunctionType
ALU = mybir.AluOpType
AX = mybir.AxisListType

ENGS = ("pe", "dve", "act", "pool", "sp")

D = 1024
KC = 8
S = 2048
LC = 256
T = S + LC
NCORE = 8
BLOCKS = [(0, 256), (256, 512), (768, 512), (1280, 512), (1792, 512)]
LAT_BLOCKS = BLOCKS[1:]
EPS = 1e-6
ARENA_BYTES = 206 * 1024


class Buf:
    def __init__(self, prog, t, name, space):
        self.prog = prog
        self.t = t
        self.name = name
        self.space = space
        self.last_w = None
        self.reads = {}
        self.dsem = None

    def __getitem__(self, idx):
        return self.t[idx]

    def view(self, name):
        b = Buf(self.prog, self.t, name, self.space)
        self.prog._register(b)
        return b


class Prog:
    def __init__(self):
        self.nc = bass.Bass("TRN2", target_bir_lowering=False)
        self.gstack = ExitStack()
        self.ops = {e: [] for e in ENGS}
        self.cnt = {e: 0 for e in ENGS}
        self.waited = {e: {} for e in ENGS}
        self.sems = {}
        self.dpool = []
        self.dcnt = {}
        self.live_dsem = set()
        self.ndsem = 0
        self.live_bufs = []
        self.phase_bufs = None
        self.uid = 0
        for e in ENGS:
            self._sem("eng_" + e)
        self.arena = self.gstack.enter_context(self.nc.sbuf_tensor("arena", [128, ARENA_BYTES // 4], F32))
        self.top = 0
        self.limit = ARENA_BYTES
        self.hiwater = 0
        self.banks = []
        for i in range(8):
            t = self.gstack.enter_context(self.nc.psum_tensor("bank%d" % i, [128, 512], F32))
            b = Buf(self, t, "bank%d" % i, "psum")
            self.live_bufs.append(b)
            self.banks.append(b)

    def _sem(self, key):
        if key not in self.sems:
            self.sems[key] = self.gstack.enter_context(self.nc.semaphore("s_" + key))
        return self.sems[key]

    def _register(self, b):
        self.live_bufs.append(b)
        if self.phase_bufs is not None:
            self.phase_bufs.append(b)

    def _name(self, name):
        self.uid += 1
        return "%s_%d" % (name, self.uid)

    def sbuf(self, name, shape, dtype=F32):
        esz = 2 if dtype == BF16 else 4
        nel = 1
        for d in shape[1:]:
            nel *= d
        nb = (nel * esz + 63) // 64 * 64
        off = self.top
        self.top += nb
        assert self.top <= self.limit, "arena overflow %s: top=%d limit=%d" % (name, self.top, self.limit)
        self.hiwater = max(self.hiwater, self.top)
        ap = self.arena[:, off // 4:(off + nb) // 4]
        if dtype != F32:
            ap = ap.bitcast(dtype)
        ap = ap[:, :nel]
        if len(shape) > 2:
            names = ["d%d" % i for i in range(len(shape) - 1)]
            pat = "p (%s) -> p %s" % (" ".join(names), " ".join(names))
            ap = ap.rearrange(pat, **{n: v for n, v in zip(names, shape[1:])})
        if shape[0] < 128:
            ap = ap[0:shape[0]]
        b = Buf(self, ap, name, "sbuf")
        b.off = off
        b.nbytes = nb
        self._register(b)
        return b

    @contextmanager
    def overlay(self, buf, start=None):
        st = (self.top, self.limit)
        self.top, self.limit = (buf.off if start is None else start), buf.off + buf.nbytes
        try:
            yield
        finally:
            buf.ov_top = self.top
            self.top, self.limit = st

    def dram(self, name, shape, dtype=F32, kind="Internal"):
        t = self.nc.dram_tensor(name, list(shape), dtype, kind=kind)
        b = Buf(self, t.ap(), name, "dram")
        self.live_bufs.append(b)
        return b

    def _need(self, eng, key, val, waits):
        if key == "eng_pe" and eng == "pe":
            return
        if self.waited[eng].get(key, 0) >= val:
            return
        self.waited[eng][key] = val
        waits[key] = max(waits.get(key, 0), val)

    def _deps(self, eng, reads, writes):
        waits = {}
        for b in reads:
            if b.last_w is not None:
                self._need(eng, b.last_w[0], b.last_w[1], waits)
            if b.space == "psum":
                for k, v in b.reads.items():
                    if k != "eng_" + eng:
                        self._need(eng, k, v, waits)
        for b in writes:
            if b.last_w is not None:
                self._need(eng, b.last_w[0], b.last_w[1], waits)
            for k, v in b.reads.items():
                self._need(eng, k, v, waits)
        return list(waits.items())

    dbg_budget = None

    def op(self, eng, fn, reads=(), writes=()):
        if self.dbg_budget is not None:
            if self.dbg_budget <= 0:
                return
            self.dbg_budget -= 1
        waits = self._deps(eng, reads, writes)
        self.cnt[eng] += 1
        key = "eng_" + eng
        val = self.cnt[eng]
        for b in reads:
            if b.reads.get(key, 0) < val:
                b.reads[key] = val
        for b in writes:
            b.last_w = (key, val)
            b.reads = {}
        self.ops[eng].append((waits, fn, key, 1))

    def dma(self, eng, out_ap, in_ap, dst, src, **kw):
        waits = self._deps(eng, [src], [dst])
        if dst.dsem is None:
            if self.dpool:
                dst.dsem = self.dpool.pop()
            else:
                self.ndsem += 1
                dst.dsem = "d%d" % self.ndsem
                self._sem(dst.dsem)
                self.dcnt[dst.dsem] = 0
            self.live_dsem.add(dst.dsem)
        key = dst.dsem
        self.dcnt[key] += 16
        val = self.dcnt[key]
        if src.reads.get(key, 0) < val:
            src.reads[key] = val
        dst.last_w = (key, val)
        dst.reads = {}

        def fn(e, out_ap=out_ap, in_ap=in_ap, kw=kw):
            return e.dma_start(out=out_ap, in_=in_ap, **kw)

        self.ops[eng].append((waits, fn, key, 16))

    def barrier(self):
        toks = [("eng_" + e, self.cnt[e]) for e in ENGS if self.cnt[e] > 0]
        toks += [(k, self.dcnt[k]) for k in sorted(self.live_dsem) if self.dcnt[k] > 0]
        for e in ENGS:
            waits = {}
            for k, v in toks:
                if k == "eng_" + e:
                    continue
                self._need(e, k, v, waits)
            if waits:
                self.ops[e].append((list(waits.items()), None, None, 0))
        for b in self.live_bufs:
            b.last_w = None
            b.reads = {}

    @contextmanager
    def phase(self):
        prev_b = self.phase_bufs
        mark = (self.top, self.limit)
        self.phase_bufs = []
        try:
            yield
        finally:
            self.barrier()
            for b in self.phase_bufs:
                if b.dsem is not None:
                    if b.dsem in self.live_dsem:
                        self.live_dsem.discard(b.dsem)
                        self.dpool.append(b.dsem)
                    b.dsem = None
            dead = set(id(b) for b in self.phase_bufs)
            self.live_bufs = [b for b in self.live_bufs if id(b) not in dead]
            self.top, self.limit = mark
            self.phase_bufs = prev_b

    def final_wait(self, eng, bufs):
        waits = {}
        for b in bufs:
            if b.last_w is not None:
                self._need(eng, b.last_w[0], b.last_w[1], waits)
        self.ops[eng].append((list(waits.items()), None, None, 0))

    def emit(self):
        nc = self.nc
        sems = self.sems

        def run(e, lst):
            for waits, fn, key, inc in lst:
                for k, v in waits:
                    e.wait_ge(sems[k], v)
                if fn is not None:
                    fn(e).then_inc(sems[key], inc)

        with nc.Block() as block:
            @block.tensor
            def _(e):
                run(e, self.ops["pe"])

            @block.vector
            def _(e):
                run(e, self.ops["dve"])

            @block.scalar
            def _(e):
                run(e, self.ops["act"])

            @block.gpsimd
            def _(e):
                run(e, self.ops["pool"])

            @block.sync
            def _(e):
                run(e, self.ops["sp"])
        self.gstack.close()
        return nc

    def mm(self, out, lhsT, rhs, start, stop, R, W):
        self.op("pe", lambda e: e.matmul(out, lhsT=lhsT, rhs=rhs, start=start, stop=stop), R, W)

    def act(self, out, in_, func, R, W, scale=1.0, bias=0.0):
        self.op("act", lambda e: e.activation(out=out, in_=in_, func=func, bias=bias, scale=scale), R, W)

    def tt(self, out, in0, in1, op, R, W, eng="dve"):
        self.op(eng, lambda e: e.tensor_tensor(out=out, in0=in0, in1=in1, op=op), R, W)

    def ts(self, out, in0, s1, s2, op0, op1, R, W, eng="dve"):
        if op1 is None:
            self.op(eng, lambda e: e.tensor_scalar(out=out, in0=in0, scalar1=s1, scalar2=None, op0=op0), R, W)
        else:
            self.op(eng, lambda e: e.tensor_scalar(out=out, in0=in0, scalar1=s1, scalar2=s2, op0=op0, op1=op1), R, W)

    def stt(self, out, in0, scalar, in1, op0, op1, R, W):
        self.op("dve", lambda e: e.scalar_tensor_tensor(out=out, in0=in0, scalar=scalar, in1=in1, op0=op0, op1=op1), R, W)

    def copy(self, out, in_, R, W, eng="dve"):
        if eng == "act":
            self.op("act", lambda e: e.copy(out=out, in_=in_), R, W)
        else:
            self.op(eng, lambda e: e.tensor_copy(out=out, in_=in_), R, W)

    def memset(self, ap, val, W, eng="dve"):
        self.op(eng, lambda e: e.memset(ap, val), (), W)


def _rope_tables():
    rows = S // 64
    row = np.repeat(np.arange(rows), 64).astype(np.float32)
    col = np.tile(np.arange(64), rows).astype(np.float32)
    inv = (10000.0 ** (-np.arange(0, 32, 2, dtype=np.float32) / 32)).astype(np.float32)
    ar = row[:, None] * inv
    ac = col[:, None] * inv
    C = np.zeros((128, S), np.float32)
    Sg = np.zeros((128, S), np.float32)
    for p in range(128):
        d = p % 64
        ang = ar if d < 32 else ac
        i = d % 16
        C[p] = np.cos(ang[:, i])
        sgn = -1.0 if (d % 32) < 16 else 1.0
        Sg[p] = sgn * np.sin(ang[:, i])
    return C, Sg


def _perm_rot(cols64):
    idx = np.arange(64)
    out = idx.copy()
    for base in (0, 32):
        out[base:base + 16] = idx[base + 16:base + 32]
        out[base + 16:base + 32] = idx[base:base + 16]
    return cols64[out]


def prep_shared(inp):
    f = np.float32
    sh = {}
    sh["mod_w"] = np.ascontiguousarray(inp["mod_w"], f)
    sh["mod_bT"] = np.ascontiguousarray(inp["mod_b"].reshape(2, 48, 128).transpose(2, 0, 1), f)
    g = np.stack([inp["norm_mix"], inp["norm_ffn"]], 0)
    sh["gT"] = np.ascontiguousarray(g.reshape(2, 2, 8, 128).transpose(3, 0, 1, 2), f)
    sh["finT"] = np.ascontiguousarray(inp["final_norm"].reshape(8, 128).T, f)
    sh["router_w"] = np.ascontiguousarray(inp["router_w"], f)
    sh["exp_w1"] = inp["exp_w1"]
    sh["exp_w3"] = inp["exp_w3"]
    sh["exp_w2"] = inp["exp_w2"]
    w_in = inp["mix_in"][0]
    q = w_in[:, 0:512]
    k = w_in[:, 512:640]
    v = w_in[:, 640:768]
    ul = w_in[:, 768:1280]
    gl = w_in[:, 1280:1792]
    qcols, qpcols = [], []
    for j in range(4):
        for h in (j, 4 + j):
            c64 = np.arange(h * 64, (h + 1) * 64)
            qcols.append(c64)
            qpcols.append(_perm_rot(c64))
    qcols = np.concatenate(qcols)
    qpcols = np.concatenate(qpcols)
    kp = np.concatenate([_perm_rot(np.arange(0, 64)), _perm_rot(np.arange(64, 128))])
    sh["w_in"] = np.ascontiguousarray(np.concatenate([q[:, qcols], q[:, qpcols], k, k[:, kp], v, ul, gl], 1), f)
    w_out = inp["mix_out"][0]
    arows = []
    for j in range(4):
        for h in (j, 4 + j):
            arows.append(np.arange(h * 64, (h + 1) * 64))
    arows = np.concatenate(arows + [np.arange(512, 1024)])
    sh["w_out"] = np.ascontiguousarray(w_out[arows], f)
    sh["sinkb"] = np.ascontiguousarray(np.broadcast_to(inp["attn_sink"][0][None, :], (128, 8)), f)
    C, Sg = _rope_tables()
    sh["ropeC"] = C
    sh["ropeS"] = Sg
    kq = np.arange(128)
    mP = (kq[:, None] >= kq[None, :]).astype(f)
    mN = (kq[:, None] <= kq[None, :]).astype(f)
    sh["maskP"] = np.ascontiguousarray(np.tile(mP, (1, 4)))
    sh["maskN"] = np.ascontiguousarray(np.tile(mN, (1, 4)))
    sh["convw"] = np.ascontiguousarray(inp["lru_conv_w"][0].reshape(4, 4, 128).transpose(2, 1, 0), f)
    sh["convb"] = np.ascontiguousarray(inp["lru_conv_b"][0].reshape(4, 128).T, f)

    def bd(w):
        o = np.zeros((128, 2, 4, 128), f)
        for d in range(2):
            for c in range(4):
                for hb in range(2):
                    o[hb * 64:(hb + 1) * 64, d, c, hb * 64:(hb + 1) * 64] = w[d, 2 * c + hb]
        return o
    sh["lru_wa"] = bd(inp["lru_wa"][0])
    sh["lru_wi"] = bd(inp["lru_wi"][0])

    def vec(a):
        return np.ascontiguousarray(a.reshape(2, 4, 128).transpose(2, 0, 1), f)
    sh["lru_ba"] = vec(inp["lru_ba"][0])
    sh["lru_bi"] = vec(inp["lru_bi"][0])
    sh["lru_lam"] = vec(inp["lru_lam"][0])
    sh["id16"] = np.eye(16, dtype=f)
    rw48 = np.zeros((2, 1024, 48), f)
    rw48[:, :, 0:16] = inp["router_w"]
    rw48[:, :, 32:48] = inp["router_w"]
    sh["router_w48"] = rw48
    bd = np.zeros((48, 48), f)
    for g_ in range(3):
        bd[g_ * 16:(g_ + 1) * 16, g_ * 16:(g_ + 1) * 16] = 1.0
    sh["bd48"] = bd
    sh["iota_j"] = np.ascontiguousarray(np.broadcast_to(np.arange(288, dtype=f)[None, :], (128, 288)))
    sh["iota_p"] = np.ascontiguousarray(np.arange(128, dtype=f)[:, None] + np.array([0, 128, 256], f)[None, :])
    def fm(v1024):
        return np.asarray(v1024, f).reshape(8, 128).T
    vecs = [inp["rw_mu"][0][i] for i in range(6)] + [inp["rw_w0"][0][0], inp["rw_w0"][0][1], inp["rw_a0"][0][0], inp["rw_a0"][0][1],
            inp["rw_kk"][0], inp["rw_ka"][0], inp["rw_rk"][0].reshape(1024), inp["rw_gn_g"][0], inp["rw_gn_b"][0]]
    sh["rw_vec"] = np.ascontiguousarray(np.stack([fm(v) for v in vecs], 1), f)
    sh["ident"] = np.eye(128, dtype=f)
    bo = np.zeros((128, 128), f)
    bo[:64, :64] = 1.0
    bo[64:, 64:] = 1.0
    sh["blockones"] = bo
    mc = np.ones((128, 256), f)
    mc[:, ::64] = 0.0
    sh["maskC"] = mc
    ii = np.arange(64)
    lt = (ii[:, None] < ii[None, :]).astype(f)
    le = (ii[:, None] <= ii[None, :]).astype(f)
    def bdm(m):
        o = np.zeros((128, 128), f)
        o[:64, :64] = m
        o[64:, 64:] = m
        return o
    sh["rmask4"] = np.ascontiguousarray(np.stack([np.concatenate([bdm(lt), bdm(le), bdm(lt), bdm(le)], 1),
                                                  np.concatenate([bdm(lt.T), bdm(le.T), bdm(lt.T), bdm(le.T)], 1)], 0), f)
    sh["rmask1"] = np.ascontiguousarray(np.stack([bdm(lt.T), bdm(lt)], 0), f)
    sh["rw_w1cat"] = np.ascontiguousarray(np.concatenate([inp["rw_w1"][0][0], inp["rw_w1"][0][1]], 1), f)
    sh["rw_a1cat"] = np.ascontiguousarray(np.concatenate([inp["rw_a1"][0][0], inp["rw_a1"][0][1]], 1), f)
    sh["rw_g1"] = np.ascontiguousarray(inp["rw_g1"][0], f)
    sh["rw_rkv"] = np.ascontiguousarray(inp["rw_rkv"][0], f)
    sh["rw_w2cat"] = np.ascontiguousarray(np.concatenate([inp["rw_w2"][0][0], inp["rw_w2"][0][1]], 0), f)
    sh["rw_a2cat"] = np.ascontiguousarray(np.concatenate([inp["rw_a2"][0][0], inp["rw_a2"][0][1]], 0), f)
    sh["rw_g2"] = np.ascontiguousarray(inp["rw_g2"][0], f)
    sh["rw_wo"] = np.ascontiguousarray(inp["rw_wo"][0], f)
    return sh


def prep_core(inp, core, ns):
    f = np.float32
    b0 = core * ns
    pc = {}
    pc["xT"] = np.ascontiguousarray(inp["x"][b0:b0 + ns].transpose(0, 2, 1), f)
    pc["cxT"] = np.ascontiguousarray(inp["ctx"][b0:b0 + ns].transpose(0, 2, 1), f)
    cc = np.zeros((8, 1024), f)
    cc[:ns] = inp["c"][b0:b0 + ns]
    cc[4] = inp["c_ctx"]
    pc["cT"] = np.ascontiguousarray(cc.reshape(8, 8, 128).transpose(2, 1, 0), f)
    return pc


class K:
    pass


GELU_C = 1.5957691216057308


def bank3(bank, a, b):
    return bank[:].rearrange("p (a b) -> p a b", a=a)


def build(ns=4, taps=None, stop_after=None, skip_l0=False):
    taps = taps or []
    P = Prog()
    k = K()
    k.P = P
    k.tap_outs = {}
    k.stop_after = stop_after
    k.skip_l0 = skip_l0
    import os as _os
    k.sparse = _os.environ.get("MOE_DENSE", "0") != "1"
    IN = "ExternalInput"
    dr = {}
    k.dr = dr

    def din(name, shape):
        dr[name] = P.dram(name, shape, F32, IN)
        return dr[name]

    din("xT", [ns, D, S]); din("cxT", [ns, D, LC]); din("cT", [128, KC, 8])
    din("mod_w", [2, D, 6 * D]); din("mod_bT", [128, 2, 48]); din("gT", [128, 2, 2, 8]); din("finT", [128, 8])
    din("router_w", [2, D, 16])
    din("exp_w1", [2, 16, D, D]); din("exp_w3", [2, 16, D, D]); din("exp_w2", [2, 16, D, D])
    din("w_in", [D, 2432]); din("w_out", [D, D]); din("sinkb", [128, 8])
    din("ropeC", [128, S]); din("ropeS", [128, S]); din("maskP", [128, 512]); din("maskN", [128, 512])
    din("convw", [128, 4, 4]); din("convb", [128, 4])
    din("lru_wa", [128, 2, 4, 128]); din("lru_wi", [128, 2, 4, 128])
    din("lru_ba", [128, 2, 4]); din("lru_bi", [128, 2, 4]); din("lru_lam", [128, 2, 4])
    din("id16", [16, 16]); din("iota_j", [128, 288]); din("iota_p", [128, 3]); din("router_w48", [2, D, 48]); din("bd48", [48, 48])
    din("rw_vec", [128, 15, KC]); din("ident", [128, 128]); din("blockones", [128, 128]); din("maskC", [128, 256])
    din("rmask4", [2, 128, 512]); din("rmask1", [2, 128, 128])
    din("rw_w1cat", [D, 128]); din("rw_a1cat", [D, 128]); din("rw_g1", [D, 160]); din("rw_rkv", [3, D, D])
    din("rw_w2cat", [128, D]); din("rw_a2cat", [128, D]); din("rw_g2", [160, D]); din("rw_wo", [D, D])
    k.xspill = P.dram("xspill", [D, S], F32, "Internal")
    k.outT = P.dram("outT", [ns, D, S], F32, "ExternalOutput")

    def tap(name, buf, ap, shape):
        if name not in taps:
            return
        o = P.dram("tap_" + name, list(shape), F32, "ExternalOutput")
        k.tap_outs[name] = o
        P.dma("pool", o[:], ap, o, buf)
    k.tap = tap

    k.ones_f = P.sbuf("ones_f", [128, 128], F32)
    P.memset(k.ones_f[:], 1.0, [k.ones_f])
    k.ones_b = P.sbuf("ones_b", [128, 64], BF16)
    P.memset(k.ones_b[:], 1.0, [k.ones_b])
    k.epsb = P.sbuf("epsb", [128, 1], F32)
    P.memset(k.epsb[:], EPS, [k.epsb])
    k.gneps = P.sbuf("gneps", [128, 1], F32)
    P.memset(k.gneps[:], 64e-5, [k.gneps])
    k.modv = modv = P.sbuf("modv", [128, 2, 48, 8], F32)
    k.Amod = Amod = P.sbuf("Amod", [128, 2, 2, 8, 8], F32)
    gT = P.sbuf("gT", [128, 2, 2, 8], F32)
    k.finT = P.sbuf("finT", [128, 8], F32)
    P.dma("sp", gT[:], dr["gT"][:], gT, dr["gT"])
    P.dma("sp", k.finT[:], dr["finT"][:], k.finT, dr["finT"])

    with P.phase():
        cT = P.sbuf("cT", [128, KC, 8], F32)
        P.dma("sp", cT[:], dr["cT"][:], cT, dr["cT"])
        sT = P.sbuf("sT", [128, KC, 8], F32)
        P.act(sT[:], cT[:], AF.Silu, [cT], [sT])
        mbT = P.sbuf("mbT", [128, 2, 48], F32)
        P.dma("sp", mbT[:], dr["mod_bT"][:], mbT, dr["mod_bT"])
        wm = [P.sbuf("wm%d" % i, [128, KC, 512], F32) for i in range(2)]
        it = 0
        for l in range(2):
            psm = P.banks[l]
            psm3 = psm[:, 0:384].rearrange("p (a b) -> p a b", a=48)
            for cb in range(12):
                w = wm[it % 2]
                it += 1
                src = dr["mod_w"][l].rearrange("(kc p) n -> p kc n", p=128)[:, :, cb * 512:(cb + 1) * 512]
                P.dma("sp", w[:], src, w, dr["mod_w"])
                for o4 in range(4):
                    oc = cb * 4 + o4
                    for kc in range(KC):
                        P.mm(psm3[:, oc, :], w[:, kc, o4 * 128:(o4 + 1) * 128], sT[:, kc, :], kc == 0, kc == KC - 1,
                             [w, sT], [psm])
            P.tt(modv[:, l], psm3, mbT[:, l, :, None].to_broadcast([128, 48, 8]), ALU.add, [psm, mbT], [modv])
            for kind in range(2):
                sc = modv[:, l, (1 + 3 * kind) * 8:(2 + 3 * kind) * 8, :]
                P.ts(Amod[:, l, kind], sc, 1.0, None, ALU.add, None, [modv], [Amod])
                P.tt(Amod[:, l, kind], Amod[:, l, kind], gT[:, kind, l, :, None].to_broadcast([128, 8, 8]), ALU.mult,
                     [Amod, gT], [Amod])
        tap("modv", modv, modv[:], [128, 2, 48, 8])

    for s in range(ns):
        sample_program(k, s)

    outs = [k.outT] + list(k.tap_outs.values())
    P.final_wait("sp", outs)
    P.final_wait("pool", outs)
    print("[build] arena hiwater %d / %d bytes, dma sems %d, instr %s" % (P.hiwater, ARENA_BYTES, P.ndsem,
                                                                        {e: len(P.ops[e]) for e in ENGS}))
    nc = P.emit()
    return nc, k


def mvec(k, l, j, kc, si):
    return k.modv[:, l, j * 8 + kc, si:si + 1]


def x_src(dr, s, b):
    t0, nb = BLOCKS[b]
    if b == 0:
        return dr["cxT"][s].rearrange("(kc p) t -> p kc t", p=128), dr["cxT"]
    return dr["xT"][s].rearrange("(kc p) t -> p kc t", p=128)[:, :, t0 - LC:t0 - LC + nb], dr["xT"]


class NormScratch:
    def __init__(self, k, bank):
        P = k.P
        self.sq = P.sbuf("sq", [128, KC, 512], F32)
        self.rs = P.sbuf("rs", [128, 512], F32)
        self.ps = bank


def rms_block(k, sc, x_ap, xbuf, nb):
    P = k.P
    P.act(sc.sq[:, :, :nb], x_ap, AF.Square, [xbuf], [sc.sq])
    for kc in range(KC):
        P.mm(sc.ps[:, :nb], k.ones_f[:], sc.sq[:, kc, :nb], kc == 0, kc == KC - 1, [k.ones_f, sc.sq], [sc.ps])
    P.act(sc.rs[:, :nb], sc.ps[:, :nb], AF.Ln, [sc.ps, k.epsb], [sc.rs], scale=1.0 / D, bias=k.epsb[:, 0:1])
    P.act(sc.rs[:, :nb], sc.rs[:, :nb], AF.Exp, [sc.rs], [sc.rs], scale=-0.5)
    P.tt(sc.sq[:, :, :nb], x_ap, sc.rs[:, None, :nb].to_broadcast([128, KC, nb]), ALU.mult, [xbuf, sc.rs], [sc.sq])
    return sc.sq


class WRing:
    def __init__(self, k, n):
        self.k = k
        self.slots = [k.P.sbuf("wslot%d" % i, [128, KC, 512], BF16) for i in range(n)]
        self.i = 0

    def load(self, w2d, wbuf, ncols=512):
        sl = self.slots[self.i % len(self.slots)]
        self.i += 1
        src = w2d.rearrange("(kc p) n -> p kc n", p=128)
        self.k.P.dma("pool", sl[:, :, :ncols], src, sl, wbuf)
        return sl


def sample_program(k, s):
    P = k.P
    dr = k.dr
    tap = k.tap if s == 0 else (lambda *a, **kw: None)
    SI = lambda b: 4 if b == 0 else s
    with P.phase():
        hT = P.sbuf("hT", [128, KC, T], BF16)
        xT = P.sbuf("xT", [128, KC, T], F32)
        if k.skip_l0:
            for b, (t0, nb) in enumerate(BLOCKS):
                src, sb_ = x_src(dr, s, b)
                P.dma("sp", xT[:, :, t0:t0 + nb], src, xT, sb_)
            layer1_and_out(k, s, hT, xT, tap)
            return
        with P.phase(), P.overlay(xT):
            sc = NormScratch(k, P.banks[0])
            xb = [P.sbuf("xb%d" % i, [128, KC, 512], F32) for i in range(2)]
            for b, (t0, nb) in enumerate(BLOCKS):
                xt = xb[b % 2]
                src, sb_ = x_src(dr, s, b)
                P.dma("sp", xt[:, :, :nb], src, xt, sb_)
                xn = rms_block(k, sc, xt[:, :, :nb], xt, nb)
                for kc in range(KC):
                    P.ts(hT[:, kc, t0:t0 + nb], xn[:, kc, :nb], k.Amod[:, 0, 0, kc, SI(b):SI(b) + 1],
                         mvec(k, 0, 0, kc, SI(b)), ALU.mult, ALU.add, [xn, k.Amod, k.modv], [hT])
            tap("L0_h", hT, hT[:], [128, KC, T])
        if k.stop_after == "L0A":
            return
        with P.phase():
            attnT = P.sbuf("attnT", [128, 4, T], BF16)
            lruT = P.sbuf("lruT", [128, 4, T], BF16)
            wr = WRing(k, 3)
            with P.phase(), P.overlay(xT):
                l0_attention(k, s, hT, attnT, wr, tap)
            if k.stop_after == "L0B":
                return
            with P.phase(), P.overlay(xT):
                l0_lru(k, s, hT, lruT, wr, tap)
            if k.stop_after == "L0C":
                return
            with P.phase():
                xb = [P.sbuf("xb%d" % i, [128, KC, 512], F32) for i in range(2)]
                wo = [wr.load(dr["w_out"][:, h * 512:(h + 1) * 512], dr["w_out"]) for h in range(2)]
                pi = 0
                for b, (t0, nb) in enumerate(BLOCKS):
                    xt = xb[b % 2]
                    src, sb_ = x_src(dr, s, b)
                    P.dma("sp", xt[:, :, :nb], src, xt, sb_)
                    for oc in range(KC):
                        ps = P.banks[pi % 4]
                        pi += 1
                        w = wo[oc // 4]
                        c0 = (oc % 4) * 128
                        for j in range(4):
                            P.mm(ps[:, :nb], w[:, j, c0:c0 + 128], attnT[:, j, t0:t0 + nb], j == 0, False, [w, attnT], [ps])
                        for c in range(4):
                            P.mm(ps[:, :nb], w[:, 4 + c, c0:c0 + 128], lruT[:, c, t0:t0 + nb], False, c == 3, [w, lruT], [ps])
                        P.stt(xT[:, oc, t0:t0 + nb], ps[:, :nb], mvec(k, 0, 2, oc, SI(b)), xt[:, oc, :nb], ALU.mult, ALU.add,
                              [ps, k.modv, xt], [xT])
                tap("L0_xmix", xT, xT[:], [128, KC, T])
        if k.stop_after == "L0D":
            return
        (moe_layer_sparse if k.sparse else moe_layer)(k, s, 0, hT, xT, True, tap)
        tap("L0_x", xT, xT[:], [128, KC, T])
        if k.stop_after == "L0F":
            return
        layer1_and_out(k, s, hT, xT, tap)


def layer1_and_out(k, s, hT, xT, tap):
    P = k.P
    layer1_mixer(k, s, hT, xT, tap)
    if k.stop_after in ("L1A", "L1B", "L1D", "L1L", "L1P"):
        return
    (moe_layer_sparse if k.sparse else moe_layer)(k, s, 1, hT, xT, False, tap)
    tap("L1_x", xT, xT[:], [128, KC, T])
    with P.phase():
        sc = NormScratch(k, P.banks[0])
        ob = [P.sbuf("ob%d" % i, [128, KC, 512], F32) for i in range(2)]
        for i, (t0, nb) in enumerate(LAT_BLOCKS):
            xn = rms_block(k, sc, xT[:, :, t0:t0 + nb], xT, nb)
            o = ob[i % 2]
            P.tt(o[:], xn[:, :, :nb], k.finT[:, :, None].to_broadcast([128, KC, nb]), ALU.mult, [xn, k.finT], [o])
            P.dma("sp", k.outT[s].rearrange("(kc p) t -> p kc t", p=128)[:, :, t0 - LC:t0 - LC + nb], o[:], k.outT, o)


def l0_attention(k, s, hT, qT, wr, tap):
    P = k.P
    dr = k.dr
    kT = P.sbuf("kT", [128, T], BF16)
    Vtok = P.sbuf("Vtok", [128, 18, 128], BF16)
    ropeC = P.sbuf("ropeC", [128, 512], F32)
    ropeS = P.sbuf("ropeS", [128, 512], F32)
    t1 = P.sbuf("rt1", [128, 512], F32)
    t2 = P.sbuf("rt2", [128, 512], F32)
    maskP = P.sbuf("maskP", [128, 4, 128], BF16)
    maskN = P.sbuf("maskN", [128, 4, 128], BF16)
    sinkb = P.sbuf("sinkb", [128, 8], F32)
    esink = P.sbuf("esink", [128, 8], F32)
    P.dma("pool", maskP[:], dr["maskP"][:].rearrange("p (a b) -> p a b", a=4), maskP, dr["maskP"])
    P.dma("pool", maskN[:], dr["maskN"][:].rearrange("p (a b) -> p a b", a=4), maskN, dr["maskN"])
    P.dma("sp", sinkb[:], dr["sinkb"][:], sinkb, dr["sinkb"])
    P.act(esink[:], sinkb[:], AF.Exp, [sinkb], [esink])
    wq = wr.load(dr["w_in"][:, 0:512], dr["w_in"])
    wqp = wr.load(dr["w_in"][:, 512:1024], dr["w_in"])
    wkv = wr.load(dr["w_in"][:, 1024:1408], dr["w_in"], 384)
    pi = 0
    for b, (t0, nb) in enumerate(BLOCKS):
        if b > 0:
            P.dma("sp", ropeC[:, :nb], dr["ropeC"][:, t0 - LC:t0 - LC + nb], ropeC, dr["ropeC"])
            P.dma("sp", ropeS[:, :nb], dr["ropeS"][:, t0 - LC:t0 - LC + nb], ropeS, dr["ropeS"])
        for j in range(5):
            w, wp, c0 = (wq, wqp, j * 128) if j < 4 else (wkv, wkv, 0)
            dst = qT[:, j, t0:t0 + nb] if j < 4 else kT[:, t0:t0 + nb]
            dbuf = qT if j < 4 else kT
            ps = P.banks[pi % 4]
            pi += 1
            for kc in range(KC):
                P.mm(ps[:, :nb], w[:, kc, c0:c0 + 128], hT[:, kc, t0:t0 + nb], kc == 0, kc == KC - 1, [w, hT], [ps])
            if b == 0:
                P.copy(dst, ps[:, :nb], [ps], [dbuf], eng="act")
            else:
                c1 = c0 if j < 4 else 128
                ps2 = P.banks[pi % 4]
                pi += 1
                for kc in range(KC):
                    P.mm(ps2[:, :nb], wp[:, kc, c1:c1 + 128], hT[:, kc, t0:t0 + nb], kc == 0, kc == KC - 1, [wp, hT], [ps2])
                P.tt(t1[:, :nb], ps[:, :nb], ropeC[:, :nb], ALU.mult, [ps, ropeC], [t1])
                P.tt(t2[:, :nb], ps2[:, :nb], ropeS[:, :nb], ALU.mult, [ps2, ropeS], [t2])
                P.tt(dst, t1[:, :nb], t2[:, :nb], ALU.add, [t1, t2], [dbuf])
        for tt_ in range(t0 // 128, (t0 + nb) // 128):
            ps = P.banks[pi % 4]
            pi += 1
            for kc in range(KC):
                P.mm(ps[:, 0:128], hT[:, kc, tt_ * 128:(tt_ + 1) * 128], wkv[:, kc, 256:384], kc == 0, kc == KC - 1,
                     [hT, wkv], [ps])
            P.copy(Vtok[:, tt_, :], ps[:, 0:128], [ps], [Vtok], eng="act")
    tap("L0_q", qT, qT[:], [128, 4, T])
    tap("L0_k", kT, kT[:], [128, T])
    tap("L0_v", Vtok, Vtok[:], [128, 18, 128])
    pT = [P.sbuf("pT%d" % i, [128, 4, 128], BF16) for i in range(2)]
    den = P.sbuf("den", [128, 4, 128], F32)
    ps_s = [P.banks[4], P.banks[5]]
    ps_num = P.banks[6]
    ps_den = P.banks[7]
    it = 0
    for qt in range(18):
        if qt < 2:
            keys = [(0, None), (1, None)]
        else:
            i = qt - 2
            keys = [(0, None), (1, None)]
            if i > 0:
                keys.append((qt - 1, maskP))
            keys.append((qt, None))
            if i < 15:
                keys.append((qt + 1, maskN))
        for g in range(2):
            pr = slice(g * 64, (g + 1) * 64)
            num3 = ps_num[pr, :].rearrange("p (a b) -> p a b", a=4)
            den3 = ps_den[pr, :].rearrange("p (a b) -> p a b", a=4)
            for idx, (kt, mk) in enumerate(keys):
                pss = ps_s[it % 2]
                pt = pT[it % 2]
                it += 1
                P.mm(pss[:].rearrange("p (a b) -> p a b", a=4), kT[pr, kt * 128:(kt + 1) * 128],
                     qT[pr, :, qt * 128:(qt + 1) * 128], True, True, [kT, qT], [pss])
                P.act(pt[:], pss[:].rearrange("p (a b) -> p a b", a=4), AF.Exp, [pss], [pt], scale=0.125)
                if mk is not None:
                    P.tt(pt[:], pt[:], mk[:], ALU.mult, [pt, mk], [pt])
                first, last = idx == 0, idx == len(keys) - 1
                P.mm(num3, Vtok[:, kt, pr], pt[:], first, last, [Vtok, pt], [ps_num])
                P.mm(den3, k.ones_b[:, 0:64], pt[:], first, last, [k.ones_b, pt], [ps_den])
            P.tt(den[pr], den3, esink[pr, g * 4:(g + 1) * 4, None].to_broadcast([64, 4, 128]), ALU.add,
                 [ps_den, esink], [den])
            P.op("dve", lambda e, pr=pr: e.reciprocal(out=den[pr], in_=den[pr]), [den], [den])
            P.tt(qT[pr, :, qt * 128:(qt + 1) * 128], num3, den[pr], ALU.mult, [ps_num, den], [qT])
    tap("L0_attn", qT, qT[:], [128, 4, T])


def l0_lru(k, s, hT, lruT, wr, tap):
    P = k.P
    dr = k.dr
    ul = P.sbuf("ul", [128, T], F32)
    u = P.sbuf("u", [128, T], F32)
    ub = P.sbuf("ub", [128, T], BF16)
    tmp = [P.sbuf("lt%d" % i, [128, 512], F32) for i in range(7)]
    t_r, t_i, t_a, t_y, t_b, t_h0, t_h1 = tmp
    convw = P.sbuf("convw", [128, 4, 4], F32)
    convb = P.sbuf("convb", [128, 4], F32)
    wa = P.sbuf("wa", [128, 2, 4, 128], BF16)
    wi = P.sbuf("wi", [128, 2, 4, 128], BF16)
    ba = P.sbuf("ba", [128, 2, 4], F32)
    bi = P.sbuf("bi", [128, 2, 4], F32)
    lam = P.sbuf("lam", [128, 2, 4], F32)
    kap = P.sbuf("kap", [128, 2, 4], F32)
    kap2 = P.sbuf("kap2", [128, 2, 4], F32)
    zz = P.sbuf("zz", [128, 2, 4], F32)
    zp = P.sbuf("zp", [128, 2, 4], F32)
    for nm, t_ in (("convw", convw), ("convb", convb), ("lru_ba", ba), ("lru_bi", bi), ("lru_lam", lam)):
        P.dma("sp", t_[:], dr[nm][:], t_, dr[nm])
    P.dma("pool", wa[:], dr["lru_wa"][:], wa, dr["lru_wa"])
    P.dma("pool", wi[:], dr["lru_wi"][:], wi, dr["lru_wi"])
    P.act(zz[:], lam[:], AF.Exp, [lam], [zz], scale=-1.0)
    P.ts(zp[:], zz[:], -0.2, 0.25, ALU.mult, ALU.add, [zz], [zp])
    for cst in (1.0 / 3.0, 0.5, 1.0):
        P.tt(zp[:], zp[:], zz[:], ALU.mult, [zp, zz], [zp])
        P.ts(zp[:], zp[:], -1.0, cst, ALU.mult, ALU.add, [zp], [zp])
    P.tt(zp[:], zp[:], zz[:], ALU.mult, [zp, zz], [zp])
    P.ts(kap[:], zp[:], -8.0, None, ALU.mult, None, [zp], [kap])
    P.ts(kap2[:], zp[:], -16.0, None, ALU.mult, None, [zp], [kap2])
    wul = wr.load(dr["w_in"][:, 1408:1920], dr["w_in"])
    wgl = wr.load(dr["w_in"][:, 1920:2432], dr["w_in"])
    pi = 0
    for c in range(4):
        for b, (t0, nb) in enumerate(BLOCKS):
            ps = P.banks[pi % 4]
            pi += 1
            for kc in range(KC):
                P.mm(ps[:, :nb], wul[:, kc, c * 128:(c + 1) * 128], hT[:, kc, t0:t0 + nb], kc == 0, kc == KC - 1, [wul, hT], [ps])
            P.copy(ul[:, t0:t0 + nb], ps[:, :nb], [ps], [ul], eng="act")
        P.act(u[:], ul[:], AF.Identity, [ul, convw, convb], [u], scale=convw[:, c, 2:3], bias=convb[:, c:c + 1])
        for (a_, e_) in ((0, LC), (LC, T)):
            P.stt(u[:, a_ + 2:e_], ul[:, a_:e_ - 2], convw[:, c, 0:1], u[:, a_ + 2:e_], ALU.mult, ALU.add, [ul, convw, u], [u])
            P.stt(u[:, a_ + 1:e_], ul[:, a_:e_ - 1], convw[:, c, 1:2], u[:, a_ + 1:e_], ALU.mult, ALU.add, [ul, convw, u], [u])
            P.stt(u[:, a_:e_ - 1], ul[:, a_ + 1:e_], convw[:, c, 3:4], u[:, a_:e_ - 1], ALU.mult, ALU.add, [ul, convw, u], [u])
        P.copy(ub[:], u[:], [u], [ub], eng="act")
        if c == 0:
            tap("L0_u0", u, u[:], [128, T])
        hsum = ul
        for d in range(2):
            order = [0, 1, 2, 3, 4] if d == 0 else [0, 4, 3, 2, 1]
            prev_h = None
            for oi, b in enumerate(order):
                t0, nb = BLOCKS[b]
                psr = P.banks[pi % 4]
                pi += 1
                psi = P.banks[pi % 4]
                pi += 1
                P.mm(psr[:, :nb], wa[:, d, c, :], ub[:, t0:t0 + nb], True, True, [wa, ub], [psr])
                P.mm(psi[:, :nb], wi[:, d, c, :], ub[:, t0:t0 + nb], True, True, [wi, ub], [psi])
                P.act(t_r[:, :nb], psr[:, :nb], AF.Sigmoid, [psr, ba], [t_r], bias=ba[:, d, c:c + 1])
                P.act(t_i[:, :nb], psi[:, :nb], AF.Sigmoid, [psi, bi], [t_i], bias=bi[:, d, c:c + 1])
                P.act(t_a[:, :nb], t_r[:, :nb], AF.Exp, [t_r, kap], [t_a], scale=kap[:, d, c:c + 1])
                P.act(t_y[:, :nb], t_r[:, :nb], AF.Exp, [t_r, kap2], [t_y], scale=kap2[:, d, c:c + 1])
                P.ts(t_y[:, :nb], t_y[:, :nb], -1.0, 1.0, ALU.mult, ALU.add, [t_y], [t_y])
                P.ts(t_y[:, :nb], t_y[:, :nb], 1e-30, None, ALU.max, None, [t_y], [t_y])
                P.act(t_y[:, :nb], t_y[:, :nb], AF.Ln, [t_y], [t_y])
                P.act(t_y[:, :nb], t_y[:, :nb], AF.Exp, [t_y], [t_y], scale=0.5)
                P.tt(t_b[:, :nb], t_y[:, :nb], t_i[:, :nb], ALU.mult, [t_y, t_i], [t_b])
                P.tt(t_b[:, :nb], t_b[:, :nb], u[:, t0:t0 + nb], ALU.mult, [t_b, u], [t_b])
                if d == 0:
                    init = 0.0 if oi == 0 else hsum[:, t0 - 1:t0]
                    P.op("dve", lambda e, t0=t0, nb=nb, init=init: e.tensor_tensor_scan(
                        out=hsum[:, t0:t0 + nb], data0=t_a[:, :nb], data1=t_b[:, :nb], initial=init,
                        op0=ALU.mult, op1=ALU.add), [t_a, t_b, hsum], [hsum])
                else:
                    th = t_h0 if oi % 2 == 0 else t_h1
                    if oi == 0:
                        init, rd = 0.0, []
                    else:
                        init, rd = prev_h[1], [prev_h[0]]
                    P.op("dve", lambda e, nb=nb, init=init, th=th: e.tensor_tensor_scan(
                        out=th[:, :nb][:, ::-1], data0=t_a[:, :nb][:, ::-1], data1=t_b[:, :nb][:, ::-1], initial=init,
                        op0=ALU.mult, op1=ALU.add), [t_a, t_b] + rd, [th])
                    prev_h = (th, th[:, 0:1])
                    P.tt(hsum[:, t0:t0 + nb], hsum[:, t0:t0 + nb], th[:, :nb], ALU.add, [hsum, th], [hsum])
        if c == 0:
            tap("L0_hsum0", hsum, hsum[:], [128, T])
        for b, (t0, nb) in enumerate(BLOCKS):
            ps = P.banks[pi % 4]
            pi += 1
            for kc in range(KC):
                P.mm(ps[:, :nb], wgl[:, kc, c * 128:(c + 1) * 128], hT[:, kc, t0:t0 + nb], kc == 0, kc == KC - 1, [wgl, hT], [ps])
            P.copy(t_r[:, :nb], ps[:, :nb], [ps], [t_r], eng="act")
            P.act(t_i[:, :nb], ps[:, :nb], AF.Square, [ps], [t_i])
            P.ts(t_i[:, :nb], t_i[:, :nb], 0.044715, 1.0, ALU.mult, ALU.add, [t_i], [t_i])
            P.tt(t_i[:, :nb], t_i[:, :nb], t_r[:, :nb], ALU.mult, [t_i, t_r], [t_i])
            P.act(t_i[:, :nb], t_i[:, :nb], AF.Sigmoid, [t_i], [t_i], scale=GELU_C)
            P.tt(t_i[:, :nb], t_i[:, :nb], t_r[:, :nb], ALU.mult, [t_i, t_r], [t_i])
            P.tt(lruT[:, c, t0:t0 + nb], t_i[:, :nb], hsum[:, t0:t0 + nb], ALU.mult, [t_i, hsum], [lruT])
    tap("L0_lru", lruT, lruT[:], [128, 4, T])


def moe_layer(k, s, l, hT, xT, with_ctx, tap):
    P = k.P
    dr = k.dr
    SI = lambda b: 4 if b == 0 else s
    blocks = list(enumerate(BLOCKS)) if with_ctx else list(enumerate(BLOCKS))[1:]
    with P.phase():
        gateT = P.sbuf("gateT", [16, T], F32)
        with P.phase():
            sc = NormScratch(k, P.banks[0])
            hf = P.sbuf("hf", [128, KC, 512], F32)
            wrt = P.sbuf("wrt", [128, KC, 16], F32)
            ones16 = P.sbuf("ones16", [16, 16], F32)
            ex = P.sbuf("ex", [16, 512], F32)
            P.memset(ones16[:], 1.0, [ones16])
            P.dma("sp", wrt[:], dr["router_w"][l].rearrange("(kc p) e -> p kc e", p=128), wrt, dr["router_w"])
            psl = P.banks[1]
            pss = P.banks[2]
            for b, (t0, nb) in blocks:
                xn = rms_block(k, sc, xT[:, :, t0:t0 + nb], xT, nb)
                for kc in range(KC):
                    P.ts(hf[:, kc, :nb], xn[:, kc, :nb], k.Amod[:, l, 1, kc, SI(b):SI(b) + 1], mvec(k, l, 3, kc, SI(b)),
                         ALU.mult, ALU.add, [xn, k.Amod, k.modv], [hf])
                P.copy(hT[:, :, t0:t0 + nb], hf[:, :, :nb], [hf], [hT], eng="act")
                for kc in range(KC):
                    P.mm(psl[0:16, :nb], wrt[:, kc, :], hf[:, kc, :nb], kc == 0, kc == KC - 1, [wrt, hf], [psl])
                P.act(ex[:, :nb], psl[0:16, :nb], AF.Exp, [psl], [ex])
                P.mm(pss[0:16, :nb], ones16[:], ex[:, :nb], True, True, [ones16, ex], [pss])
                P.op("dve", lambda e, nb=nb: e.reciprocal(out=sc.rs[0:16, :nb], in_=pss[0:16, :nb]), [pss], [sc.rs])
                P.tt(gateT[:, t0:t0 + nb], ex[:, :nb], sc.rs[0:16, :nb], ALU.mult, [ex, sc.rs], [gateT])
            tap("L%d_aff" % l, gateT, gateT[:], [16, T])
            work = P.sbuf("work", [16, S], F32)
            m8 = P.sbuf("m8", [16, 8], F32)
            segs = [(LC, S, 8)] + ([(0, LC, 8)] if with_ctx else [])
            for (a0, n, _) in segs:
                cap = 2 * n // 16
                P.copy(work[:, :n], gateT[:, a0:a0 + n], [gateT], [work])
                for r in range(cap // 8):
                    P.op("dve", lambda e, n=n: e.max(out=m8[:], in_=work[:, :n]), [work], [m8])
                    if r < cap // 8 - 1:
                        P.op("dve", lambda e, n=n: e.match_replace(out=work[:, :n], in_to_replace=m8[:], in_values=work[:, :n],
                                                                   imm_value=-1.0), [work, m8], [work])
                P.stt(gateT[:, a0:a0 + n], gateT[:, a0:a0 + n], m8[:, 7:8], gateT[:, a0:a0 + n], ALU.is_ge, ALU.mult,
                      [gateT, m8], [gateT])
            tap("L%d_gate" % l, gateT, gateT[:], [16, T])
            tap("L%d_hffn" % l, hT, hT[:], [128, KC, T])
        with P.phase():
            wr = WRing(k, 4)
            id16 = P.sbuf("id16", [16, 16], F32)
            P.dma("sp", id16[:], dr["id16"][:], id16, dr["id16"])
            hid = P.sbuf("hid", [128, KC, T], BF16)
            gb = [P.sbuf("gb%d" % i, [128, 512], F32) for i in range(2)]
            s1 = [P.sbuf("s1_%d" % i, [128, 512], F32) for i in range(2)]
            pi = 0
            it = 0
            for e in range(16):
                for h in range(2):
                    w1 = wr.load(dr["exp_w1"][l, e][:, h * 512:(h + 1) * 512], dr["exp_w1"])
                    w3 = wr.load(dr["exp_w3"][l, e][:, h * 512:(h + 1) * 512], dr["exp_w3"])
                    for b, (t0, nb) in blocks:
                        g_ = gb[it % 2]
                        it += 1
                        psg = P.banks[6 + it % 2]
                        P.mm(psg[:, :nb], id16[:, e:e + 1].to_broadcast([16, 128]), gateT[:, t0:t0 + nb], True, True,
                             [id16, gateT], [psg])
                        P.copy(g_[:, :nb], psg[:, :nb], [psg], [g_], eng="act")
                        for f4 in range(4):
                            f = h * 4 + f4
                            ps1 = P.banks[pi % 6]
                            ps3 = P.banks[(pi + 1) % 6]
                            pi += 2
                            st = s1[(pi // 2) % 2]
                            for kc in range(KC):
                                P.mm(ps1[:, :nb], w1[:, kc, f4 * 128:(f4 + 1) * 128], hT[:, kc, t0:t0 + nb], kc == 0, kc == KC - 1,
                                     [w1, hT], [ps1])
                            for kc in range(KC):
                                P.mm(ps3[:, :nb], w3[:, kc, f4 * 128:(f4 + 1) * 128], hT[:, kc, t0:t0 + nb], kc == 0, kc == KC - 1,
                                     [w3, hT], [ps3])
                            P.act(st[:, :nb], ps1[:, :nb], AF.Silu, [ps1], [st])
                            P.tt(st[:, :nb], st[:, :nb], ps3[:, :nb], ALU.mult, [st, ps3], [st])
                            P.tt(hid[:, f, t0:t0 + nb], st[:, :nb], g_[:, :nb], ALU.mult, [st, g_], [hid])
                for h2 in range(2):
                    w2 = wr.load(dr["exp_w2"][l, e][:, h2 * 512:(h2 + 1) * 512], dr["exp_w2"])
                    for b, (t0, nb) in blocks:
                        for o4 in range(4):
                            oc = h2 * 4 + o4
                            psy = P.banks[pi % 6]
                            pi += 1
                            for f in range(KC):
                                P.mm(psy[:, :nb], w2[:, f, o4 * 128:(o4 + 1) * 128], hid[:, f, t0:t0 + nb], f == 0, f == KC - 1,
                                     [w2, hid], [psy])
                            P.stt(xT[:, oc, t0:t0 + nb], psy[:, :nb], mvec(k, l, 5, oc, SI(b)), xT[:, oc, t0:t0 + nb],
                                  ALU.mult, ALU.add, [psy, k.modv, xT], [xT])


def moe_layer_sparse(k, s, l, hT, xT, with_ctx, tap):
    P = k.P
    dr = k.dr
    SI = lambda b: 4 if b == 0 else s
    blocks = list(enumerate(BLOCKS)) if with_ctx else list(enumerate(BLOCKS))[1:]
    nslot = 288 if with_ctx else 256
    jcs = [(0, 128), (128, 128)] + ([(256, 32)] if with_ctx else [])
    tiles = list(range(18)) if with_ctx else list(range(2, 18))
    with P.phase():
        gp = P.sbuf("gp", [48, T], F32)
        pos_tok = P.sbuf("pos_tok", [128, 18, 16], F32)
        with P.overlay(hT):
            h_tok = P.sbuf("h_tok", [128, 18, D], BF16)
        gateT = gp
        with P.phase():
            sc = NormScratch(k, P.banks[0])
            hf = P.sbuf("hf", [128, KC, 512], F32)
            wrt = P.sbuf("wrt", [128, KC, 48], F32)
            ones16 = P.sbuf("ones48", [48, 48], F32)
            ex = P.sbuf("ex", [48, 512], F32)
            identf = P.sbuf("identf", [128, 128], F32)
            P.dma("sp", identf[:], dr["ident"][:], identf, dr["ident"])
            P.dma("sp", ones16[:], dr["bd48"][:], ones16, dr["bd48"])
            P.dma("sp", wrt[:], dr["router_w48"][l].rearrange("(kc p) e -> p kc e", p=128), wrt, dr["router_w48"])
            psl = P.banks[1]
            pss = P.banks[2]
            ti = 0
            for b, (t0, nb) in blocks:
                xn = rms_block(k, sc, xT[:, :, t0:t0 + nb], xT, nb)
                for kc in range(KC):
                    P.ts(hf[:, kc, :nb], xn[:, kc, :nb], k.Amod[:, l, 1, kc, SI(b):SI(b) + 1], mvec(k, l, 3, kc, SI(b)),
                         ALU.mult, ALU.add, [xn, k.Amod, k.modv], [hf])
                for kc in range(KC):
                    P.mm(psl[0:48, :nb], wrt[:, kc, :], hf[:, kc, :nb], kc == 0, kc == KC - 1, [wrt, hf], [psl])
                P.act(ex[:, :nb], psl[0:48, :nb], AF.Exp, [psl], [ex])
                P.mm(pss[0:48, :nb], ones16[:], ex[:, :nb], True, True, [ones16, ex], [pss])
                P.op("dve", lambda e, nb=nb: e.reciprocal(out=sc.rs[0:48, :nb], in_=pss[0:48, :nb]), [pss], [sc.rs])
                P.tt(gateT[0:48, t0:t0 + nb], ex[:, :nb], sc.rs[0:48, :nb], ALU.mult, [ex, sc.rs], [gateT])
                for tl in range(nb // 128):
                    tile = t0 // 128 + tl
                    for half in range(2):
                        bk = P.banks[3 + ti % 4]
                        ti += 1
                        for q in range(4):
                            kc = half * 4 + q
                            P.op("pe", lambda e, bk=bk, q=q, kc=kc, tl=tl: e.transpose(
                                out=bk[:, q * 128:(q + 1) * 128], in_=hf[:, kc, tl * 128:(tl + 1) * 128], identity=identf[:]),
                                [hf, identf], [bk])
                        P.copy(h_tok[:, tile, half * 512:(half + 1) * 512], bk[:, :], [bk], [h_tok],
                               eng="act" if ti % 2 == 0 else "dve")
            work = P.sbuf("work", [48, S], F32)
            m8 = P.sbuf("m8", [48, 8], F32)
            segs = [(LC, S)] + ([(0, LC)] if with_ctx else [])
            for (a0, n) in segs:
                cap = 2 * n // 16
                P.copy(work[:, :n], gateT[0:48, a0:a0 + n], [gateT], [work])
                for r in range(cap // 8):
                    P.op("dve", lambda e, n=n: e.max(out=m8[:], in_=work[:, :n]), [work], [m8])
                    if r < cap // 8 - 1:
                        P.op("dve", lambda e, n=n: e.match_replace(out=work[:, :n], in_to_replace=m8[:], in_values=work[:, :n],
                                                                   imm_value=-1.0), [work, m8], [work])
                P.stt(gateT[0:48, a0:a0 + n], gateT[0:48, a0:a0 + n], m8[:, 7:8], gateT[0:48, a0:a0 + n], ALU.is_ge, ALU.mult,
                      [gateT, m8], [gateT])
                P.ts(work[32:48, :n], gateT[32:48, a0:a0 + n], 0.0, None, ALU.is_gt, None, [gateT], [work])
                P.op("dve", lambda e, a0=a0, n=n: e.tensor_tensor_scan(out=gp[32:48, a0:a0 + n], data0=work[32:48, :n], data1=work[32:48, :n],
                                                                       initial=0.0, op0=ALU.add, op1=ALU.max), [work], [gp])
                if a0 == 0:
                    P.ts(gp[32:48, a0:a0 + n], gp[32:48, a0:a0 + n], 256.0, None, ALU.add, None, [gp], [gp])
                P.tt(gp[32:48, a0:a0 + n], gp[32:48, a0:a0 + n], work[32:48, :n], ALU.mult, [gp, work], [gp])
                P.ts(gp[32:48, a0:a0 + n], gp[32:48, a0:a0 + n], -1.0, None, ALU.add, None, [gp], [gp])
            tap("L%d_gate" % l, gp, gp[0:16, :], [16, T])
            bk = P.banks[1]
            bk3 = bk[:, 0:288].rearrange("p (a b) -> p a b", a=18)
            for tile in tiles:
                P.op("pe", lambda e, tile=tile: e.transpose(out=bk3[:, tile, :], in_=gp[32:48, tile * 128:(tile + 1) * 128],
                                                            identity=identf[32:48, 32:48]), [gp, identf], [bk])
            P.copy(pos_tok[:, tiles[0]:18, :], bk3[:, tiles[0]:18, :], [bk], [pos_tok])
        with P.phase():
            wr = WRing(k, 3)
            id48 = P.sbuf("id48", [48, 48], F32)
            P.dma("sp", id48[:], dr["ident"][0:48, 0:48], id48, dr["ident"])
            iota_j = P.sbuf("iota_j", [128, 288], F32)
            iota_p = P.sbuf("iota_p", [128, 3], F32)
            P.dma("sp", iota_j[:], dr["iota_j"][:], iota_j, dr["iota_j"])
            P.dma("sp", iota_p[:], dr["iota_p"][:], iota_p, dr["iota_p"])
            Sel = P.sbuf("Sel", [128, 18, nslot], BF16)
            SelT = P.sbuf("SelT", [128, len(jcs), T], BF16)
            xin = P.sbuf("xin", [128, KC, nslot], BF16)
            hid = P.sbuf("hid", [128, KC, nslot], BF16)
            y_tok = P.sbuf("y_tok", [128, len(jcs), D], BF16)
            gb = P.sbuf("gb", [128, 512], F32)
            s1 = [P.sbuf("s1_%d" % i, [128, nslot], F32) for i in range(2)]
            pi = 0
            it = 0
            def build_sel(e_):
                for tile in tiles:
                    P.ts(Sel[:, tile, :], iota_j[:, :nslot], pos_tok[:, tile, e_:e_ + 1], None, ALU.is_equal, None,
                         [iota_j, pos_tok], [Sel])

            build_sel(0)
            for e in range(16):
                for b, (t0, nb) in blocks:
                    psp = P.banks[6]
                    psg = P.banks[7]
                    P.mm(psp[:, :nb], id48[:, 32 + e:33 + e].to_broadcast([48, 128]), gp[:, t0:t0 + nb], True, True, [id48, gp], [psp])
                    P.mm(psg[:, :nb], id48[:, e:e + 1].to_broadcast([48, 128]), gp[:, t0:t0 + nb], True, True, [id48, gp], [psg])
                    P.copy(gb[:, :nb], psg[:, :nb], [psg], [gb], eng="act")
                    for ji, (j0, nj) in enumerate(jcs):
                        P.stt(SelT[0:nj, ji, t0:t0 + nb], psp[0:nj, :nb], iota_p[0:nj, ji:ji + 1], gb[0:nj, :nb], ALU.is_equal, ALU.mult,
                              [psp, iota_p, gb], [SelT])
                for kc in range(KC):
                    ps = P.banks[pi % 6]
                    pi += 1
                    for i_, tile in enumerate(tiles):
                        P.mm(ps[:, :nslot], h_tok[:, tile, kc * 128:(kc + 1) * 128], Sel[:, tile, :], i_ == 0, i_ == len(tiles) - 1,
                             [h_tok, Sel], [ps])
                    P.copy(xin[:, kc, :], ps[:, :nslot], [ps], [xin], eng="act" if kc % 2 == 0 else "dve")
                if e + 1 < 16:
                    build_sel(e + 1)
                for h in range(2):
                    w1 = wr.load(dr["exp_w1"][l, e][:, h * 512:(h + 1) * 512], dr["exp_w1"])
                    w3 = wr.load(dr["exp_w3"][l, e][:, h * 512:(h + 1) * 512], dr["exp_w3"])
                    for f4 in range(4):
                        f = h * 4 + f4
                        ps1 = P.banks[pi % 6]
                        ps3 = P.banks[(pi + 1) % 6]
                        pi += 2
                        st = s1[(pi // 2) % 2]
                        for kc in range(KC):
                            P.mm(ps1[:, :nslot], w1[:, kc, f4 * 128:(f4 + 1) * 128], xin[:, kc, :], kc == 0, kc == KC - 1, [w1, xin], [ps1])
                        for kc in range(KC):
                            P.mm(ps3[:, :nslot], w3[:, kc, f4 * 128:(f4 + 1) * 128], xin[:, kc, :], kc == 0, kc == KC - 1, [w3, xin], [ps3])
                        P.act(st[:], ps1[:, :nslot], AF.Silu, [ps1], [st])
                        P.tt(hid[:, f, :], st[:], ps3[:, :nslot], ALU.mult, [st, ps3], [hid])
                for h2 in range(2):
                    w2 = wr.load(dr["exp_w2"][l, e][:, h2 * 512:(h2 + 1) * 512], dr["exp_w2"])
                    for ji, (j0, nj) in enumerate(jcs):
                        psy = P.banks[pi % 6]
                        pi += 1
                        for f in range(KC):
                            P.mm(psy[0:nj, :], hid[:, f, j0:j0 + nj], w2[:, f, :], f == 0, f == KC - 1, [hid, w2], [psy])
                        P.copy(y_tok[0:nj, ji, h2 * 512:(h2 + 1) * 512], psy[0:nj, :], [psy], [y_tok], eng="act" if ji % 2 == 0 else "dve")
                for b, (t0, nb) in blocks:
                    for oc in range(KC):
                        pso = P.banks[pi % 6]
                        pi += 1
                        for ji, (j0, nj) in enumerate(jcs):
                            P.mm(pso[:, :nb], y_tok[0:nj, ji, oc * 128:(oc + 1) * 128], SelT[0:nj, ji, t0:t0 + nb], ji == 0, ji == len(jcs) - 1,
                                 [y_tok, SelT], [pso])
                        P.stt(xT[:, oc, t0:t0 + nb], pso[:, :nb], mvec(k, l, 5, oc, SI(b)), xT[:, oc, t0:t0 + nb],
                              ALU.mult, ALU.add, [pso, k.modv, xT], [xT])


RB = [(0, 256)] + [(256 + 256 * i, 256) for i in range(8)]
V_MU, V_W0, V_A0, V_KK, V_KA, V_RK, V_GG, V_GB = 0, 6, 8, 10, 11, 12, 13, 14
NEG_EXP_HALF = -0.6065306597126334


def layer1_mixer(k, s, hT, xT, tap):
    P = k.P
    dr = k.dr
    SI = lambda b: 4 if b == 0 else s
    with P.phase():
        sc = NormScratch(k, P.banks[0])
        for b, (t0, nb) in enumerate(BLOCKS):
            xn = rms_block(k, sc, xT[:, :, t0:t0 + nb], xT, nb)
            for kc in range(KC):
                P.ts(hT[:, kc, t0:t0 + nb], xn[:, kc, :nb], k.Amod[:, 1, 0, kc, SI(b):SI(b) + 1],
                     mvec(k, 1, 0, kc, SI(b)), ALU.mult, ALU.add, [xn, k.Amod, k.modv], [hT])
        P.dma("sp", k.xspill[:].rearrange("(kc p) t -> p kc t", p=128), xT[:, :, LC:T], k.xspill, xT)
        tap("L1_h", hT, hT[:], [128, KC, T])
    if k.stop_after == "L1A":
        return
    with P.phase():
        outT = P.sbuf("outT", [128, KC, S], BF16)
        with P.phase():
            rwkv_core(k, s, hT, xT, outT, tap)
        if k.stop_after in ("L1B", "L1L", "L1P"):
            return
        with P.phase():
            wr = WRing(k, 2)
            wo = [wr.load(dr["rw_wo"][:, h * 512:(h + 1) * 512], dr["rw_wo"]) for h in range(2)]
            P.dma("sp", xT[:, :, LC:T], k.xspill[:].rearrange("(kc p) t -> p kc t", p=128), xT, k.xspill)
            pi = 0
            for b, (t0, nb) in list(enumerate(BLOCKS))[1:]:
                for oc in range(KC):
                    ps = P.banks[pi % 4]
                    pi += 1
                    w = wo[oc // 4]
                    c0 = (oc % 4) * 128
                    for c in range(KC):
                        P.mm(ps[:, :nb], w[:, c, c0:c0 + 128], outT[:, c, t0 - LC:t0 - LC + nb], c == 0, c == KC - 1,
                             [w, outT], [ps])
                    P.stt(xT[:, oc, t0:t0 + nb], ps[:, :nb], mvec(k, 1, 2, oc, s), xT[:, oc, t0:t0 + nb], ALU.mult, ALU.add,
                          [ps, k.modv, xT], [xT])
            tap("L1_xmix", xT, xT[:], [128, KC, T])


def rwkv_core(k, s, hT, xT, outT, tap):
    P = k.P
    dr = k.dr
    with P.overlay(xT):
        lw1 = P.sbuf("lw1", [128, T], BF16)
        la1 = P.sbuf("la1", [128, T], BF16)
        lg1 = P.sbuf("lg1", [128, T], BF16)
        lg1b = P.sbuf("lg1b", [32, T], BF16)
        y_acc = P.sbuf("y_acc", [128, S], F32)
        bon = P.sbuf("bon", [128, S], F32)
    ov_mark = xT.ov_top
    vec = P.sbuf("rwvec", [128, 15, KC], F32)
    P.dma("sp", vec[:], dr["rw_vec"][:], vec, dr["rw_vec"])
    omk = P.sbuf("omk", [128, KC], F32)
    P.ts(omk[:], vec[:, V_KA, :], -1.0, 1.0, ALU.mult, ALU.add, [vec], [omk])
    omu = P.sbuf("omu", [128, 6, KC], F32)
    hmu = P.sbuf("hmu", [128, 6, KC], F32)
    P.ts(omu[:], vec[:, 0:6, :], -1.0, 1.0, ALU.mult, ALU.add, [vec], [omu])
    P.ts(hmu[:], vec[:, 0:6, :], 0.5, None, ALU.mult, None, [vec], [hmu])
    identb = P.sbuf("identb", [128, 128], BF16)
    P.dma("pool", identb[:], dr["ident"][:], identb, dr["ident"])
    identf = P.sbuf("identf", [128, 128], F32)
    P.dma("sp", identf[:], dr["ident"][:], identf, dr["ident"])
    bones = P.sbuf("bones", [128, 128], F32)
    P.dma("sp", bones[:], dr["blockones"][:], bones, dr["blockones"])
    maskC = P.sbuf("maskC", [128, 256], F32)
    P.dma("sp", maskC[:], dr["maskC"][:], maskC, dr["maskC"])
    m4 = [P.sbuf("m4_%d" % d, [128, 512], BF16) for d in range(2)]
    m1 = [P.sbuf("m1_%d" % d, [128, 128], BF16) for d in range(2)]
    for d in range(2):
        P.dma("pool", m4[d][:], dr["rmask4"][d], m4[d], dr["rmask4"])
        P.dma("pool", m1[d][:], dr["rmask1"][d], m1[d], dr["rmask1"])

    def vcol(i, c):
        return vec[:, i, c:c + 1]

    def load_scaled(name, src2d, ncols, mu_i):
        wa = P.sbuf(name + "a", [128, KC, ncols], BF16)
        wb = P.sbuf(name + "b", [128, KC, ncols], BF16)
        P.dma("pool", wa[:], src2d.rearrange("(kc p) n -> p kc n", p=128), wa, dr["rw_rkv"])
        for kc in range(KC):
            P.ts(wb[:, kc, :], wa[:, kc, :], hmu[:, mu_i, kc:kc + 1], None, ALU.mult, None, [wa, hmu], [wb])
        for kc in range(KC):
            P.ts(wa[:, kc, :], wa[:, kc, :], omu[:, mu_i, kc:kc + 1], None, ALU.mult, None, [wa, omu], [wa])
        return wa, wb

    def proj_mix(ps_ap, psbuf, wa, wb, c0, m, t0, nb):
        lo = 1 if t0 in (0, LC) else 0
        hi = nb - 1 if (t0 + nb) in (LC, T) else nb
        for kc in range(KC):
            P.mm(ps_ap, wa[:, kc, c0:c0 + m], hT[:, kc, t0:t0 + nb], kc == 0, False, [wa, hT], [psbuf])
        for kc in range(KC):
            P.mm(ps_ap[:, lo:nb], wb[:, kc, c0:c0 + m], hT[:, kc, t0 + lo - 1:t0 + nb - 1], False, False, [wb, hT], [psbuf])
        for kc in range(KC):
            P.mm(ps_ap[:, 0:hi], wb[:, kc, c0:c0 + m], hT[:, kc, t0 + 1:t0 + hi + 1], False, kc == KC - 1, [wb, hT], [psbuf])

    with P.phase():
        w1, w1s = load_scaled("w1c", dr["rw_w1cat"][:], 128, 1)
        a1, a1s = load_scaled("a1c", dr["rw_a1cat"][:], 128, 4)
        g1, g1s = load_scaled("g1c", dr["rw_g1"][:], 160, 5)
        pi = 0
        for (t0, nb) in BLOCKS:
            ps = P.banks[pi % 4]; pi += 1
            proj_mix(ps[:, :nb], ps, w1, w1s, 0, 128, t0, nb)
            P.act(lw1[:, t0:t0 + nb], ps[:, :nb], AF.Tanh, [ps], [lw1])
            ps = P.banks[pi % 4]; pi += 1
            proj_mix(ps[:, :nb], ps, a1, a1s, 0, 128, t0, nb)
            P.copy(la1[:, t0:t0 + nb], ps[:, :nb], [ps], [la1], eng="act")
            ps = P.banks[pi % 4]; pi += 1
            proj_mix(ps[:, :nb], ps, g1, g1s, 0, 128, t0, nb)
            P.act(lg1[:, t0:t0 + nb], ps[:, :nb], AF.Sigmoid, [ps], [lg1])
            ps = P.banks[pi % 4]; pi += 1
            proj_mix(ps[0:32, :nb], ps, g1, g1s, 128, 32, t0, nb)
            P.act(lg1b[:, t0:t0 + nb], ps[0:32, :nb], AF.Sigmoid, [ps], [lg1b])
    if k.stop_after == "L1L":
        tap("L1_lw1", lw1, lw1[:], [128, T])
        return
    for c in range(KC if k.stop_after != "L1P" else 1):
        with P.phase():
            rwkv_pair(k, s, c, hT, xT, ov_mark, lw1, la1, lg1, lg1b, y_acc, bon, outT, vec, omk, identb, identf, bones, maskC,
                      m4, m1, load_scaled, proj_mix, vcol, tap)
    tap("L1_og", outT, outT[:], [128, KC, S])


class DirBufs:
    def __init__(self, P, d):
        NB = 256
        self.f = [P.sbuf("rf%d_%d" % (d, i), [128, NB], F32) for i in range(10)]
        self.Vb = P.sbuf("Vb%d" % d, [128, NB], BF16)
        self.ARz = [P.sbuf("zar%d_%d" % (d, h), [128, 2, NB], BF16) for h in range(2)]
        self.BTz, self.KTz = [[P.sbuf("z%d_%d_%d" % (d, i, h), [128, NB], BF16) for h in range(2)] for i in range(2)]
        self.stk = [P.sbuf("stk%d_%d" % (d, i), [128, 192], BF16) for i in range(4)]
        self.Vz = [[P.sbuf("Vz%d_%d_%d" % (d, i, h), [128, 64], BF16) for h in range(2)] for i in range(4)]
        self.Uz = [P.sbuf("Uz%d_%d" % (d, h), [128, 64], BF16) for h in range(2)]
        self.mats = [P.sbuf("mats%d_%d" % (d, i), [128, 640], BF16) for i in range(4)]
        self.Lf = [P.sbuf("Lf%d_%d" % (d, i), [128, 256], F32) for i in range(4)]
        self.sq = [[P.sbuf("sq%d_%d_%d" % (d, i, j), [128, 256], F32) for j in range(2)] for i in range(4)]
        self.ZT = [P.sbuf("ZT%d_%d" % (d, i), [128, 128], F32) for i in range(4)]
        self.T0 = P.sbuf("T0_%d" % d, [128, 64], F32)
        self.T0s = P.sbuf("T0s_%d" % d, [128, 64], F32)
        self.T0b = P.sbuf("T0b_%d" % d, [128, 64], BF16)
        self.Xb = P.sbuf("Xb_%d" % d, [128, 64], F32)
        zs = [t_ for l_ in (self.ARz, self.BTz, self.KTz) for t_ in l_] + [v_ for l_ in self.Vz for v_ in l_] + self.Uz
        for t_ in zs + [self.T0, self.T0b]:
            P.memset(t_[:], 0.0, [t_])


def rwkv_pair(k, s, c, hT, xT, ov_mark, lw1, la1, lg1, lg1b, y_acc, bon, outT, vec, omk, identb, identf, bones, maskC, m4, m1,
              load_scaled, proj_mix, vcol, tap):
    P = k.P
    dr = k.dr
    cs_ = slice(c * 128, (c + 1) * 128)
    wr_, wrs = load_scaled("wr", dr["rw_rkv"][0][:, cs_], 128, 0)
    wk_, wks = load_scaled("wk", dr["rw_rkv"][1][:, cs_], 128, 2)
    wv_, wvs = load_scaled("wv", dr["rw_rkv"][2][:, cs_], 128, 3)
    w2c = P.sbuf("w2c", [128, 128], BF16)
    a2c = P.sbuf("a2c", [128, 128], BF16)
    g2a = P.sbuf("g2a", [128, 128], BF16)
    g2b = P.sbuf("g2b", [32, 128], BF16)
    P.dma("pool", w2c[:], dr["rw_w2cat"][:, cs_], w2c, dr["rw_w2cat"])
    P.dma("pool", a2c[:], dr["rw_a2cat"][:, cs_], a2c, dr["rw_a2cat"])
    P.dma("pool", g2a[:], dr["rw_g2"][0:128, cs_], g2a, dr["rw_g2"])
    P.dma("pool", g2b[:], dr["rw_g2"][128:160, cs_], g2b, dr["rw_g2"])
    P.memset(y_acc[:], 0.0, [y_acc])
    P.memset(bon[:], 0.0, [bon])
    bufs = [DirBufs(P, 0)]
    with P.overlay(xT, start=ov_mark):
        bufs.append(DirBufs(P, 1))
    sqb = bufs[0].f[0]

    def dir_gen(d):
        W = bufs[d]
        B = P.banks[4 * d:4 * d + 4]
        r_f, k_f, v_f, kk_f, ic_f, lw_f, cs_f, e1, e2, tmp = W.f
        ARz, BTz, KTz, Vb, stk, Vz, Uz, mats, Lf, sq, ZT = (W.ARz, W.BTz, W.KTz, W.Vb, W.stk, W.Vz, W.Uz, W.mats, W.Lf, W.sq, W.ZT)
        T0, T0s, T0b, Xb = W.T0, W.T0s, W.T0b, W.Xb
        order = list(range(9)) if d == 0 else [0] + list(range(8, 0, -1))
        for b in order:
            t0, nb = RB[b]
            lat = b > 0
            l0 = t0 - LC
            proj_mix(B[0][:, :nb], B[0], wr_, wrs, 0, 128, t0, nb)
            yield
            proj_mix(B[1][:, :nb], B[1], wk_, wks, 0, 128, t0, nb)
            yield
            proj_mix(B[2][:, :nb], B[2], wv_, wvs, 0, 128, t0, nb)
            pr_d = slice(d * 64, (d + 1) * 64)
            P.mm(B[3][:, 0:nb], w2c[pr_d, :], lw1[pr_d, t0:t0 + nb], True, True, [w2c, lw1], [B[3]])
            P.mm(B[3][:, 256:256 + nb], a2c[pr_d, :], la1[pr_d, t0:t0 + nb], True, True, [a2c, la1], [B[3]])
            P.copy(r_f[:], B[0][:, :nb], [B[0]], [r_f], eng="act")
            P.copy(k_f[:], B[1][:, :nb], [B[1]], [k_f], eng="act")
            P.copy(v_f[:], B[2][:, :nb], [B[2]], [v_f], eng="act")
            P.copy(Vb[:], v_f[:], [v_f], [Vb])
            yield
            P.act(lw_f[:], B[3][:, 0:nb], AF.Sigmoid, [B[3], vec], [lw_f], bias=vcol(V_W0 + d, c))
            P.ts(lw_f[:], lw_f[:], NEG_EXP_HALF, None, ALU.mult, None, [lw_f], [lw_f])
            P.act(ic_f[:], B[3][:, 256:256 + nb], AF.Sigmoid, [B[3], vec], [ic_f], bias=vcol(V_A0 + d, c))
            P.ts(kk_f[:], k_f[:], vcol(V_KK, c), None, ALU.mult, None, [k_f, vec], [kk_f])
            P.tt(tmp[:], kk_f[:], kk_f[:], ALU.mult, [kk_f], [tmp])
            P.mm(B[0][:, :nb], bones[:], tmp[:], True, True, [bones, tmp], [B[0]])
            yield
            P.ts(e1[:], B[0][:, :nb], 1e-18, None, ALU.max, None, [B[0]], [e1])
            P.act(e1[:], e1[:], AF.Ln, [e1], [e1])
            P.act(e1[:], e1[:], AF.Exp, [e1], [e1], scale=-0.5)
            P.tt(kk_f[:], kk_f[:], e1[:], ALU.mult, [kk_f, e1], [kk_f])
            P.ts(tmp[:], ic_f[:], vcol(V_KA, c), omk[:, c:c + 1], ALU.mult, ALU.add, [ic_f, vec, omk], [tmp])
            P.tt(k_f[:], k_f[:], tmp[:], ALU.mult, [k_f, tmp], [k_f])
            if lat:
                P.stt(tmp[:], r_f[:], vcol(V_RK, c), k_f[:], ALU.mult, ALU.mult, [r_f, vec, k_f], [tmp])
                P.mm(B[1][:, :nb], bones[:], tmp[:], True, True, [bones, tmp], [B[1]])
                yield
                P.tt(tmp[:], B[1][:, :nb], v_f[:], ALU.mult, [B[1], v_f], [tmp])
                P.tt(bon[:, l0:l0 + nb], bon[:, l0:l0 + nb], tmp[:], ALU.add, [bon, tmp], [bon])
            if d == 0:
                P.op("dve", lambda e: e.tensor_tensor_scan(out=cs_f[:], data0=maskC[:], data1=lw_f[:], initial=0.0,
                                                           op0=ALU.mult, op1=ALU.add), [maskC, lw_f], [cs_f])
            else:
                P.op("dve", lambda e: e.tensor_tensor_scan(out=cs_f[:, ::-1], data0=maskC[:], data1=lw_f[:, ::-1], initial=0.0,
                                                           op0=ALU.mult, op1=ALU.add), [maskC, lw_f], [cs_f])
            P.tt(e2[:], cs_f[:], lw_f[:], ALU.subtract, [cs_f, lw_f], [e2])
            P.act(e2[:], e2[:], AF.Exp, [e2], [e2])
            for h in range(2):
                pr = slice(h * 64, (h + 1) * 64)
                P.stt(ARz[h][pr, 0, :], kk_f[pr], -1.0, e2[pr], ALU.mult, ALU.mult, [kk_f, e2], [ARz[h]])
            P.act(e1[:], cs_f[:], AF.Exp, [cs_f], [e1])
            for h in range(2):
                pr = slice(h * 64, (h + 1) * 64)
                P.tt(ARz[h][pr, 1, :], r_f[pr], e1[pr], ALU.mult, [r_f, e1], [ARz[h]])
            P.act(e2[:], cs_f[:], AF.Exp, [cs_f], [e2], scale=-1.0)
            P.tt(tmp[:], kk_f[:], ic_f[:], ALU.mult, [kk_f, ic_f], [tmp])
            for h in range(2):
                pr = slice(h * 64, (h + 1) * 64)
                P.tt(BTz[h][pr], tmp[pr], e2[pr], ALU.mult, [tmp, e2], [BTz[h]])
                P.tt(KTz[h][pr], k_f[pr], e2[pr], ALU.mult, [k_f, e2], [KTz[h]])
            yield
            nch = nb // 64
            for n in range(nch):
                cols = slice(n * 64, (n + 1) * 64)
                pb = B[n % 2]
                for h in range(2):
                    pr = slice(h * 64, (h + 1) * 64)
                    for mi, src in enumerate((Vb, BTz[h], KTz[h])):
                        P.mm(pb[pr, mi * 64:(mi + 1) * 64], src[:, cols], identb[:, pr], True, True, [src, identb], [pb])
                P.copy(stk[n][:], pb[:, 0:192], [pb], [stk[n]], eng="act")
                for h in range(2):
                    pr = slice(h * 64, (h + 1) * 64)
                    P.copy(Vz[n][h][pr, :], stk[n][pr, 0:64], [stk[n]], [Vz[n][h]])
                yield
            for n in range(nch):
                cols = slice(n * 64, (n + 1) * 64)
                pa = B[2 + n % 2]
                pb = B[n % 2]
                for h in range(2):
                    pr = slice(h * 64, (h + 1) * 64)
                    hc2 = slice(h * 64, (h + 1) * 64)
                    P.mm(pa[pr, 0:256].rearrange("p (m c) -> p m c", m=2)[:, :, hc2], BTz[h][:, cols], ARz[h][:, :, cols], True, True,
                         [BTz[h], ARz[h]], [pa])
                    P.mm(pa[pr, 256:512].rearrange("p (m c) -> p m c", m=2)[:, :, hc2], KTz[h][:, cols], ARz[h][:, :, cols], True, True,
                         [KTz[h], ARz[h]], [pa])
                    P.mm(pb[pr, 256 + h * 64:256 + (h + 1) * 64], ARz[h][:, 0, cols], BTz[h][:, cols], True, True,
                         [ARz[h], BTz[h]], [pb])
                P.tt(Lf[n][:, 0:128], pa[:, 0:128], m4[d][:, 0:128], ALU.mult, [pa, m4[d]], [Lf[n]])
                P.tt(Lf[n][:, 128:256], pb[:, 256:384], m1[d][:], ALU.mult, [pb, m1[d]], [Lf[n]])
                P.tt(mats[n][:, 128:512], pa[:, 128:512], m4[d][:, 128:512], ALU.mult, [pa, m4[d]], [mats[n]])
                yield
            cur = []
            for n in range(nch):
                P.tt(ZT[n][:], Lf[n][:, 0:128], identf[:], ALU.add, [Lf[n], identf], [ZT[n]])
                cur.append((Lf[n], 128, 0))
            for lvl in range(5):
                last = lvl == 4
                for n in range(nch):
                    src, oL, oLT = cur[n]
                    dst = sq[n][lvl % 2]
                    pa = B[n % 2]
                    P.mm(pa[:, 0:128], src[:, oLT:oLT + 128], src[:, oL:oL + 128], True, True, [src], [pa])
                    if not last:
                        P.mm(pa[:, 128:256], src[:, oL:oL + 128], src[:, oLT:oLT + 128], True, True, [src], [pa])
                        P.copy(dst[:, 0:256], pa[:, 0:256], [pa], [dst], eng="act")
                    else:
                        P.copy(dst[:, 0:128], pa[:, 0:128], [pa], [dst], eng="act")
                    cur[n] = (dst, 0, 128)
                    yield
                for n in range(nch):
                    src, oL, oLT = cur[n]
                    pz = B[2 + n % 2]
                    P.mm(pz[:, 0:128], src[:, oL:oL + 128], ZT[n][:], True, True, [src, ZT[n]], [pz])
                    P.tt(ZT[n][:], ZT[n][:], pz[:, 0:128], ALU.add, [ZT[n], pz], [ZT[n]])
                    yield
            chunks = list(range(nch)) if d == 0 else list(range(nch - 1, -1, -1))
            for n in chunks:
                cols = slice(n * 64, (n + 1) * 64)
                dcol = (n * 64 + 63) if d == 0 else n * 64
                dC = e1[:, dcol:dcol + 1]
                pX = B[0]
                for h in range(2):
                    pr = slice(h * 64, (h + 1) * 64)
                    P.mm(pX[pr, 0:64], ARz[h][:, 0, cols], T0b[:, :], True, False, [ARz[h], T0b], [pX])
                P.mm(pX[:, 0:64], mats[n][:, 256:384], stk[n][:, 0:64], False, True, [mats[n], stk[n]], [pX])
                P.ts(T0s[:], T0[:], dC, None, ALU.mult, None, [T0, e1], [T0s])
                yield
                P.copy(Xb[:], pX[:, 0:64], [pX], [Xb], eng="act")
                pU = B[1]
                P.mm(pU[:, 0:64], ZT[n][:], Xb[:], True, True, [ZT[n], Xb], [pU])
                yield
                P.copy(Uz[0][0:64, :], pU[0:64, 0:64], [pU], [Uz[0]], eng="act")
                P.copy(Uz[1][64:128, :], pU[64:128, 0:64], [pU], [Uz[1]])
                pT = B[3]
                for h in range(2):
                    pr = slice(h * 64, (h + 1) * 64)
                    P.mm(pT[pr, 0:64], stk[n][:, 64:128], Uz[h][:, :], True, False, [stk[n], Uz[h]], [pT])
                    P.mm(pT[pr, 0:64], stk[n][:, 128:192], Vz[n][h][:, :], False, True, [stk[n], Vz[n][h]], [pT])
                if lat:
                    pY = B[2]
                    for h in range(2):
                        pr = slice(h * 64, (h + 1) * 64)
                        P.mm(pY[pr, 0:64], T0b[:, :], ARz[h][:, 1, cols], True, False, [T0b, ARz[h]], [pY])
                        P.mm(pY[pr, 0:64], Uz[h][:, :], mats[n][:, 128 + h * 64:128 + (h + 1) * 64], False, False,
                             [Uz[h], mats[n]], [pY])
                        P.mm(pY[pr, 0:64], stk[n][:, 0:64], mats[n][:, 384 + h * 64:384 + (h + 1) * 64], False, True,
                             [stk[n], mats[n]], [pY])
                yield
                P.stt(T0[:], pT[:, 0:64], dC, T0s[:], ALU.mult, ALU.add, [pT, e1, T0s], [T0])
                P.copy(T0b[:], T0[:], [T0], [T0b], eng="act")
                if lat:
                    yc = slice(l0 + n * 64, l0 + (n + 1) * 64)
                    P.tt(y_acc[:, yc], y_acc[:, yc], pY[:, 0:64], ALU.add, [y_acc, pY], [y_acc])
                yield

    gens = [dir_gen(0), dir_gen(1)]
    alive = [True, True]
    while any(alive):
        for gi, g in enumerate(gens):
            if alive[gi]:
                try:
                    next(g)
                except StopIteration:
                    alive[gi] = False
    if c == 0:
        tap("L1_y0", y_acc, y_acc[:], [128, S])
        tap("L1_bon0", bon, bon[:], [128, S])
    B = P.banks
    sqb2 = bufs[1].f[0]
    for i in range(4):
        l0 = i * 512
        t0 = LC + l0
        nb = 512
        yb = y_acc[:, l0:l0 + nb]
        sq_ = [bufs[0].f[0], bufs[0].f[1]]
        for hf in range(2):
            c0_ = l0 + hf * 256
            ybh = y_acc[:, c0_:c0_ + 256]
            sc_ = bufs[hf].f[0]
            P.mm(B[0][:, hf * 256:(hf + 1) * 256], bones[:], ybh, True, True, [bones, y_acc], [B[0]])
            P.stt(ybh, B[0][:, hf * 256:(hf + 1) * 256], -1.0 / 64, ybh, ALU.mult, ALU.add, [B[0], y_acc], [y_acc])
            P.tt(sc_[:], ybh, ybh, ALU.mult, [y_acc], [sc_])
            P.mm(B[1][:, hf * 256:(hf + 1) * 256], bones[:], sc_[:], True, True, [bones, sc_], [B[1]])
            P.act(sc_[:], B[1][:, hf * 256:(hf + 1) * 256], AF.Ln, [B[1], k.gneps], [sc_], scale=1.0 / 64, bias=k.gneps[:, 0:1])
            P.act(sc_[:], sc_[:], AF.Exp, [sc_], [sc_], scale=-0.5)
            P.tt(ybh, ybh, sc_[:], ALU.mult, [y_acc, sc_], [y_acc])
        P.ts(yb, yb, vcol(V_GG, c), vcol(V_GB, c), ALU.mult, ALU.add, [y_acc, vec], [y_acc])
        P.tt(yb, yb, bon[:, l0:l0 + nb], ALU.add, [y_acc, bon], [y_acc])
        P.mm(B[2][:, :nb], g2a[:], lg1[:, t0:t0 + nb], True, False, [g2a, lg1], [B[2]])
        P.mm(B[2][:, :nb], g2b[:], lg1b[:, t0:t0 + nb], False, True, [g2b, lg1b], [B[2]])
        P.tt(outT[:, c, l0:l0 + nb], yb, B[2][:, :nb], ALU.mult, [y_acc, B[2]], [outT])


_CACHE = {}


def kernel(**inputs):
    inp = {k_: np.asarray(v) for k_, v in inputs.items()}
    B = inp["x"].shape[0]
    ns = B // NCORE
    if "nc" not in _CACHE:
        _CACHE["nc"] = build(ns=ns)[0]
    nc = _CACHE["nc"]
    sh = prep_shared(inp)
    in_maps = []
    for core in range(NCORE):
        m = dict(sh)
        m.update(prep_core(inp, core, ns))
        in_maps.append(m)
    res = run_bass_kernel_spmd(nc, in_maps, core_ids=list(range(NCORE)))
    out = np.empty((B, S, D), np.float32)
    for core in range(NCORE):
        o = res.results[core]["outT"]
        for s in range(ns):
            out[core * ns + s] = o[s].T
    return out
```
